# Optimizing a Trainium2 kernel written in Bass

```python
import math
import jax
import jax.numpy as jnp
from jax import lax
import numpy as np

D_MODEL = 1024
BATCH = 2
SEQ = 8192
DEPTH = 2

N_EVEN = (DEPTH + 1) // 2
N_ODD = DEPTH // 2
Q_BLOCK = 128
EPS = 1e-6
SB_HEADS = 8
SB_HEAD_DIM = 64
SB_WIDTH = SB_HEADS * SB_HEAD_DIM
MLA_HEADS = 8
MLA_Q_RANK = 256
MLA_KV_RANK = 128
MLA_NOPE_DIM = 64
MLA_ROPE_DIM = 32
MLA_QK_DIM = MLA_NOPE_DIM + MLA_ROPE_DIM
MLA_V_DIM = 64
MLA_WIDTH = MLA_HEADS * MLA_V_DIM
ROPE_THETA = 10000.0
OFF_SB_K = SB_WIDTH
OFF_SB_V = 2 * SB_WIDTH
OFF_CQ = 3 * SB_WIDTH
OFF_CKV = OFF_CQ + MLA_Q_RANK
OFF_KR = OFF_CKV + MLA_KV_RANK
IN_COLS = OFF_KR + MLA_ROPE_DIM
MIX_WIDTH = SB_WIDTH + MLA_WIDTH
SSM_WIDTH = D_MODEL // 2
SSM_GROUP = 16
SSM_GROUPS = SSM_WIDTH // SSM_GROUP
SSM_STATE = 64
DT_MIN = 1e-3
DT_MAX = 1e-1
EIG_RE_MAX = -1e-4
D_FF = 7 * D_MODEL // 2
N_EXPERTS = 8
TOP_K = 2
EXPERT_BLOCK = 128

kernel_name = 'hybrid_stickbreak_mla_s5_moe'


def _rms_norm(x, gain):
    xf = x.astype(jnp.float32)
    y = xf * lax.rsqrt(jnp.mean(xf * xf, axis=-1, keepdims=True) + EPS)
    return (y * gain.astype(jnp.float32)).astype(x.dtype)


def _to_heads(t, heads):
    b, s, _ = t.shape
    return t.reshape(b, s, heads, -1).transpose(0, 2, 1, 3).astype(jnp.float32)


def _from_heads(t):
    b, h, s, d = t.shape
    return t.transpose(0, 2, 1, 3).reshape(b, s, h * d)


def _sweep_query_blocks(block_fn, q, k, v):
    b, h, s, dq = q.shape
    nb = s // Q_BLOCK
    qb = q.reshape(b, h, nb, Q_BLOCK, dq).transpose(2, 0, 1, 3, 4)
    starts = jnp.arange(nb, dtype=jnp.int32) * Q_BLOCK
    out = lax.map(lambda args: block_fn(args[0], args[1], k, v), (qb, starts))
    return out.transpose(1, 2, 0, 3, 4).reshape(b, h, s, v.shape[-1])


def _stick_breaking_block(q_blk, t0, k, v):
    s_len = k.shape[2]
    t_idx = t0 + jnp.arange(Q_BLOCK, dtype=jnp.int32)
    s_idx = jnp.arange(s_len, dtype=jnp.int32)
    earlier = s_idx[None, :] < t_idx[:, None]
    z = jnp.einsum('bhqd,bhkd->bhqk', q_blk, k) * (q_blk.shape[-1] ** -0.5)
    log_fail = jnp.where(earlier, jax.nn.log_sigmoid(-z), 0.0)
    log_stick = lax.cumsum(log_fail, axis=3, reverse=True) - log_fail
    w = jnp.where(earlier, jnp.exp(jax.nn.log_sigmoid(z) + log_stick), 0.0)
    return jnp.einsum('bhqk,bhkd->bhqd', w, v)


def _causal_softmax_block(q_blk, t0, k, v):
    s_len = k.shape[2]
    t_idx = t0 + jnp.arange(Q_BLOCK, dtype=jnp.int32)
    s_idx = jnp.arange(s_len, dtype=jnp.int32)
    visible = s_idx[None, :] <= t_idx[:, None]
    z = jnp.einsum('bhqd,bhkd->bhqk', q_blk, k) * (q_blk.shape[-1] ** -0.5)
    p = jax.nn.softmax(jnp.where(visible, z, -jnp.inf), axis=-1)
    return jnp.einsum('bhqk,bhkd->bhqd', p, v)


def _rope_tables(s_len):
    inv_freq = ROPE_THETA ** (-jnp.arange(0, MLA_ROPE_DIM, 2, dtype=jnp.float32) / MLA_ROPE_DIM)
    ang = jnp.arange(s_len, dtype=jnp.float32)[:, None] * inv_freq[None, :]
    return jnp.cos(ang), jnp.sin(ang)


def _rope_tail(t, cos, sin):
    half = MLA_ROPE_DIM // 2
    nope = t[..., :MLA_NOPE_DIM]
    r1 = t[..., MLA_NOPE_DIM:MLA_NOPE_DIM + half]
    r2 = t[..., MLA_NOPE_DIM + half:]
    c = cos[None, :, None, :]
    s = sin[None, :, None, :]
    return jnp.concatenate([nope, r1 * c - r2 * s, r2 * c + r1 * s], axis=-1)


def _attention_layer(x, norm_g, w_in, q_lat_g, w_q_up, kv_lat_g, w_kv_up, q_g, k_g, w_out):
    b, s, _ = x.shape
    h = _rms_norm(x, norm_g) @ w_in
    o_sb = _sweep_query_blocks(
        _stick_breaking_block,
        _to_heads(h[..., :OFF_SB_K], SB_HEADS),
        _to_heads(h[..., OFF_SB_K:OFF_SB_V], SB_HEADS),
        _to_heads(h[..., OFF_SB_V:OFF_CQ], SB_HEADS))
    c_q = _rms_norm(h[..., OFF_CQ:OFF_CKV], q_lat_g)
    c_kv = _rms_norm(h[..., OFF_CKV:OFF_KR], kv_lat_g)
    k_rope = h[..., OFF_KR:]
    q = (c_q @ w_q_up).reshape(b, s, MLA_HEADS, MLA_QK_DIM)
    kv = (c_kv @ w_kv_up).reshape(b, s, MLA_HEADS, MLA_NOPE_DIM + MLA_V_DIM)
    k = jnp.concatenate(
        [kv[..., :MLA_NOPE_DIM],
         jnp.broadcast_to(k_rope[:, :, None, :], (b, s, MLA_HEADS, MLA_ROPE_DIM))], axis=-1)
    v = kv[..., MLA_NOPE_DIM:].astype(jnp.float32)
    q = _rms_norm(q, q_g).astype(jnp.float32)
    k = _rms_norm(k, k_g).astype(jnp.float32)
    cos, sin = _rope_tables(s)
    q = _rope_tail(q, cos, sin)
    k = _rope_tail(k, cos, sin)
    o_mla = _sweep_query_blocks(
        _causal_softmax_block,
        q.transpose(0, 2, 1, 3), k.transpose(0, 2, 1, 3), v.transpose(0, 2, 1, 3))
    merged = jnp.concatenate([_from_heads(o_sb), _from_heads(o_mla)], axis=-1)
    return x + merged.astype(x.dtype) @ w_out


def _dense_swiglu_layer(x, norm_g, w_gate, w_up, w_down):
    xn = _rms_norm(x, norm_g)
    return x + (jax.nn.silu(xn @ w_gate) * (xn @ w_up)) @ w_down


def _s5_discretize(a_re, a_im, log_dt, b_re, b_im):
    a_re = jnp.minimum(a_re.astype(jnp.float32), EIG_RE_MAX)
    a_im = a_im.astype(jnp.float32)
    dt = jnp.exp(log_dt.astype(jnp.float32))[:, None]
    mag = jnp.exp(a_re * dt)
    lam_re = mag * jnp.cos(a_im * dt)
    lam_im = mag * jnp.sin(a_im * dt)
    den = a_re * a_re + a_im * a_im
    num_re = lam_re - 1.0
    coef_re = ((num_re * a_re + lam_im * a_im) / den)[..., None]
    coef_im = ((lam_im * a_re - num_re * a_im) / den)[..., None]
    b_re = b_re.astype(jnp.float32)
    b_im = b_im.astype(jnp.float32)
    bb_re = coef_re * b_re - coef_im * b_im
    bb_im = coef_re * b_im + coef_im * b_re
    return lam_re, lam_im, bb_re, bb_im


def _complex_affine_combine(first, second):
    a1r, a1i, b1r, b1i = first
    a2r, a2i, b2r, b2i = second
    return (a2r * a1r - a2i * a1i,
            a2r * a1i + a2i * a1r,
            a2r * b1r - a2i * b1i + b2r,
            a2r * b1i + a2i * b1r + b2i)


def _s5_layer(x, norm_g, w_in, a_re, a_im, log_dt, b_re, b_im, c_re, c_im, d_skip, w_glu):
    b, s, _ = x.shape
    u = (_rms_norm(x, norm_g) @ w_in).astype(jnp.float32)
    lam_re, lam_im, bb_re, bb_im = _s5_discretize(a_re, a_im, log_dt, b_re, b_im)
    ug = u.reshape(b, s, SSM_GROUPS, SSM_GROUP)
    bu_re = jnp.einsum('bsgc,gnc->bsgn', ug, bb_re)
    bu_im = jnp.einsum('bsgc,gnc->bsgn', ug, bb_im)
    full = bu_re.shape
    _, _, h_re, h_im = lax.associative_scan(
        _complex_affine_combine,
        (jnp.broadcast_to(lam_re, full), jnp.broadcast_to(lam_im, full), bu_re, bu_im),
        axis=1)
    y = (jnp.einsum('bsgn,gcn->bsgc', h_re, c_re.astype(jnp.float32))
         - jnp.einsum('bsgn,gcn->bsgc', h_im, c_im.astype(jnp.float32))).reshape(b, s, SSM_WIDTH)
    y = jax.nn.gelu(y + d_skip.astype(jnp.float32) * u).astype(x.dtype)
    z = y @ w_glu
    return x + z[..., :D_MODEL] * jax.nn.sigmoid(z[..., D_MODEL:])


def _moe_swiglu_layer(x, norm_g, w_router, w_gate, w_up, w_down):
    b, s, d = x.shape
    xt = _rms_norm(x, norm_g).reshape(-1, d)
    n_tok = xt.shape[0]
    logits = xt.astype(jnp.float32) @ w_router.astype(jnp.float32)
    top_vals, top_idx = lax.top_k(logits, TOP_K)
    gates = jax.nn.softmax(top_vals, axis=-1)
    n_assign = n_tok * TOP_K
    flat_e = top_idx.reshape(-1).astype(jnp.int32)
    flat_tok = jnp.repeat(jnp.arange(n_tok, dtype=jnp.int32), TOP_K)
    flat_w = gates.reshape(-1)
    order = jnp.argsort(flat_e)
    e_sorted = flat_e[order]
    counts = jnp.bincount(flat_e, length=N_EXPERTS).astype(jnp.int32)
    padded = (counts + EXPERT_BLOCK - 1) // EXPERT_BLOCK * EXPERT_BLOCK
    pad_end = jnp.cumsum(padded)
    pad_start = pad_end - padded
    unpad_start = jnp.cumsum(counts) - counts
    dest = pad_start[e_sorted] + jnp.arange(n_assign, dtype=jnp.int32) - unpad_start[e_sorted]
    n_slots = (n_assign + EXPERT_BLOCK - 1) // EXPERT_BLOCK * EXPERT_BLOCK + N_EXPERTS * EXPERT_BLOCK
    n_blocks = n_slots // EXPERT_BLOCK
    slot_tok = jnp.zeros((n_slots,), jnp.int32).at[dest].set(flat_tok[order])
    slot_w = jnp.zeros((n_slots,), jnp.float32).at[dest].set(flat_w[order])
    blk_start = jnp.arange(n_blocks, dtype=jnp.int32) * EXPERT_BLOCK
    blk_expert = jnp.clip(jnp.searchsorted(pad_end, blk_start, side='right'), 0, N_EXPERTS - 1)
    xs = xt[slot_tok].reshape(n_blocks, EXPERT_BLOCK, d)

    def expert_block(args):
        xb, e = args
        return (jax.nn.silu(xb @ w_gate[e]) * (xb @ w_up[e])) @ w_down[e]

    ys = lax.map(expert_block, (xs, blk_expert)).reshape(n_slots, d)
    out = jax.ops.segment_sum(ys.astype(jnp.float32) * slot_w[:, None], slot_tok, num_segments=n_tok)
    return x + out.reshape(b, s, d).astype(x.dtype)


def setup_inputs(seed: int = 0) -> dict:
    key = jax.random.key(seed)
    ks = jax.random.split(key, 30)
    f32 = jnp.float32

    def nrm(k, shape, scale):
        return jax.random.normal(k, shape, f32) * scale

    def gain(k, shape):
        return 1.0 + 0.02 * jax.random.normal(k, shape, f32)

    ne, no = N_EVEN, N_ODD
    return {
        'x': jax.random.normal(ks[0], (BATCH, SEQ, D_MODEL), f32),
        'att_norm': gain(ks[1], (ne, D_MODEL)),
        'att_w_in': nrm(ks[2], (ne, D_MODEL, IN_COLS), D_MODEL ** -0.5),
        'att_q_latent_norm': gain(ks[3], (ne, MLA_Q_RANK)),
        'att_w_q_up': nrm(ks[4], (ne, MLA_Q_RANK, MLA_HEADS * MLA_QK_DIM), MLA_Q_RANK ** -0.5),
        'att_kv_latent_norm': gain(ks[5], (ne, MLA_KV_RANK)),
        'att_w_kv_up': nrm(ks[6], (ne, MLA_KV_RANK, MLA_HEADS * (MLA_NOPE_DIM + MLA_V_DIM)), MLA_KV_RANK ** -0.5),
        'att_q_norm': gain(ks[7], (ne, MLA_QK_DIM)),
        'att_k_norm': gain(ks[8], (ne, MLA_QK_DIM)),
        'att_w_out': nrm(ks[9], (ne, MIX_WIDTH, D_MODEL), MIX_WIDTH ** -0.5),
        'dffn_norm': gain(ks[10], (ne, D_MODEL)),
        'dffn_w_gate': nrm(ks[11], (ne, D_MODEL, D_FF), D_MODEL ** -0.5),
        'dffn_w_up': nrm(ks[12], (ne, D_MODEL, D_FF), D_MODEL ** -0.5),
        'dffn_w_down': nrm(ks[13], (ne, D_FF, D_MODEL), D_FF ** -0.5),
        'ssm_norm': gain(ks[14], (no, D_MODEL)),
        'ssm_w_in': nrm(ks[15], (no, D_MODEL, SSM_WIDTH), D_MODEL ** -0.5),
        'ssm_a_re': -0.5 + nrm(ks[16], (no, SSM_GROUPS, SSM_STATE), 0.01),
        'ssm_a_im': math.pi * jnp.arange(SSM_STATE, dtype=f32)[None, None, :]
                    + nrm(ks[17], (no, SSM_GROUPS, SSM_STATE), 0.01),
        'ssm_log_dt': jax.random.uniform(ks[18], (no, SSM_GROUPS), f32,
                                         minval=math.log(DT_MIN), maxval=math.log(DT_MAX)),
        'ssm_b_re': nrm(ks[19], (no, SSM_GROUPS, SSM_STATE, SSM_GROUP), (2 * SSM_GROUP) ** -0.5),
        'ssm_b_im': nrm(ks[20], (no, SSM_GROUPS, SSM_STATE, SSM_GROUP), (2 * SSM_GROUP) ** -0.5),
        'ssm_c_re': nrm(ks[21], (no, SSM_GROUPS, SSM_GROUP, SSM_STATE), 0.5),
        'ssm_c_im': nrm(ks[22], (no, SSM_GROUPS, SSM_GROUP, SSM_STATE), 0.5),
        'ssm_d': nrm(ks[23], (no, SSM_WIDTH), 1.0),
        'ssm_w_glu': nrm(ks[24], (no, SSM_WIDTH, 2 * D_MODEL), SSM_WIDTH ** -0.5),
        'moe_norm': gain(ks[25], (no, D_MODEL)),
        'moe_router': nrm(ks[26], (no, D_MODEL, N_EXPERTS), D_MODEL ** -0.5),
        'moe_w_gate': nrm(ks[27], (no, N_EXPERTS, D_MODEL, D_FF), D_MODEL ** -0.5),
        'moe_w_up': nrm(ks[28], (no, N_EXPERTS, D_MODEL, D_FF), D_MODEL ** -0.5),
        'moe_w_down': nrm(ks[29], (no, N_EXPERTS, D_FF, D_MODEL), D_FF ** -0.5),
    }


def reference(x, att_norm, att_w_in, att_q_latent_norm, att_w_q_up, att_kv_latent_norm, att_w_kv_up,
              att_q_norm, att_k_norm, att_w_out, dffn_norm, dffn_w_gate, dffn_w_up, dffn_w_down,
              ssm_norm, ssm_w_in, ssm_a_re, ssm_a_im, ssm_log_dt, ssm_b_re, ssm_b_im, ssm_c_re, ssm_c_im,
              ssm_d, ssm_w_glu, moe_norm, moe_router, moe_w_gate, moe_w_up, moe_w_down):
    h = x
    for layer in range(DEPTH):
        i = layer // 2
        if layer % 2 == 0:
            h = _attention_layer(h, att_norm[i], att_w_in[i], att_q_latent_norm[i], att_w_q_up[i],
                                 att_kv_latent_norm[i], att_w_kv_up[i], att_q_norm[i], att_k_norm[i],
                                 att_w_out[i])
            h = _dense_swiglu_layer(h, dffn_norm[i], dffn_w_gate[i], dffn_w_up[i], dffn_w_down[i])
        else:
            h = _s5_layer(h, ssm_norm[i], ssm_w_in[i], ssm_a_re[i], ssm_a_im[i], ssm_log_dt[i],
                          ssm_b_re[i], ssm_b_im[i], ssm_c_re[i], ssm_c_im[i], ssm_d[i], ssm_w_glu[i])
            h = _moe_swiglu_layer(h, moe_norm[i], moe_router[i], moe_w_gate[i], moe_w_up[i], moe_w_down[i])
    return h
```

```python
import contextlib
import numpy as np
import concourse.bass as bass
import concourse.mybir as mybir
from concourse.bass_utils import run_bass_kernel_spmd

F32 = mybir.dt.float32
BF16 = mybir.dt.bfloat16
I32 = mybir.dt.int32
AF = mybir.ActivationFunctionType
ALU = mybir.AluOpType
AX = mybir.AxisListType

D_MODEL = 1024
D_FF = 3584
EPS = 1e-6
NCORES = 8

COMPUTE = ("pe", "act", "dve", "pool")
NDSEM = 8


class Prog:
    def __init__(self, nc):
        self.nc = nc
        self.engs = ("pe", "act", "dve", "pool", "sp")
        self.q = {e: [] for e in self.engs}
        self.cnt = {e: 0 for e in COMPUTE}
        self.known = {e: {} for e in self.engs}
        self.res = {}
        self.dcnt = {}
        self.drr = {e: 0 for e in self.engs}
        self.sems = {}
        self.n_wait = 0
        self.n_op = 0

    def _deps(self, R, W):
        deps = {}
        for r in R:
            st = self.res.get(r)
            if st is not None and st[0] is not None:
                k, v = st[0]
                if deps.get(k, 0) < v:
                    deps[k] = v
        for w in W:
            st = self.res.get(w)
            if st is not None:
                if st[0] is not None:
                    k, v = st[0]
                    if deps.get(k, 0) < v:
                        deps[k] = v
                for k, v in st[1].items():
                    if deps.get(k, 0) < v:
                        deps[k] = v
        return deps

    def _record(self, tok, R, W):
        k, v = tok
        for r in R:
            st = self.res.get(r)
            if st is None:
                st = [None, {}]
                self.res[r] = st
            if st[1].get(k, 0) < v:
                st[1][k] = v
        for w in W:
            self.res[w] = [tok, {}]

    def _emit_waits(self, eng, deps):
        kn = self.known[eng]
        for k, v in deps.items():
            if k == eng and eng == "pe":
                continue
            if kn.get(k, 0) >= v:
                continue
            kn[k] = v
            self.q[eng].append(("w", k, v))
            self.n_wait += 1

    def op(self, eng, fn, R=(), W=()):
        deps = self._deps(R, W)
        self._emit_waits(eng, deps)
        self.cnt[eng] += 1
        tok = (eng, self.cnt[eng])
        self.q[eng].append(("o", fn, eng, 1))
        self._record(tok, R, W)
        self.n_op += 1
        return tok

    def dma(self, eng, out, in_, R=(), W=(), **kw):
        deps = self._deps(R, W)
        j = self.drr[eng]
        self.drr[eng] = (j + 1) % NDSEM
        key = ("d", eng, j)
        prev = self.dcnt.get(key, 0)
        if prev:
            deps[key] = max(deps.get(key, 0), prev * 16)
        self._emit_waits(eng, deps)
        self.dcnt[key] = prev + 1
        tok = (key, (prev + 1) * 16)
        self.q[eng].append(("o", lambda e: e.dma_start(out=out, in_=in_, **kw), key, 16))
        self._record(tok, R, W)
        self.n_op += 1
        return tok

    def finish(self, eng="sp"):
        deps = {k: c * 16 for k, c in self.dcnt.items()}
        self._emit_waits(eng, deps)

    def emit(self):
        nc = self.nc
        with contextlib.ExitStack() as es:
            keys = list(COMPUTE) + list(self.dcnt.keys())
            for k in keys:
                nm = k if isinstance(k, str) else "d_%s_%d" % (k[1], k[2])
                self.sems[k] = es.enter_context(nc.semaphore("s_" + nm))
            block = es.enter_context(nc.Block())
            handles = {"pe": block.tensor, "act": block.scalar, "dve": block.vector,
                       "pool": block.gpsimd, "sp": block.sync}
            sems = self.sems
            for eng in self.engs:
                items = self.q[eng]

                def body(e, items=items):
                    for it in items:
                        if it[0] == "w":
                            e.wait_ge(sems[it[1]], it[2])
                        else:
                            it[1](e).then_inc(sems[it[2]], it[3])
                handles[eng](body)


class Rot:
    def __init__(self, nc, name, shape, dtype, n, psum=False):
        self.tiles = []
        for i in range(n):
            if psum:
                t = nc.alloc_psum_tensor("%s%d" % (name, i), shape, dtype)
            else:
                t = nc.alloc_sbuf_tensor("%s%d" % (name, i), shape, dtype)
            self.tiles.append(t)
        self.name = name
        self.i = 0

    def next(self):
        i = self.i % len(self.tiles)
        self.i += 1
        return self.tiles[i], (self.name, i)


class Ctx:
    def __init__(self, nc, n_ps=8):
        self.nc = nc
        self.P = Prog(nc)
        P = self.P
        self.ps = Rot(nc, "ps", [128, 512], F32, n_ps, psum=True)
        self.ones_bf = nc.alloc_sbuf_tensor("ones_bf", [128, 128], BF16)
        self.ones_f = nc.alloc_sbuf_tensor("ones_f", [128, 128], F32)
        self.ident_f = nc.alloc_sbuf_tensor("ident_f", [128, 128], F32)
        self.eps_t = nc.alloc_sbuf_tensor("eps_t", [128, 1], F32)
        P.op("pool", lambda e: e.memset(self.ones_bf[:], 1.0), W=["ones_bf"])
        P.op("pool", lambda e: e.memset(self.ones_f[:], 1.0), W=["ones_f"])
        P.op("pool", lambda e: e.memset(self.eps_t[:], EPS), W=["eps_t"])
        P.op("pool", lambda e: e.memset(self.ident_f[:], 1.0), W=["ident_f"])
        P.op("pool", lambda e: e.affine_select(out=self.ident_f[:], in_=self.ident_f[:], pattern=[[-1, 128]],
                                               compare_op=ALU.is_equal, fill=0.0, base=0, channel_multiplier=1),
             R=["ident_f"], W=["ident_f"])
        self.sq = Rot(nc, "sq", [128, 8, 512], BF16, 1)
        self.rt = Rot(nc, "rt", [128, 512], F32, 2)


def emit_rmsnorm_fm(C, hT, hkeys, nk, tok0, ntok, gain_sb, gkey, xnT, xkeys, xtok0, D, npart=128):
    P = C.P
    sq, sqk = C.sq.next()
    P.op("act", lambda e: e.activation(out=sq[:npart, 0:nk, 0:ntok], in_=hT[:npart, 0:nk, tok0:tok0 + ntok], func=AF.Square),
         R=list(hkeys), W=[sqk])
    ps, psk = C.ps.next()
    for k in range(nk):
        P.op("pe", lambda e, k=k: e.matmul(ps[:npart, 0:ntok], lhsT=C.ones_bf[:npart, :npart], rhs=sq[:npart, k, 0:ntok],
                                          start=(k == 0), stop=(k == nk - 1)),
             R=[sqk, "ones_bf"], W=[psk])
    rt, rtk = C.rt.next()
    P.op("act", lambda e: e.activation(out=rt[:npart, 0:ntok], in_=ps[:npart, 0:ntok], func=AF.Sqrt,
                                       bias=C.eps_t[:npart, 0:1], scale=1.0 / D),
         R=[psk, "eps_t"], W=[rtk])
    P.op("dve", lambda e: e.reciprocal(out=rt[:npart, 0:ntok], in_=rt[:npart, 0:ntok]), R=[rtk], W=[rtk])
    for k in range(nk):
        P.op("dve", lambda e, k=k: e.scalar_tensor_tensor(out=xnT[:npart, k, xtok0:xtok0 + ntok],
                                                          in0=hT[:npart, k, tok0:tok0 + ntok],
                                                          scalar=gain_sb[:npart, k:k + 1], in1=rt[:npart, 0:ntok],
                                                          op0=ALU.mult, op1=ALU.mult),
             R=[hkeys[k], rtk, gkey], W=[xkeys[k]])


def load_vec_fm(C, name, dram_vec_ap, n):
    nk = max(1, n // 128)
    npart = min(128, n)
    t = C.nc.alloc_sbuf_tensor(name, [128, nk], F32)
    C.P.dma("sp", t[:npart, :], dram_vec_ap.rearrange("(k p) -> p k", p=npart), W=[name], allow_slow_non_contiguous=True)
    return t


def emit_ffn(C, xnT, xkeys, hT, hkeys, T, wg, wu, wd, pools, gate_bc=None):
    P = C.P
    NTG = T // 512
    hidT, hidkeys = pools["hidT"], pools["hidkeys"]
    for wb in range(7):
        wgt, wgk = pools["wgu"].next()
        P.dma("pool", wgt[:], wg[:, wb * 512:(wb + 1) * 512].rearrange("(k p) n -> p k n", p=128), W=[wgk])
        wut, wuk = pools["wgu"].next()
        P.dma("pool", wut[:], wu[:, wb * 512:(wb + 1) * 512].rearrange("(k p) n -> p k n", p=128), W=[wuk])
        for m in range(4):
            mm = wb * 4 + m
            for tg in range(NTG):
                pg, pgk = C.ps.next()
                for k in range(8):
                    P.op("pe", lambda e, k=k, pg=pg, wgt=wgt, m=m, tg=tg: e.matmul(
                        pg[:, :], lhsT=wgt[:, k, m * 128:(m + 1) * 128], rhs=xnT[:, k, tg * 512:(tg + 1) * 512],
                        start=(k == 0), stop=(k == 7)), R=[wgk, xkeys(k, tg)], W=[pgk])
                pu, puk = C.ps.next()
                for k in range(8):
                    P.op("pe", lambda e, k=k, pu=pu, wut=wut, m=m, tg=tg: e.matmul(
                        pu[:, :], lhsT=wut[:, k, m * 128:(m + 1) * 128], rhs=xnT[:, k, tg * 512:(tg + 1) * 512],
                        start=(k == 0), stop=(k == 7)), R=[wuk, xkeys(k, tg)], W=[puk])
                sg, sgk = pools["sg"].next()
                P.op("act", lambda e, sg=sg, pg=pg: e.activation(out=sg[:, :], in_=pg[:, :], func=AF.Silu), R=[pgk], W=[sgk])
                P.op("dve", lambda e, sg=sg, pu=pu, mm=mm, tg=tg: e.tensor_tensor(
                    out=hidT[:, mm, tg * 512:(tg + 1) * 512], in0=sg[:, :], in1=pu[:, :], op=ALU.mult),
                    R=[sgk, puk], W=[hidkeys[mm] + (tg,)])
    for dm in range(8):
        wdt, wdk = pools["wd"].next()
        P.dma("pool", wdt[:], wd[:, dm * 128:(dm + 1) * 128].rearrange("(k p) n -> p k n", p=128), W=[wdk])
        for tg in range(NTG):
            po, pok = C.ps.next()
            for k in range(28):
                P.op("pe", lambda e, k=k, po=po, wdt=wdt, tg=tg: e.matmul(
                    po[:, :], lhsT=wdt[:, k, :], rhs=hidT[:, k, tg * 512:(tg + 1) * 512],
                    start=(k == 0), stop=(k == 27)), R=[wdk, hidkeys[k] + (tg,)], W=[pok])
            if gate_bc is None:
                P.op("dve", lambda e, po=po, dm=dm, tg=tg: e.tensor_tensor(
                    out=hT[:, dm, tg * 512:(tg + 1) * 512], in0=hT[:, dm, tg * 512:(tg + 1) * 512], in1=po[:, :], op=ALU.add),
                    R=[pok, hkeys(dm, tg)], W=[hkeys(dm, tg)])
            else:
                gt, gkf = gate_bc
                tmp, tmpk = pools["sg"].next()
                P.op("dve", lambda e, po=po, tmp=tmp, gt=gt, tg=tg: e.tensor_tensor(
                    out=tmp[:, :], in0=po[:, :], in1=gt[:, tg * 512:(tg + 1) * 512], op=ALU.mult),
                    R=[pok, gkf(tg)], W=[tmpk])
                P.op("pool", lambda e, tmp=tmp, dm=dm, tg=tg: e.tensor_tensor(
                    out=hT[:, dm, tg * 512:(tg + 1) * 512], in0=hT[:, dm, tg * 512:(tg + 1) * 512], in1=tmp[:, :], op=ALU.add),
                    R=[tmpk, hkeys(dm, tg)], W=[hkeys(dm, tg)])


def ffn_pools(nc, T):
    return {
        "hidT": nc.alloc_sbuf_tensor("hidT", [128, 28, T], BF16),
        "hidkeys": [("hid", k) for k in range(28)],
        "wgu": Rot(nc, "wgu", [128, 8, 512], BF16, 4),
        "wd": Rot(nc, "wd", [128, 28, 128], BF16, 2),
        "sg": Rot(nc, "sg", [128, 512], F32, 3),
    }


def emit_load_tm_to_fm(C, src_dram, hT, hkeys, ntiles, stage):
    P = C.P
    for t in range(ntiles):
        st, stk = stage.next()
        P.dma("sp", st[:], src_dram[t * 128:(t + 1) * 128, :], W=[stk])
        for half in range(2):
            ps, psk = C.ps.next()
            for kk in range(4):
                k = half * 4 + kk
                P.op("pe", lambda e, ps=ps, st=st, k=k, kk=kk: e.transpose(out=ps[:, kk * 128:(kk + 1) * 128], in_=st[:, k * 128:(k + 1) * 128],
                                                                           identity=C.ident_f[:]),
                     R=[stk, "ident_f"], W=[psk])
            P.op("dve" if half == 0 else "act",
                 (lambda e, ps=ps, half=half, t=t: e.tensor_copy(out=hT[:, half * 4:half * 4 + 4, t * 128:(t + 1) * 128],
                                                                 in_=ps[:, :].rearrange("p (k n) -> p k n", k=4))) if half == 0 else
                 (lambda e, ps=ps, half=half, t=t: e.copy(out=hT[:, half * 4:half * 4 + 4, t * 128:(t + 1) * 128],
                                                          in_=ps[:, :].rearrange("p (k n) -> p k n", k=4))),
                 R=[psk], W=[hkeys(half * 4 + kk, t // 4) for kk in range(4)])


def emit_store_fm_to_tm(C, hT, hkeys, dst_dram, ntiles, stage):
    P = C.P
    for t in range(ntiles):
        st, stk = stage.next()
        for half in range(2):
            ps, psk = C.ps.next()
            for kk in range(4):
                k = half * 4 + kk
                P.op("pe", lambda e, ps=ps, k=k, kk=kk, t=t: e.transpose(out=ps[:, kk * 128:(kk + 1) * 128], in_=hT[:, k, t * 128:(t + 1) * 128],
                                                                         identity=C.ident_f[:]),
                     R=[hkeys(k, t // 4), "ident_f"], W=[psk])
            if half == 0:
                P.op("dve", lambda e, ps=ps, st=st: e.tensor_copy(out=st[:, 0:512], in_=ps[:, :]), R=[psk], W=[stk + (0,)])
            else:
                P.op("act", lambda e, ps=ps, st=st: e.copy(out=st[:, 512:1024], in_=ps[:, :]), R=[psk], W=[stk + (1,)])
        P.dma("sp", dst_dram[t * 128:(t + 1) * 128, :], st[:], R=[stk + (0,), stk + (1,)], W=[("out", t)])


def build_L3(T):
    nc = bass.Bass("TRN2", target_bir_lowering=False)
    x = nc.dram_tensor("x", [T, 1024], F32, kind="ExternalInput").ap()
    mT = nc.dram_tensor("mT", [1024, T], F32, kind="ExternalInput").ap()
    w_out = nc.dram_tensor("w_out", [1024, 1024], F32, kind="ExternalInput").ap()
    n1 = nc.dram_tensor("dffn_norm", [1024], F32, kind="ExternalInput").ap()
    wg = nc.dram_tensor("wg", [1024, D_FF], F32, kind="ExternalInput").ap()
    wu = nc.dram_tensor("wu", [1024, D_FF], F32, kind="ExternalInput").ap()
    wd = nc.dram_tensor("wd", [D_FF, 1024], F32, kind="ExternalInput").ap()
    n2 = nc.dram_tensor("ssm_norm", [1024], F32, kind="ExternalInput").ap()
    w_sin = nc.dram_tensor("w_sin", [1024, 512], F32, kind="ExternalInput").ap()
    h2T = nc.dram_tensor("h2T", [1024, T], F32, kind="ExternalOutput").ap()
    uT = nc.dram_tensor("uT", [512, T], F32, kind="ExternalOutput").ap()
    C = Ctx(nc)
    P = C.P
    TG = 1024
    hT = nc.alloc_sbuf_tensor("hT", [128, 8, TG], F32)
    xnT = nc.alloc_sbuf_tensor("xnT", [128, 8, TG], BF16)
    pools = ffn_pools(nc, TG)
    stage = Rot(nc, "stage", [128, 1024], F32, 2)
    mts = Rot(nc, "mts", [128, 8, 512], BF16, 2)
    uo = Rot(nc, "uo", [128, 512], F32, 2)
    g1 = load_vec_fm(C, "g1", n1, 1024)
    g2 = load_vec_fm(C, "g2", n2, 1024)
    hk = lambda k, tg: ("h", k, tg)
    xk = lambda k, tg: ("xn", k, tg)
    for grp in range(T // TG):
        t0 = grp * TG
        emit_load_tm_to_fm(C, x[t0:t0 + TG, :], hT, hk, TG // 128, stage)
        wo = []
        for hf in range(2):
            wt, wk = pools["wgu"].next()
            P.dma("pool", wt[:], w_out[:, hf * 512:(hf + 1) * 512].rearrange("(k p) n -> p k n", p=128), W=[wk])
            wo.append((wt, wk))
        for tg in range(TG // 512):
            mt, mk = mts.next()
            P.dma("pool", mt[:], mT[:, t0 + tg * 512:t0 + (tg + 1) * 512].rearrange("(k p) n -> p k n", p=128), W=[mk])
            for dm in range(8):
                wt, wk = wo[dm // 4]
                po, pok = C.ps.next()
                for k in range(8):
                    P.op("pe", lambda e, k=k, po=po, wt=wt, mt=mt, dm=dm: e.matmul(
                        po[:, :], lhsT=wt[:, k, (dm % 4) * 128:(dm % 4 + 1) * 128], rhs=mt[:, k, :],
                        start=(k == 0), stop=(k == 7)), R=[wk, mk], W=[pok])
                P.op("dve", lambda e, po=po, dm=dm, tg=tg: e.tensor_tensor(
                    out=hT[:, dm, tg * 512:(tg + 1) * 512], in0=hT[:, dm, tg * 512:(tg + 1) * 512], in1=po[:, :], op=ALU.add),
                    R=[pok, hk(dm, tg)], W=[hk(dm, tg)])
        for tg in range(TG // 512):
            emit_rmsnorm_fm(C, hT, [hk(k, tg) for k in range(8)], 8, tg * 512, 512, g1, "g1",
                            xnT, [xk(k, tg) for k in range(8)], tg * 512, 1024)
        emit_ffn(C, xnT, xk, hT, hk, TG, wg, wu, wd, pools)
        for k in range(8):
            P.dma("sp", h2T[k * 128:(k + 1) * 128, t0:t0 + TG], hT[:, k, :], R=[hk(k, tg) for tg in range(TG // 512)], W=[("h2o", k)])
        for tg in range(TG // 512):
            emit_rmsnorm_fm(C, hT, [hk(k, tg) for k in range(8)], 8, tg * 512, 512, g2, "g2",
                            xnT, [xk(k, tg) for k in range(8)], tg * 512, 1024)
        wt, wk = pools["wgu"].next()
        P.dma("pool", wt[:], w_sin.rearrange("(k p) n -> p k n", p=128), W=[wk])
        for tg in range(TG // 512):
            for c in range(4):
                po, pok = C.ps.next()
                for k in range(8):
                    P.op("pe", lambda e, k=k, po=po, wt=wt, c=c, tg=tg: e.matmul(
                        po[:, :], lhsT=wt[:, k, c * 128:(c + 1) * 128], rhs=xnT[:, k, tg * 512:(tg + 1) * 512],
                        start=(k == 0), stop=(k == 7)), R=[wk, xk(k, tg)], W=[pok])
                ut, uk = uo.next()
                P.op("act", lambda e, ut=ut, po=po: e.copy(out=ut[:, :], in_=po[:, :]), R=[pok], W=[uk])
                P.dma("sp", uT[c * 128:(c + 1) * 128, t0 + tg * 512:t0 + (tg + 1) * 512], ut[:, :], R=[uk], W=[("uo", c, tg, grp)])
    P.finish("sp")
    P.emit()
    return nc


def build_L5(T, n_exp=8):
    nc = bass.Bass("TRN2", target_bir_lowering=False)
    h2T = nc.dram_tensor("h2T", [1024, T], F32, kind="ExternalInput").ap()
    yT = nc.dram_tensor("yT", [512, T], F32, kind="ExternalInput").ap()
    w_glu = nc.dram_tensor("w_glu", [512, 2048], F32, kind="ExternalInput").ap()
    n1 = nc.dram_tensor("moe_norm", [1024], F32, kind="ExternalInput").ap()
    w_r = nc.dram_tensor("w_r", [1024, 8], F32, kind="ExternalInput").ap()
    wg = nc.dram_tensor("wg", [8, 1024, D_FF], F32, kind="ExternalInput").ap()
    wu = nc.dram_tensor("wu", [8, 1024, D_FF], F32, kind="ExternalInput").ap()
    wd = nc.dram_tensor("wd", [8, D_FF, 1024], F32, kind="ExternalInput").ap()
    out = nc.dram_tensor("out", [T, 1024], F32, kind="ExternalOutput").ap()
    C = Ctx(nc)
    P = C.P
    TG = 1024
    NTG = TG // 512
    hT = nc.alloc_sbuf_tensor("hT", [128, 8, TG], F32)
    xnT = nc.alloc_sbuf_tensor("xnT", [128, 8, TG], BF16)
    pools = ffn_pools(nc, TG)
    stage = Rot(nc, "stage", [128, 1024], F32, 1)
    yts = Rot(nc, "yts", [128, 4, 512], BF16, 1)
    wglu = nc.alloc_sbuf_tensor("wglu", [128, 4, 2048], BF16)
    wrg = nc.alloc_sbuf_tensor("wrg", [128, 8, 8], F32)
    sel = nc.alloc_sbuf_tensor("sel", [8, 8, 128], F32)
    gT = nc.alloc_sbuf_tensor("gT", [8, TG], F32)
    gbc = nc.alloc_sbuf_tensor("gbc", [128, TG], F32)
    sm = Rot(nc, "sm", [128, 64], F32, 2)
    g1 = load_vec_fm(C, "g1", n1, 1024)
    P.dma("pool", wglu[:], w_glu.rearrange("(k p) n -> p k n", p=128), W=["wglu"])
    P.dma("sp", wrg[:], w_r.rearrange("(k p) n -> p k n", p=128), W=["wrg"], allow_slow_non_contiguous=True)
    for k in range(8):
        P.op("dve", lambda e, k=k: e.tensor_scalar(out=wrg[:, k, :], in0=wrg[:, k, :], scalar1=g1[:, k:k + 1], scalar2=None, op0=ALU.mult),
             R=["wrg", "g1"], W=["wrg"])
    P.op("pool", lambda e: e.memset(sel[:], 1.0), W=["sel"])
    P.op("pool", lambda e: e.affine_select(out=sel[:], in_=sel[:], pattern=[[-1, 8], [0, 128]], compare_op=ALU.is_equal, fill=0.0,
                                           base=0, channel_multiplier=1), R=["sel"], W=["sel"])
    hk = lambda k, tg: ("h", k, tg)
    xk = lambda k, tg: ("xn", k, tg)
    for grp in range(T // TG):
        t0 = grp * TG
        for k in range(8):
            P.dma("sp", hT[:, k, :], h2T[k * 128:(k + 1) * 128, t0:t0 + TG], W=[hk(k, tg) for tg in range(NTG)])
        for tg in range(NTG):
            yt, yk = yts.next()
            P.dma("pool", yt[:], yT[:, t0 + tg * 512:t0 + (tg + 1) * 512].rearrange("(k p) n -> p k n", p=128), W=[yk])
            for dm in range(8):
                p1, p1k = C.ps.next()
                for k in range(4):
                    P.op("pe", lambda e, k=k, p1=p1, yt=yt, dm=dm: e.matmul(p1[:, :], lhsT=wglu[:, k, dm * 128:(dm + 1) * 128], rhs=yt[:, k, :],
                                                                          start=(k == 0), stop=(k == 3)), R=["wglu", yk], W=[p1k])
                p2, p2k = C.ps.next()
                for k in range(4):
                    P.op("pe", lambda e, k=k, p2=p2, yt=yt, dm=dm: e.matmul(p2[:, :], lhsT=wglu[:, k, 1024 + dm * 128:1024 + (dm + 1) * 128], rhs=yt[:, k, :],
                                                                          start=(k == 0), stop=(k == 3)), R=["wglu", yk], W=[p2k])
                sg, sgk = pools["sg"].next()
                P.op("act", lambda e, sg=sg, p2=p2: e.activation(out=sg[:, :], in_=p2[:, :], func=AF.Sigmoid), R=[p2k], W=[sgk])
                P.op("dve", lambda e, sg=sg, p1=p1: e.tensor_tensor(out=sg[:, :], in0=sg[:, :], in1=p1[:, :], op=ALU.mult), R=[sgk, p1k], W=[sgk])
                P.op("pool", lambda e, sg=sg, dm=dm, tg=tg: e.tensor_tensor(out=hT[:, dm, tg * 512:(tg + 1) * 512], in0=hT[:, dm, tg * 512:(tg + 1) * 512],
                                                                        in1=sg[:, :], op=ALU.add), R=[sgk, hk(dm, tg)], W=[hk(dm, tg)])
        for tg in range(NTG):
            emit_rmsnorm_fm(C, hT, [hk(k, tg) for k in range(8)], 8, tg * 512, 512, g1, "g1",
                            xnT, [xk(k, tg) for k in range(8)], tg * 512, 1024)
        for tt in range(TG // 128):
            tg = tt // 4
            pl, plk = C.ps.next()
            for k in range(8):
                P.op("pe", lambda e, k=k, pl=pl, tt=tt: e.matmul(pl[:, 0:8], lhsT=hT[:, k, tt * 128:(tt + 1) * 128], rhs=wrg[:, k, :],
                                                             start=(k == 0), stop=(k == 7)), R=[hk(k, tg), "wrg"], W=[plk])
            pss, pssk = C.ps.next()
            sq, sqk = C.sq.next()
            P.op("act", lambda e, sq=sq, tt=tt: e.activation(out=sq[:, :, 0:128], in_=hT[:, :, tt * 128:(tt + 1) * 128], func=AF.Square),
                 R=[hk(k, tg) for k in range(8)], W=[sqk])
            for k in range(8):
                P.op("pe", lambda e, k=k, pss=pss, sq=sq: e.matmul(pss[:, 0:1], lhsT=sq[:, k, 0:128], rhs=C.ones_bf[:, 0:1],
                                                               start=(k == 0), stop=(k == 7)), R=[sqk, "ones_bf"], W=[pssk])
            s, sk = sm.next()
            P.op("act", lambda e, s=s, pss=pss: e.activation(out=s[:, 0:1], in_=pss[:, 0:1], func=AF.Sqrt, bias=C.eps_t[:, 0:1], scale=1.0 / 1024),
                 R=[pssk, "eps_t"], W=[sk])
            P.op("dve", lambda e, s=s: e.reciprocal(out=s[:, 0:1], in_=s[:, 0:1]), R=[sk], W=[sk])
            P.op("dve", lambda e, s=s, pl=pl: e.tensor_scalar(out=s[:, 8:16], in0=pl[:, 0:8], scalar1=s[:, 0:1], scalar2=None, op0=ALU.mult),
                 R=[sk, plk], W=[sk])
            P.op("dve", lambda e, s=s: e.max(out=s[:, 16:24], in_=s[:, 8:16]), R=[sk], W=[sk])
            P.op("dve", lambda e, s=s: e.tensor_scalar(out=s[:, 24:25], in0=s[:, 16:17], scalar1=-1.0, scalar2=None, op0=ALU.mult), R=[sk], W=[sk])
            P.op("act", lambda e, s=s: e.activation(out=s[:, 32:40], in_=s[:, 8:16], func=AF.Exp, bias=s[:, 24:25], scale=1.0), R=[sk], W=[sk])
            P.op("dve", lambda e, s=s: e.tensor_scalar(out=s[:, 40:48], in0=s[:, 8:16], scalar1=s[:, 17:18], scalar2=None, op0=ALU.is_ge), R=[sk], W=[sk])
            P.op("dve", lambda e, s=s: e.tensor_tensor(out=s[:, 32:40], in0=s[:, 32:40], in1=s[:, 40:48], op=ALU.mult), R=[sk], W=[sk])
            P.op("dve", lambda e, s=s: e.reduce_sum(out=s[:, 48:49], in_=s[:, 32:40], axis=AX.X), R=[sk], W=[sk])
            P.op("dve", lambda e, s=s: e.reciprocal(out=s[:, 48:49], in_=s[:, 48:49]), R=[sk], W=[sk])
            P.op("dve", lambda e, s=s: e.tensor_scalar(out=s[:, 32:40], in0=s[:, 32:40], scalar1=s[:, 48:49], scalar2=None, op0=ALU.mult), R=[sk], W=[sk])
            pt, ptk = C.ps.next()
            P.op("pe", lambda e, pt=pt, s=s: e.transpose(out=pt[0:8, 0:128], in_=s[:, 32:40], identity=C.ident_f[:]), R=[sk, "ident_f"], W=[ptk])
            P.op("act", lambda e, pt=pt, tt=tt: e.copy(out=gT[0:8, tt * 128:(tt + 1) * 128], in_=pt[0:8, 0:128]), R=[ptk], W=[("gT", tg)])
        for ex in range(n_exp):
            for tg in range(NTG):
                pb, pbk = C.ps.next()
                P.op("pe", lambda e, pb=pb, ex=ex, tg=tg: e.matmul(pb[:, :], lhsT=sel[0:8, ex, :], rhs=gT[0:8, tg * 512:(tg + 1) * 512],
                                                               start=True, stop=True), R=["sel", ("gT", tg)], W=[pbk])
                P.op("act", lambda e, pb=pb, tg=tg: e.copy(out=gbc[:, tg * 512:(tg + 1) * 512], in_=pb[:, :]), R=[pbk], W=[("gbc", tg)])
            emit_ffn(C, xnT, xk, hT, hk, TG, wg[ex], wu[ex], wd[ex], pools, gate_bc=(gbc, lambda tg: ("gbc", tg)))
        emit_store_fm_to_tm(C, hT, hk, out[t0:t0 + TG, :], TG // 128, stage)
    P.finish("sp")
    P.emit()
    return nc


def build_L1(T):
    nc = bass.Bass("TRN2", target_bir_lowering=False)
    dt = lambda n, s, k="ExternalInput": nc.dram_tensor(n, s, F32, kind=k).ap()
    x = dt("x", [T, 1024])
    n0 = dt("att_norm", [1024])
    w_in = dt("w_in", [1024, 1952])
    nq = dt("q_lat_norm", [256])
    w_qup = dt("w_q_up", [256, 768])
    nkv = dt("kv_lat_norm", [128])
    w_kvup = dt("w_kv_up", [128, 1024])
    gq = dt("q_norm", [96])
    gk = dt("k_norm", [96])
    cq_t = dt("cq_t", [96, T]); sq_t = dt("sq_t", [96, T])
    ck_t = dt("ck_t", [96, T]); sk_t = dt("sk_t", [96, T])
    pm = dt("pm", [96, 96])
    sbqT = dt("sbqT", [512, T], "ExternalOutput")
    sbkT = dt("sbkT", [512, T], "ExternalOutput")
    sbv = dt("sbv", [T, 512], "ExternalOutput")
    mqT = dt("mqT", [8, 96, T], "ExternalOutput")
    mkT = dt("mkT", [8, 96, T], "ExternalOutput")
    mv = dt("mv", [T, 512], "ExternalOutput")
    C = Ctx(nc)
    P = C.P
    A = nc.alloc_sbuf_tensor
    hT = A("hT", [128, 8, 512], F32)
    xnT = A("xnT", [128, 8, 512], BF16)
    win = A("win", [128, 8, 1952], BF16)
    wkr = A("wkr", [128, 8, 96], BF16)
    wqup = A("wqup", [128, 2, 768], BF16)
    wkn = A("wkn", [128, 8, 96], BF16)
    wkv = A("wkv", [128, 8, 64], BF16)
    pmt = A("pmt", [96, 96], BF16)
    lat = A("lat", [128, 3, 512], F32)
    latn = A("latn", [128, 3, 512], BF16)
    krp = A("krp", [96, 512], F32)
    hr = A("hr", [96, 1, 512], F32)
    hn = A("hn", [96, 1, 512], BF16)
    tabs = A("tabs", [96, 4, 512], F32)
    t1 = Rot(nc, "t1", [96, 512], F32, 2)
    t2 = Rot(nc, "t2", [96, 512], F32, 2)
    ob = Rot(nc, "ob", [128, 512], F32, 3)
    stage = Rot(nc, "stage", [128, 1024], F32, 2)
    g0 = load_vec_fm(C, "g0", n0, 1024)
    gql = load_vec_fm(C, "gql", nq, 256)
    gkl = load_vec_fm(C, "gkl", nkv, 128)
    gqh = load_vec_fm(C, "gqh", gq, 96)
    gkh = load_vec_fm(C, "gkh", gk, 96)
    P.dma("pool", win[:], w_in.rearrange("(k p) n -> p k n", p=128), W=["win"])
    P.op("pool", lambda e: e.memset(wkr[:], 0.0), W=["wkr"])
    P.dma("pool", wkr[:, :, 64:96], w_in[:, 1920:1952].rearrange("(k p) n -> p k n", p=128), R=["wkr"], W=["wkr"])
    P.dma("pool", wqup[:], w_qup.rearrange("(k p) n -> p k n", p=128), W=["wqup"])
    P.op("pool", lambda e: e.memset(wkn[:], 0.0), W=["wkn"])
    P.dma("pool", wkn[:, :, 0:64], w_kvup.rearrange("k (h c) -> k h c", c=128)[:, :, 0:64], R=["wkn"], W=["wkn"])
    P.dma("pool", wkv[:], w_kvup.rearrange("k (h c) -> k h c", c=128)[:, :, 64:128], W=["wkv"])
    P.dma("pool", pmt[:], pm, W=["pmt"])
    hk = lambda k, tg: ("h", k)
    for tg in range(T // 512):
        t0 = tg * 512
        emit_load_tm_to_fm(C, x[t0:t0 + 512, :], hT, hk, 4, stage)
        emit_rmsnorm_fm(C, hT, [hk(k, 0) for k in range(8)], 8, 0, 512, g0, "g0", xnT, [("xn", k) for k in range(8)], 0, 1024)
        XR = [("xn", k) for k in range(8)]
        for i, tab in enumerate((cq_t, sq_t, ck_t, sk_t)):
            P.dma("sp", tabs[:, i, :], tab[:, t0:t0 + 512], W=[("tabs", i)])

        def proj_fm(col0, ncols, wt=win, wkey="win"):
            po, pok = C.ps.next()
            for k in range(8):
                P.op("pe", lambda e, k=k, po=po: e.matmul(po[0:ncols, :], lhsT=wt[:, k, col0:col0 + ncols], rhs=xnT[:, k, :],
                                                          start=(k == 0), stop=(k == 7)), R=[wkey] + XR, W=[pok])
            return po, pok
        for c in range(8):
            po, pok = proj_fm(c * 128, 128)
            o, okk = ob.next()
            P.op("act", lambda e, o=o, po=po: e.copy(out=o[:, :], in_=po[:, :]), R=[pok], W=[okk])
            dst = sbqT if c < 4 else sbkT
            P.dma("sp", dst[(c % 4) * 128:(c % 4 + 1) * 128, t0:t0 + 512], o[:, :], R=[okk], W=[("o1", c, tg)])
        for tt in range(4):
            po, pok = C.ps.next()
            for k in range(8):
                P.op("pe", lambda e, k=k, po=po, tt=tt: e.matmul(po[:, :], lhsT=xnT[:, k, tt * 128:(tt + 1) * 128], rhs=win[:, k, 1024:1536],
                                                             start=(k == 0), stop=(k == 7)), R=["win"] + XR, W=[pok])
            o, okk = ob.next()
            P.op("dve", lambda e, o=o, po=po: e.tensor_copy(out=o[:, :], in_=po[:, :]), R=[pok], W=[okk])
            P.dma("sp", sbv[t0 + tt * 128:t0 + (tt + 1) * 128, :], o[:, :], R=[okk], W=[("o2", tt, tg)])
        for c in range(3):
            po, pok = proj_fm(1536 + c * 128, 128)
            P.op("act", lambda e, po=po, c=c: e.copy(out=lat[:, c, :], in_=po[:, :]), R=[pok], W=[("lat", c)])
        emit_rmsnorm_fm(C, lat, [("lat", 0), ("lat", 1)], 2, 0, 512, gql, "gql", latn, [("latn", 0), ("latn", 1)], 0, 256)
        emit_rmsnorm_fm(C, lat[:, 2:3, :], [("lat", 2)], 1, 0, 512, gkl, "gkl", latn[:, 2:3, :], [("latn", 2)], 0, 128)
        po, pok = proj_fm(0, 96, wt=wkr, wkey="wkr")
        P.op("act", lambda e, po=po: e.copy(out=krp[:, :], in_=po[0:96, :]), R=[pok], W=["krp"])
        for tt in range(4):
            po, pok = C.ps.next()
            P.op("pe", lambda e, po=po, tt=tt: e.matmul(po[:, :], lhsT=latn[:, 2, tt * 128:(tt + 1) * 128], rhs=wkv[:, :, :],
                                                    start=True, stop=True), R=["wkv", ("latn", 2)], W=[pok])
            o, okk = ob.next()
            P.op("dve", lambda e, o=o, po=po: e.tensor_copy(out=o[:, :], in_=po[:, :]), R=[pok], W=[okk])
            P.dma("sp", mv[t0 + tt * 128:t0 + (tt + 1) * 128, :], o[:, :], R=[okk], W=[("o3", tt, tg)])
        for h in range(8):
            for which in range(2):
                po, pok = C.ps.next()
                if which == 0:
                    for k in range(2):
                        P.op("pe", lambda e, k=k, po=po, h=h: e.matmul(po[0:96, :], lhsT=wqup[:, k, h * 96:(h + 1) * 96], rhs=latn[:, k, :],
                                                                   start=(k == 0), stop=(k == 1)), R=["wqup", ("latn", 0), ("latn", 1)], W=[pok])
                    P.op("act", lambda e, po=po: e.copy(out=hr[:, 0, :], in_=po[0:96, :]), R=[pok], W=["hr"])
                else:
                    P.op("pe", lambda e, po=po, h=h: e.matmul(po[0:96, :], lhsT=wkn[:, h, :], rhs=latn[:, 2, :], start=True, stop=True),
                         R=["wkn", ("latn", 2)], W=[pok])
                    P.op("dve", lambda e, po=po: e.tensor_tensor(out=hr[:, 0, :], in0=po[0:96, :], in1=krp[:, :], op=ALU.add),
                         R=[pok, "krp"], W=["hr"])
                emit_rmsnorm_fm(C, hr, ["hr"], 1, 0, 512, gqh if which == 0 else gkh, "gqh" if which == 0 else "gkh",
                                hn, ["hn"], 0, 96, npart=96)
                pp, ppk = C.ps.next()
                P.op("pe", lambda e, pp=pp: e.matmul(pp[0:96, :], lhsT=pmt[:, :], rhs=hn[:, 0, :], start=True, stop=True), R=["pmt", "hn"], W=[ppk])
                a, ak = t1.next()
                b, bk = t2.next()
                ci, si = (0, 1) if which == 0 else (2, 3)
                P.op("pool", lambda e, a=a, ci=ci: e.tensor_tensor(out=a[:, :], in0=hn[:, 0, :], in1=tabs[:, ci, :], op=ALU.mult),
                     R=["hn", ("tabs", ci)], W=[ak])
                P.op("dve", lambda e, b=b, pp=pp, si=si: e.tensor_tensor(out=b[:, :], in0=pp[0:96, :], in1=tabs[:, si, :], op=ALU.mult),
                     R=[ppk, ("tabs", si)], W=[bk])
                P.op("pool", lambda e, a=a, b=b: e.tensor_tensor(out=a[:, :], in0=a[:, :], in1=b[:, :], op=ALU.add), R=[ak, bk], W=[ak])
                dst = mqT if which == 0 else mkT
                P.dma("sp", dst[h, :, t0:t0 + 512], a[:, :], R=[ak], W=[("o4", h, which, tg)])
    P.finish("sp")
    P.emit()
    return nc


def rope_tables(S):
    inv_freq = (10000.0 ** (-np.arange(0, 32, 2, dtype=np.float32) / np.float32(32))).astype(np.float32)
    ang = (np.arange(S, dtype=np.float32)[:, None] * inv_freq[None, :]).astype(np.float32)
    cos = np.cos(ang).astype(np.float32).T
    sin = np.sin(ang).astype(np.float32).T
    Ct = np.ones((96, S), np.float32); St = np.zeros((96, S), np.float32)
    Ct[64:80] = cos; Ct[80:96] = cos
    St[64:80] = sin; St[80:96] = sin
    pm = np.zeros((96, 96), np.float32)
    for i in range(16):
        pm[80 + i, 64 + i] = -1.0
        pm[64 + i, 80 + i] = 1.0
    return Ct, St, pm


def build_L2(S, n_sb=2, n_mla=2):
    nc = bass.Bass("TRN2", target_bir_lowering=False)
    dt = lambda n, s, k="ExternalInput": nc.dram_tensor(n, s, F32, kind=k).ap()
    sbqT = dt("sbqT", [2, 64, S]); sbkT = dt("sbkT", [2, 64, S]); sbv = dt("sbv", [2, S, 64])
    mqT = dt("mqT", [2, 96, S]); mkT = dt("mkT", [2, 96, S]); mv = dt("mv", [2, S, 64])
    oT = dt("oT", [4, 64, S], "ExternalOutput")
    C = Ctx(nc, n_ps=5)
    P = C.P
    A = nc.alloc_sbuf_tensor
    NB = S // 128
    NQG = S // 512
    acc = Rot(nc, "acc", [128, 512], F32, 2, psum=True)
    qT = A("qT", [96, S], BF16)
    kT = A("kT", [96, S], BF16)
    va = A("va", [128, NB, 65], BF16)
    mle = A("mle", [128, 4, 512], BF16)
    mlt = A("mlt", [128, 4, 512], BF16)
    uin = A("uin", [128, 128], BF16)
    et = Rot(nc, "et", [128, 512], F32, 3)
    spt = Rot(nc, "spt", [128, 512], BF16, 3)
    xt = Rot(nc, "xt", [128, 512], F32, 2)
    wt = Rot(nc, "wt", [128, 512], BF16, 3)
    Rt = Rot(nc, "Rt", [128, 512], BF16, 2)
    ot = Rot(nc, "ot", [128, 512], F32, 2)
    rr = A("rr", [128, 512], F32)
    bcs = A("bcs", [64, 512], F32)
    P.op("pool", lambda e: e.memset(va[:], 1.0), W=["va"])
    P.op("pool", lambda e: e.memset(mle[:], 1.0), W=["mle"])
    P.op("pool", lambda e: e.memset(mlt[:], 1.0), W=["mlt"])
    P.op("pool", lambda e: e.memset(uin[:], 1.0), W=["uin"])
    for d in range(4):
        P.op("pool", lambda e, d=d: e.affine_select(out=mle[:, d, :], in_=mle[:, d, :], pattern=[[1, 512]], compare_op=ALU.is_ge, fill=0.0,
                                                    base=-128 * d, channel_multiplier=-1), R=["mle"], W=["mle"])
        P.op("pool", lambda e, d=d: e.affine_select(out=mlt[:, d, :], in_=mlt[:, d, :], pattern=[[1, 512]], compare_op=ALU.is_gt, fill=0.0,
                                                    base=-128 * d, channel_multiplier=-1), R=["mlt"], W=["mlt"])
    P.op("pool", lambda e: e.affine_select(out=uin[:], in_=uin[:], pattern=[[-1, 128]], compare_op=ALU.is_ge, fill=0.0,
                                           base=0, channel_multiplier=1), R=["uin"], W=["uin"])
    for hd in range(n_sb + n_mla):
        is_sb = hd < n_sb
        hh = hd if is_sb else hd - n_sb
        dq = 64 if is_sb else 96
        qsrc, ksrc, vsrc = (sbqT, sbkT, sbv) if is_sb else (mqT, mkT, mv)
        for c4 in range(4):
            sl = slice(c4 * (S // 4), (c4 + 1) * (S // 4))
            P.dma("pool", qT[0:dq, sl], qsrc[hh, :, sl], W=[("qT", c4)])
            P.dma("pool", kT[0:dq, sl], ksrc[hh, :, sl], W=[("kT", c4)])
        P.dma("pool", va[:, :, 0:64], vsrc[hh].rearrange("(kb p) c -> p kb c", p=128), R=["va"], W=["va"])
        qkeys = lambda qg: [("qT", qg * 4 // NQG)]
        kkeys = lambda kb: [("kT", kb * 4 // NB)]
        for qg in range(NQG):
            nkb = 4 * (qg + 1)
            op_, opk = acc.next()
            if is_sb:
                Rprev = None
                for i, kb in enumerate(range(nkb - 1, -1, -1)):
                    d = kb - 4 * qg
                    zp, zpk = C.ps.next()
                    P.op("pe", lambda e, zp=zp, kb=kb, qg=qg: e.matmul(zp[:, :], lhsT=kT[0:64, kb * 128:(kb + 1) * 128], rhs=qT[0:64, qg * 512:(qg + 1) * 512],
                                                                   start=True, stop=True), R=qkeys(qg) + kkeys(kb), W=[zpk])
                    e_, ek = et.next()
                    P.op("act", lambda e, e_=e_, zp=zp: e.activation(out=e_[:, :], in_=zp[:, :], func=AF.Exp, scale=0.125), R=[zpk], W=[ek])
                    sp, spk = spt.next()
                    P.op("act", lambda e, e_=e_, sp=sp: e.activation(out=sp[:, :], in_=e_[:, :], func=AF.Ln, bias=C.ones_f[:, 0:1], scale=1.0),
                         R=[ek, "ones_f"], W=[spk])
                    if d >= 0:
                        P.op("pool", lambda e, sp=sp, d=d: e.tensor_tensor(out=sp[:, :], in0=sp[:, :], in1=mlt[:, d, :], op=ALU.mult),
                             R=[spk, "mlt"], W=[spk])
                    cs, csk = C.ps.next()
                    P.op("pe", lambda e, cs=cs, sp=sp, i=i: e.matmul(cs[:, :], lhsT=uin[:, :], rhs=sp[:, :], start=True, stop=(i == 0)),
                         R=[spk, "uin"], W=[csk])
                    if i > 0:
                        Rp, Rpk = Rprev
                        P.op("pe", lambda e, cs=cs, Rp=Rp: e.matmul(cs[:, :], lhsT=C.ones_bf[:, :], rhs=Rp[:, :], start=False, stop=True),
                             R=[Rpk, "ones_bf"], W=[csk])
                    x_, xk_ = xt.next()
                    P.op("act", lambda e, x_=x_, cs=cs: e.activation(out=x_[:, :], in_=cs[:, :], func=AF.Exp, scale=-1.0), R=[csk], W=[xk_])
                    w_, wk_ = wt.next()
                    P.op("dve", lambda e, w_=w_, e_=e_, x_=x_: e.tensor_tensor(out=w_[:, :], in0=e_[:, :], in1=x_[:, :], op=ALU.mult), R=[ek, xk_], W=[wk_])
                    if d >= 0:
                        P.op("dve", lambda e, w_=w_, d=d: e.tensor_tensor(out=w_[:, :], in0=w_[:, :], in1=mlt[:, d, :], op=ALU.mult),
                             R=[wk_, "mlt"], W=[wk_])
                    if kb > 0:
                        Rn, Rnk = Rt.next()
                        if i == 0:
                            P.op("pool", lambda e, Rn=Rn, sp=sp: e.tensor_copy(out=Rn[:, :], in_=sp[:, :]), R=[spk], W=[Rnk])
                        else:
                            Rp, Rpk = Rprev
                            P.op("pool", lambda e, Rn=Rn, Rp=Rp, sp=sp: e.tensor_tensor(out=Rn[:, :], in0=Rp[:, :], in1=sp[:, :], op=ALU.add),
                                 R=[spk, Rpk], W=[Rnk])
                        Rprev = (Rn, Rnk)
                    P.op("pe", lambda e, op_=op_, w_=w_, kb=kb, i=i: e.matmul(op_[0:64, :], lhsT=va[:, kb, 0:64], rhs=w_[:, :],
                                                                          start=(i == 0), stop=(kb == 0)), R=[wk_, "va"], W=[opk])
                o_, ok_ = ot.next()
                P.op("act", lambda e, o_=o_, op_=op_: e.copy(out=o_[0:64, :], in_=op_[0:64, :]), R=[opk], W=[ok_])
            else:
                for kb in range(nkb):
                    d = kb - 4 * qg
                    zp, zpk = C.ps.next()
                    P.op("pe", lambda e, zp=zp, kb=kb, qg=qg: e.matmul(zp[:, :], lhsT=kT[0:96, kb * 128:(kb + 1) * 128], rhs=qT[0:96, qg * 512:(qg + 1) * 512],
                                                                   start=True, stop=True), R=qkeys(qg) + kkeys(kb), W=[zpk])
                    w_, wk_ = wt.next()
                    P.op("act", lambda e, w_=w_, zp=zp: e.activation(out=w_[:, :], in_=zp[:, :], func=AF.Exp), R=[zpk], W=[wk_])
                    if d >= 0:
                        P.op("dve", lambda e, w_=w_, d=d: e.tensor_tensor(out=w_[:, :], in0=w_[:, :], in1=mle[:, d, :], op=ALU.mult),
                             R=[wk_, "mle"], W=[wk_])
                    P.op("pe", lambda e, op_=op_, w_=w_, kb=kb, nkb=nkb: e.matmul(op_[0:65, :], lhsT=va[:, kb, 0:65], rhs=w_[:, :],
                                                                              start=(kb == 0), stop=(kb == nkb - 1)), R=[wk_, "va"], W=[opk])
                P.op("dve", lambda e, op_=op_: e.reciprocal(out=rr[64:65, :], in_=op_[64:65, :]), R=[opk], W=["rr"])
                bc, bck = C.ps.next()
                P.op("pe", lambda e, bc=bc: e.matmul(bc[0:64, :], lhsT=C.ones_f[64:65, 0:64], rhs=rr[64:65, :], start=True, stop=True),
                     R=["rr", "ones_f"], W=[bck])
                P.op("act", lambda e, bc=bc: e.copy(out=bcs[:, :], in_=bc[0:64, :]), R=[bck], W=["bcs"])
                o_, ok_ = ot.next()
                P.op("dve", lambda e, o_=o_, op_=op_: e.tensor_tensor(out=o_[0:64, :], in0=op_[0:64, :], in1=bcs[:, :], op=ALU.mult),
                     R=[opk, "bcs"], W=[ok_])
            P.dma("sp", oT[hd, :, qg * 512:(qg + 1) * 512], o_[0:64, :], R=[ok_], W=[("oo", hd, qg)])
    P.finish("sp")
    P.emit()
    return nc


TWO_PI = 6.283185307179586
PI = 3.141592653589793
LCH = 512


def build_L4(S):
    nc = bass.Bass("TRN2", target_bir_lowering=False)
    dt = lambda n, s, k="ExternalInput": nc.dram_tensor(n, s, F32, kind=k).ap()
    uT = dt("uT", [128, S])
    a_re = dt("a_re", [128, 4]); a_im = dt("a_im", [128, 4]); ldt = dt("ldt", [128, 4])
    b_re = dt("b_re", [4, 128, 16]); b_im = dt("b_im", [4, 128, 16])
    ct_re = dt("ct_re", [4, 128, 16]); ct_im = dt("ct_im", [4, 128, 16])
    dsk = dt("dsk", [128])
    yT = dt("yT", [128, S], "ExternalOutput")
    C = Ctx(nc)
    P = C.P
    A = nc.alloc_sbuf_tensor
    NCH = S // LCH
    ub = A("ub", [128, S], BF16)
    P.dma("pool", ub[:], uT, W=["ub"])
    par = A("par", [128, 16, 4], F32)
    AR, AI, DT, ARD, TH, LRE, LIM, NUM, DEN, CRE, CIM, TMP, TMP2, MRE, MIM, NMIM = range(16)
    pk = lambda i: ("par", i)
    P.dma("sp", par[:, AR, :], a_re, W=[pk(AR)])
    P.dma("sp", par[:, AI, :], a_im, W=[pk(AI)])
    P.dma("sp", par[:, DT, :], ldt, W=[pk(DT)])
    dvec = load_vec_fm(C, "dvec", dsk, 128)
    cpi = A("cpi", [128, 1], F32)
    P.op("pool", lambda e: e.memset(cpi[:], PI), W=["cpi"])
    bst = A("bst", [128, 4, 4, 16], F32)
    for i, src in enumerate((b_re, b_im, ct_re, ct_im)):
        P.dma("sp", bst[:, i, :, :], src.rearrange("j p c -> p j c"), W=[("bst", i)], allow_slow_non_contiguous=True)
    io_i = A("io_i", [128, LCH], I32)
    io_f = A("io_f", [128, LCH], F32)
    P.op("pool", lambda e: e.iota(io_i[:], pattern=[[1, LCH]], base=0, channel_multiplier=0), W=["io_i"])
    P.op("dve", lambda e: e.tensor_copy(out=io_f[:], in_=io_i[:]), R=["io_i"], W=["io_f"])
    onesL = A("onesL", [128, LCH], F32)
    P.op("pool", lambda e: e.memset(onesL[:], 1.0), W=["onesL"])

    def ts(out, in0, s1, s2, o0, o1=None, R=(), W=()):
        if o1 is None:
            P.op("dve", lambda e: e.tensor_scalar(out=out, in0=in0, scalar1=s1, scalar2=None, op0=o0), R=R, W=W)
        else:
            P.op("dve", lambda e: e.tensor_scalar(out=out, in0=in0, scalar1=s1, scalar2=s2, op0=o0, op1=o1), R=R, W=W)

    def tt(out, in0, in1, o, R=(), W=(), eng="dve"):
        P.op(eng, lambda e: e.tensor_tensor(out=out, in0=in0, in1=in1, op=o), R=R, W=W)

    pv = lambda i: par[:, i, :]
    P.op("act", lambda e: e.activation(out=pv(DT), in_=pv(DT), func=AF.Exp), R=[pk(DT)], W=[pk(DT)])
    ts(pv(AR), pv(AR), -1e-4, None, ALU.min, R=[pk(AR)], W=[pk(AR)])
    tt(pv(ARD), pv(AR), pv(DT), ALU.mult, R=[pk(AR), pk(DT)], W=[pk(ARD)])
    tt(pv(TH), pv(AI), pv(DT), ALU.mult, R=[pk(AI), pk(DT)], W=[pk(TH)])
    tab = A("tab", [128, 4, 4, LCH], F32)
    scr = Rot(nc, "scr", [128, LCH], F32, 8)
    scri = Rot(nc, "scri", [128, LCH], I32, 2)
    nard = A("nard", [128, 4], F32)
    ts(nard[:, :], pv(ARD), -1.0, None, ALU.mult, R=[pk(ARD)], W=["nard"])
    def sin_of(ang, angk):
        t, tk = scr.next()
        ki, kik = scri.next()
        ts(t[:, :], ang[:, :], 1.0 / TWO_PI, None, ALU.mult, R=[angk], W=[tk])
        P.op("dve", lambda e: e.tensor_copy(out=ki[:, :], in_=t[:, :]), R=[tk], W=[kik])
        P.op("dve", lambda e: e.tensor_copy(out=t[:, :], in_=ki[:, :]), R=[kik], W=[tk])
        P.op("dve", lambda e: e.scalar_tensor_tensor(out=ang[:, :], in0=t[:, :], scalar=-TWO_PI, in1=ang[:, :], op0=ALU.mult, op1=ALU.add),
             R=[tk, angk], W=[angk])
        ts(t[:, :], ang[:, :], PI, -TWO_PI, ALU.is_gt, ALU.mult, R=[angk], W=[tk])
        tt(ang[:, :], ang[:, :], t[:, :], ALU.add, R=[angk, tk], W=[angk])
        ts(t[:, :], ang[:, :], -PI, TWO_PI, ALU.is_lt, ALU.mult, R=[angk], W=[tk])
        tt(ang[:, :], ang[:, :], t[:, :], ALU.add, R=[angk, tk], W=[angk])
        ts(ang[:, :], ang[:, :], PI, -PI, ALU.min, ALU.max, R=[angk], W=[angk])
        P.op("act", lambda e: e.activation(out=t[:, :], in_=ang[:, :], func=AF.Sin), R=[angk], W=[tk])
        return t, tk

    for j in range(4):
        ang, angk = scr.next()
        ts(ang[:, :], io_f[:, :], par[:, TH, j:j + 1], None, ALU.mult, R=["io_f", pk(TH)], W=[angk])
        sn, snk = sin_of(ang, angk)
        ang2, ang2k = scr.next()
        ts(ang2[:, :], io_f[:, :], par[:, TH, j:j + 1], PI / 2, ALU.mult, ALU.add, R=["io_f", pk(TH)], W=[ang2k])
        cs, csk = sin_of(ang2, ang2k)
        mg, mgk = scr.next()
        P.op("act", lambda e, mg=mg, j=j: e.activation(out=mg[:, :], in_=io_f[:, :], func=AF.Exp, scale=par[:, ARD, j:j + 1]),
             R=["io_f", pk(ARD)], W=[mgk])
        tt(tab[:, 2, j, :], mg[:, :], cs[:, :], ALU.mult, R=[mgk, csk], W=[("tab", 2, j)])
        tt(tab[:, 3, j, :], mg[:, :], sn[:, :], ALU.mult, R=[mgk, snk], W=[("tab", 3, j)])
        mg2, mg2k = scr.next()
        P.op("act", lambda e, mg2=mg2, j=j: e.activation(out=mg2[:, :], in_=io_f[:, :], func=AF.Exp, scale=nard[:, j:j + 1]),
             R=["io_f", "nard"], W=[mg2k])
        tt(tab[:, 0, j, :], mg2[:, :], cs[:, :], ALU.mult, R=[mg2k, csk], W=[("tab", 0, j)])
        P.op("dve", lambda e, mg2=mg2, sn=sn, j=j: e.scalar_tensor_tensor(out=tab[:, 1, j, :], in0=mg2[:, :], scalar=-1.0, in1=sn[:, :],
                                                                          op0=ALU.mult, op1=ALU.mult), R=[mg2k, snk], W=[("tab", 1, j)])
    for j in range(4):
        P.op("dve", lambda e, j=j: e.tensor_copy(out=par[:, LRE, j:j + 1], in_=tab[:, 2, j, 1:2]), R=[("tab", 2, j)], W=[pk(LRE)])
        P.op("dve", lambda e, j=j: e.tensor_copy(out=par[:, LIM, j:j + 1], in_=tab[:, 3, j, 1:2]), R=[("tab", 3, j)], W=[pk(LIM)])
    for j in range(4):
        l5r = tab[:, 2, j, LCH - 1:LCH]; l5i = tab[:, 3, j, LCH - 1:LCH]
        RK = [pk(LRE), pk(LIM), ("tab", 2, j), ("tab", 3, j)]
        tt(par[:, TMP, j:j + 1], par[:, LRE, j:j + 1], l5r, ALU.mult, R=RK, W=[pk(TMP)])
        tt(par[:, TMP2, j:j + 1], par[:, LIM, j:j + 1], l5i, ALU.mult, R=RK, W=[pk(TMP2)])
        tt(par[:, MRE, j:j + 1], par[:, TMP, j:j + 1], par[:, TMP2, j:j + 1], ALU.subtract, R=[pk(TMP), pk(TMP2)], W=[pk(MRE)])
        tt(par[:, TMP, j:j + 1], par[:, LRE, j:j + 1], l5i, ALU.mult, R=RK + [pk(MRE)], W=[pk(TMP)])
        tt(par[:, TMP2, j:j + 1], par[:, LIM, j:j + 1], l5r, ALU.mult, R=RK + [pk(MRE)], W=[pk(TMP2)])
        tt(par[:, MIM, j:j + 1], par[:, TMP, j:j + 1], par[:, TMP2, j:j + 1], ALU.add, R=[pk(TMP), pk(TMP2)], W=[pk(MIM)])
    ts(pv(NMIM), pv(MIM), -1.0, None, ALU.mult, R=[pk(MIM)], W=[pk(NMIM)])
    ts(pv(NUM), pv(LRE), -1.0, None, ALU.add, R=[pk(LRE)], W=[pk(NUM)])
    tt(pv(DEN), pv(AR), pv(AR), ALU.mult, R=[pk(AR)], W=[pk(DEN)])
    tt(pv(TMP), pv(AI), pv(AI), ALU.mult, R=[pk(AI), pk(MIM), pk(MRE)], W=[pk(TMP)])
    tt(pv(DEN), pv(DEN), pv(TMP), ALU.add, R=[pk(DEN), pk(TMP)], W=[pk(DEN)])
    P.op("dve", lambda e: e.reciprocal(out=pv(DEN), in_=pv(DEN)), R=[pk(DEN)], W=[pk(DEN)])
    tt(pv(TMP), pv(NUM), pv(AR), ALU.mult, R=[pk(NUM), pk(AR), pk(DEN)], W=[pk(TMP)])
    tt(pv(TMP2), pv(LIM), pv(AI), ALU.mult, R=[pk(LIM), pk(AI), pk(NMIM)], W=[pk(TMP2)])
    tt(pv(CRE), pv(TMP), pv(TMP2), ALU.add, R=[pk(TMP), pk(TMP2)], W=[pk(CRE)])
    tt(pv(CRE), pv(CRE), pv(DEN), ALU.mult, R=[pk(CRE), pk(DEN)], W=[pk(CRE)])
    tt(pv(TMP), pv(LIM), pv(AR), ALU.mult, R=[pk(LIM), pk(AR), pk(CRE)], W=[pk(TMP)])
    tt(pv(TMP2), pv(NUM), pv(AI), ALU.mult, R=[pk(NUM), pk(AI), pk(CRE)], W=[pk(TMP2)])
    tt(pv(CIM), pv(TMP), pv(TMP2), ALU.subtract, R=[pk(TMP), pk(TMP2)], W=[pk(CIM)])
    tt(pv(CIM), pv(CIM), pv(DEN), ALU.mult, R=[pk(CIM), pk(DEN)], W=[pk(CIM)])
    bfull = A("bfull", [128, 2, 4, 128], F32)
    P.op("pool", lambda e: e.memset(bfull[:], 0.0), W=["bfull"])
    BT = A("BT", [128, 2, 4, 128], BF16)
    CTt = A("CTt", [128, 2, 4, 128], BF16)
    P.op("pool", lambda e: e.memset(CTt[:], 0.0), W=["CTt"])
    t16 = Rot(nc, "t16", [128, 16], F32, 4)
    for j in range(4):
        for g in range(2):
            ps_ = slice(g * 64, (g + 1) * 64)
            c0 = 32 * j + 16 * g
            for which in range(2):
                ta, tak = t16.next()
                tb, tbk = t16.next()
                s_a = bst[ps_, 0 if which == 0 else 1, j, :]
                s_b = bst[ps_, 1 if which == 0 else 0, j, :]
                ts(ta[ps_, :], s_a, par[ps_, CRE, j:j + 1], None, ALU.mult, R=[("bst", 0), ("bst", 1), pk(CRE)], W=[tak])
                ts(tb[ps_, :], s_b, par[ps_, CIM, j:j + 1], None, ALU.mult, R=[("bst", 0), ("bst", 1), pk(CIM)], W=[tbk])
                tt(bfull[ps_, which, j, c0:c0 + 16], ta[ps_, :], tb[ps_, :], ALU.subtract if which == 0 else ALU.add,
                   R=[tak, tbk, "bfull"], W=["bfull"])
            P.op("dve", lambda e, ps_=ps_, j=j, c0=c0: e.tensor_copy(out=CTt[ps_, 0, j, c0:c0 + 16], in_=bst[ps_, 2, j, :]),
                 R=[("bst", 2), "CTt"], W=["CTt"])
            ts(CTt[ps_, 1, j, c0:c0 + 16], bst[ps_, 3, j, :], -1.0, None, ALU.mult, R=[("bst", 3), "CTt"], W=["CTt"])
    for j in range(4):
        for which in range(2):
            pt, ptk = C.ps.next()
            P.op("pe", lambda e, pt=pt, which=which, j=j: e.transpose(out=pt[:, 0:128], in_=bfull[:, which, j, :], identity=C.ident_f[:]),
                 R=["bfull", "ident_f"], W=[ptk])
            P.op("act", lambda e, pt=pt, which=which, j=j: e.copy(out=BT[:, which, j, :], in_=pt[:, 0:128]), R=[ptk], W=[("BT", which, j)])
    G = A("G", [128, NCH + 1, 4, 2], F32)
    P.op("pool", lambda e: e.memset(G[:], 0.0), W=["G"])
    pt_ = Rot(nc, "pt_", [128, LCH], F32, 4)
    Pre = Rot(nc, "Pre", [128, LCH], F32, 2)
    Pim = Rot(nc, "Pim", [128, LCH], F32, 2)
    Sre = Rot(nc, "Sre", [128, LCH], F32, 2)
    Sim = Rot(nc, "Sim", [128, LCH], F32, 2)
    hre = Rot(nc, "hre", [128, LCH], BF16, 8)
    him = Rot(nc, "him", [128, LCH], BF16, 8)
    yv = Rot(nc, "yv", [128, LCH], F32, 2)
    gt = Rot(nc, "gt", [128, LCH], F32, 2)
    sml = Rot(nc, "sml", [128, 2], F32, 4)
    for ch in range(NCH):
        c0 = ch * LCH
        hs = []
        for j in range(4):
            bre, brek = C.ps.next()
            P.op("pe", lambda e, bre=bre, j=j, c0=c0: e.matmul(bre[:, :], lhsT=BT[:, 0, j, :], rhs=ub[:, c0:c0 + LCH], start=True, stop=True),
                 R=[("BT", 0, j), "ub"], W=[brek])
            bim, bimk = C.ps.next()
            P.op("pe", lambda e, bim=bim, j=j, c0=c0: e.matmul(bim[:, :], lhsT=BT[:, 1, j, :], rhs=ub[:, c0:c0 + LCH], start=True, stop=True),
                 R=[("BT", 1, j), "ub"], W=[bimk])
            a1, a1k = pt_.next(); a2, a2k = pt_.next(); a3, a3k = pt_.next(); a4, a4k = pt_.next()
            tt(a1[:, :], bre[:, :], tab[:, 0, j, :], ALU.mult, R=[brek, ("tab", 0, j)], W=[a1k])
            tt(a2[:, :], bim[:, :], tab[:, 1, j, :], ALU.mult, R=[bimk, ("tab", 1, j)], W=[a2k])
            tt(a3[:, :], bim[:, :], tab[:, 0, j, :], ALU.mult, R=[bimk, ("tab", 0, j)], W=[a3k])
            tt(a4[:, :], bre[:, :], tab[:, 1, j, :], ALU.mult, R=[brek, ("tab", 1, j)], W=[a4k])
            pr, prk = Pre.next(); pi_, pik = Pim.next()
            tt(pr[:, :], a1[:, :], a2[:, :], ALU.subtract, R=[a1k, a2k], W=[prk], eng="pool")
            tt(pi_[:, :], a3[:, :], a4[:, :], ALU.add, R=[a3k, a4k], W=[pik], eng="pool")
            sr, srk = Sre.next(); si, sik = Sim.next()
            P.op("dve", lambda e, sr=sr, pr=pr, ch=ch, j=j: e.tensor_tensor_scan(out=sr[:, :], data0=onesL[:, :], data1=pr[:, :],
                                                                                initial=G[:, ch, j, 0:1], op0=ALU.mult, op1=ALU.add),
                 R=[prk, "onesL", ("G", ch, j), "G"], W=[srk])
            P.op("dve", lambda e, si=si, pi_=pi_, ch=ch, j=j: e.tensor_tensor_scan(out=si[:, :], data0=onesL[:, :], data1=pi_[:, :],
                                                                                  initial=G[:, ch, j, 1:2], op0=ALU.mult, op1=ALU.add),
                 R=[pik, "onesL", ("G", ch, j), "G"], W=[sik])
            sm_, smk = sml.next()
            ts(sm_[:, 0:1], sr[:, LCH - 1:LCH], par[:, MRE, j:j + 1], None, ALU.mult, R=[srk, pk(MRE)], W=[smk])
            ts(sm_[:, 1:2], si[:, LCH - 1:LCH], par[:, MRE, j:j + 1], None, ALU.mult, R=[sik, pk(MRE)], W=[smk])
            P.op("dve", lambda e, sm_=sm_, si=si, ch=ch, j=j: e.scalar_tensor_tensor(out=G[:, ch + 1, j, 0:1], in0=si[:, LCH - 1:LCH],
                                                                                    scalar=par[:, NMIM, j:j + 1], in1=sm_[:, 0:1],
                                                                                    op0=ALU.mult, op1=ALU.add),
                 R=[smk, sik, pk(NMIM), "G"], W=[("G", ch + 1, j, 0)])
            P.op("dve", lambda e, sm_=sm_, sr=sr, ch=ch, j=j: e.scalar_tensor_tensor(out=G[:, ch + 1, j, 1:2], in0=sr[:, LCH - 1:LCH],
                                                                                    scalar=par[:, MIM, j:j + 1], in1=sm_[:, 1:2],
                                                                                    op0=ALU.mult, op1=ALU.add),
                 R=[smk, srk, pk(MIM), "G"], W=[("G", ch + 1, j, 1)])
            P.res[("G", ch + 1, j)] = P.res[("G", ch + 1, j, 1)]
            b1, b1k = pt_.next(); b2, b2k = pt_.next(); b3, b3k = pt_.next(); b4, b4k = pt_.next()
            tt(b1[:, :], sr[:, :], tab[:, 2, j, :], ALU.mult, R=[srk, ("tab", 2, j)], W=[b1k], eng="pool")
            tt(b2[:, :], si[:, :], tab[:, 3, j, :], ALU.mult, R=[sik, ("tab", 3, j)], W=[b2k], eng="pool")
            tt(b3[:, :], si[:, :], tab[:, 2, j, :], ALU.mult, R=[sik, ("tab", 2, j)], W=[b3k], eng="pool")
            tt(b4[:, :], sr[:, :], tab[:, 3, j, :], ALU.mult, R=[srk, ("tab", 3, j)], W=[b4k], eng="pool")
            hr_, hrk = hre.next(); hi_, hik = him.next()
            tt(hr_[:, :], b1[:, :], b2[:, :], ALU.subtract, R=[b1k, b2k], W=[hrk])
            tt(hi_[:, :], b3[:, :], b4[:, :], ALU.add, R=[b3k, b4k], W=[hik])
            hs.append((hr_, hrk, hi_, hik))
        yp, ypk = C.ps.next()
        for j in range(4):
            hr_, hrk, hi_, hik = hs[j]
            P.op("pe", lambda e, yp=yp, hr_=hr_, j=j: e.matmul(yp[:, :], lhsT=CTt[:, 0, j, :], rhs=hr_[:, :], start=(j == 0), stop=False),
                 R=["CTt", hrk], W=[ypk])
            P.op("pe", lambda e, yp=yp, hi_=hi_, j=j: e.matmul(yp[:, :], lhsT=CTt[:, 1, j, :], rhs=hi_[:, :], start=False, stop=(j == 3)),
                 R=["CTt", hik], W=[ypk])
        y_, yk_ = yv.next()
        P.op("dve", lambda e, y_=y_, yp=yp, c0=c0: e.scalar_tensor_tensor(out=y_[:, :], in0=ub[:, c0:c0 + LCH], scalar=dvec[:, 0:1], in1=yp[:, :],
                                                                          op0=ALU.mult, op1=ALU.add), R=[ypk, "ub", "dvec"], W=[yk_])
        g_, gk_ = gt.next()
        tt(g_[:, :], y_[:, :], y_[:, :], ALU.mult, R=[yk_], W=[gk_], eng="pool")
        ts(g_[:, :], g_[:, :], 0.044715, 1.0, ALU.mult, ALU.add, R=[gk_], W=[gk_])
        tt(g_[:, :], g_[:, :], y_[:, :], ALU.mult, R=[gk_, yk_], W=[gk_], eng="pool")
        P.op("act", lambda e, g_=g_: e.activation(out=g_[:, :], in_=g_[:, :], func=AF.Sigmoid, scale=1.5957691216057308), R=[gk_], W=[gk_])
        tt(y_[:, :], y_[:, :], g_[:, :], ALU.mult, R=[gk_, yk_], W=[yk_])
        P.dma("sp", yT[:, c0:c0 + LCH], y_[:, :], R=[yk_], W=[("yo", ch)])
    P.finish("sp")
    P.emit()
    return nc


def s5_core_inputs(ins, b, gq, uT_b):
    gs = slice(8 * gq, 8 * gq + 8)
    def st(a):
        return np.ascontiguousarray(a[gs].reshape(4, 128).T)
    d = dict(uT=np.ascontiguousarray(uT_b[128 * gq:128 * gq + 128]),
             a_re=st(ins["ssm_a_re"][0]), a_im=st(ins["ssm_a_im"][0]),
             ldt=np.ascontiguousarray(np.repeat(ins["ssm_log_dt"][0][gs].reshape(4, 2, 1), 64, axis=2).reshape(4, 128).T),
             b_re=np.ascontiguousarray(ins["ssm_b_re"][0][gs].reshape(4, 128, 16)),
             b_im=np.ascontiguousarray(ins["ssm_b_im"][0][gs].reshape(4, 128, 16)),
             ct_re=np.ascontiguousarray(ins["ssm_c_re"][0][gs].transpose(0, 2, 1).reshape(4, 128, 16)),
             ct_im=np.ascontiguousarray(ins["ssm_c_im"][0][gs].transpose(0, 2, 1).reshape(4, 128, 16)),
             dsk=np.ascontiguousarray(ins["ssm_d"][0][128 * gq:128 * gq + 128]))
    return d


SEQ = 8192
BATCH = 2
TPC = BATCH * SEQ // NCORES
CPB = NCORES // BATCH


def _run(nc, in_maps):
    res = run_bass_kernel_spmd(nc, in_maps, core_ids=list(range(NCORES)))
    return res.results


def kernel(**ins):
    ins = {k: np.ascontiguousarray(np.asarray(v, dtype=np.float32)) for k, v in ins.items()}
    x = ins["x"].reshape(BATCH * SEQ, D_MODEL)
    ca = np.ascontiguousarray
    Ct, St, pm = rope_tables(SEQ)
    sc = np.float32(96 ** -0.5)
    Cq, Sq = ca(Ct * sc), ca(St * sc)
    nc1 = build_L1(TPC)
    maps = []
    for c in range(NCORES):
        p0 = (c % CPB) * TPC
        sl = slice(p0, p0 + TPC)
        maps.append(dict(x=x[c * TPC:(c + 1) * TPC], att_norm=ins["att_norm"][0], w_in=ins["att_w_in"][0],
                         q_lat_norm=ins["att_q_latent_norm"][0], w_q_up=ins["att_w_q_up"][0],
                         kv_lat_norm=ins["att_kv_latent_norm"][0], w_kv_up=ins["att_w_kv_up"][0],
                         q_norm=ins["att_q_norm"][0], k_norm=ins["att_k_norm"][0],
                         cq_t=ca(Cq[:, sl]), sq_t=ca(Sq[:, sl]), ck_t=ca(Ct[:, sl]), sk_t=ca(St[:, sl]), pm=pm))
    r1 = _run(nc1, maps)
    del nc1
    cat = lambda name, b, axis: np.concatenate([r1[b * CPB + i][name] for i in range(CPB)], axis=axis)
    nc2 = build_L2(SEQ)
    maps = []
    for b in range(BATCH):
        sbqT = cat("sbqT", b, 1); sbkT = cat("sbkT", b, 1); sbv = cat("sbv", b, 0)
        mqT = cat("mqT", b, 2); mkT = cat("mkT", b, 2); mv = cat("mv", b, 0)
        for g in range(CPB):
            maps.append(dict(sbqT=ca(sbqT[128 * g:128 * g + 128].reshape(2, 64, SEQ)),
                             sbkT=ca(sbkT[128 * g:128 * g + 128].reshape(2, 64, SEQ)),
                             sbv=ca(sbv[:, 128 * g:128 * g + 128].reshape(SEQ, 2, 64).transpose(1, 0, 2)),
                             mqT=ca(mqT[2 * g:2 * g + 2]), mkT=ca(mkT[2 * g:2 * g + 2]),
                             mv=ca(mv[:, 128 * g:128 * g + 128].reshape(SEQ, 2, 64).transpose(1, 0, 2))))
    r2 = _run(nc2, maps)
    del nc2, r1
    mT = []
    for b in range(BATCH):
        m = np.empty((1024, SEQ), np.float32)
        for g in range(CPB):
            o = r2[b * CPB + g]["oT"]
            m[128 * g:128 * g + 128] = o[0:2].reshape(128, SEQ)
            m[512 + 128 * g:512 + 128 * g + 128] = o[2:4].reshape(128, SEQ)
        mT.append(m)
    nc3 = build_L3(TPC)
    maps = []
    for c in range(NCORES):
        p0 = (c % CPB) * TPC
        maps.append(dict(x=x[c * TPC:(c + 1) * TPC], mT=ca(mT[c // CPB][:, p0:p0 + TPC]), w_out=ins["att_w_out"][0],
                         dffn_norm=ins["dffn_norm"][0], wg=ins["dffn_w_gate"][0], wu=ins["dffn_w_up"][0], wd=ins["dffn_w_down"][0],
                         ssm_norm=ins["ssm_norm"][0], w_sin=ins["ssm_w_in"][0]))
    r3 = _run(nc3, maps)
    del nc3, r2
    nc4 = build_L4(SEQ)
    maps = []
    for b in range(BATCH):
        uT_b = np.concatenate([r3[b * CPB + i]["uT"] for i in range(CPB)], axis=1)
        for gq in range(CPB):
            maps.append(s5_core_inputs(ins, b, gq, uT_b))
    r4 = _run(nc4, maps)
    del nc4
    nc5 = build_L5(TPC)
    maps = []
    for c in range(NCORES):
        b = c // CPB
        p0 = (c % CPB) * TPC
        yT = np.concatenate([r4[b * CPB + gq]["yT"][:, p0:p0 + TPC] for gq in range(CPB)], axis=0)
        maps.append(dict(h2T=r3[c]["h2T"], yT=ca(yT), w_glu=ins["ssm_w_glu"][0], moe_norm=ins["moe_norm"][0], w_r=ins["moe_router"][0],
                         wg=ins["moe_w_gate"][0], wu=ins["moe_w_up"][0], wd=ins["moe_w_down"][0]))
    r5 = _run(nc5, maps)
    out = np.concatenate([r5[c]["out"] for c in range(NCORES)], axis=0).reshape(BATCH, SEQ, D_MODEL)
    return out.astype(np.float32)
```

```python
import contextlib
import numpy as np
import concourse.bass as bass
import concourse.mybir as mybir
from concourse.bass_utils import run_bass_kernel_spmd

F32 = mybir.dt.float32
BF16 = mybir.dt.bfloat16
I32 = mybir.dt.int32
AF = mybir.ActivationFunctionType
ALU = mybir.AluOpType
AX = mybir.AxisListType

D_MODEL = 1024
D_FF = 3584
EPS = 1e-6
NCORES = 8

COMPUTE = ("pe", "act", "dve", "pool")
NDSEM = 8


class Prog:
    def __init__(self, nc):
        self.nc = nc
        self.engs = ("pe", "act", "dve", "pool", "sp")
        self.q = {e: [] for e in self.engs}
        self.cnt = {e: 0 for e in COMPUTE}
        self.known = {e: {} for e in self.engs}
        self.res = {}
        self.dcnt = {}
        self.drr = {e: 0 for e in self.engs}
        self.sems = {}
        self.n_wait = 0
        self.n_op = 0

    def _deps(self, R, W):
        deps = {}
        for r in R:
            st = self.res.get(r)
            if st is not None and st[0] is not None:
                k, v = st[0]
                if deps.get(k, 0) < v:
                    deps[k] = v
        for w in W:
            st = self.res.get(w)
            if st is not None:
                if st[0] is not None:
                    k, v = st[0]
                    if deps.get(k, 0) < v:
                        deps[k] = v
                for k, v in st[1].items():
                    if deps.get(k, 0) < v:
                        deps[k] = v
        return deps

    def _record(self, tok, R, W):
        k, v = tok
        for r in R:
            st = self.res.get(r)
            if st is None:
                st = [None, {}]
                self.res[r] = st
            if st[1].get(k, 0) < v:
                st[1][k] = v
        for w in W:
            self.res[w] = [tok, {}]

    def _emit_waits(self, eng, deps):
        kn = self.known[eng]
        for k, v in deps.items():
            if k == eng and eng == "pe":
                continue
            if kn.get(k, 0) >= v:
                continue
            kn[k] = v
            self.q[eng].append(("w", k, v))
            self.n_wait += 1

    def op(self, eng, fn, R=(), W=()):
        deps = self._deps(R, W)
        self._emit_waits(eng, deps)
        self.cnt[eng] += 1
        tok = (eng, self.cnt[eng])
        self.q[eng].append(("o", fn, eng, 1))
        self._record(tok, R, W)
        self.n_op += 1
        return tok

    def dma(self, eng, out, in_, R=(), W=(), **kw):
        deps = self._deps(R, W)
        j = self.drr[eng]
        self.drr[eng] = (j + 1) % NDSEM
        key = ("d", eng, j)
        prev = self.dcnt.get(key, 0)
        if prev:
            deps[key] = max(deps.get(key, 0), prev * 16)
        self._emit_waits(eng, deps)
        self.dcnt[key] = prev + 1
        tok = (key, (prev + 1) * 16)
        self.q[eng].append(("o", lambda e: e.dma_start(out=out, in_=in_, **kw), key, 16))
        self._record(tok, R, W)
        self.n_op += 1
        return tok

    def finish(self, eng="sp"):
        deps = {k: c * 16 for k, c in self.dcnt.items()}
        self._emit_waits(eng, deps)

    def emit(self):
        nc = self.nc
        with contextlib.ExitStack() as es:
            keys = list(COMPUTE) + list(self.dcnt.keys())
            for k in keys:
                nm = k if isinstance(k, str) else "d_%s_%d" % (k[1], k[2])
                self.sems[k] = es.enter_context(nc.semaphore("s_" + nm))
            block = es.enter_context(nc.Block())
            handles = {"pe": block.tensor, "act": block.scalar, "dve": block.vector,
                       "pool": block.gpsimd, "sp": block.sync}
            sems = self.sems
            for eng in self.engs:
                items = self.q[eng]

                def body(e, items=items):
                    for it in items:
                        if it[0] == "w":
                            e.wait_ge(sems[it[1]], it[2])
                        else:
                            it[1](e).then_inc(sems[it[2]], it[3])
                handles[eng](body)


class Rot:
    def __init__(self, nc, name, shape, dtype, n, psum=False):
        self.tiles = []
        for i in range(n):
            if psum:
                t = nc.alloc_psum_tensor("%s%d" % (name, i), shape, dtype)
            else:
                t = nc.alloc_sbuf_tensor("%s%d" % (name, i), shape, dtype)
            self.tiles.append(t)
        self.name = name
        self.i = 0

    def next(self):
        i = self.i % len(self.tiles)
        self.i += 1
        return self.tiles[i], (self.name, i)


class Ctx:
    def __init__(self, nc, n_ps=8):
        self.nc = nc
        self.P = Prog(nc)
        P = self.P
        self.ps = Rot(nc, "ps", [128, 512], F32, n_ps, psum=True)
        self.ones_bf = nc.alloc_sbuf_tensor("ones_bf", [128, 128], BF16)
        self.ones_f = nc.alloc_sbuf_tensor("ones_f", [128, 128], F32)
        self.ident_f = nc.alloc_sbuf_tensor("ident_f", [128, 128], F32)
        self.eps_t = nc.alloc_sbuf_tensor("eps_t", [128, 1], F32)
        P.op("pool", lambda e: e.memset(self.ones_bf[:], 1.0), W=["ones_bf"])
        P.op("pool", lambda e: e.memset(self.ones_f[:], 1.0), W=["ones_f"])
        P.op("pool", lambda e: e.memset(self.eps_t[:], EPS), W=["eps_t"])
        P.op("pool", lambda e: e.memset(self.ident_f[:], 1.0), W=["ident_f"])
        P.op("pool", lambda e: e.affine_select(out=self.ident_f[:], in_=self.ident_f[:], pattern=[[-1, 128]],
                                               compare_op=ALU.is_equal, fill=0.0, base=0, channel_multiplier=1),
             R=["ident_f"], W=["ident_f"])
        self.sq = Rot(nc, "sq", [128, 8, 512], BF16, 1)
        self.rt = Rot(nc, "rt", [128, 512], F32, 2)


def emit_rmsnorm_fm(C, hT, hkeys, nk, tok0, ntok, gain_sb, gkey, xnT, xkeys, xtok0, D, npart=128):
    P = C.P
    sq, sqk = C.sq.next()
    P.op("act", lambda e: e.activation(out=sq[:npart, 0:nk, 0:ntok], in_=hT[:npart, 0:nk, tok0:tok0 + ntok], func=AF.Square),
         R=list(hkeys), W=[sqk])
    ps, psk = C.ps.next()
    for k in range(nk):
        P.op("pe", lambda e, k=k: e.matmul(ps[:npart, 0:ntok], lhsT=C.ones_bf[:npart, :npart], rhs=sq[:npart, k, 0:ntok],
                                          start=(k == 0), stop=(k == nk - 1)),
             R=[sqk, "ones_bf"], W=[psk])
    rt, rtk = C.rt.next()
    P.op("act", lambda e: e.activation(out=rt[:npart, 0:ntok], in_=ps[:npart, 0:ntok], func=AF.Sqrt,
                                       bias=C.eps_t[:npart, 0:1], scale=1.0 / D),
         R=[psk, "eps_t"], W=[rtk])
    P.op("dve", lambda e: e.reciprocal(out=rt[:npart, 0:ntok], in_=rt[:npart, 0:ntok]), R=[rtk], W=[rtk])
    for k in range(nk):
        P.op("dve", lambda e, k=k: e.scalar_tensor_tensor(out=xnT[:npart, k, xtok0:xtok0 + ntok],
                                                          in0=hT[:npart, k, tok0:tok0 + ntok],
                                                          scalar=gain_sb[:npart, k:k + 1], in1=rt[:npart, 0:ntok],
                                                          op0=ALU.mult, op1=ALU.mult),
             R=[hkeys[k], rtk, gkey], W=[xkeys[k]])


def load_vec_fm(C, name, dram_vec_ap, n):
    nk = max(1, n // 128)
    npart = min(128, n)
    t = C.nc.alloc_sbuf_tensor(name, [128, nk], F32)
    C.P.dma("sp", t[:npart, :], dram_vec_ap.rearrange("(k p) -> p k", p=npart), W=[name], allow_slow_non_contiguous=True)
    return t


def emit_ffn(C, xnT, xkeys, hT, hkeys, T, wg, wu, wd, pools, gate_bc=None):
    P = C.P
    NTG = T // 512
    hidT, hidkeys = pools["hidT"], pools["hidkeys"]
    for wb in range(7):
        wgt, wgk = pools["wgu"].next()
        P.dma("pool", wgt[:], wg[:, wb * 512:(wb + 1) * 512].rearrange("(k p) n -> p k n", p=128), W=[wgk])
        wut, wuk = pools["wgu"].next()
        P.dma("pool", wut[:], wu[:, wb * 512:(wb + 1) * 512].rearrange("(k p) n -> p k n", p=128), W=[wuk])
        for m in range(4):
            mm = wb * 4 + m
            for tg in range(NTG):
                pg, pgk = C.ps.next()
                for k in range(8):
                    P.op("pe", lambda e, k=k, pg=pg, wgt=wgt, m=m, tg=tg: e.matmul(
                        pg[:, :], lhsT=wgt[:, k, m * 128:(m + 1) * 128], rhs=xnT[:, k, tg * 512:(tg + 1) * 512],
                        start=(k == 0), stop=(k == 7)), R=[wgk, xkeys(k, tg)], W=[pgk])
                pu, puk = C.ps.next()
                for k in range(8):
                    P.op("pe", lambda e, k=k, pu=pu, wut=wut, m=m, tg=tg: e.matmul(
                        pu[:, :], lhsT=wut[:, k, m * 128:(m + 1) * 128], rhs=xnT[:, k, tg * 512:(tg + 1) * 512],
                        start=(k == 0), stop=(k == 7)), R=[wuk, xkeys(k, tg)], W=[puk])
                sg, sgk = pools["sg"].next()
                P.op("act", lambda e, sg=sg, pg=pg: e.activation(out=sg[:, :], in_=pg[:, :], func=AF.Silu), R=[pgk], W=[sgk])
                P.op("dve", lambda e, sg=sg, pu=pu, mm=mm, tg=tg: e.tensor_tensor(
                    out=hidT[:, mm, tg * 512:(tg + 1) * 512], in0=sg[:, :], in1=pu[:, :], op=ALU.mult),
                    R=[sgk, puk], W=[hidkeys[mm] + (tg,)])
    for dm in range(8):
        wdt, wdk = pools["wd"].next()
        P.dma("pool", wdt[:], wd[:, dm * 128:(dm + 1) * 128].rearrange("(k p) n -> p k n", p=128), W=[wdk])
        for tg in range(NTG):
            po, pok = C.ps.next()
            for k in range(28):
                P.op("pe", lambda e, k=k, po=po, wdt=wdt, tg=tg: e.matmul(
                    po[:, :], lhsT=wdt[:, k, :], rhs=hidT[:, k, tg * 512:(tg + 1) * 512],
                    start=(k == 0), stop=(k == 27)), R=[wdk, hidkeys[k] + (tg,)], W=[pok])
            if gate_bc is None:
                P.op("dve", lambda e, po=po, dm=dm, tg=tg: e.tensor_tensor(
                    out=hT[:, dm, tg * 512:(tg + 1) * 512], in0=hT[:, dm, tg * 512:(tg + 1) * 512], in1=po[:, :], op=ALU.add),
                    R=[pok, hkeys(dm, tg)], W=[hkeys(dm, tg)])
            else:
                gt, gkf = gate_bc
                tmp, tmpk = pools["sg"].next()
                P.op("dve", lambda e, po=po, tmp=tmp, gt=gt, tg=tg: e.tensor_tensor(
                    out=tmp[:, :], in0=po[:, :], in1=gt[:, tg * 512:(tg + 1) * 512], op=ALU.mult),
                    R=[pok, gkf(tg)], W=[tmpk])
                P.op("pool", lambda e, tmp=tmp, dm=dm, tg=tg: e.tensor_tensor(
                    out=hT[:, dm, tg * 512:(tg + 1) * 512], in0=hT[:, dm, tg * 512:(tg + 1) * 512], in1=tmp[:, :], op=ALU.add),
                    R=[tmpk, hkeys(dm, tg)], W=[hkeys(dm, tg)])


def ffn_pools(nc, T):
    return {
        "hidT": nc.alloc_sbuf_tensor("hidT", [128, 28, T], BF16),
        "hidkeys": [("hid", k) for k in range(28)],
        "wgu": Rot(nc, "wgu", [128, 8, 512], BF16, 4),
        "wd": Rot(nc, "wd", [128, 28, 128], BF16, 2),
        "sg": Rot(nc, "sg", [128, 512], F32, 3),
    }


def emit_load_tm_to_fm(C, src_dram, hT, hkeys, ntiles, stage):
    P = C.P
    for t in range(ntiles):
        st, stk = stage.next()
        P.dma("sp", st[:], src_dram[t * 128:(t + 1) * 128, :], W=[stk])
        for half in range(2):
            ps, psk = C.ps.next()
            for kk in range(4):
                k = half * 4 + kk
                P.op("pe", lambda e, ps=ps, st=st, k=k, kk=kk: e.transpose(out=ps[:, kk * 128:(kk + 1) * 128], in_=st[:, k * 128:(k + 1) * 128],
                                                                           identity=C.ident_f[:]),
                     R=[stk, "ident_f"], W=[psk])
            P.op("dve" if half == 0 else "act",
                 (lambda e, ps=ps, half=half, t=t: e.tensor_copy(out=hT[:, half * 4:half * 4 + 4, t * 128:(t + 1) * 128],
                                                                 in_=ps[:, :].rearrange("p (k n) -> p k n", k=4))) if half == 0 else
                 (lambda e, ps=ps, half=half, t=t: e.copy(out=hT[:, half * 4:half * 4 + 4, t * 128:(t + 1) * 128],
                                                          in_=ps[:, :].rearrange("p (k n) -> p k n", k=4))),
                 R=[psk], W=[hkeys(half * 4 + kk, t // 4) for kk in range(4)])


def emit_store_fm_to_tm(C, hT, hkeys, dst_dram, ntiles, stage):
    P = C.P
    for t in range(ntiles):
        st, stk = stage.next()
        for half in range(2):
            ps, psk = C.ps.next()
            for kk in range(4):
                k = half * 4 + kk
                P.op("pe", lambda e, ps=ps, k=k, kk=kk, t=t: e.transpose(out=ps[:, kk * 128:(kk + 1) * 128], in_=hT[:, k, t * 128:(t + 1) * 128],
                                                                         identity=C.ident_f[:]),
                     R=[hkeys(k, t // 4), "ident_f"], W=[psk])
            if half == 0:
                P.op("dve", lambda e, ps=ps, st=st: e.tensor_copy(out=st[:, 0:512], in_=ps[:, :]), R=[psk], W=[stk + (0,)])
            else:
                P.op("act", lambda e, ps=ps, st=st: e.copy(out=st[:, 512:1024], in_=ps[:, :]), R=[psk], W=[stk + (1,)])
        P.dma("sp", dst_dram[t * 128:(t + 1) * 128, :], st[:], R=[stk + (0,), stk + (1,)], W=[("out", t)])


def build_L3(T):
    nc = bass.Bass("TRN2", target_bir_lowering=False)
    x = nc.dram_tensor("x", [T, 1024], F32, kind="ExternalInput").ap()
    mT = nc.dram_tensor("mT", [1024, T], F32, kind="ExternalInput").ap()
    w_out = nc.dram_tensor("w_out", [1024, 1024], F32, kind="ExternalInput").ap()
    n1 = nc.dram_tensor("dffn_norm", [1024], F32, kind="ExternalInput").ap()
    wg = nc.dram_tensor("wg", [1024, D_FF], F32, kind="ExternalInput").ap()
    wu = nc.dram_tensor("wu", [1024, D_FF], F32, kind="ExternalInput").ap()
    wd = nc.dram_tensor("wd", [D_FF, 1024], F32, kind="ExternalInput").ap()
    n2 = nc.dram_tensor("ssm_norm", [1024], F32, kind="ExternalInput").ap()
    w_sin = nc.dram_tensor("w_sin", [1024, 512], F32, kind="ExternalInput").ap()
    h2T = nc.dram_tensor("h2T", [1024, T], F32, kind="ExternalOutput").ap()
    uT = nc.dram_tensor("uT", [512, T], F32, kind="ExternalOutput").ap()
    C = Ctx(nc)
    P = C.P
    TG = 1024
    hT = nc.alloc_sbuf_tensor("hT", [128, 8, TG], F32)
    xnT = nc.alloc_sbuf_tensor("xnT", [128, 8, TG], BF16)
    pools = ffn_pools(nc, TG)
    stage = Rot(nc, "stage", [128, 1024], F32, 2)
    mts = Rot(nc, "mts", [128, 8, 512], BF16, 2)
    uo = Rot(nc, "uo", [128, 512], F32, 2)
    g1 = load_vec_fm(C, "g1", n1, 1024)
    g2 = load_vec_fm(C, "g2", n2, 1024)
    hk = lambda k, tg: ("h", k, tg)
    xk = lambda k, tg: ("xn", k, tg)
    for grp in range(T // TG):
        t0 = grp * TG
        emit_load_tm_to_fm(C, x[t0:t0 + TG, :], hT, hk, TG // 128, stage)
        wo = []
        for hf in range(2):
            wt, wk = pools["wgu"].next()
            P.dma("pool", wt[:], w_out[:, hf * 512:(hf + 1) * 512].rearrange("(k p) n -> p k n", p=128), W=[wk])
            wo.append((wt, wk))
        for tg in range(TG // 512):
            mt, mk = mts.next()
            P.dma("pool", mt[:], mT[:, t0 + tg * 512:t0 + (tg + 1) * 512].rearrange("(k p) n -> p k n", p=128), W=[mk])
            for dm in range(8):
                wt, wk = wo[dm // 4]
                po, pok = C.ps.next()
                for k in range(8):
                    P.op("pe", lambda e, k=k, po=po, wt=wt, mt=mt, dm=dm: e.matmul(
                        po[:, :], lhsT=wt[:, k, (dm % 4) * 128:(dm % 4 + 1) * 128], rhs=mt[:, k, :],
                        start=(k == 0), stop=(k == 7)), R=[wk, mk], W=[pok])
                P.op("dve", lambda e, po=po, dm=dm, tg=tg: e.tensor_tensor(
                    out=hT[:, dm, tg * 512:(tg + 1) * 512], in0=hT[:, dm, tg * 512:(tg + 1) * 512], in1=po[:, :], op=ALU.add),
                    R=[pok, hk(dm, tg)], W=[hk(dm, tg)])
        for tg in range(TG // 512):
            emit_rmsnorm_fm(C, hT, [hk(k, tg) for k in range(8)], 8, tg * 512, 512, g1, "g1",
                            xnT, [xk(k, tg) for k in range(8)], tg * 512, 1024)
        emit_ffn(C, xnT, xk, hT, hk, TG, wg, wu, wd, pools)
        for k in range(8):
            P.dma("sp", h2T[k * 128:(k + 1) * 128, t0:t0 + TG], hT[:, k, :], R=[hk(k, tg) for tg in range(TG // 512)], W=[("h2o", k)])
        for tg in range(TG // 512):
            emit_rmsnorm_fm(C, hT, [hk(k, tg) for k in range(8)], 8, tg * 512, 512, g2, "g2",
                            xnT, [xk(k, tg) for k in range(8)], tg * 512, 1024)
        wt, wk = pools["wgu"].next()
        P.dma("pool", wt[:], w_sin.rearrange("(k p) n -> p k n", p=128), W=[wk])
        for tg in range(TG // 512):
            for c in range(4):
                po, pok = C.ps.next()
                for k in range(8):
                    P.op("pe", lambda e, k=k, po=po, wt=wt, c=c, tg=tg: e.matmul(
                        po[:, :], lhsT=wt[:, k, c * 128:(c + 1) * 128], rhs=xnT[:, k, tg * 512:(tg + 1) * 512],
                        start=(k == 0), stop=(k == 7)), R=[wk, xk(k, tg)], W=[pok])
                ut, uk = uo.next()
                P.op("act", lambda e, ut=ut, po=po: e.copy(out=ut[:, :], in_=po[:, :]), R=[pok], W=[uk])
                P.dma("sp", uT[c * 128:(c + 1) * 128, t0 + tg * 512:t0 + (tg + 1) * 512], ut[:, :], R=[uk], W=[("uo", c, tg, grp)])
    P.finish("sp")
    P.emit()
    return nc


def build_L5(T, n_exp=8):
    nc = bass.Bass("TRN2", target_bir_lowering=False)
    h2T = nc.dram_tensor("h2T", [1024, T], F32, kind="ExternalInput").ap()
    yT = nc.dram_tensor("yT", [512, T], F32, kind="ExternalInput").ap()
    w_glu = nc.dram_tensor("w_glu", [512, 2048], F32, kind="ExternalInput").ap()
    n1 = nc.dram_tensor("moe_norm", [1024], F32, kind="ExternalInput").ap()
    w_r = nc.dram_tensor("w_r", [1024, 8], F32, kind="ExternalInput").ap()
    wg = nc.dram_tensor("wg", [8, 1024, D_FF], F32, kind="ExternalInput").ap()
    wu = nc.dram_tensor("wu", [8, 1024, D_FF], F32, kind="ExternalInput").ap()
    wd = nc.dram_tensor("wd", [8, D_FF, 1024], F32, kind="ExternalInput").ap()
    out = nc.dram_tensor("out", [T, 1024], F32, kind="ExternalOutput").ap()
    C = Ctx(nc)
    P = C.P
    TG = 1024
    NTG = TG // 512
    hT = nc.alloc_sbuf_tensor("hT", [128, 8, TG], F32)
    xnT = nc.alloc_sbuf_tensor("xnT", [128, 8, TG], BF16)
    pools = ffn_pools(nc, TG)
    stage = Rot(nc, "stage", [128, 1024], F32, 1)
    yts = Rot(nc, "yts", [128, 4, 512], BF16, 1)
    wglu = nc.alloc_sbuf_tensor("wglu", [128, 4, 2048], BF16)
    wrg = nc.alloc_sbuf_tensor("wrg", [128, 8, 8], F32)
    sel = nc.alloc_sbuf_tensor("sel", [8, 8, 128], F32)
    gT = nc.alloc_sbuf_tensor("gT", [8, TG], F32)
    gbc = nc.alloc_sbuf_tensor("gbc", [128, TG], F32)
    sm = Rot(nc, "sm", [128, 64], F32, 2)
    g1 = load_vec_fm(C, "g1", n1, 1024)
    P.dma("pool", wglu[:], w_glu.rearrange("(k p) n -> p k n", p=128), W=["wglu"])
    P.dma("sp", wrg[:], w_r.rearrange("(k p) n -> p k n", p=128), W=["wrg"], allow_slow_non_contiguous=True)
    for k in range(8):
        P.op("dve", lambda e, k=k: e.tensor_scalar(out=wrg[:, k, :], in0=wrg[:, k, :], scalar1=g1[:, k:k + 1], scalar2=None, op0=ALU.mult),
             R=["wrg", "g1"], W=["wrg"])
    P.op("pool", lambda e: e.memset(sel[:], 1.0), W=["sel"])
    P.op("pool", lambda e: e.affine_select(out=sel[:], in_=sel[:], pattern=[[-1, 8], [0, 128]], compare_op=ALU.is_equal, fill=0.0,
                                           base=0, channel_multiplier=1), R=["sel"], W=["sel"])
    hk = lambda k, tg: ("h", k, tg)
    xk = lambda k, tg: ("xn", k, tg)
    for grp in range(T // TG):
        t0 = grp * TG
        for k in range(8):
            P.dma("sp", hT[:, k, :], h2T[k * 128:(k + 1) * 128, t0:t0 + TG], W=[hk(k, tg) for tg in range(NTG)])
        for tg in range(NTG):
            yt, yk = yts.next()
            P.dma("pool", yt[:], yT[:, t0 + tg * 512:t0 + (tg + 1) * 512].rearrange("(k p) n -> p k n", p=128), W=[yk])
            for dm in range(8):
                p1, p1k = C.ps.next()
                for k in range(4):
                    P.op("pe", lambda e, k=k, p1=p1, yt=yt, dm=dm: e.matmul(p1[:, :], lhsT=wglu[:, k, dm * 128:(dm + 1) * 128], rhs=yt[:, k, :],
                                                                          start=(k == 0), stop=(k == 3)), R=["wglu", yk], W=[p1k])
                p2, p2k = C.ps.next()
                for k in range(4):
                    P.op("pe", lambda e, k=k, p2=p2, yt=yt, dm=dm: e.matmul(p2[:, :], lhsT=wglu[:, k, 1024 + dm * 128:1024 + (dm + 1) * 128], rhs=yt[:, k, :],
                                                                          start=(k == 0), stop=(k == 3)), R=["wglu", yk], W=[p2k])
                sg, sgk = pools["sg"].next()
                P.op("act", lambda e, sg=sg, p2=p2: e.activation(out=sg[:, :], in_=p2[:, :], func=AF.Sigmoid), R=[p2k], W=[sgk])
                P.op("dve", lambda e, sg=sg, p1=p1: e.tensor_tensor(out=sg[:, :], in0=sg[:, :], in1=p1[:, :], op=ALU.mult), R=[sgk, p1k], W=[sgk])
                P.op("pool", lambda e, sg=sg, dm=dm, tg=tg: e.tensor_tensor(out=hT[:, dm, tg * 512:(tg + 1) * 512], in0=hT[:, dm, tg * 512:(tg + 1) * 512],
                                                                        in1=sg[:, :], op=ALU.add), R=[sgk, hk(dm, tg)], W=[hk(dm, tg)])
        for tg in range(NTG):
            emit_rmsnorm_fm(C, hT, [hk(k, tg) for k in range(8)], 8, tg * 512, 512, g1, "g1",
                            xnT, [xk(k, tg) for k in range(8)], tg * 512, 1024)
        for tt in range(TG // 128):
            tg = tt // 4
            pl, plk = C.ps.next()
            for k in range(8):
                P.op("pe", lambda e, k=k, pl=pl, tt=tt: e.matmul(pl[:, 0:8], lhsT=hT[:, k, tt * 128:(tt + 1) * 128], rhs=wrg[:, k, :],
                                                             start=(k == 0), stop=(k == 7)), R=[hk(k, tg), "wrg"], W=[plk])
            pss, pssk = C.ps.next()
            sq, sqk = C.sq.next()
            P.op("act", lambda e, sq=sq, tt=tt: e.activation(out=sq[:, :, 0:128], in_=hT[:, :, tt * 128:(tt + 1) * 128], func=AF.Square),
                 R=[hk(k, tg) for k in range(8)], W=[sqk])
            for k in range(8):
                P.op("pe", lambda e, k=k, pss=pss, sq=sq: e.matmul(pss[:, 0:1], lhsT=sq[:, k, 0:128], rhs=C.ones_bf[:, 0:1],
                                                               start=(k == 0), stop=(k == 7)), R=[sqk, "ones_bf"], W=[pssk])
            s, sk = sm.next()
            P.op("act", lambda e, s=s, pss=pss: e.activation(out=s[:, 0:1], in_=pss[:, 0:1], func=AF.Sqrt, bias=C.eps_t[:, 0:1], scale=1.0 / 1024),
                 R=[pssk, "eps_t"], W=[sk])
            P.op("dve", lambda e, s=s: e.reciprocal(out=s[:, 0:1], in_=s[:, 0:1]), R=[sk], W=[sk])
            P.op("dve", lambda e, s=s, pl=pl: e.tensor_scalar(out=s[:, 8:16], in0=pl[:, 0:8], scalar1=s[:, 0:1], scalar2=None, op0=ALU.mult),
                 R=[sk, plk], W=[sk])
            P.op("dve", lambda e, s=s: e.max(out=s[:, 16:24], in_=s[:, 8:16]), R=[sk], W=[sk])
            P.op("dve", lambda e, s=s: e.tensor_scalar(out=s[:, 24:25], in0=s[:, 16:17], scalar1=-1.0, scalar2=None, op0=ALU.mult), R=[sk], W=[sk])
            P.op("act", lambda e, s=s: e.activation(out=s[:, 32:40], in_=s[:, 8:16], func=AF.Exp, bias=s[:, 24:25], scale=1.0), R=[sk], W=[sk])
            P.op("dve", lambda e, s=s: e.tensor_scalar(out=s[:, 40:48], in0=s[:, 8:16], scalar1=s[:, 17:18], scalar2=None, op0=ALU.is_ge), R=[sk], W=[sk])
            P.op("dve", lambda e, s=s: e.tensor_tensor(out=s[:, 32:40], in0=s[:, 32:40], in1=s[:, 40:48], op=ALU.mult), R=[sk], W=[sk])
            P.op("dve", lambda e, s=s: e.reduce_sum(out=s[:, 48:49], in_=s[:, 32:40], axis=AX.X), R=[sk], W=[sk])
            P.op("dve", lambda e, s=s: e.reciprocal(out=s[:, 48:49], in_=s[:, 48:49]), R=[sk], W=[sk])
            P.op("dve", lambda e, s=s: e.tensor_scalar(out=s[:, 32:40], in0=s[:, 32:40], scalar1=s[:, 48:49], scalar2=None, op0=ALU.mult), R=[sk], W=[sk])
            pt, ptk = C.ps.next()
            P.op("pe", lambda e, pt=pt, s=s: e.transpose(out=pt[0:8, 0:128], in_=s[:, 32:40], identity=C.ident_f[:]), R=[sk, "ident_f"], W=[ptk])
            P.op("act", lambda e, pt=pt, tt=tt: e.copy(out=gT[0:8, tt * 128:(tt + 1) * 128], in_=pt[0:8, 0:128]), R=[ptk], W=[("gT", tg)])
        for ex in range(n_exp):
            for tg in range(NTG):
                pb, pbk = C.ps.next()
                P.op("pe", lambda e, pb=pb, ex=ex, tg=tg: e.matmul(pb[:, :], lhsT=sel[0:8, ex, :], rhs=gT[0:8, tg * 512:(tg + 1) * 512],
                                                               start=True, stop=True), R=["sel", ("gT", tg)], W=[pbk])
                P.op("act", lambda e, pb=pb, tg=tg: e.copy(out=gbc[:, tg * 512:(tg + 1) * 512], in_=pb[:, :]), R=[pbk], W=[("gbc", tg)])
            emit_ffn(C, xnT, xk, hT, hk, TG, wg[ex], wu[ex], wd[ex], pools, gate_bc=(gbc, lambda tg: ("gbc", tg)))
        emit_store_fm_to_tm(C, hT, hk, out[t0:t0 + TG, :], TG // 128, stage)
    P.finish("sp")
    P.emit()
    return nc


def build_L1(T):
    nc = bass.Bass("TRN2", target_bir_lowering=False)
    dt = lambda n, s, k="ExternalInput": nc.dram_tensor(n, s, F32, kind=k).ap()
    x = dt("x", [T, 1024])
    n0 = dt("att_norm", [1024])
    w_in = dt("w_in", [1024, 1952])
    nq = dt("q_lat_norm", [256])
    w_qup = dt("w_q_up", [256, 768])
    nkv = dt("kv_lat_norm", [128])
    w_kvup = dt("w_kv_up", [128, 1024])
    gq = dt("q_norm", [96])
    gk = dt("k_norm", [96])
    cq_t = dt("cq_t", [96, T]); sq_t = dt("sq_t", [96, T])
    ck_t = dt("ck_t", [96, T]); sk_t = dt("sk_t", [96, T])
    pm = dt("pm", [96, 96])
    sbqT = dt("sbqT", [512, T], "ExternalOutput")
    sbkT = dt("sbkT", [512, T], "ExternalOutput")
    sbv = dt("sbv", [T, 512], "ExternalOutput")
    mqT = dt("mqT", [8, 96, T], "ExternalOutput")
    mkT = dt("mkT", [8, 96, T], "ExternalOutput")
    mv = dt("mv", [T, 512], "ExternalOutput")
    C = Ctx(nc)
    P = C.P
    A = nc.alloc_sbuf_tensor
    hT = A("hT", [128, 8, 512], F32)
    xnT = A("xnT", [128, 8, 512], BF16)
    win = A("win", [128, 8, 1952], BF16)
    wkr = A("wkr", [128, 8, 96], BF16)
    wqup = A("wqup", [128, 2, 768], BF16)
    wkn = A("wkn", [128, 8, 96], BF16)
    wkv = A("wkv", [128, 8, 64], BF16)
    pmt = A("pmt", [96, 96], BF16)
    lat = A("lat", [128, 3, 512], F32)
    latn = A("latn", [128, 3, 512], BF16)
    krp = A("krp", [96, 512], F32)
    hr = A("hr", [96, 1, 512], F32)
    hn = A("hn", [96, 1, 512], BF16)
    tabs = A("tabs", [96, 4, 512], F32)
    t1 = Rot(nc, "t1", [96, 512], F32, 2)
    t2 = Rot(nc, "t2", [96, 512], F32, 2)
    ob = Rot(nc, "ob", [128, 512], F32, 3)
    stage = Rot(nc, "stage", [128, 1024], F32, 2)
    g0 = load_vec_fm(C, "g0", n0, 1024)
    gql = load_vec_fm(C, "gql", nq, 256)
    gkl = load_vec_fm(C, "gkl", nkv, 128)
    gqh = load_vec_fm(C, "gqh", gq, 96)
    gkh = load_vec_fm(C, "gkh", gk, 96)
    P.dma("pool", win[:], w_in.rearrange("(k p) n -> p k n", p=128), W=["win"])
    P.op("pool", lambda e: e.memset(wkr[:], 0.0), W=["wkr"])
    P.dma("pool", wkr[:, :, 64:96], w_in[:, 1920:1952].rearrange("(k p) n -> p k n", p=128), R=["wkr"], W=["wkr"])
    P.dma("pool", wqup[:], w_qup.rearrange("(k p) n -> p k n", p=128), W=["wqup"])
    P.op("pool", lambda e: e.memset(wkn[:], 0.0), W=["wkn"])
    P.dma("pool", wkn[:, :, 0:64], w_kvup.rearrange("k (h c) -> k h c", c=128)[:, :, 0:64], R=["wkn"], W=["wkn"])
    P.dma("pool", wkv[:], w_kvup.rearrange("k (h c) -> k h c", c=128)[:, :, 64:128], W=["wkv"])
    P.dma("pool", pmt[:], pm, W=["pmt"])
    hk = lambda k, tg: ("h", k)
    for tg in range(T // 512):
        t0 = tg * 512
        emit_load_tm_to_fm(C, x[t0:t0 + 512, :], hT, hk, 4, stage)
        emit_rmsnorm_fm(C, hT, [hk(k, 0) for k in range(8)], 8, 0, 512, g0, "g0", xnT, [("xn", k) for k in range(8)], 0, 1024)
        XR = [("xn", k) for k in range(8)]
        for i, tab in enumerate((cq_t, sq_t, ck_t, sk_t)):
            P.dma("sp", tabs[:, i, :], tab[:, t0:t0 + 512], W=[("tabs", i)])

        def proj_fm(col0, ncols, wt=win, wkey="win"):
            po, pok = C.ps.next()
            for k in range(8):
                P.op("pe", lambda e, k=k, po=po: e.matmul(po[0:ncols, :], lhsT=wt[:, k, col0:col0 + ncols], rhs=xnT[:, k, :],
                                                          start=(k == 0), stop=(k == 7)), R=[wkey] + XR, W=[pok])
            return po, pok
        for c in range(8):
            po, pok = proj_fm(c * 128, 128)
            o, okk = ob.next()
            P.op("act", lambda e, o=o, po=po: e.copy(out=o[:, :], in_=po[:, :]), R=[pok], W=[okk])
            dst = sbqT if c < 4 else sbkT
            P.dma("sp", dst[(c % 4) * 128:(c % 4 + 1) * 128, t0:t0 + 512], o[:, :], R=[okk], W=[("o1", c, tg)])
        for tt in range(4):
            po, pok = C.ps.next()
            for k in range(8):
                P.op("pe", lambda e, k=k, po=po, tt=tt: e.matmul(po[:, :], lhsT=xnT[:, k, tt * 128:(tt + 1) * 128], rhs=win[:, k, 1024:1536],
                                                             start=(k == 0), stop=(k == 7)), R=["win"] + XR, W=[pok])
            o, okk = ob.next()
            P.op("dve", lambda e, o=o, po=po: e.tensor_copy(out=o[:, :], in_=po[:, :]), R=[pok], W=[okk])
            P.dma("sp", sbv[t0 + tt * 128:t0 + (tt + 1) * 128, :], o[:, :], R=[okk], W=[("o2", tt, tg)])
        for c in range(3):
            po, pok = proj_fm(1536 + c * 128, 128)
            P.op("act", lambda e, po=po, c=c: e.copy(out=lat[:, c, :], in_=po[:, :]), R=[pok], W=[("lat", c)])
        emit_rmsnorm_fm(C, lat, [("lat", 0), ("lat", 1)], 2, 0, 512, gql, "gql", latn, [("latn", 0), ("latn", 1)], 0, 256)
        emit_rmsnorm_fm(C, lat[:, 2:3, :], [("lat", 2)], 1, 0, 512, gkl, "gkl", latn[:, 2:3, :], [("latn", 2)], 0, 128)
        po, pok = proj_fm(0, 96, wt=wkr, wkey="wkr")
        P.op("act", lambda e, po=po: e.copy(out=krp[:, :], in_=po[0:96, :]), R=[pok], W=["krp"])
        for tt in range(4):
            po, pok = C.ps.next()
            P.op("pe", lambda e, po=po, tt=tt: e.matmul(po[:, :], lhsT=latn[:, 2, tt * 128:(tt + 1) * 128], rhs=wkv[:, :, :],
                                                    start=True, stop=True), R=["wkv", ("latn", 2)], W=[pok])
            o, okk = ob.next()
            P.op("dve", lambda e, o=o, po=po: e.tensor_copy(out=o[:, :], in_=po[:, :]), R=[pok], W=[okk])
            P.dma("sp", mv[t0 + tt * 128:t0 + (tt + 1) * 128, :], o[:, :], R=[okk], W=[("o3", tt, tg)])
        for h in range(8):
            for which in range(2):
                po, pok = C.ps.next()
                if which == 0:
                    for k in range(2):
                        P.op("pe", lambda e, k=k, po=po, h=h: e.matmul(po[0:96, :], lhsT=wqup[:, k, h * 96:(h + 1) * 96], rhs=latn[:, k, :],
                                                                   start=(k == 0), stop=(k == 1)), R=["wqup", ("latn", 0), ("latn", 1)], W=[pok])
                    P.op("act", lambda e, po=po: e.copy(out=hr[:, 0, :], in_=po[0:96, :]), R=[pok], W=["hr"])
                else:
                    P.op("pe", lambda e, po=po, h=h: e.matmul(po[0:96, :], lhsT=wkn[:, h, :], rhs=latn[:, 2, :], start=True, stop=True),
                         R=["wkn", ("latn", 2)], W=[pok])
                    P.op("dve", lambda e, po=po: e.tensor_tensor(out=hr[:, 0, :], in0=po[0:96, :], in1=krp[:, :], op=ALU.add),
                         R=[pok, "krp"], W=["hr"])
                emit_rmsnorm_fm(C, hr, ["hr"], 1, 0, 512, gqh if which == 0 else gkh, "gqh" if which == 0 else "gkh",
                                hn, ["hn"], 0, 96, npart=96)
                pp, ppk = C.ps.next()
                P.op("pe", lambda e, pp=pp: e.matmul(pp[0:96, :], lhsT=pmt[:, :], rhs=hn[:, 0, :], start=True, stop=True), R=["pmt", "hn"], W=[ppk])
                a, ak = t1.next()
                b, bk = t2.next()
                ci, si = (0, 1) if which == 0 else (2, 3)
                P.op("pool", lambda e, a=a, ci=ci: e.tensor_tensor(out=a[:, :], in0=hn[:, 0, :], in1=tabs[:, ci, :], op=ALU.mult),
                     R=["hn", ("tabs", ci)], W=[ak])
                P.op("dve", lambda e, b=b, pp=pp, si=si: e.tensor_tensor(out=b[:, :], in0=pp[0:96, :], in1=tabs[:, si, :], op=ALU.mult),
                     R=[ppk, ("tabs", si)], W=[bk])
                P.op("pool", lambda e, a=a, b=b: e.tensor_tensor(out=a[:, :], in0=a[:, :], in1=b[:, :], op=ALU.add), R=[ak, bk], W=[ak])
                dst = mqT if which == 0 else mkT
                P.dma("sp", dst[h, :, t0:t0 + 512], a[:, :], R=[ak], W=[("o4", h, which, tg)])
    P.finish("sp")
    P.emit()
    return nc


def rope_tables(S):
    inv_freq = (10000.0 ** (-np.arange(0, 32, 2, dtype=np.float32) / np.float32(32))).astype(np.float32)
    ang = (np.arange(S, dtype=np.float32)[:, None] * inv_freq[None, :]).astype(np.float32)
    cos = np.cos(ang).astype(np.float32).T
    sin = np.sin(ang).astype(np.float32).T
    Ct = np.ones((96, S), np.float32); St = np.zeros((96, S), np.float32)
    Ct[64:80] = cos; Ct[80:96] = cos
    St[64:80] = sin; St[80:96] = sin
    pm = np.zeros((96, 96), np.float32)
    for i in range(16):
        pm[80 + i, 64 + i] = -1.0
        pm[64 + i, 80 + i] = 1.0
    return Ct, St, pm


def build_L2(S, n_sb=2, n_mla=2):
    nc = bass.Bass("TRN2", target_bir_lowering=False)
    dt = lambda n, s, k="ExternalInput": nc.dram_tensor(n, s, F32, kind=k).ap()
    sbqT = dt("sbqT", [2, 64, S]); sbkT = dt("sbkT", [2, 64, S]); sbv = dt("sbv", [2, S, 64])
    mqT = dt("mqT", [2, 96, S]); mkT = dt("mkT", [2, 96, S]); mv = dt("mv", [2, S, 64])
    oT = dt("oT", [4, 64, S], "ExternalOutput")
    C = Ctx(nc, n_ps=3)
    P = C.P
    A = nc.alloc_sbuf_tensor
    NB = S // 128
    NQG = S // 512
    argp = Rot(nc, "argp", [128, 512], F32, 3, psum=True)
    acc = Rot(nc, "acc", [128, 512], F32, 2, psum=True)
    qT = A("qT", [128, S], BF16)
    kT = A("kT", [128, S], BF16)
    va = A("va", [128, NB, 65], BF16)
    mle = A("mle", [128, 4, 512], BF16)
    mlt = A("mlt", [128, 4, 512], BF16)
    for c4 in range(4):
        sl = slice(c4 * (S // 4), (c4 + 1) * (S // 4))
        P.op("pool", lambda e, sl=sl: e.memset(qT[:, sl], 0.0), W=[("qT", c4)])
        P.op("pool", lambda e, sl=sl: e.memset(kT[:, sl], 0.0), W=[("kT", c4)])
    nuin = A("nuin", [128, 128], BF16)
    nones = A("nones", [128, 128], BF16)
    et = Rot(nc, "et", [128, 512], F32, 2)
    spt = Rot(nc, "spt", [128, 512], BF16, 3)
    wt = Rot(nc, "wt", [128, 512], BF16, 3)
    Rt = Rot(nc, "Rt", [128, 512], BF16, 3)
    ot = Rot(nc, "ot", [128, 512], F32, 2)
    rr = A("rr", [128, 512], F32)
    bcs = A("bcs", [64, 512], F32)
    P.op("pool", lambda e: e.memset(va[:], 1.0), W=["va"])
    P.op("pool", lambda e: e.memset(mle[:], 1.0), W=["mle"])
    P.op("pool", lambda e: e.memset(mlt[:], 1.0), W=["mlt"])
    P.op("pool", lambda e: e.memset(nuin[:], -1.0), W=["nuin"])
    P.op("pool", lambda e: e.memset(nones[:], -1.0), W=["nones"])
    for d in range(4):
        P.op("pool", lambda e, d=d: e.affine_select(out=mle[:, d, :], in_=mle[:, d, :], pattern=[[1, 512]], compare_op=ALU.is_ge, fill=0.0,
                                                    base=-128 * d, channel_multiplier=-1), R=["mle"], W=["mle"])
        P.op("pool", lambda e, d=d: e.affine_select(out=mlt[:, d, :], in_=mlt[:, d, :], pattern=[[1, 512]], compare_op=ALU.is_gt, fill=0.0,
                                                    base=-128 * d, channel_multiplier=-1), R=["mlt"], W=["mlt"])
    P.op("pool", lambda e: e.affine_select(out=nuin[:], in_=nuin[:], pattern=[[-1, 128]], compare_op=ALU.is_ge, fill=0.0,
                                           base=0, channel_multiplier=1), R=["nuin"], W=["nuin"])
    CW = S // 4
    qkeys = lambda qg: [("qT", c) for c in range((qg * 512) // CW, (qg * 512 + 511) // CW + 1)]
    kkeys = lambda kb: [("kT", c) for c in range((kb * 128) // CW, (kb * 128 + 127) // CW + 1)]
    for hd in range(n_sb + n_mla):
        is_sb = hd < n_sb
        hh = hd if is_sb else hd - n_sb
        dq = 64 if is_sb else 96
        qsrc, ksrc, vsrc = (sbqT, sbkT, sbv) if is_sb else (mqT, mkT, mv)
        for c4 in range(4):
            sl = slice(c4 * (S // 4), (c4 + 1) * (S // 4))
            P.dma("pool", qT[0:dq, sl], qsrc[hh, :, sl], W=[("qT", c4)])
            if is_sb:
                P.op("dve", lambda e, sl=sl: e.tensor_scalar(out=qT[0:64, sl], in0=qT[0:64, sl], scalar1=0.125, scalar2=None, op0=ALU.mult),
                     R=[("qT", c4)], W=[("qT", c4)])
            P.dma("pool", kT[0:dq, sl], ksrc[hh, :, sl], W=[("kT", c4)])
        P.dma("pool", va[:, :, 0:64], vsrc[hh].rearrange("(kb p) c -> p kb c", p=128), R=["va"], W=["va"])
        blocks = []
        for qg in range(NQG):
            nkb = 4 * (qg + 1)
            order = range(nkb - 1, -1, -1) if is_sb else range(nkb)
            for i, kb in enumerate(order):
                blocks.append((qg, i, kb, nkb))
        nblk = len(blocks)
        st = {}
        qstate = {}

        def stage_z(t):
            qg, i, kb, nkb = blocks[t]
            zp, zpk = C.ps.next()
            P.op("pe", lambda e: e.matmul(zp[:, :], lhsT=kT[0:dq, kb * 128:(kb + 1) * 128], rhs=qT[0:dq, qg * 512:(qg + 1) * 512],
                                          start=True, stop=True), R=qkeys(qg) + kkeys(kb), W=[zpk])
            st[t] = dict(zp=zp, zpk=zpk)

        def stage_a_sb(t):
            qg, i, kb, nkb = blocks[t]
            d = kb - 4 * qg
            s_ = st[t]
            zp, zpk = s_["zp"], s_["zpk"]
            e_, ek = et.next()
            P.op("act", lambda e: e.activation(out=e_[:, :], in_=zp[:, :], func=AF.Exp), R=[zpk], W=[ek])
            s_["e"] = (e_, ek)

        def stage_a2_sb(t):
            qg, i, kb, nkb = blocks[t]
            d = kb - 4 * qg
            s_ = st[t]
            e_, ek = s_["e"]
            sp, spk = spt.next()
            P.op("act", lambda e: e.activation(out=sp[:, :], in_=e_[:, :], func=AF.Ln, bias=C.ones_f[:, 0:1], scale=1.0),
                 R=[ek, "ones_f"], W=[spk])
            if d >= 0:
                P.op("pool", lambda e: e.tensor_tensor(out=sp[:, :], in0=sp[:, :], in1=mlt[:, d, :], op=ALU.mult), R=[spk, "mlt"], W=[spk])
            ap_, apk = argp.next()
            P.op("pe", lambda e: e.matmul(ap_[:, :], lhsT=kT[:, kb * 128:(kb + 1) * 128], rhs=qT[:, qg * 512:(qg + 1) * 512],
                                          start=True, stop=False), R=qkeys(qg) + kkeys(kb), W=[apk])
            P.op("pe", lambda e: e.matmul(ap_[:, :], lhsT=nuin[:, :], rhs=sp[:, :], start=False, stop=(i == 0)), R=[spk, "nuin"], W=[apk])
            if i > 0:
                Rp, Rpk = qstate[qg]["R"]
                P.op("pe", lambda e: e.matmul(ap_[:, :], lhsT=nones[:, :], rhs=Rp[:, :], start=False, stop=True), R=[Rpk, "nones"], W=[apk])
            if kb > 0:
                Rn, Rnk = Rt.next()
                if i == 0:
                    P.op("pool", lambda e: e.tensor_copy(out=Rn[:, :], in_=sp[:, :]), R=[spk], W=[Rnk])
                else:
                    Rp, Rpk = qstate[qg]["R"]
                    P.op("pool", lambda e: e.tensor_tensor(out=Rn[:, :], in0=Rp[:, :], in1=sp[:, :], op=ALU.add), R=[spk, Rpk], W=[Rnk])
                qstate.setdefault(qg, {})["R"] = (Rn, Rnk)
            s_["arg"] = (ap_, apk)

        def stage_b(t):
            qg, i, kb, nkb = blocks[t]
            d = kb - 4 * qg
            s_ = st[t]
            if i == 0:
                qstate.setdefault(qg, {})["acc"] = acc.next()
            op_, opk = qstate[qg]["acc"]
            src, srck = s_["arg"] if is_sb else (s_["zp"], s_["zpk"])
            w_, wk_ = wt.next()
            P.op("act", lambda e: e.activation(out=w_[:, :], in_=src[:, :], func=AF.Exp), R=[srck], W=[wk_])
            if d >= 0:
                mk_ = mlt if is_sb else mle
                P.op("dve", lambda e: e.tensor_tensor(out=w_[:, :], in0=w_[:, :], in1=mk_[:, d, :], op=ALU.mult),
                     R=[wk_, "mlt" if is_sb else "mle"], W=[wk_])
            last = (i == nkb - 1)
            nv = 64 if is_sb else 65
            P.op("pe", lambda e: e.matmul(op_[0:nv, :], lhsT=va[:, kb, 0:nv], rhs=w_[:, :], start=(i == 0), stop=last), R=[wk_, "va"], W=[opk])
            if last:
                o_, ok_ = ot.next()
                if is_sb:
                    P.op("dve", lambda e: e.tensor_copy(out=o_[0:64, :], in_=op_[0:64, :]), R=[opk], W=[ok_])
                else:
                    P.op("dve", lambda e: e.reciprocal(out=rr[64:65, :], in_=op_[64:65, :]), R=[opk], W=["rr"])
                    bc, bck = argp.next()
                    P.op("pe", lambda e: e.matmul(bc[0:64, :], lhsT=C.ones_f[64:65, 0:64], rhs=rr[64:65, :], start=True, stop=True),
                         R=["rr", "ones_f"], W=[bck])
                    P.op("dve", lambda e: e.tensor_copy(out=bcs[:, :], in_=bc[0:64, :]), R=[bck], W=["bcs"])
                    P.op("dve", lambda e: e.tensor_tensor(out=o_[0:64, :], in0=op_[0:64, :], in1=bcs[:, :], op=ALU.mult), R=[opk, "bcs"], W=[ok_])
                P.dma("sp", oT[hd, :, qg * 512:(qg + 1) * 512], o_[0:64, :], R=[ok_], W=[("oo", hd, qg)])
            del st[t]

        for t in range(-2, nblk):
            if 0 <= t + 2 < nblk:
                stage_z(t + 2)
            if is_sb:
                if 0 <= t + 1 < nblk:
                    stage_a_sb(t + 1)
                if 0 <= t:
                    stage_b(t)
                if 0 <= t + 1 < nblk:
                    stage_a2_sb(t + 1)
            else:
                if 0 <= t:
                    stage_b(t)
    P.finish("sp")
    P.emit()
    return nc


TWO_PI = 6.283185307179586
PI = 3.141592653589793
LCH = 512


def build_L4(S):
    nc = bass.Bass("TRN2", target_bir_lowering=False)
    dt = lambda n, s, k="ExternalInput": nc.dram_tensor(n, s, F32, kind=k).ap()
    uT = dt("uT", [128, S])
    a_re = dt("a_re", [128, 4]); a_im = dt("a_im", [128, 4]); ldt = dt("ldt", [128, 4])
    b_re = dt("b_re", [4, 128, 16]); b_im = dt("b_im", [4, 128, 16])
    ct_re = dt("ct_re", [4, 128, 16]); ct_im = dt("ct_im", [4, 128, 16])
    dsk = dt("dsk", [128])
    yT = dt("yT", [128, S], "ExternalOutput")
    C = Ctx(nc)
    P = C.P
    A = nc.alloc_sbuf_tensor
    NCH = S // LCH
    ub = A("ub", [128, S], BF16)
    P.dma("pool", ub[:], uT, W=["ub"])
    par = A("par", [128, 16, 4], F32)
    AR, AI, DT, ARD, TH, LRE, LIM, NUM, DEN, CRE, CIM, TMP, TMP2, MRE, MIM, NMIM = range(16)
    pk = lambda i: ("par", i)
    P.dma("sp", par[:, AR, :], a_re, W=[pk(AR)])
    P.dma("sp", par[:, AI, :], a_im, W=[pk(AI)])
    P.dma("sp", par[:, DT, :], ldt, W=[pk(DT)])
    dvec = load_vec_fm(C, "dvec", dsk, 128)
    cpi = A("cpi", [128, 1], F32)
    P.op("pool", lambda e: e.memset(cpi[:], PI), W=["cpi"])
    bst = A("bst", [128, 4, 4, 16], F32)
    for i, src in enumerate((b_re, b_im, ct_re, ct_im)):
        P.dma("sp", bst[:, i, :, :], src.rearrange("j p c -> p j c"), W=[("bst", i)], allow_slow_non_contiguous=True)
    io_i = A("io_i", [128, LCH], I32)
    io_f = A("io_f", [128, LCH], F32)
    P.op("pool", lambda e: e.iota(io_i[:], pattern=[[1, LCH]], base=0, channel_multiplier=0), W=["io_i"])
    P.op("dve", lambda e: e.tensor_copy(out=io_f[:], in_=io_i[:]), R=["io_i"], W=["io_f"])
    onesL = A("onesL", [128, LCH], F32)
    P.op("pool", lambda e: e.memset(onesL[:], 1.0), W=["onesL"])

    def ts(out, in0, s1, s2, o0, o1=None, R=(), W=()):
        if o1 is None:
            P.op("dve", lambda e: e.tensor_scalar(out=out, in0=in0, scalar1=s1, scalar2=None, op0=o0), R=R, W=W)
        else:
            P.op("dve", lambda e: e.tensor_scalar(out=out, in0=in0, scalar1=s1, scalar2=s2, op0=o0, op1=o1), R=R, W=W)

    def tt(out, in0, in1, o, R=(), W=(), eng="dve"):
        P.op(eng, lambda e: e.tensor_tensor(out=out, in0=in0, in1=in1, op=o), R=R, W=W)

    pv = lambda i: par[:, i, :]
    P.op("act", lambda e: e.activation(out=pv(DT), in_=pv(DT), func=AF.Exp), R=[pk(DT)], W=[pk(DT)])
    ts(pv(AR), pv(AR), -1e-4, None, ALU.min, R=[pk(AR)], W=[pk(AR)])
    tt(pv(ARD), pv(AR), pv(DT), ALU.mult, R=[pk(AR), pk(DT)], W=[pk(ARD)])
    tt(pv(TH), pv(AI), pv(DT), ALU.mult, R=[pk(AI), pk(DT)], W=[pk(TH)])
    tab = A("tab", [128, 4, 4, LCH], F32)
    scr = Rot(nc, "scr", [128, LCH], F32, 8)
    scri = Rot(nc, "scri", [128, LCH], I32, 2)
    nard = A("nard", [128, 4], F32)
    ts(nard[:, :], pv(ARD), -1.0, None, ALU.mult, R=[pk(ARD)], W=["nard"])
    def sin_of(ang, angk):
        t, tk = scr.next()
        ki, kik = scri.next()
        ts(t[:, :], ang[:, :], 1.0 / TWO_PI, None, ALU.mult, R=[angk], W=[tk])
        P.op("dve", lambda e: e.tensor_copy(out=ki[:, :], in_=t[:, :]), R=[tk], W=[kik])
        P.op("dve", lambda e: e.tensor_copy(out=t[:, :], in_=ki[:, :]), R=[kik], W=[tk])
        P.op("dve", lambda e: e.scalar_tensor_tensor(out=ang[:, :], in0=t[:, :], scalar=-TWO_PI, in1=ang[:, :], op0=ALU.mult, op1=ALU.add),
             R=[tk, angk], W=[angk])
        ts(t[:, :], ang[:, :], PI, -TWO_PI, ALU.is_gt, ALU.mult, R=[angk], W=[tk])
        tt(ang[:, :], ang[:, :], t[:, :], ALU.add, R=[angk, tk], W=[angk])
        ts(t[:, :], ang[:, :], -PI, TWO_PI, ALU.is_lt, ALU.mult, R=[angk], W=[tk])
        tt(ang[:, :], ang[:, :], t[:, :], ALU.add, R=[angk, tk], W=[angk])
        ts(ang[:, :], ang[:, :], PI, -PI, ALU.min, ALU.max, R=[angk], W=[angk])
        P.op("act", lambda e: e.activation(out=t[:, :], in_=ang[:, :], func=AF.Sin), R=[angk], W=[tk])
        return t, tk

    for j in range(4):
        ang, angk = scr.next()
        ts(ang[:, :], io_f[:, :], par[:, TH, j:j + 1], None, ALU.mult, R=["io_f", pk(TH)], W=[angk])
        sn, snk = sin_of(ang, angk)
        ang2, ang2k = scr.next()
        ts(ang2[:, :], io_f[:, :], par[:, TH, j:j + 1], PI / 2, ALU.mult, ALU.add, R=["io_f", pk(TH)], W=[ang2k])
        cs, csk = sin_of(ang2, ang2k)
        mg, mgk = scr.next()
        P.op("act", lambda e, mg=mg, j=j: e.activation(out=mg[:, :], in_=io_f[:, :], func=AF.Exp, scale=par[:, ARD, j:j + 1]),
             R=["io_f", pk(ARD)], W=[mgk])
        tt(tab[:, 2, j, :], mg[:, :], cs[:, :], ALU.mult, R=[mgk, csk], W=[("tab", 2, j)])
        tt(tab[:, 3, j, :], mg[:, :], sn[:, :], ALU.mult, R=[mgk, snk], W=[("tab", 3, j)])
        mg2, mg2k = scr.next()
        P.op("act", lambda e, mg2=mg2, j=j: e.activation(out=mg2[:, :], in_=io_f[:, :], func=AF.Exp, scale=nard[:, j:j + 1]),
             R=["io_f", "nard"], W=[mg2k])
        tt(tab[:, 0, j, :], mg2[:, :], cs[:, :], ALU.mult, R=[mg2k, csk], W=[("tab", 0, j)])
        P.op("dve", lambda e, mg2=mg2, sn=sn, j=j: e.scalar_tensor_tensor(out=tab[:, 1, j, :], in0=mg2[:, :], scalar=-1.0, in1=sn[:, :],
                                                                          op0=ALU.mult, op1=ALU.mult), R=[mg2k, snk], W=[("tab", 1, j)])
    for j in range(4):
        P.op("dve", lambda e, j=j: e.tensor_copy(out=par[:, LRE, j:j + 1], in_=tab[:, 2, j, 1:2]), R=[("tab", 2, j)], W=[pk(LRE)])
        P.op("dve", lambda e, j=j: e.tensor_copy(out=par[:, LIM, j:j + 1], in_=tab[:, 3, j, 1:2]), R=[("tab", 3, j)], W=[pk(LIM)])
    for j in range(4):
        l5r = tab[:, 2, j, LCH - 1:LCH]; l5i = tab[:, 3, j, LCH - 1:LCH]
        RK = [pk(LRE), pk(LIM), ("tab", 2, j), ("tab", 3, j)]
        tt(par[:, TMP, j:j + 1], par[:, LRE, j:j + 1], l5r, ALU.mult, R=RK, W=[pk(TMP)])
        tt(par[:, TMP2, j:j + 1], par[:, LIM, j:j + 1], l5i, ALU.mult, R=RK, W=[pk(TMP2)])
        tt(par[:, MRE, j:j + 1], par[:, TMP, j:j + 1], par[:, TMP2, j:j + 1], ALU.subtract, R=[pk(TMP), pk(TMP2)], W=[pk(MRE)])
        tt(par[:, TMP, j:j + 1], par[:, LRE, j:j + 1], l5i, ALU.mult, R=RK + [pk(MRE)], W=[pk(TMP)])
        tt(par[:, TMP2, j:j + 1], par[:, LIM, j:j + 1], l5r, ALU.mult, R=RK + [pk(MRE)], W=[pk(TMP2)])
        tt(par[:, MIM, j:j + 1], par[:, TMP, j:j + 1], par[:, TMP2, j:j + 1], ALU.add, R=[pk(TMP), pk(TMP2)], W=[pk(MIM)])
    ts(pv(NMIM), pv(MIM), -1.0, None, ALU.mult, R=[pk(MIM)], W=[pk(NMIM)])
    ts(pv(NUM), pv(LRE), -1.0, None, ALU.add, R=[pk(LRE)], W=[pk(NUM)])
    tt(pv(DEN), pv(AR), pv(AR), ALU.mult, R=[pk(AR)], W=[pk(DEN)])
    tt(pv(TMP), pv(AI), pv(AI), ALU.mult, R=[pk(AI), pk(MIM), pk(MRE)], W=[pk(TMP)])
    tt(pv(DEN), pv(DEN), pv(TMP), ALU.add, R=[pk(DEN), pk(TMP)], W=[pk(DEN)])
    P.op("dve", lambda e: e.reciprocal(out=pv(DEN), in_=pv(DEN)), R=[pk(DEN)], W=[pk(DEN)])
    tt(pv(TMP), pv(NUM), pv(AR), ALU.mult, R=[pk(NUM), pk(AR), pk(DEN)], W=[pk(TMP)])
    tt(pv(TMP2), pv(LIM), pv(AI), ALU.mult, R=[pk(LIM), pk(AI), pk(NMIM)], W=[pk(TMP2)])
    tt(pv(CRE), pv(TMP), pv(TMP2), ALU.add, R=[pk(TMP), pk(TMP2)], W=[pk(CRE)])
    tt(pv(CRE), pv(CRE), pv(DEN), ALU.mult, R=[pk(CRE), pk(DEN)], W=[pk(CRE)])
    tt(pv(TMP), pv(LIM), pv(AR), ALU.mult, R=[pk(LIM), pk(AR), pk(CRE)], W=[pk(TMP)])
    tt(pv(TMP2), pv(NUM), pv(AI), ALU.mult, R=[pk(NUM), pk(AI), pk(CRE)], W=[pk(TMP2)])
    tt(pv(CIM), pv(TMP), pv(TMP2), ALU.subtract, R=[pk(TMP), pk(TMP2)], W=[pk(CIM)])
    tt(pv(CIM), pv(CIM), pv(DEN), ALU.mult, R=[pk(CIM), pk(DEN)], W=[pk(CIM)])
    bfull = A("bfull", [128, 2, 4, 128], F32)
    P.op("pool", lambda e: e.memset(bfull[:], 0.0), W=["bfull"])
    BT = A("BT", [128, 2, 4, 128], BF16)
    CTt = A("CTt", [128, 2, 4, 128], BF16)
    P.op("pool", lambda e: e.memset(CTt[:], 0.0), W=["CTt"])
    t16 = Rot(nc, "t16", [128, 16], F32, 4)
    for j in range(4):
        for g in range(2):
            ps_ = slice(g * 64, (g + 1) * 64)
            c0 = 32 * j + 16 * g
            for which in range(2):
                ta, tak = t16.next()
                tb, tbk = t16.next()
                s_a = bst[ps_, 0 if which == 0 else 1, j, :]
                s_b = bst[ps_, 1 if which == 0 else 0, j, :]
                ts(ta[ps_, :], s_a, par[ps_, CRE, j:j + 1], None, ALU.mult, R=[("bst", 0), ("bst", 1), pk(CRE)], W=[tak])
                ts(tb[ps_, :], s_b, par[ps_, CIM, j:j + 1], None, ALU.mult, R=[("bst", 0), ("bst", 1), pk(CIM)], W=[tbk])
                tt(bfull[ps_, which, j, c0:c0 + 16], ta[ps_, :], tb[ps_, :], ALU.subtract if which == 0 else ALU.add,
                   R=[tak, tbk, "bfull"], W=["bfull"])
            P.op("dve", lambda e, ps_=ps_, j=j, c0=c0: e.tensor_copy(out=CTt[ps_, 0, j, c0:c0 + 16], in_=bst[ps_, 2, j, :]),
                 R=[("bst", 2), "CTt"], W=["CTt"])
            ts(CTt[ps_, 1, j, c0:c0 + 16], bst[ps_, 3, j, :], -1.0, None, ALU.mult, R=[("bst", 3), "CTt"], W=["CTt"])
    for j in range(4):
        for which in range(2):
            pt, ptk = C.ps.next()
            P.op("pe", lambda e, pt=pt, which=which, j=j: e.transpose(out=pt[:, 0:128], in_=bfull[:, which, j, :], identity=C.ident_f[:]),
                 R=["bfull", "ident_f"], W=[ptk])
            P.op("act", lambda e, pt=pt, which=which, j=j: e.copy(out=BT[:, which, j, :], in_=pt[:, 0:128]), R=[ptk], W=[("BT", which, j)])
    G = A("G", [128, NCH + 1, 4, 2], F32)
    P.op("pool", lambda e: e.memset(G[:], 0.0), W=["G"])
    pt_ = Rot(nc, "pt_", [128, LCH], F32, 4)
    Pre = Rot(nc, "Pre", [128, LCH], F32, 2)
    Pim = Rot(nc, "Pim", [128, LCH], F32, 2)
    Sre = Rot(nc, "Sre", [128, LCH], F32, 2)
    Sim = Rot(nc, "Sim", [128, LCH], F32, 2)
    hre = Rot(nc, "hre", [128, LCH], BF16, 8)
    him = Rot(nc, "him", [128, LCH], BF16, 8)
    yv = Rot(nc, "yv", [128, LCH], F32, 2)
    gt = Rot(nc, "gt", [128, LCH], F32, 2)
    sml = Rot(nc, "sml", [128, 2], F32, 4)
    for ch in range(NCH):
        c0 = ch * LCH
        hs = []
        for j in range(4):
            bre, brek = C.ps.next()
            P.op("pe", lambda e, bre=bre, j=j, c0=c0: e.matmul(bre[:, :], lhsT=BT[:, 0, j, :], rhs=ub[:, c0:c0 + LCH], start=True, stop=True),
                 R=[("BT", 0, j), "ub"], W=[brek])
            bim, bimk = C.ps.next()
            P.op("pe", lambda e, bim=bim, j=j, c0=c0: e.matmul(bim[:, :], lhsT=BT[:, 1, j, :], rhs=ub[:, c0:c0 + LCH], start=True, stop=True),
                 R=[("BT", 1, j), "ub"], W=[bimk])
            a1, a1k = pt_.next(); a2, a2k = pt_.next(); a3, a3k = pt_.next(); a4, a4k = pt_.next()
            tt(a1[:, :], bre[:, :], tab[:, 0, j, :], ALU.mult, R=[brek, ("tab", 0, j)], W=[a1k])
            tt(a2[:, :], bim[:, :], tab[:, 1, j, :], ALU.mult, R=[bimk, ("tab", 1, j)], W=[a2k])
            tt(a3[:, :], bim[:, :], tab[:, 0, j, :], ALU.mult, R=[bimk, ("tab", 0, j)], W=[a3k])
            tt(a4[:, :], bre[:, :], tab[:, 1, j, :], ALU.mult, R=[brek, ("tab", 1, j)], W=[a4k])
            pr, prk = Pre.next(); pi_, pik = Pim.next()
            tt(pr[:, :], a1[:, :], a2[:, :], ALU.subtract, R=[a1k, a2k], W=[prk], eng="pool")
            tt(pi_[:, :], a3[:, :], a4[:, :], ALU.add, R=[a3k, a4k], W=[pik], eng="pool")
            sr, srk = Sre.next(); si, sik = Sim.next()
            P.op("dve", lambda e, sr=sr, pr=pr, ch=ch, j=j: e.tensor_tensor_scan(out=sr[:, :], data0=onesL[:, :], data1=pr[:, :],
                                                                                initial=G[:, ch, j, 0:1], op0=ALU.mult, op1=ALU.add),
                 R=[prk, "onesL", ("G", ch, j), "G"], W=[srk])
            P.op("dve", lambda e, si=si, pi_=pi_, ch=ch, j=j: e.tensor_tensor_scan(out=si[:, :], data0=onesL[:, :], data1=pi_[:, :],
                                                                                  initial=G[:, ch, j, 1:2], op0=ALU.mult, op1=ALU.add),
                 R=[pik, "onesL", ("G", ch, j), "G"], W=[sik])
            sm_, smk = sml.next()
            ts(sm_[:, 0:1], sr[:, LCH - 1:LCH], par[:, MRE, j:j + 1], None, ALU.mult, R=[srk, pk(MRE)], W=[smk])
            ts(sm_[:, 1:2], si[:, LCH - 1:LCH], par[:, MRE, j:j + 1], None, ALU.mult, R=[sik, pk(MRE)], W=[smk])
            P.op("dve", lambda e, sm_=sm_, si=si, ch=ch, j=j: e.scalar_tensor_tensor(out=G[:, ch + 1, j, 0:1], in0=si[:, LCH - 1:LCH],
                                                                                    scalar=par[:, NMIM, j:j + 1], in1=sm_[:, 0:1],
                                                                                    op0=ALU.mult, op1=ALU.add),
                 R=[smk, sik, pk(NMIM), "G"], W=[("G", ch + 1, j, 0)])
            P.op("dve", lambda e, sm_=sm_, sr=sr, ch=ch, j=j: e.scalar_tensor_tensor(out=G[:, ch + 1, j, 1:2], in0=sr[:, LCH - 1:LCH],
                                                                                    scalar=par[:, MIM, j:j + 1], in1=sm_[:, 1:2],
                                                                                    op0=ALU.mult, op1=ALU.add),
                 R=[smk, srk, pk(MIM), "G"], W=[("G", ch + 1, j, 1)])
            P.res[("G", ch + 1, j)] = P.res[("G", ch + 1, j, 1)]
            b1, b1k = pt_.next(); b2, b2k = pt_.next(); b3, b3k = pt_.next(); b4, b4k = pt_.next()
            tt(b1[:, :], sr[:, :], tab[:, 2, j, :], ALU.mult, R=[srk, ("tab", 2, j)], W=[b1k], eng="pool")
            tt(b2[:, :], si[:, :], tab[:, 3, j, :], ALU.mult, R=[sik, ("tab", 3, j)], W=[b2k], eng="pool")
            tt(b3[:, :], si[:, :], tab[:, 2, j, :], ALU.mult, R=[sik, ("tab", 2, j)], W=[b3k], eng="pool")
            tt(b4[:, :], sr[:, :], tab[:, 3, j, :], ALU.mult, R=[srk, ("tab", 3, j)], W=[b4k], eng="pool")
            hr_, hrk = hre.next(); hi_, hik = him.next()
            tt(hr_[:, :], b1[:, :], b2[:, :], ALU.subtract, R=[b1k, b2k], W=[hrk])
            tt(hi_[:, :], b3[:, :], b4[:, :], ALU.add, R=[b3k, b4k], W=[hik])
            hs.append((hr_, hrk, hi_, hik))
        yp, ypk = C.ps.next()
        for j in range(4):
            hr_, hrk, hi_, hik = hs[j]
            P.op("pe", lambda e, yp=yp, hr_=hr_, j=j: e.matmul(yp[:, :], lhsT=CTt[:, 0, j, :], rhs=hr_[:, :], start=(j == 0), stop=False),
                 R=["CTt", hrk], W=[ypk])
            P.op("pe", lambda e, yp=yp, hi_=hi_, j=j: e.matmul(yp[:, :], lhsT=CTt[:, 1, j, :], rhs=hi_[:, :], start=False, stop=(j == 3)),
                 R=["CTt", hik], W=[ypk])
        y_, yk_ = yv.next()
        P.op("dve", lambda e, y_=y_, yp=yp, c0=c0: e.scalar_tensor_tensor(out=y_[:, :], in0=ub[:, c0:c0 + LCH], scalar=dvec[:, 0:1], in1=yp[:, :],
                                                                          op0=ALU.mult, op1=ALU.add), R=[ypk, "ub", "dvec"], W=[yk_])
        g_, gk_ = gt.next()
        tt(g_[:, :], y_[:, :], y_[:, :], ALU.mult, R=[yk_], W=[gk_], eng="pool")
        ts(g_[:, :], g_[:, :], 0.044715, 1.0, ALU.mult, ALU.add, R=[gk_], W=[gk_])
        tt(g_[:, :], g_[:, :], y_[:, :], ALU.mult, R=[gk_, yk_], W=[gk_], eng="pool")
        P.op("act", lambda e, g_=g_: e.activation(out=g_[:, :], in_=g_[:, :], func=AF.Sigmoid, scale=1.5957691216057308), R=[gk_], W=[gk_])
        tt(y_[:, :], y_[:, :], g_[:, :], ALU.mult, R=[gk_, yk_], W=[yk_])
        P.dma("sp", yT[:, c0:c0 + LCH], y_[:, :], R=[yk_], W=[("yo", ch)])
    P.finish("sp")
    P.emit()
    return nc


def s5_core_inputs(ins, b, gq, uT_b):
    gs = slice(8 * gq, 8 * gq + 8)
    def st(a):
        return np.ascontiguousarray(a[gs].reshape(4, 128).T)
    d = dict(uT=np.ascontiguousarray(uT_b[128 * gq:128 * gq + 128]),
             a_re=st(ins["ssm_a_re"][0]), a_im=st(ins["ssm_a_im"][0]),
             ldt=np.ascontiguousarray(np.repeat(ins["ssm_log_dt"][0][gs].reshape(4, 2, 1), 64, axis=2).reshape(4, 128).T),
             b_re=np.ascontiguousarray(ins["ssm_b_re"][0][gs].reshape(4, 128, 16)),
             b_im=np.ascontiguousarray(ins["ssm_b_im"][0][gs].reshape(4, 128, 16)),
             ct_re=np.ascontiguousarray(ins["ssm_c_re"][0][gs].transpose(0, 2, 1).reshape(4, 128, 16)),
             ct_im=np.ascontiguousarray(ins["ssm_c_im"][0][gs].transpose(0, 2, 1).reshape(4, 128, 16)),
             dsk=np.ascontiguousarray(ins["ssm_d"][0][128 * gq:128 * gq + 128]))
    return d


SEQ = 8192
BATCH = 2
TPC = BATCH * SEQ // NCORES
CPB = NCORES // BATCH


def _run(nc, in_maps):
    res = run_bass_kernel_spmd(nc, in_maps, core_ids=list(range(NCORES)))
    return res.results


def kernel(**ins):
    ins = {k: np.ascontiguousarray(np.asarray(v, dtype=np.float32)) for k, v in ins.items()}
    x = ins["x"].reshape(BATCH * SEQ, D_MODEL)
    ca = np.ascontiguousarray
    Ct, St, pm = rope_tables(SEQ)
    sc = np.float32(96 ** -0.5)
    Cq, Sq = ca(Ct * sc), ca(St * sc)
    nc1 = build_L1(TPC)
    maps = []
    for c in range(NCORES):
        p0 = (c % CPB) * TPC
        sl = slice(p0, p0 + TPC)
        maps.append(dict(x=x[c * TPC:(c + 1) * TPC], att_norm=ins["att_norm"][0], w_in=ins["att_w_in"][0],
                         q_lat_norm=ins["att_q_latent_norm"][0], w_q_up=ins["att_w_q_up"][0],
                         kv_lat_norm=ins["att_kv_latent_norm"][0], w_kv_up=ins["att_w_kv_up"][0],
                         q_norm=ins["att_q_norm"][0], k_norm=ins["att_k_norm"][0],
                         cq_t=ca(Cq[:, sl]), sq_t=ca(Sq[:, sl]), ck_t=ca(Ct[:, sl]), sk_t=ca(St[:, sl]), pm=pm))
    r1 = _run(nc1, maps)
    del nc1
    cat = lambda name, b, axis: np.concatenate([r1[b * CPB + i][name] for i in range(CPB)], axis=axis)
    nc2 = build_L2(SEQ)
    maps = []
    for b in range(BATCH):
        sbqT = cat("sbqT", b, 1); sbkT = cat("sbkT", b, 1); sbv = cat("sbv", b, 0)
        mqT = cat("mqT", b, 2); mkT = cat("mkT", b, 2); mv = cat("mv", b, 0)
        for g in range(CPB):
            maps.append(dict(sbqT=ca(sbqT[128 * g:128 * g + 128].reshape(2, 64, SEQ)),
                             sbkT=ca(sbkT[128 * g:128 * g + 128].reshape(2, 64, SEQ)),
                             sbv=ca(sbv[:, 128 * g:128 * g + 128].reshape(SEQ, 2, 64).transpose(1, 0, 2)),
                             mqT=ca(mqT[2 * g:2 * g + 2]), mkT=ca(mkT[2 * g:2 * g + 2]),
                             mv=ca(mv[:, 128 * g:128 * g + 128].reshape(SEQ, 2, 64).transpose(1, 0, 2))))
    r2 = _run(nc2, maps)
    del nc2, r1
    mT = []
    for b in range(BATCH):
        m = np.empty((1024, SEQ), np.float32)
        for g in range(CPB):
            o = r2[b * CPB + g]["oT"]
            m[128 * g:128 * g + 128] = o[0:2].reshape(128, SEQ)
            m[512 + 128 * g:512 + 128 * g + 128] = o[2:4].reshape(128, SEQ)
        mT.append(m)
    nc3 = build_L3(TPC)
    maps = []
    for c in range(NCORES):
        p0 = (c % CPB) * TPC
        maps.append(dict(x=x[c * TPC:(c + 1) * TPC], mT=ca(mT[c // CPB][:, p0:p0 + TPC]), w_out=ins["att_w_out"][0],
                         dffn_norm=ins["dffn_norm"][0], wg=ins["dffn_w_gate"][0], wu=ins["dffn_w_up"][0], wd=ins["dffn_w_down"][0],
                         ssm_norm=ins["ssm_norm"][0], w_sin=ins["ssm_w_in"][0]))
    r3 = _run(nc3, maps)
    del nc3, r2
    nc4 = build_L4(SEQ)
    maps = []
    for b in range(BATCH):
        uT_b = np.concatenate([r3[b * CPB + i]["uT"] for i in range(CPB)], axis=1)
        for gq in range(CPB):
            maps.append(s5_core_inputs(ins, b, gq, uT_b))
    r4 = _run(nc4, maps)
    del nc4
    nc5 = build_L5(TPC)
    maps = []
    for c in range(NCORES):
        b = c // CPB
        p0 = (c % CPB) * TPC
        yT = np.concatenate([r4[b * CPB + gq]["yT"][:, p0:p0 + TPC] for gq in range(CPB)], axis=0)
        maps.append(dict(h2T=r3[c]["h2T"], yT=ca(yT), w_glu=ins["ssm_w_glu"][0], moe_norm=ins["moe_norm"][0], w_r=ins["moe_router"][0],
                         wg=ins["moe_w_gate"][0], wu=ins["moe_w_up"][0], wd=ins["moe_w_down"][0]))
    r5 = _run(nc5, maps)
    out = np.concatenate([r5[c]["out"] for c in range(NCORES)], axis=0).reshape(BATCH, SEQ, D_MODEL)
    return out.astype(np.float32)
```

```python
import contextlib
import numpy as np
import concourse.bass as bass
import concourse.mybir as mybir
from concourse.bass_utils import run_bass_kernel_spmd

F32 = mybir.dt.float32
BF16 = mybir.dt.bfloat16
I32 = mybir.dt.int32
AF = mybir.ActivationFunctionType
ALU = mybir.AluOpType
AX = mybir.AxisListType

D_MODEL = 1024
D_FF = 3584
EPS = 1e-6
NCORES = 8

COMPUTE = ("pe", "act", "dve", "pool")
NDSEM = 8


class Prog:
    def __init__(self, nc):
        self.nc = nc
        self.engs = ("pe", "act", "dve", "pool", "sp")
        self.q = {e: [] for e in self.engs}
        self.cnt = {e: 0 for e in COMPUTE}
        self.known = {e: {} for e in self.engs}
        self.res = {}
        self.dcnt = {}
        self.drr = {e: 0 for e in self.engs}
        self.sems = {}
        self.n_wait = 0
        self.n_op = 0

    def _deps(self, R, W):
        deps = {}
        for r in R:
            st = self.res.get(r)
            if st is not None and st[0] is not None:
                k, v = st[0]
                if deps.get(k, 0) < v:
                    deps[k] = v
        for w in W:
            st = self.res.get(w)
            if st is not None:
                if st[0] is not None:
                    k, v = st[0]
                    if deps.get(k, 0) < v:
                        deps[k] = v
                for k, v in st[1].items():
                    if deps.get(k, 0) < v:
                        deps[k] = v
        return deps

    def _record(self, tok, R, W):
        k, v = tok
        for r in R:
            st = self.res.get(r)
            if st is None:
                st = [None, {}]
                self.res[r] = st
            if st[1].get(k, 0) < v:
                st[1][k] = v
        for w in W:
            self.res[w] = [tok, {}]

    def _emit_waits(self, eng, deps):
        kn = self.known[eng]
        for k, v in deps.items():
            if k == eng and eng == "pe":
                continue
            if kn.get(k, 0) >= v:
                continue
            kn[k] = v
            self.q[eng].append(("w", k, v))
            self.n_wait += 1

    def op(self, eng, fn, R=(), W=()):
        deps = self._deps(R, W)
        self._emit_waits(eng, deps)
        self.cnt[eng] += 1
        tok = (eng, self.cnt[eng])
        self.q[eng].append(("o", fn, eng, 1))
        self._record(tok, R, W)
        self.n_op += 1
        return tok

    def dma(self, eng, out, in_, R=(), W=(), **kw):
        deps = self._deps(R, W)
        j = self.drr[eng]
        self.drr[eng] = (j + 1) % NDSEM
        key = ("d", eng, j)
        prev = self.dcnt.get(key, 0)
        if prev:
            deps[key] = max(deps.get(key, 0), prev * 16)
        self._emit_waits(eng, deps)
        self.dcnt[key] = prev + 1
        tok = (key, (prev + 1) * 16)
        self.q[eng].append(("o", lambda e: e.dma_start(out=out, in_=in_, **kw), key, 16))
        self._record(tok, R, W)
        self.n_op += 1
        return tok

    def finish(self, eng="sp"):
        deps = {k: c * 16 for k, c in self.dcnt.items()}
        self._emit_waits(eng, deps)

    def emit(self):
        nc = self.nc
        with contextlib.ExitStack() as es:
            keys = list(COMPUTE) + list(self.dcnt.keys())
            for k in keys:
                nm = k if isinstance(k, str) else "d_%s_%d" % (k[1], k[2])
                self.sems[k] = es.enter_context(nc.semaphore("s_" + nm))
            block = es.enter_context(nc.Block())
            handles = {"pe": block.tensor, "act": block.scalar, "dve": block.vector,
                       "pool": block.gpsimd, "sp": block.sync}
            sems = self.sems
            for eng in self.engs:
                items = self.q[eng]

                def body(e, items=items):
                    for it in items:
                        if it[0] == "w":
                            e.wait_ge(sems[it[1]], it[2])
                        else:
                            it[1](e).then_inc(sems[it[2]], it[3])
                handles[eng](body)


class Rot:
    def __init__(self, nc, name, shape, dtype, n, psum=False):
        self.tiles = []
        for i in range(n):
            if psum:
                t = nc.alloc_psum_tensor("%s%d" % (name, i), shape, dtype)
            else:
                t = nc.alloc_sbuf_tensor("%s%d" % (name, i), shape, dtype)
            self.tiles.append(t)
        self.name = name
        self.i = 0

    def next(self):
        i = self.i % len(self.tiles)
        self.i += 1
        return self.tiles[i], (self.name, i)


class Ctx:
    def __init__(self, nc, n_ps=8):
        self.nc = nc
        self.P = Prog(nc)
        P = self.P
        self.ps = Rot(nc, "ps", [128, 512], F32, n_ps, psum=True)
        self.ones_bf = nc.alloc_sbuf_tensor("ones_bf", [128, 128], BF16)
        self.ones_f = nc.alloc_sbuf_tensor("ones_f", [128, 128], F32)
        self.ident_f = nc.alloc_sbuf_tensor("ident_f", [128, 128], F32)
        self.eps_t = nc.alloc_sbuf_tensor("eps_t", [128, 1], F32)
        P.op("pool", lambda e: e.memset(self.ones_bf[:], 1.0), W=["ones_bf"])
        P.op("pool", lambda e: e.memset(self.ones_f[:], 1.0), W=["ones_f"])
        P.op("pool", lambda e: e.memset(self.eps_t[:], EPS), W=["eps_t"])
        P.op("pool", lambda e: e.memset(self.ident_f[:], 1.0), W=["ident_f"])
        P.op("pool", lambda e: e.affine_select(out=self.ident_f[:], in_=self.ident_f[:], pattern=[[-1, 128]],
                                               compare_op=ALU.is_equal, fill=0.0, base=0, channel_multiplier=1),
             R=["ident_f"], W=["ident_f"])
        self.sq = Rot(nc, "sq", [128, 8, 512], BF16, 1)
        self.rt = Rot(nc, "rt", [128, 512], F32, 2)


def emit_rmsnorm_fm(C, hT, hkeys, nk, tok0, ntok, gain_sb, gkey, xnT, xkeys, xtok0, D, npart=128):
    P = C.P
    sq, sqk = C.sq.next()
    P.op("act", lambda e: e.activation(out=sq[:npart, 0:nk, 0:ntok], in_=hT[:npart, 0:nk, tok0:tok0 + ntok], func=AF.Square),
         R=list(hkeys), W=[sqk])
    ps, psk = C.ps.next()
    for k in range(nk):
        P.op("pe", lambda e, k=k: e.matmul(ps[:npart, 0:ntok], lhsT=C.ones_bf[:npart, :npart], rhs=sq[:npart, k, 0:ntok],
                                          start=(k == 0), stop=(k == nk - 1)),
             R=[sqk, "ones_bf"], W=[psk])
    rt, rtk = C.rt.next()
    P.op("act", lambda e: e.activation(out=rt[:npart, 0:ntok], in_=ps[:npart, 0:ntok], func=AF.Sqrt,
                                       bias=C.eps_t[:npart, 0:1], scale=1.0 / D),
         R=[psk, "eps_t"], W=[rtk])
    P.op("dve", lambda e: e.reciprocal(out=rt[:npart, 0:ntok], in_=rt[:npart, 0:ntok]), R=[rtk], W=[rtk])
    for k in range(nk):
        P.op("dve", lambda e, k=k: e.scalar_tensor_tensor(out=xnT[:npart, k, xtok0:xtok0 + ntok],
                                                          in0=hT[:npart, k, tok0:tok0 + ntok],
                                                          scalar=gain_sb[:npart, k:k + 1], in1=rt[:npart, 0:ntok],
                                                          op0=ALU.mult, op1=ALU.mult),
             R=[hkeys[k], rtk, gkey], W=[xkeys[k]])


def load_vec_fm(C, name, dram_vec_ap, n):
    nk = max(1, n // 128)
    npart = min(128, n)
    t = C.nc.alloc_sbuf_tensor(name, [128, nk], F32)
    C.P.dma("sp", t[:npart, :], dram_vec_ap.rearrange("(k p) -> p k", p=npart), W=[name], allow_slow_non_contiguous=True)
    return t


def emit_ffn(C, xnT, xkeys, hT, hkeys, T, wg, wu, wd, pools, gate_bc=None):
    P = C.P
    NTG = T // 512
    hidT, hidkeys = pools["hidT"], pools["hidkeys"]
    for wb in range(7):
        wgt, wgk = pools["wgu"].next()
        P.dma("pool", wgt[:], wg[:, wb * 512:(wb + 1) * 512].rearrange("(k p) n -> p k n", p=128), W=[wgk])
        wut, wuk = pools["wgu"].next()
        P.dma("pool", wut[:], wu[:, wb * 512:(wb + 1) * 512].rearrange("(k p) n -> p k n", p=128), W=[wuk])
        for m in range(4):
            mm = wb * 4 + m
            for tg in range(NTG):
                pg, pgk = C.ps.next()
                for k in range(8):
                    P.op("pe", lambda e, k=k, pg=pg, wgt=wgt, m=m, tg=tg: e.matmul(
                        pg[:, :], lhsT=wgt[:, k, m * 128:(m + 1) * 128], rhs=xnT[:, k, tg * 512:(tg + 1) * 512],
                        start=(k == 0), stop=(k == 7)), R=[wgk, xkeys(k, tg)], W=[pgk])
                pu, puk = C.ps.next()
                for k in range(8):
                    P.op("pe", lambda e, k=k, pu=pu, wut=wut, m=m, tg=tg: e.matmul(
                        pu[:, :], lhsT=wut[:, k, m * 128:(m + 1) * 128], rhs=xnT[:, k, tg * 512:(tg + 1) * 512],
                        start=(k == 0), stop=(k == 7)), R=[wuk, xkeys(k, tg)], W=[puk])
                sg, sgk = pools["sg"].next()
                P.op("act", lambda e, sg=sg, pg=pg: e.activation(out=sg[:, :], in_=pg[:, :], func=AF.Silu), R=[pgk], W=[sgk])
                P.op("dve", lambda e, sg=sg, pu=pu, mm=mm, tg=tg: e.tensor_tensor(
                    out=hidT[:, mm, tg * 512:(tg + 1) * 512], in0=sg[:, :], in1=pu[:, :], op=ALU.mult),
                    R=[sgk, puk], W=[hidkeys[mm] + (tg,)])
    for dm in range(8):
        wdt, wdk = pools["wd"].next()
        P.dma("pool", wdt[:], wd[:, dm * 128:(dm + 1) * 128].rearrange("(k p) n -> p k n", p=128), W=[wdk])
        for tg in range(NTG):
            po, pok = C.ps.next()
            for k in range(28):
                P.op("pe", lambda e, k=k, po=po, wdt=wdt, tg=tg: e.matmul(
                    po[:, :], lhsT=wdt[:, k, :], rhs=hidT[:, k, tg * 512:(tg + 1) * 512],
                    start=(k == 0), stop=(k == 27)), R=[wdk, hidkeys[k] + (tg,)], W=[pok])
            if gate_bc is None:
                P.op("dve", lambda e, po=po, dm=dm, tg=tg: e.tensor_tensor(
                    out=hT[:, dm, tg * 512:(tg + 1) * 512], in0=hT[:, dm, tg * 512:(tg + 1) * 512], in1=po[:, :], op=ALU.add),
                    R=[pok, hkeys(dm, tg)], W=[hkeys(dm, tg)])
            else:
                gt, gkf = gate_bc
                tmp, tmpk = pools["sg"].next()
                P.op("dve", lambda e, po=po, tmp=tmp, gt=gt, tg=tg: e.tensor_tensor(
                    out=tmp[:, :], in0=po[:, :], in1=gt[:, tg * 512:(tg + 1) * 512], op=ALU.mult),
                    R=[pok, gkf(tg)], W=[tmpk])
                P.op("dve", lambda e, tmp=tmp, dm=dm, tg=tg: e.tensor_tensor(
                    out=hT[:, dm, tg * 512:(tg + 1) * 512], in0=hT[:, dm, tg * 512:(tg + 1) * 512], in1=tmp[:, :], op=ALU.add),
                    R=[tmpk, hkeys(dm, tg)], W=[hkeys(dm, tg)])


def ffn_pools(nc, T):
    return {
        "hidT": nc.alloc_sbuf_tensor("hidT", [128, 28, T], BF16),
        "hidkeys": [("hid", k) for k in range(28)],
        "wgu": Rot(nc, "wgu", [128, 8, 512], BF16, 4),
        "wd": Rot(nc, "wd", [128, 28, 128], BF16, 2),
        "sg": Rot(nc, "sg", [128, 512], F32, 3),
    }


def emit_load_tm_to_fm(C, src_dram, hT, hkeys, ntiles, stage):
    P = C.P
    for t in range(ntiles):
        st, stk = stage.next()
        P.dma("sp", st[:], src_dram[t * 128:(t + 1) * 128, :], W=[stk])
        for half in range(2):
            ps, psk = C.ps.next()
            for kk in range(4):
                k = half * 4 + kk
                P.op("pe", lambda e, ps=ps, st=st, k=k, kk=kk: e.transpose(out=ps[:, kk * 128:(kk + 1) * 128], in_=st[:, k * 128:(k + 1) * 128],
                                                                           identity=C.ident_f[:]),
                     R=[stk, "ident_f"], W=[psk])
            P.op("dve" if half == 0 else "act",
                 (lambda e, ps=ps, half=half, t=t: e.tensor_copy(out=hT[:, half * 4:half * 4 + 4, t * 128:(t + 1) * 128],
                                                                 in_=ps[:, :].rearrange("p (k n) -> p k n", k=4))) if half == 0 else
                 (lambda e, ps=ps, half=half, t=t: e.copy(out=hT[:, half * 4:half * 4 + 4, t * 128:(t + 1) * 128],
                                                          in_=ps[:, :].rearrange("p (k n) -> p k n", k=4))),
                 R=[psk], W=[hkeys(half * 4 + kk, t // 4) for kk in range(4)])


def emit_store_fm_to_tm(C, hT, hkeys, dst_dram, ntiles, stage):
    P = C.P
    for t in range(ntiles):
        st, stk = stage.next()
        for half in range(2):
            ps, psk = C.ps.next()
            for kk in range(4):
                k = half * 4 + kk
                P.op("pe", lambda e, ps=ps, k=k, kk=kk, t=t: e.transpose(out=ps[:, kk * 128:(kk + 1) * 128], in_=hT[:, k, t * 128:(t + 1) * 128],
                                                                         identity=C.ident_f[:]),
                     R=[hkeys(k, t // 4), "ident_f"], W=[psk])
            if half == 0:
                P.op("dve", lambda e, ps=ps, st=st: e.tensor_copy(out=st[:, 0:512], in_=ps[:, :]), R=[psk], W=[stk + (0,)])
            else:
                P.op("act", lambda e, ps=ps, st=st: e.copy(out=st[:, 512:1024], in_=ps[:, :]), R=[psk], W=[stk + (1,)])
        P.dma("sp", dst_dram[t * 128:(t + 1) * 128, :], st[:], R=[stk + (0,), stk + (1,)], W=[("out", t)])


def build_L3(T):
    nc = bass.Bass("TRN2", target_bir_lowering=False)
    x = nc.dram_tensor("x", [T, 1024], F32, kind="ExternalInput").ap()
    mT = nc.dram_tensor("mT", [1024, T], F32, kind="ExternalInput").ap()
    w_out = nc.dram_tensor("w_out", [1024, 1024], F32, kind="ExternalInput").ap()
    n1 = nc.dram_tensor("dffn_norm", [1024], F32, kind="ExternalInput").ap()
    wg = nc.dram_tensor("wg", [1024, D_FF], F32, kind="ExternalInput").ap()
    wu = nc.dram_tensor("wu", [1024, D_FF], F32, kind="ExternalInput").ap()
    wd = nc.dram_tensor("wd", [D_FF, 1024], F32, kind="ExternalInput").ap()
    n2 = nc.dram_tensor("ssm_norm", [1024], F32, kind="ExternalInput").ap()
    w_sin = nc.dram_tensor("w_sin", [1024, 512], F32, kind="ExternalInput").ap()
    h2T = nc.dram_tensor("h2T", [1024, T], F32, kind="ExternalOutput").ap()
    uT = nc.dram_tensor("uT", [512, T], F32, kind="ExternalOutput").ap()
    C = Ctx(nc)
    P = C.P
    TG = 1024
    hT = nc.alloc_sbuf_tensor("hT", [128, 8, TG], F32)
    xnT = nc.alloc_sbuf_tensor("xnT", [128, 8, TG], BF16)
    pools = ffn_pools(nc, TG)
    stage = Rot(nc, "stage", [128, 1024], F32, 2)
    mts = Rot(nc, "mts", [128, 8, 512], BF16, 2)
    uo = Rot(nc, "uo", [128, 512], F32, 2)
    g1 = load_vec_fm(C, "g1", n1, 1024)
    g2 = load_vec_fm(C, "g2", n2, 1024)
    hk = lambda k, tg: ("h", k, tg)
    xk = lambda k, tg: ("xn", k, tg)
    for grp in range(T // TG):
        t0 = grp * TG
        emit_load_tm_to_fm(C, x[t0:t0 + TG, :], hT, hk, TG // 128, stage)
        wo = []
        for hf in range(2):
            wt, wk = pools["wgu"].next()
            P.dma("pool", wt[:], w_out[:, hf * 512:(hf + 1) * 512].rearrange("(k p) n -> p k n", p=128), W=[wk])
            wo.append((wt, wk))
        for tg in range(TG // 512):
            mt, mk = mts.next()
            P.dma("pool", mt[:], mT[:, t0 + tg * 512:t0 + (tg + 1) * 512].rearrange("(k p) n -> p k n", p=128), W=[mk])
            for dm in range(8):
                wt, wk = wo[dm // 4]
                po, pok = C.ps.next()
                for k in range(8):
                    P.op("pe", lambda e, k=k, po=po, wt=wt, mt=mt, dm=dm: e.matmul(
                        po[:, :], lhsT=wt[:, k, (dm % 4) * 128:(dm % 4 + 1) * 128], rhs=mt[:, k, :],
                        start=(k == 0), stop=(k == 7)), R=[wk, mk], W=[pok])
                P.op("dve", lambda e, po=po, dm=dm, tg=tg: e.tensor_tensor(
                    out=hT[:, dm, tg * 512:(tg + 1) * 512], in0=hT[:, dm, tg * 512:(tg + 1) * 512], in1=po[:, :], op=ALU.add),
                    R=[pok, hk(dm, tg)], W=[hk(dm, tg)])
        for tg in range(TG // 512):
            emit_rmsnorm_fm(C, hT, [hk(k, tg) for k in range(8)], 8, tg * 512, 512, g1, "g1",
                            xnT, [xk(k, tg) for k in range(8)], tg * 512, 1024)
        emit_ffn(C, xnT, xk, hT, hk, TG, wg, wu, wd, pools)
        for k in range(8):
            P.dma("sp", h2T[k * 128:(k + 1) * 128, t0:t0 + TG], hT[:, k, :], R=[hk(k, tg) for tg in range(TG // 512)], W=[("h2o", k)])
        for tg in range(TG // 512):
            emit_rmsnorm_fm(C, hT, [hk(k, tg) for k in range(8)], 8, tg * 512, 512, g2, "g2",
                            xnT, [xk(k, tg) for k in range(8)], tg * 512, 1024)
        wt, wk = pools["wgu"].next()
        P.dma("pool", wt[:], w_sin.rearrange("(k p) n -> p k n", p=128), W=[wk])
        for tg in range(TG // 512):
            for c in range(4):
                po, pok = C.ps.next()
                for k in range(8):
                    P.op("pe", lambda e, k=k, po=po, wt=wt, c=c, tg=tg: e.matmul(
                        po[:, :], lhsT=wt[:, k, c * 128:(c + 1) * 128], rhs=xnT[:, k, tg * 512:(tg + 1) * 512],
                        start=(k == 0), stop=(k == 7)), R=[wk, xk(k, tg)], W=[pok])
                ut, uk = uo.next()
                P.op("act", lambda e, ut=ut, po=po: e.copy(out=ut[:, :], in_=po[:, :]), R=[pok], W=[uk])
                P.dma("sp", uT[c * 128:(c + 1) * 128, t0 + tg * 512:t0 + (tg + 1) * 512], ut[:, :], R=[uk], W=[("uo", c, tg, grp)])
    P.finish("sp")
    P.emit()
    return nc


def build_L5(T, n_exp=8):
    nc = bass.Bass("TRN2", target_bir_lowering=False)
    h2T = nc.dram_tensor("h2T", [1024, T], F32, kind="ExternalInput").ap()
    yT = nc.dram_tensor("yT", [512, T], F32, kind="ExternalInput").ap()
    w_glu = nc.dram_tensor("w_glu", [512, 2048], F32, kind="ExternalInput").ap()
    n1 = nc.dram_tensor("moe_norm", [1024], F32, kind="ExternalInput").ap()
    w_r = nc.dram_tensor("w_r", [1024, 8], F32, kind="ExternalInput").ap()
    wg = nc.dram_tensor("wg", [8, 1024, D_FF], F32, kind="ExternalInput").ap()
    wu = nc.dram_tensor("wu", [8, 1024, D_FF], F32, kind="ExternalInput").ap()
    wd = nc.dram_tensor("wd", [8, D_FF, 1024], F32, kind="ExternalInput").ap()
    out = nc.dram_tensor("out", [T, 1024], F32, kind="ExternalOutput").ap()
    C = Ctx(nc)
    P = C.P
    TG = 1024
    NTG = TG // 512
    hT = nc.alloc_sbuf_tensor("hT", [128, 8, TG], F32)
    xnT = nc.alloc_sbuf_tensor("xnT", [128, 8, TG], BF16)
    pools = ffn_pools(nc, TG)
    stage = Rot(nc, "stage", [128, 1024], F32, 1)
    yts = Rot(nc, "yts", [128, 4, 512], BF16, 1)
    wglu = nc.alloc_sbuf_tensor("wglu", [128, 4, 2048], BF16)
    wrg = nc.alloc_sbuf_tensor("wrg", [128, 8, 8], F32)
    sel = nc.alloc_sbuf_tensor("sel", [8, 8, 128], F32)
    gT = nc.alloc_sbuf_tensor("gT", [8, TG], F32)
    gbc = nc.alloc_sbuf_tensor("gbc", [128, TG], F32)
    sm = Rot(nc, "sm", [128, 64], F32, 2)
    g1 = load_vec_fm(C, "g1", n1, 1024)
    P.dma("pool", wglu[:], w_glu.rearrange("(k p) n -> p k n", p=128), W=["wglu"])
    P.dma("sp", wrg[:], w_r.rearrange("(k p) n -> p k n", p=128), W=["wrg"], allow_slow_non_contiguous=True)
    for k in range(8):
        P.op("dve", lambda e, k=k: e.tensor_scalar(out=wrg[:, k, :], in0=wrg[:, k, :], scalar1=g1[:, k:k + 1], scalar2=None, op0=ALU.mult),
             R=["wrg", "g1"], W=["wrg"])
    P.op("pool", lambda e: e.memset(sel[:], 1.0), W=["sel"])
    P.op("pool", lambda e: e.affine_select(out=sel[:], in_=sel[:], pattern=[[-1, 8], [0, 128]], compare_op=ALU.is_equal, fill=0.0,
                                           base=0, channel_multiplier=1), R=["sel"], W=["sel"])
    hk = lambda k, tg: ("h", k, tg)
    xk = lambda k, tg: ("xn", k, tg)
    for grp in range(T // TG):
        t0 = grp * TG
        for k in range(8):
            P.dma("sp", hT[:, k, :], h2T[k * 128:(k + 1) * 128, t0:t0 + TG], W=[hk(k, tg) for tg in range(NTG)])
        for tg in range(NTG):
            yt, yk = yts.next()
            P.dma("pool", yt[:], yT[:, t0 + tg * 512:t0 + (tg + 1) * 512].rearrange("(k p) n -> p k n", p=128), W=[yk])
            for dm in range(8):
                p1, p1k = C.ps.next()
                for k in range(4):
                    P.op("pe", lambda e, k=k, p1=p1, yt=yt, dm=dm: e.matmul(p1[:, :], lhsT=wglu[:, k, dm * 128:(dm + 1) * 128], rhs=yt[:, k, :],
                                                                          start=(k == 0), stop=(k == 3)), R=["wglu", yk], W=[p1k])
                p2, p2k = C.ps.next()
                for k in range(4):
                    P.op("pe", lambda e, k=k, p2=p2, yt=yt, dm=dm: e.matmul(p2[:, :], lhsT=wglu[:, k, 1024 + dm * 128:1024 + (dm + 1) * 128], rhs=yt[:, k, :],
                                                                          start=(k == 0), stop=(k == 3)), R=["wglu", yk], W=[p2k])
                sg, sgk = pools["sg"].next()
                P.op("act", lambda e, sg=sg, p2=p2: e.activation(out=sg[:, :], in_=p2[:, :], func=AF.Sigmoid), R=[p2k], W=[sgk])
                P.op("dve", lambda e, sg=sg, p1=p1: e.tensor_tensor(out=sg[:, :], in0=sg[:, :], in1=p1[:, :], op=ALU.mult), R=[sgk, p1k], W=[sgk])
                P.op("dve", lambda e, sg=sg, dm=dm, tg=tg: e.tensor_tensor(out=hT[:, dm, tg * 512:(tg + 1) * 512], in0=hT[:, dm, tg * 512:(tg + 1) * 512],
                                                                       in1=sg[:, :], op=ALU.add), R=[sgk, hk(dm, tg)], W=[hk(dm, tg)])
        for tg in range(NTG):
            emit_rmsnorm_fm(C, hT, [hk(k, tg) for k in range(8)], 8, tg * 512, 512, g1, "g1",
                            xnT, [xk(k, tg) for k in range(8)], tg * 512, 1024)
        for tt in range(TG // 128):
            tg = tt // 4
            pl, plk = C.ps.next()
            for k in range(8):
                P.op("pe", lambda e, k=k, pl=pl, tt=tt: e.matmul(pl[:, 0:8], lhsT=hT[:, k, tt * 128:(tt + 1) * 128], rhs=wrg[:, k, :],
                                                             start=(k == 0), stop=(k == 7)), R=[hk(k, tg), "wrg"], W=[plk])
            pss, pssk = C.ps.next()
            sq, sqk = C.sq.next()
            P.op("act", lambda e, sq=sq, tt=tt: e.activation(out=sq[:, :, 0:128], in_=hT[:, :, tt * 128:(tt + 1) * 128], func=AF.Square),
                 R=[hk(k, tg) for k in range(8)], W=[sqk])
            for k in range(8):
                P.op("pe", lambda e, k=k, pss=pss, sq=sq: e.matmul(pss[:, 0:1], lhsT=sq[:, k, 0:128], rhs=C.ones_bf[:, 0:1],
                                                               start=(k == 0), stop=(k == 7)), R=[sqk, "ones_bf"], W=[pssk])
            s, sk = sm.next()
            P.op("act", lambda e, s=s, pss=pss: e.activation(out=s[:, 0:1], in_=pss[:, 0:1], func=AF.Sqrt, bias=C.eps_t[:, 0:1], scale=1.0 / 1024),
                 R=[pssk, "eps_t"], W=[sk])
            P.op("dve", lambda e, s=s: e.reciprocal(out=s[:, 0:1], in_=s[:, 0:1]), R=[sk], W=[sk])
            P.op("dve", lambda e, s=s, pl=pl: e.tensor_scalar(out=s[:, 8:16], in0=pl[:, 0:8], scalar1=s[:, 0:1], scalar2=None, op0=ALU.mult),
                 R=[sk, plk], W=[sk])
            P.op("dve", lambda e, s=s: e.max(out=s[:, 16:24], in_=s[:, 8:16]), R=[sk], W=[sk])
            P.op("dve", lambda e, s=s: e.tensor_scalar(out=s[:, 24:25], in0=s[:, 16:17], scalar1=-1.0, scalar2=None, op0=ALU.mult), R=[sk], W=[sk])
            P.op("act", lambda e, s=s: e.activation(out=s[:, 32:40], in_=s[:, 8:16], func=AF.Exp, bias=s[:, 24:25], scale=1.0), R=[sk], W=[sk])
            P.op("dve", lambda e, s=s: e.tensor_scalar(out=s[:, 40:48], in0=s[:, 8:16], scalar1=s[:, 17:18], scalar2=None, op0=ALU.is_ge), R=[sk], W=[sk])
            P.op("dve", lambda e, s=s: e.tensor_tensor(out=s[:, 32:40], in0=s[:, 32:40], in1=s[:, 40:48], op=ALU.mult), R=[sk], W=[sk])
            P.op("dve", lambda e, s=s: e.reduce_sum(out=s[:, 48:49], in_=s[:, 32:40], axis=AX.X), R=[sk], W=[sk])
            P.op("dve", lambda e, s=s: e.reciprocal(out=s[:, 48:49], in_=s[:, 48:49]), R=[sk], W=[sk])
            P.op("dve", lambda e, s=s: e.tensor_scalar(out=s[:, 32:40], in0=s[:, 32:40], scalar1=s[:, 48:49], scalar2=None, op0=ALU.mult), R=[sk], W=[sk])
            pt, ptk = C.ps.next()
            P.op("pe", lambda e, pt=pt, s=s: e.transpose(out=pt[0:8, 0:128], in_=s[:, 32:40], identity=C.ident_f[:]), R=[sk, "ident_f"], W=[ptk])
            P.op("act", lambda e, pt=pt, tt=tt: e.copy(out=gT[0:8, tt * 128:(tt + 1) * 128], in_=pt[0:8, 0:128]), R=[ptk], W=[("gT", tg)])
        for ex in range(n_exp):
            for tg in range(NTG):
                pb, pbk = C.ps.next()
                P.op("pe", lambda e, pb=pb, ex=ex, tg=tg: e.matmul(pb[:, :], lhsT=sel[0:8, ex, :], rhs=gT[0:8, tg * 512:(tg + 1) * 512],
                                                               start=True, stop=True), R=["sel", ("gT", tg)], W=[pbk])
                P.op("act", lambda e, pb=pb, tg=tg: e.copy(out=gbc[:, tg * 512:(tg + 1) * 512], in_=pb[:, :]), R=[pbk], W=[("gbc", tg)])
            emit_ffn(C, xnT, xk, hT, hk, TG, wg[ex], wu[ex], wd[ex], pools, gate_bc=(gbc, lambda tg: ("gbc", tg)))
        emit_store_fm_to_tm(C, hT, hk, out[t0:t0 + TG, :], TG // 128, stage)
    P.finish("sp")
    P.emit()
    return nc


def build_L1(T):
    nc = bass.Bass("TRN2", target_bir_lowering=False)
    dt = lambda n, s, k="ExternalInput": nc.dram_tensor(n, s, F32, kind=k).ap()
    x = dt("x", [T, 1024])
    n0 = dt("att_norm", [1024])
    w_in = dt("w_in", [1024, 1952])
    nq = dt("q_lat_norm", [256])
    w_qup = dt("w_q_up", [256, 768])
    nkv = dt("kv_lat_norm", [128])
    w_kvup = dt("w_kv_up", [128, 1024])
    gq = dt("q_norm", [96])
    gk = dt("k_norm", [96])
    cq_t = dt("cq_t", [96, T]); sq_t = dt("sq_t", [96, T])
    ck_t = dt("ck_t", [96, T]); sk_t = dt("sk_t", [96, T])
    pm = dt("pm", [96, 96])
    dtb = lambda n, s: nc.dram_tensor(n, s, BF16, kind="ExternalOutput").ap()
    sbqT = dtb("sbqT", [512, T])
    sbkT = dtb("sbkT", [512, T])
    sbv = dtb("sbv", [T, 512])
    mqT = dtb("mqT", [8, 96, T])
    mkT = dtb("mkT", [8, 96, T])
    mv = dtb("mv", [T, 512])
    C = Ctx(nc)
    P = C.P
    A = nc.alloc_sbuf_tensor
    hT = A("hT", [128, 8, 512], F32)
    xnT = A("xnT", [128, 8, 512], BF16)
    win = A("win", [128, 8, 1952], BF16)
    wkr = A("wkr", [128, 8, 96], BF16)
    wqup = A("wqup", [128, 2, 768], BF16)
    wkn = A("wkn", [128, 8, 96], BF16)
    wkv = A("wkv", [128, 8, 64], BF16)
    pmt = A("pmt", [96, 96], BF16)
    lat = A("lat", [128, 3, 512], F32)
    latn = A("latn", [128, 3, 512], BF16)
    krp = A("krp", [96, 512], F32)
    hr = A("hr", [96, 1, 512], F32)
    hn = A("hn", [96, 1, 512], BF16)
    tabs = A("tabs", [96, 4, 512], F32)
    t1 = Rot(nc, "t1", [96, 512], F32, 2)
    t2 = Rot(nc, "t2", [96, 512], F32, 2)
    ob = Rot(nc, "ob", [128, 512], BF16, 3)
    t3 = Rot(nc, "t3", [96, 512], BF16, 2)
    stage = Rot(nc, "stage", [128, 1024], F32, 2)
    g0 = load_vec_fm(C, "g0", n0, 1024)
    gql = load_vec_fm(C, "gql", nq, 256)
    gkl = load_vec_fm(C, "gkl", nkv, 128)
    gqh = load_vec_fm(C, "gqh", gq, 96)
    gkh = load_vec_fm(C, "gkh", gk, 96)
    P.dma("pool", win[:], w_in.rearrange("(k p) n -> p k n", p=128), W=["win"])
    P.op("pool", lambda e: e.memset(wkr[:], 0.0), W=["wkr"])
    P.dma("pool", wkr[:, :, 64:96], w_in[:, 1920:1952].rearrange("(k p) n -> p k n", p=128), R=["wkr"], W=["wkr"])
    P.dma("pool", wqup[:], w_qup.rearrange("(k p) n -> p k n", p=128), W=["wqup"])
    P.op("pool", lambda e: e.memset(wkn[:], 0.0), W=["wkn"])
    P.dma("pool", wkn[:, :, 0:64], w_kvup.rearrange("k (h c) -> k h c", c=128)[:, :, 0:64], R=["wkn"], W=["wkn"])
    P.dma("pool", wkv[:], w_kvup.rearrange("k (h c) -> k h c", c=128)[:, :, 64:128], W=["wkv"])
    P.dma("pool", pmt[:], pm, W=["pmt"])
    hk = lambda k, tg: ("h", k)
    for tg in range(T // 512):
        t0 = tg * 512
        emit_load_tm_to_fm(C, x[t0:t0 + 512, :], hT, hk, 4, stage)
        emit_rmsnorm_fm(C, hT, [hk(k, 0) for k in range(8)], 8, 0, 512, g0, "g0", xnT, [("xn", k) for k in range(8)], 0, 1024)
        XR = [("xn", k) for k in range(8)]
        for i, tab in enumerate((cq_t, sq_t, ck_t, sk_t)):
            P.dma("sp", tabs[:, i, :], tab[:, t0:t0 + 512], W=[("tabs", i)])

        def proj_fm(col0, ncols, wt=win, wkey="win"):
            po, pok = C.ps.next()
            for k in range(8):
                P.op("pe", lambda e, k=k, po=po: e.matmul(po[0:ncols, :], lhsT=wt[:, k, col0:col0 + ncols], rhs=xnT[:, k, :],
                                                          start=(k == 0), stop=(k == 7)), R=[wkey] + XR, W=[pok])
            return po, pok
        for c in range(8):
            po, pok = proj_fm(c * 128, 128)
            o, okk = ob.next()
            P.op("act", lambda e, o=o, po=po, c=c: e.mul(out=o[:, :], in_=po[:, :], mul=(0.125 if c < 4 else 1.0)), R=[pok], W=[okk])
            dst = sbqT if c < 4 else sbkT
            P.dma("sp", dst[(c % 4) * 128:(c % 4 + 1) * 128, t0:t0 + 512], o[:, :], R=[okk], W=[("o1", c, tg)])
        for tt in range(4):
            po, pok = C.ps.next()
            for k in range(8):
                P.op("pe", lambda e, k=k, po=po, tt=tt: e.matmul(po[:, :], lhsT=xnT[:, k, tt * 128:(tt + 1) * 128], rhs=win[:, k, 1024:1536],
                                                             start=(k == 0), stop=(k == 7)), R=["win"] + XR, W=[pok])
            o, okk = ob.next()
            P.op("dve", lambda e, o=o, po=po: e.tensor_copy(out=o[:, :], in_=po[:, :]), R=[pok], W=[okk])
            P.dma("sp", sbv[t0 + tt * 128:t0 + (tt + 1) * 128, :], o[:, :], R=[okk], W=[("o2", tt, tg)])
        for c in range(3):
            po, pok = proj_fm(1536 + c * 128, 128)
            P.op("act", lambda e, po=po, c=c: e.copy(out=lat[:, c, :], in_=po[:, :]), R=[pok], W=[("lat", c)])
        emit_rmsnorm_fm(C, lat, [("lat", 0), ("lat", 1)], 2, 0, 512, gql, "gql", latn, [("latn", 0), ("latn", 1)], 0, 256)
        emit_rmsnorm_fm(C, lat[:, 2:3, :], [("lat", 2)], 1, 0, 512, gkl, "gkl", latn[:, 2:3, :], [("latn", 2)], 0, 128)
        po, pok = proj_fm(0, 96, wt=wkr, wkey="wkr")
        P.op("act", lambda e, po=po: e.copy(out=krp[:, :], in_=po[0:96, :]), R=[pok], W=["krp"])
        for tt in range(4):
            po, pok = C.ps.next()
            P.op("pe", lambda e, po=po, tt=tt: e.matmul(po[:, :], lhsT=latn[:, 2, tt * 128:(tt + 1) * 128], rhs=wkv[:, :, :],
                                                    start=True, stop=True), R=["wkv", ("latn", 2)], W=[pok])
            o, okk = ob.next()
            P.op("dve", lambda e, o=o, po=po: e.tensor_copy(out=o[:, :], in_=po[:, :]), R=[pok], W=[okk])
            P.dma("sp", mv[t0 + tt * 128:t0 + (tt + 1) * 128, :], o[:, :], R=[okk], W=[("o3", tt, tg)])
        for h in range(8):
            for which in range(2):
                po, pok = C.ps.next()
                if which == 0:
                    for k in range(2):
                        P.op("pe", lambda e, k=k, po=po, h=h: e.matmul(po[0:96, :], lhsT=wqup[:, k, h * 96:(h + 1) * 96], rhs=latn[:, k, :],
                                                                   start=(k == 0), stop=(k == 1)), R=["wqup", ("latn", 0), ("latn", 1)], W=[pok])
                    P.op("act", lambda e, po=po: e.copy(out=hr[:, 0, :], in_=po[0:96, :]), R=[pok], W=["hr"])
                else:
                    P.op("pe", lambda e, po=po, h=h: e.matmul(po[0:96, :], lhsT=wkn[:, h, :], rhs=latn[:, 2, :], start=True, stop=True),
                         R=["wkn", ("latn", 2)], W=[pok])
                    P.op("dve", lambda e, po=po: e.tensor_tensor(out=hr[:, 0, :], in0=po[0:96, :], in1=krp[:, :], op=ALU.add),
                         R=[pok, "krp"], W=["hr"])
                emit_rmsnorm_fm(C, hr, ["hr"], 1, 0, 512, gqh if which == 0 else gkh, "gqh" if which == 0 else "gkh",
                                hn, ["hn"], 0, 96, npart=96)
                pp, ppk = C.ps.next()
                P.op("pe", lambda e, pp=pp: e.matmul(pp[0:96, :], lhsT=pmt[:, :], rhs=hn[:, 0, :], start=True, stop=True), R=["pmt", "hn"], W=[ppk])
                a, ak = t1.next()
                b, bk = t2.next()
                ci, si = (0, 1) if which == 0 else (2, 3)
                P.op("pool", lambda e, a=a, ci=ci: e.tensor_tensor(out=a[:, :], in0=hn[:, 0, :], in1=tabs[:, ci, :], op=ALU.mult),
                     R=["hn", ("tabs", ci)], W=[ak])
                P.op("dve", lambda e, b=b, pp=pp, si=si: e.tensor_tensor(out=b[:, :], in0=pp[0:96, :], in1=tabs[:, si, :], op=ALU.mult),
                     R=[ppk, ("tabs", si)], W=[bk])
                a3, a3k = t3.next()
                P.op("pool", lambda e, a=a, b=b, a3=a3: e.tensor_tensor(out=a3[:, :], in0=a[:, :], in1=b[:, :], op=ALU.add), R=[ak, bk], W=[a3k])
                dst = mqT if which == 0 else mkT
                P.dma("sp", dst[h, :, t0:t0 + 512], a3[:, :], R=[a3k], W=[("o4", h, which, tg)])
    P.finish("sp")
    P.emit()
    return nc


def rope_tables(S):
    inv_freq = (10000.0 ** (-np.arange(0, 32, 2, dtype=np.float32) / np.float32(32))).astype(np.float32)
    ang = (np.arange(S, dtype=np.float32)[:, None] * inv_freq[None, :]).astype(np.float32)
    cos = np.cos(ang).astype(np.float32).T
    sin = np.sin(ang).astype(np.float32).T
    Ct = np.ones((96, S), np.float32); St = np.zeros((96, S), np.float32)
    Ct[64:80] = cos; Ct[80:96] = cos
    St[64:80] = sin; St[80:96] = sin
    pm = np.zeros((96, 96), np.float32)
    for i in range(16):
        pm[80 + i, 64 + i] = -1.0
        pm[64 + i, 80 + i] = 1.0
    return Ct, St, pm


def build_L2(S, n_sb=2, n_mla=2):
    nc = bass.Bass("TRN2", target_bir_lowering=False)
    dt = lambda n, s, k="ExternalInput": nc.dram_tensor(n, s, F32, kind=k).ap()
    dtb = lambda n, s: nc.dram_tensor(n, s, BF16, kind="ExternalInput").ap()
    sbqT = dtb("sbqT", [2, 64, S]); sbkT = dtb("sbkT", [2, 64, S]); sbv = dtb("sbv", [2, S, 64])
    mqT = dtb("mqT", [2, 96, S]); mkT = dtb("mkT", [2, 96, S]); mv = dtb("mv", [2, S, 64])
    oT = dt("oT", [4, 64, S], "ExternalOutput")
    C = Ctx(nc, n_ps=3)
    P = C.P
    A = nc.alloc_sbuf_tensor
    NB = S // 128
    NQG = S // 512
    argp = Rot(nc, "argp", [128, 512], F32, 3, psum=True)
    acc = Rot(nc, "acc", [128, 512], F32, 2, psum=True)
    qTs = [A("qT%d" % i, [128, S], BF16) for i in range(2)]
    kTs = [A("kT%d" % i, [128, S], BF16) for i in range(2)]
    vas = [A("va%d" % i, [128, NB, 128], BF16) for i in range(2)]
    mle = A("mle", [128, 4, 512], BF16)
    mlt = A("mlt", [128, 4, 512], BF16)
    for bi in range(2):
        for c4 in range(4):
            sl = slice(c4 * (S // 4), (c4 + 1) * (S // 4))
            P.op("pool", lambda e, sl=sl, bi=bi: e.memset(qTs[bi][:, sl], 0.0), W=[("qT", bi, c4)])
            P.op("pool", lambda e, sl=sl, bi=bi: e.memset(kTs[bi][:, sl], 0.0), W=[("kT", bi, c4)])
        P.op("pool", lambda e, bi=bi: e.memset(vas[bi][:], 0.0), W=[("va", bi)])
        P.op("pool", lambda e, bi=bi: e.memset(vas[bi][:, :, 64:65], 1.0), R=[("va", bi)], W=[("va", bi)])
    nuin = A("nuin", [128, 128], BF16)
    nones = A("nones", [128, 128], BF16)
    et = Rot(nc, "et", [128, 512], F32, 2)
    spt = Rot(nc, "spt", [128, 512], BF16, 3)
    wt = Rot(nc, "wt", [128, 512], BF16, 3)
    Rt = Rot(nc, "Rt", [128, 512], BF16, 3)
    ot = Rot(nc, "ot", [128, 512], F32, 2)
    rr = A("rr", [128, 512], F32)
    bcs = A("bcs", [64, 512], F32)
    P.op("pool", lambda e: e.memset(mle[:], 1.0), W=["mle"])
    P.op("pool", lambda e: e.memset(mlt[:], 1.0), W=["mlt"])
    P.op("pool", lambda e: e.memset(nuin[:], -1.0), W=["nuin"])
    P.op("pool", lambda e: e.memset(nones[:], -1.0), W=["nones"])
    for d in range(4):
        P.op("pool", lambda e, d=d: e.affine_select(out=mle[:, d, :], in_=mle[:, d, :], pattern=[[1, 512]], compare_op=ALU.is_ge, fill=0.0,
                                                    base=-128 * d, channel_multiplier=-1), R=["mle"], W=["mle"])
        P.op("pool", lambda e, d=d: e.affine_select(out=mlt[:, d, :], in_=mlt[:, d, :], pattern=[[1, 512]], compare_op=ALU.is_gt, fill=0.0,
                                                    base=-128 * d, channel_multiplier=-1), R=["mlt"], W=["mlt"])
    P.op("pool", lambda e: e.affine_select(out=nuin[:], in_=nuin[:], pattern=[[-1, 128]], compare_op=ALU.is_ge, fill=0.0,
                                           base=0, channel_multiplier=1), R=["nuin"], W=["nuin"])
    CW = S // 4
    NH = n_sb + n_mla

    def head_cfg(hd):
        is_sb = hd < n_sb
        hh = hd if is_sb else hd - n_sb
        return is_sb, hh, (64 if is_sb else 96), ((sbqT, sbkT, sbv) if is_sb else (mqT, mkT, mv))

    def emit_loads(hd):
        is_sb, hh, dq, (qsrc, ksrc, vsrc) = head_cfg(hd)
        bi = hd % 2
        for c4 in range(4):
            sl = slice(c4 * CW, (c4 + 1) * CW)
            P.dma("sp", qTs[bi][0:dq, sl], qsrc[hh, :, sl], W=[("qT", bi, c4)])
            P.dma("sp", kTs[bi][0:dq, sl], ksrc[hh, :, sl], W=[("kT", bi, c4)])
        P.dma("sp", vas[bi][:, :, 0:64], vsrc[hh].rearrange("(kb p) c -> p kb c", p=128), R=[("va", bi)], W=[("va", bi)])

    emit_loads(0)
    for hd in range(NH):
        is_sb, hh, dq, _ = head_cfg(hd)
        bi = hd % 2
        qT, kT, va = qTs[bi], kTs[bi], vas[bi]
        vak = ("va", bi)
        qkeys = lambda qg, bi=bi: [("qT", bi, c) for c in range((qg * 512) // CW, (qg * 512 + 511) // CW + 1)]
        kkeys = lambda kb, bi=bi: [("kT", bi, c) for c in range((kb * 128) // CW, (kb * 128 + 127) // CW + 1)]
        if hd + 1 < NH:
            emit_loads(hd + 1)
        blocks = []
        for qg in range(NQG):
            nkb = 4 * (qg + 1)
            order = range(nkb - 1, -1, -1) if is_sb else range(nkb)
            for i, kb in enumerate(order):
                blocks.append((qg, i, kb, nkb))
        nblk = len(blocks)
        st = {}
        qstate = {}

        def stage_z(t):
            qg, i, kb, nkb = blocks[t]
            kTl, qTl, val, dql, sbl, hdl = kT, qT, va, dq, is_sb, hd
            zp, zpk = C.ps.next()
            kq = 128
            P.op("pe", lambda e: e.matmul(zp[:, :], lhsT=kTl[0:kq, kb * 128:(kb + 1) * 128], rhs=qTl[0:kq, qg * 512:(qg + 1) * 512],
                                          start=True, stop=True), R=qkeys(qg) + kkeys(kb), W=[zpk])
            st[t] = dict(zp=zp, zpk=zpk)

        def stage_a_sb(t):
            qg, i, kb, nkb = blocks[t]
            kTl, qTl, val, dql, sbl, hdl = kT, qT, va, dq, is_sb, hd
            d = kb - 4 * qg
            s_ = st[t]
            zp, zpk = s_["zp"], s_["zpk"]
            e_, ek = et.next()
            P.op("act", lambda e: e.activation(out=e_[:, :], in_=zp[:, :], func=AF.Exp), R=[zpk], W=[ek])
            s_["e"] = (e_, ek)

        def stage_a2_sb(t):
            qg, i, kb, nkb = blocks[t]
            kTl, qTl, val, dql, sbl, hdl = kT, qT, va, dq, is_sb, hd
            d = kb - 4 * qg
            s_ = st[t]
            e_, ek = s_["e"]
            sp, spk = spt.next()
            P.op("act", lambda e: e.activation(out=sp[:, :], in_=e_[:, :], func=AF.Ln, bias=C.ones_f[:, 0:1], scale=1.0),
                 R=[ek, "ones_f"], W=[spk])
            if d >= 0:
                P.op("pool", lambda e: e.tensor_tensor(out=sp[:, :], in0=sp[:, :], in1=mlt[:, d, :], op=ALU.mult), R=[spk, "mlt"], W=[spk])
            ap_, apk = argp.next()
            P.op("pe", lambda e: e.matmul(ap_[:, :], lhsT=kTl[:, kb * 128:(kb + 1) * 128], rhs=qTl[:, qg * 512:(qg + 1) * 512],
                                          start=True, stop=False), R=qkeys(qg) + kkeys(kb), W=[apk])
            P.op("pe", lambda e: e.matmul(ap_[:, :], lhsT=nuin[:, :], rhs=sp[:, :], start=False, stop=(i == 0)), R=[spk, "nuin"], W=[apk])
            if i > 0:
                Rp, Rpk = qstate[qg]["R"]
                P.op("pe", lambda e: e.matmul(ap_[:, :], lhsT=nones[:, :], rhs=Rp[:, :], start=False, stop=True), R=[Rpk, "nones"], W=[apk])
            if kb > 0:
                Rn, Rnk = Rt.next()
                if i == 0:
                    P.op("pool", lambda e: e.tensor_copy(out=Rn[:, :], in_=sp[:, :]), R=[spk], W=[Rnk])
                else:
                    Rp, Rpk = qstate[qg]["R"]
                    P.op("pool", lambda e: e.tensor_tensor(out=Rn[:, :], in0=Rp[:, :], in1=sp[:, :], op=ALU.add), R=[spk, Rpk], W=[Rnk])
                qstate.setdefault(qg, {})["R"] = (Rn, Rnk)
            s_["arg"] = (ap_, apk)

        def stage_b(t):
            qg, i, kb, nkb = blocks[t]
            kTl, qTl, val, dql, sbl, hdl = kT, qT, va, dq, is_sb, hd
            d = kb - 4 * qg
            s_ = st[t]
            if i == 0:
                qstate.setdefault(qg, {})["acc"] = acc.next()
            op_, opk = qstate[qg]["acc"]
            src, srck = s_["arg"] if is_sb else (s_["zp"], s_["zpk"])
            w_, wk_ = wt.next()
            P.op("act", lambda e: e.activation(out=w_[:, :], in_=src[:, :], func=AF.Exp), R=[srck], W=[wk_])
            if d >= 0:
                mk_ = mlt if is_sb else mle
                P.op("dve", lambda e: e.tensor_tensor(out=w_[:, :], in0=w_[:, :], in1=mk_[:, d, :], op=ALU.mult),
                     R=[wk_, "mlt" if is_sb else "mle"], W=[wk_])
            last = (i == nkb - 1)
            nv = 64 if is_sb else 65
            P.op("pe", lambda e: e.matmul(op_[:, :], lhsT=val[:, kb, :], rhs=w_[:, :], start=(i == 0), stop=last), R=[wk_, vak], W=[opk])
            if last:
                o_, ok_ = ot.next()
                if is_sb:
                    P.op("dve", lambda e: e.tensor_copy(out=o_[0:64, :], in_=op_[0:64, :]), R=[opk], W=[ok_])
                else:
                    P.op("dve", lambda e: e.reciprocal(out=rr[64:65, :], in_=op_[64:65, :]), R=[opk], W=["rr"])
                    bc, bck = argp.next()
                    P.op("pe", lambda e: e.matmul(bc[0:64, :], lhsT=C.ones_f[64:65, 0:64], rhs=rr[64:65, :], start=True, stop=True),
                         R=["rr", "ones_f"], W=[bck])
                    P.op("dve", lambda e: e.tensor_copy(out=bcs[:, :], in_=bc[0:64, :]), R=[bck], W=["bcs"])
                    P.op("dve", lambda e: e.tensor_tensor(out=o_[0:64, :], in0=op_[0:64, :], in1=bcs[:, :], op=ALU.mult), R=[opk, "bcs"], W=[ok_])
                P.dma("sp", oT[hdl, :, qg * 512:(qg + 1) * 512], o_[0:64, :], R=[ok_], W=[("oo", hd, qg)])
            del st[t]

        for t in range(-2, nblk):
            if 0 <= t + 2 < nblk:
                stage_z(t + 2)
            if is_sb:
                if 0 <= t + 1 < nblk:
                    stage_a_sb(t + 1)
                if 0 <= t:
                    stage_b(t)
                if 0 <= t + 1 < nblk:
                    stage_a2_sb(t + 1)
            else:
                if 0 <= t:
                    stage_b(t)
    P.finish("sp")
    P.emit()
    return nc


TWO_PI = 6.283185307179586
PI = 3.141592653589793
LCH = 512


def build_L4(S):
    nc = bass.Bass("TRN2", target_bir_lowering=False)
    dt = lambda n, s, k="ExternalInput": nc.dram_tensor(n, s, F32, kind=k).ap()
    uT = dt("uT", [128, S])
    a_re = dt("a_re", [128, 4]); a_im = dt("a_im", [128, 4]); ldt = dt("ldt", [128, 4])
    b_re = dt("b_re", [4, 128, 16]); b_im = dt("b_im", [4, 128, 16])
    ct_re = dt("ct_re", [4, 128, 16]); ct_im = dt("ct_im", [4, 128, 16])
    dsk = dt("dsk", [128])
    yT = dt("yT", [128, S], "ExternalOutput")
    C = Ctx(nc)
    P = C.P
    A = nc.alloc_sbuf_tensor
    NCH = S // LCH
    ub = A("ub", [128, S], BF16)
    P.dma("pool", ub[:], uT, W=["ub"])
    par = A("par", [128, 16, 4], F32)
    AR, AI, DT, ARD, TH, LRE, LIM, NUM, DEN, CRE, CIM, TMP, TMP2, MRE, MIM, NMIM = range(16)
    pk = lambda i: ("par", i)
    P.dma("sp", par[:, AR, :], a_re, W=[pk(AR)])
    P.dma("sp", par[:, AI, :], a_im, W=[pk(AI)])
    P.dma("sp", par[:, DT, :], ldt, W=[pk(DT)])
    dvec = load_vec_fm(C, "dvec", dsk, 128)
    cpi = A("cpi", [128, 1], F32)
    P.op("pool", lambda e: e.memset(cpi[:], PI), W=["cpi"])
    bst = A("bst", [128, 4, 4, 16], F32)
    for i, src in enumerate((b_re, b_im, ct_re, ct_im)):
        P.dma("sp", bst[:, i, :, :], src.rearrange("j p c -> p j c"), W=[("bst", i)], allow_slow_non_contiguous=True)
    io_i = A("io_i", [128, LCH], I32)
    io_f = A("io_f", [128, LCH], F32)
    P.op("pool", lambda e: e.iota(io_i[:], pattern=[[1, LCH]], base=0, channel_multiplier=0), W=["io_i"])
    P.op("dve", lambda e: e.tensor_copy(out=io_f[:], in_=io_i[:]), R=["io_i"], W=["io_f"])
    onesL = A("onesL", [128, LCH], F32)
    P.op("pool", lambda e: e.memset(onesL[:], 1.0), W=["onesL"])

    def ts(out, in0, s1, s2, o0, o1=None, R=(), W=()):
        if o1 is None:
            P.op("dve", lambda e: e.tensor_scalar(out=out, in0=in0, scalar1=s1, scalar2=None, op0=o0), R=R, W=W)
        else:
            P.op("dve", lambda e: e.tensor_scalar(out=out, in0=in0, scalar1=s1, scalar2=s2, op0=o0, op1=o1), R=R, W=W)

    def tt(out, in0, in1, o, R=(), W=(), eng="dve"):
        P.op(eng, lambda e: e.tensor_tensor(out=out, in0=in0, in1=in1, op=o), R=R, W=W)

    pv = lambda i: par[:, i, :]
    P.op("act", lambda e: e.activation(out=pv(DT), in_=pv(DT), func=AF.Exp), R=[pk(DT)], W=[pk(DT)])
    ts(pv(AR), pv(AR), -1e-4, None, ALU.min, R=[pk(AR)], W=[pk(AR)])
    tt(pv(ARD), pv(AR), pv(DT), ALU.mult, R=[pk(AR), pk(DT)], W=[pk(ARD)])
    tt(pv(TH), pv(AI), pv(DT), ALU.mult, R=[pk(AI), pk(DT)], W=[pk(TH)])
    tab = A("tab", [128, 4, 4, LCH], F32)
    scr = Rot(nc, "scr", [128, LCH], F32, 8)
    scri = Rot(nc, "scri", [128, LCH], I32, 2)
    nard = A("nard", [128, 4], F32)
    ts(nard[:, :], pv(ARD), -1.0, None, ALU.mult, R=[pk(ARD)], W=["nard"])
    def sin_of(ang, angk):
        t, tk = scr.next()
        ki, kik = scri.next()
        ts(t[:, :], ang[:, :], 1.0 / TWO_PI, None, ALU.mult, R=[angk], W=[tk])
        P.op("dve", lambda e: e.tensor_copy(out=ki[:, :], in_=t[:, :]), R=[tk], W=[kik])
        P.op("dve", lambda e: e.tensor_copy(out=t[:, :], in_=ki[:, :]), R=[kik], W=[tk])
        P.op("dve", lambda e: e.scalar_tensor_tensor(out=ang[:, :], in0=t[:, :], scalar=-TWO_PI, in1=ang[:, :], op0=ALU.mult, op1=ALU.add),
             R=[tk, angk], W=[angk])
        ts(t[:, :], ang[:, :], PI, -TWO_PI, ALU.is_gt, ALU.mult, R=[angk], W=[tk])
        tt(ang[:, :], ang[:, :], t[:, :], ALU.add, R=[angk, tk], W=[angk])
        ts(t[:, :], ang[:, :], -PI, TWO_PI, ALU.is_lt, ALU.mult, R=[angk], W=[tk])
        tt(ang[:, :], ang[:, :], t[:, :], ALU.add, R=[angk, tk], W=[angk])
        ts(ang[:, :], ang[:, :], PI, -PI, ALU.min, ALU.max, R=[angk], W=[angk])
        P.op("act", lambda e: e.activation(out=t[:, :], in_=ang[:, :], func=AF.Sin), R=[angk], W=[tk])
        return t, tk

    for j in range(4):
        ang, angk = scr.next()
        ts(ang[:, :], io_f[:, :], par[:, TH, j:j + 1], None, ALU.mult, R=["io_f", pk(TH)], W=[angk])
        sn, snk = sin_of(ang, angk)
        ang2, ang2k = scr.next()
        ts(ang2[:, :], io_f[:, :], par[:, TH, j:j + 1], PI / 2, ALU.mult, ALU.add, R=["io_f", pk(TH)], W=[ang2k])
        cs, csk = sin_of(ang2, ang2k)
        mg, mgk = scr.next()
        P.op("act", lambda e, mg=mg, j=j: e.activation(out=mg[:, :], in_=io_f[:, :], func=AF.Exp, scale=par[:, ARD, j:j + 1]),
             R=["io_f", pk(ARD)], W=[mgk])
        tt(tab[:, 2, j, :], mg[:, :], cs[:, :], ALU.mult, R=[mgk, csk], W=[("tab", 2, j)])
        tt(tab[:, 3, j, :], mg[:, :], sn[:, :], ALU.mult, R=[mgk, snk], W=[("tab", 3, j)])
        mg2, mg2k = scr.next()
        P.op("act", lambda e, mg2=mg2, j=j: e.activation(out=mg2[:, :], in_=io_f[:, :], func=AF.Exp, scale=nard[:, j:j + 1]),
             R=["io_f", "nard"], W=[mg2k])
        tt(tab[:, 0, j, :], mg2[:, :], cs[:, :], ALU.mult, R=[mg2k, csk], W=[("tab", 0, j)])
        P.op("dve", lambda e, mg2=mg2, sn=sn, j=j: e.scalar_tensor_tensor(out=tab[:, 1, j, :], in0=mg2[:, :], scalar=-1.0, in1=sn[:, :],
                                                                          op0=ALU.mult, op1=ALU.mult), R=[mg2k, snk], W=[("tab", 1, j)])
    for j in range(4):
        P.op("dve", lambda e, j=j: e.tensor_copy(out=par[:, LRE, j:j + 1], in_=tab[:, 2, j, 1:2]), R=[("tab", 2, j)], W=[pk(LRE)])
        P.op("dve", lambda e, j=j: e.tensor_copy(out=par[:, LIM, j:j + 1], in_=tab[:, 3, j, 1:2]), R=[("tab", 3, j)], W=[pk(LIM)])
    for j in range(4):
        l5r = tab[:, 2, j, LCH - 1:LCH]; l5i = tab[:, 3, j, LCH - 1:LCH]
        RK = [pk(LRE), pk(LIM), ("tab", 2, j), ("tab", 3, j)]
        tt(par[:, TMP, j:j + 1], par[:, LRE, j:j + 1], l5r, ALU.mult, R=RK, W=[pk(TMP)])
        tt(par[:, TMP2, j:j + 1], par[:, LIM, j:j + 1], l5i, ALU.mult, R=RK, W=[pk(TMP2)])
        tt(par[:, MRE, j:j + 1], par[:, TMP, j:j + 1], par[:, TMP2, j:j + 1], ALU.subtract, R=[pk(TMP), pk(TMP2)], W=[pk(MRE)])
        tt(par[:, TMP, j:j + 1], par[:, LRE, j:j + 1], l5i, ALU.mult, R=RK + [pk(MRE)], W=[pk(TMP)])
        tt(par[:, TMP2, j:j + 1], par[:, LIM, j:j + 1], l5r, ALU.mult, R=RK + [pk(MRE)], W=[pk(TMP2)])
        tt(par[:, MIM, j:j + 1], par[:, TMP, j:j + 1], par[:, TMP2, j:j + 1], ALU.add, R=[pk(TMP), pk(TMP2)], W=[pk(MIM)])
    ts(pv(NMIM), pv(MIM), -1.0, None, ALU.mult, R=[pk(MIM)], W=[pk(NMIM)])
    ts(pv(NUM), pv(LRE), -1.0, None, ALU.add, R=[pk(LRE)], W=[pk(NUM)])
    tt(pv(DEN), pv(AR), pv(AR), ALU.mult, R=[pk(AR)], W=[pk(DEN)])
    tt(pv(TMP), pv(AI), pv(AI), ALU.mult, R=[pk(AI), pk(MIM), pk(MRE)], W=[pk(TMP)])
    tt(pv(DEN), pv(DEN), pv(TMP), ALU.add, R=[pk(DEN), pk(TMP)], W=[pk(DEN)])
    P.op("dve", lambda e: e.reciprocal(out=pv(DEN), in_=pv(DEN)), R=[pk(DEN)], W=[pk(DEN)])
    tt(pv(TMP), pv(NUM), pv(AR), ALU.mult, R=[pk(NUM), pk(AR), pk(DEN)], W=[pk(TMP)])
    tt(pv(TMP2), pv(LIM), pv(AI), ALU.mult, R=[pk(LIM), pk(AI), pk(NMIM)], W=[pk(TMP2)])
    tt(pv(CRE), pv(TMP), pv(TMP2), ALU.add, R=[pk(TMP), pk(TMP2)], W=[pk(CRE)])
    tt(pv(CRE), pv(CRE), pv(DEN), ALU.mult, R=[pk(CRE), pk(DEN)], W=[pk(CRE)])
    tt(pv(TMP), pv(LIM), pv(AR), ALU.mult, R=[pk(LIM), pk(AR), pk(CRE)], W=[pk(TMP)])
    tt(pv(TMP2), pv(NUM), pv(AI), ALU.mult, R=[pk(NUM), pk(AI), pk(CRE)], W=[pk(TMP2)])
    tt(pv(CIM), pv(TMP), pv(TMP2), ALU.subtract, R=[pk(TMP), pk(TMP2)], W=[pk(CIM)])
    tt(pv(CIM), pv(CIM), pv(DEN), ALU.mult, R=[pk(CIM), pk(DEN)], W=[pk(CIM)])
    bfull = A("bfull", [128, 2, 4, 128], F32)
    P.op("pool", lambda e: e.memset(bfull[:], 0.0), W=["bfull"])
    BT = A("BT", [128, 2, 4, 128], BF16)
    CTt = A("CTt", [128, 2, 4, 128], BF16)
    P.op("pool", lambda e: e.memset(CTt[:], 0.0), W=["CTt"])
    t16 = Rot(nc, "t16", [128, 16], F32, 4)
    for j in range(4):
        for g in range(2):
            ps_ = slice(g * 64, (g + 1) * 64)
            c0 = 32 * j + 16 * g
            for which in range(2):
                ta, tak = t16.next()
                tb, tbk = t16.next()
                s_a = bst[ps_, 0 if which == 0 else 1, j, :]
                s_b = bst[ps_, 1 if which == 0 else 0, j, :]
                ts(ta[ps_, :], s_a, par[ps_, CRE, j:j + 1], None, ALU.mult, R=[("bst", 0), ("bst", 1), pk(CRE)], W=[tak])
                ts(tb[ps_, :], s_b, par[ps_, CIM, j:j + 1], None, ALU.mult, R=[("bst", 0), ("bst", 1), pk(CIM)], W=[tbk])
                tt(bfull[ps_, which, j, c0:c0 + 16], ta[ps_, :], tb[ps_, :], ALU.subtract if which == 0 else ALU.add,
                   R=[tak, tbk, "bfull"], W=["bfull"])
            P.op("dve", lambda e, ps_=ps_, j=j, c0=c0: e.tensor_copy(out=CTt[ps_, 0, j, c0:c0 + 16], in_=bst[ps_, 2, j, :]),
                 R=[("bst", 2), "CTt"], W=["CTt"])
            ts(CTt[ps_, 1, j, c0:c0 + 16], bst[ps_, 3, j, :], -1.0, None, ALU.mult, R=[("bst", 3), "CTt"], W=["CTt"])
    for j in range(4):
        for which in range(2):
            pt, ptk = C.ps.next()
            P.op("pe", lambda e, pt=pt, which=which, j=j: e.transpose(out=pt[:, 0:128], in_=bfull[:, which, j, :], identity=C.ident_f[:]),
                 R=["bfull", "ident_f"], W=[ptk])
            P.op("act", lambda e, pt=pt, which=which, j=j: e.copy(out=BT[:, which, j, :], in_=pt[:, 0:128]), R=[ptk], W=[("BT", which, j)])
    G = A("G", [128, NCH + 1, 4, 2], F32)
    P.op("pool", lambda e: e.memset(G[:], 0.0), W=["G"])
    pt_ = Rot(nc, "pt_", [128, LCH], F32, 4)
    Pre = Rot(nc, "Pre", [128, LCH], F32, 2)
    Pim = Rot(nc, "Pim", [128, LCH], F32, 2)
    Sre = Rot(nc, "Sre", [128, LCH], F32, 2)
    Sim = Rot(nc, "Sim", [128, LCH], F32, 2)
    hre = Rot(nc, "hre", [128, LCH], BF16, 8)
    him = Rot(nc, "him", [128, LCH], BF16, 8)
    yv = Rot(nc, "yv", [128, LCH], F32, 2)
    gt = Rot(nc, "gt", [128, LCH], F32, 2)
    sml = Rot(nc, "sml", [128, 2], F32, 4)
    for ch in range(NCH):
        c0 = ch * LCH
        hs = []
        for j in range(4):
            bre, brek = C.ps.next()
            P.op("pe", lambda e, bre=bre, j=j, c0=c0: e.matmul(bre[:, :], lhsT=BT[:, 0, j, :], rhs=ub[:, c0:c0 + LCH], start=True, stop=True),
                 R=[("BT", 0, j), "ub"], W=[brek])
            bim, bimk = C.ps.next()
            P.op("pe", lambda e, bim=bim, j=j, c0=c0: e.matmul(bim[:, :], lhsT=BT[:, 1, j, :], rhs=ub[:, c0:c0 + LCH], start=True, stop=True),
                 R=[("BT", 1, j), "ub"], W=[bimk])
            a1, a1k = pt_.next(); a2, a2k = pt_.next(); a3, a3k = pt_.next(); a4, a4k = pt_.next()
            tt(a1[:, :], bre[:, :], tab[:, 0, j, :], ALU.mult, R=[brek, ("tab", 0, j)], W=[a1k])
            tt(a2[:, :], bim[:, :], tab[:, 1, j, :], ALU.mult, R=[bimk, ("tab", 1, j)], W=[a2k])
            tt(a3[:, :], bim[:, :], tab[:, 0, j, :], ALU.mult, R=[bimk, ("tab", 0, j)], W=[a3k])
            tt(a4[:, :], bre[:, :], tab[:, 1, j, :], ALU.mult, R=[brek, ("tab", 1, j)], W=[a4k])
            pr, prk = Pre.next(); pi_, pik = Pim.next()
            tt(pr[:, :], a1[:, :], a2[:, :], ALU.subtract, R=[a1k, a2k], W=[prk], eng="pool")
            tt(pi_[:, :], a3[:, :], a4[:, :], ALU.add, R=[a3k, a4k], W=[pik], eng="pool")
            sr, srk = Sre.next(); si, sik = Sim.next()
            P.op("dve", lambda e, sr=sr, pr=pr, ch=ch, j=j: e.tensor_tensor_scan(out=sr[:, :], data0=onesL[:, :], data1=pr[:, :],
                                                                                initial=G[:, ch, j, 0:1], op0=ALU.mult, op1=ALU.add),
                 R=[prk, "onesL", ("G", ch, j), "G"], W=[srk])
            P.op("dve", lambda e, si=si, pi_=pi_, ch=ch, j=j: e.tensor_tensor_scan(out=si[:, :], data0=onesL[:, :], data1=pi_[:, :],
                                                                                  initial=G[:, ch, j, 1:2], op0=ALU.mult, op1=ALU.add),
                 R=[pik, "onesL", ("G", ch, j), "G"], W=[sik])
            sm_, smk = sml.next()
            ts(sm_[:, 0:1], sr[:, LCH - 1:LCH], par[:, MRE, j:j + 1], None, ALU.mult, R=[srk, pk(MRE)], W=[smk])
            ts(sm_[:, 1:2], si[:, LCH - 1:LCH], par[:, MRE, j:j + 1], None, ALU.mult, R=[sik, pk(MRE)], W=[smk])
            P.op("dve", lambda e, sm_=sm_, si=si, ch=ch, j=j: e.scalar_tensor_tensor(out=G[:, ch + 1, j, 0:1], in0=si[:, LCH - 1:LCH],
                                                                                    scalar=par[:, NMIM, j:j + 1], in1=sm_[:, 0:1],
                                                                                    op0=ALU.mult, op1=ALU.add),
                 R=[smk, sik, pk(NMIM), "G"], W=[("G", ch + 1, j, 0)])
            P.op("dve", lambda e, sm_=sm_, sr=sr, ch=ch, j=j: e.scalar_tensor_tensor(out=G[:, ch + 1, j, 1:2], in0=sr[:, LCH - 1:LCH],
                                                                                    scalar=par[:, MIM, j:j + 1], in1=sm_[:, 1:2],
                                                                                    op0=ALU.mult, op1=ALU.add),
                 R=[smk, srk, pk(MIM), "G"], W=[("G", ch + 1, j, 1)])
            P.res[("G", ch + 1, j)] = P.res[("G", ch + 1, j, 1)]
            b1, b1k = pt_.next(); b2, b2k = pt_.next(); b3, b3k = pt_.next(); b4, b4k = pt_.next()
            tt(b1[:, :], sr[:, :], tab[:, 2, j, :], ALU.mult, R=[srk, ("tab", 2, j)], W=[b1k], eng="pool")
            tt(b2[:, :], si[:, :], tab[:, 3, j, :], ALU.mult, R=[sik, ("tab", 3, j)], W=[b2k], eng="pool")
            tt(b3[:, :], si[:, :], tab[:, 2, j, :], ALU.mult, R=[sik, ("tab", 2, j)], W=[b3k], eng="pool")
            tt(b4[:, :], sr[:, :], tab[:, 3, j, :], ALU.mult, R=[srk, ("tab", 3, j)], W=[b4k], eng="pool")
            hr_, hrk = hre.next(); hi_, hik = him.next()
            tt(hr_[:, :], b1[:, :], b2[:, :], ALU.subtract, R=[b1k, b2k], W=[hrk])
            tt(hi_[:, :], b3[:, :], b4[:, :], ALU.add, R=[b3k, b4k], W=[hik])
            hs.append((hr_, hrk, hi_, hik))
        yp, ypk = C.ps.next()
        for j in range(4):
            hr_, hrk, hi_, hik = hs[j]
            P.op("pe", lambda e, yp=yp, hr_=hr_, j=j: e.matmul(yp[:, :], lhsT=CTt[:, 0, j, :], rhs=hr_[:, :], start=(j == 0), stop=False),
                 R=["CTt", hrk], W=[ypk])
            P.op("pe", lambda e, yp=yp, hi_=hi_, j=j: e.matmul(yp[:, :], lhsT=CTt[:, 1, j, :], rhs=hi_[:, :], start=False, stop=(j == 3)),
                 R=["CTt", hik], W=[ypk])
        y_, yk_ = yv.next()
        P.op("dve", lambda e, y_=y_, yp=yp, c0=c0: e.scalar_tensor_tensor(out=y_[:, :], in0=ub[:, c0:c0 + LCH], scalar=dvec[:, 0:1], in1=yp[:, :],
                                                                          op0=ALU.mult, op1=ALU.add), R=[ypk, "ub", "dvec"], W=[yk_])
        g_, gk_ = gt.next()
        tt(g_[:, :], y_[:, :], y_[:, :], ALU.mult, R=[yk_], W=[gk_], eng="pool")
        ts(g_[:, :], g_[:, :], 0.044715, 1.0, ALU.mult, ALU.add, R=[gk_], W=[gk_])
        tt(g_[:, :], g_[:, :], y_[:, :], ALU.mult, R=[gk_, yk_], W=[gk_], eng="pool")
        P.op("act", lambda e, g_=g_: e.activation(out=g_[:, :], in_=g_[:, :], func=AF.Sigmoid, scale=1.5957691216057308), R=[gk_], W=[gk_])
        tt(y_[:, :], y_[:, :], g_[:, :], ALU.mult, R=[gk_, yk_], W=[yk_])
        P.dma("sp", yT[:, c0:c0 + LCH], y_[:, :], R=[yk_], W=[("yo", ch)])
    P.finish("sp")
    P.emit()
    return nc


def s5_core_inputs(ins, b, gq, uT_b):
    gs = slice(8 * gq, 8 * gq + 8)
    def st(a):
        return np.ascontiguousarray(a[gs].reshape(4, 128).T)
    d = dict(uT=np.ascontiguousarray(uT_b[128 * gq:128 * gq + 128]),
             a_re=st(ins["ssm_a_re"][0]), a_im=st(ins["ssm_a_im"][0]),
             ldt=np.ascontiguousarray(np.repeat(ins["ssm_log_dt"][0][gs].reshape(4, 2, 1), 64, axis=2).reshape(4, 128).T),
             b_re=np.ascontiguousarray(ins["ssm_b_re"][0][gs].reshape(4, 128, 16)),
             b_im=np.ascontiguousarray(ins["ssm_b_im"][0][gs].reshape(4, 128, 16)),
             ct_re=np.ascontiguousarray(ins["ssm_c_re"][0][gs].transpose(0, 2, 1).reshape(4, 128, 16)),
             ct_im=np.ascontiguousarray(ins["ssm_c_im"][0][gs].transpose(0, 2, 1).reshape(4, 128, 16)),
             dsk=np.ascontiguousarray(ins["ssm_d"][0][128 * gq:128 * gq + 128]))
    return d


SEQ = 8192
BATCH = 2
TPC = BATCH * SEQ // NCORES
CPB = NCORES // BATCH


def _run(nc, in_maps):
    res = run_bass_kernel_spmd(nc, in_maps, core_ids=list(range(NCORES)))
    return res.results


def kernel(**ins):
    ins = {k: np.ascontiguousarray(np.asarray(v, dtype=np.float32)) for k, v in ins.items()}
    x = ins["x"].reshape(BATCH * SEQ, D_MODEL)
    ca = np.ascontiguousarray
    Ct, St, pm = rope_tables(SEQ)
    sc = np.float32(96 ** -0.5)
    Cq, Sq = ca(Ct * sc), ca(St * sc)
    nc1 = build_L1(TPC)
    maps = []
    for c in range(NCORES):
        p0 = (c % CPB) * TPC
        sl = slice(p0, p0 + TPC)
        maps.append(dict(x=x[c * TPC:(c + 1) * TPC], att_norm=ins["att_norm"][0], w_in=ins["att_w_in"][0],
                         q_lat_norm=ins["att_q_latent_norm"][0], w_q_up=ins["att_w_q_up"][0],
                         kv_lat_norm=ins["att_kv_latent_norm"][0], w_kv_up=ins["att_w_kv_up"][0],
                         q_norm=ins["att_q_norm"][0], k_norm=ins["att_k_norm"][0],
                         cq_t=ca(Cq[:, sl]), sq_t=ca(Sq[:, sl]), ck_t=ca(Ct[:, sl]), sk_t=ca(St[:, sl]), pm=pm))
    r1 = _run(nc1, maps)
    del nc1
    cat = lambda name, b, axis: np.concatenate([r1[b * CPB + i][name] for i in range(CPB)], axis=axis)
    nc2 = build_L2(SEQ)
    maps = []
    for b in range(BATCH):
        sbqT = cat("sbqT", b, 1); sbkT = cat("sbkT", b, 1); sbv = cat("sbv", b, 0)
        mqT = cat("mqT", b, 2); mkT = cat("mkT", b, 2); mv = cat("mv", b, 0)
        for g in range(CPB):
            maps.append(dict(sbqT=ca(sbqT[128 * g:128 * g + 128].reshape(2, 64, SEQ)),
                             sbkT=ca(sbkT[128 * g:128 * g + 128].reshape(2, 64, SEQ)),
                             sbv=ca(sbv[:, 128 * g:128 * g + 128].reshape(SEQ, 2, 64).transpose(1, 0, 2)),
                             mqT=ca(mqT[2 * g:2 * g + 2]), mkT=ca(mkT[2 * g:2 * g + 2]),
                             mv=ca(mv[:, 128 * g:128 * g + 128].reshape(SEQ, 2, 64).transpose(1, 0, 2))))
    r2 = _run(nc2, maps)
    del nc2, r1
    mT = []
    for b in range(BATCH):
        m = np.empty((1024, SEQ), np.float32)
        for g in range(CPB):
            o = r2[b * CPB + g]["oT"]
            m[128 * g:128 * g + 128] = o[0:2].reshape(128, SEQ)
            m[512 + 128 * g:512 + 128 * g + 128] = o[2:4].reshape(128, SEQ)
        mT.append(m)
    nc3 = build_L3(TPC)
    maps = []
    for c in range(NCORES):
        p0 = (c % CPB) * TPC
        maps.append(dict(x=x[c * TPC:(c + 1) * TPC], mT=ca(mT[c // CPB][:, p0:p0 + TPC]), w_out=ins["att_w_out"][0],
                         dffn_norm=ins["dffn_norm"][0], wg=ins["dffn_w_gate"][0], wu=ins["dffn_w_up"][0], wd=ins["dffn_w_down"][0],
                         ssm_norm=ins["ssm_norm"][0], w_sin=ins["ssm_w_in"][0]))
    r3 = _run(nc3, maps)
    del nc3, r2
    nc4 = build_L4(SEQ)
    maps = []
    for b in range(BATCH):
        uT_b = np.concatenate([r3[b * CPB + i]["uT"] for i in range(CPB)], axis=1)
        for gq in range(CPB):
            maps.append(s5_core_inputs(ins, b, gq, uT_b))
    r4 = _run(nc4, maps)
    del nc4
    nc5 = build_L5(TPC)
    maps = []
    for c in range(NCORES):
        b = c // CPB
        p0 = (c % CPB) * TPC
        yT = np.concatenate([r4[b * CPB + gq]["yT"][:, p0:p0 + TPC] for gq in range(CPB)], axis=0)
        maps.append(dict(h2T=r3[c]["h2T"], yT=ca(yT), w_glu=ins["ssm_w_glu"][0], moe_norm=ins["moe_norm"][0], w_r=ins["moe_router"][0],
                         wg=ins["moe_w_gate"][0], wu=ins["moe_w_up"][0], wd=ins["moe_w_down"][0]))
    r5 = _run(nc5, maps)
    out = np.concatenate([r5[c]["out"] for c in range(NCORES)], axis=0).reshape(BATCH, SEQ, D_MODEL)
    return out.astype(np.float32)
```

```python
import contextlib
import numpy as np
import concourse.bass as bass
import concourse.mybir as mybir
from concourse.bass_utils import run_bass_kernel_spmd

F32 = mybir.dt.float32
BF16 = mybir.dt.bfloat16
I32 = mybir.dt.int32
AF = mybir.ActivationFunctionType
ALU = mybir.AluOpType
AX = mybir.AxisListType

D_MODEL = 1024
D_FF = 3584
EPS = 1e-6
NCORES = 8

COMPUTE = ("pe", "act", "dve", "pool")
NDSEM = 8


class Prog:
    def __init__(self, nc):
        self.nc = nc
        self.engs = ("pe", "act", "dve", "pool", "sp")
        self.q = {e: [] for e in self.engs}
        self.cnt = {e: 0 for e in COMPUTE}
        self.known = {e: {} for e in self.engs}
        self.res = {}
        self.dcnt = {}
        self.drr = {e: 0 for e in self.engs}
        self.sems = {}
        self.n_wait = 0
        self.n_op = 0

    def _deps(self, R, W):
        deps = {}
        for r in R:
            st = self.res.get(r)
            if st is not None and st[0] is not None:
                k, v = st[0]
                if deps.get(k, 0) < v:
                    deps[k] = v
        for w in W:
            st = self.res.get(w)
            if st is not None:
                if st[0] is not None:
                    k, v = st[0]
                    if deps.get(k, 0) < v:
                        deps[k] = v
                for k, v in st[1].items():
                    if deps.get(k, 0) < v:
                        deps[k] = v
        return deps

    def _record(self, tok, R, W):
        k, v = tok
        for r in R:
            st = self.res.get(r)
            if st is None:
                st = [None, {}]
                self.res[r] = st
            if st[1].get(k, 0) < v:
                st[1][k] = v
        for w in W:
            self.res[w] = [tok, {}]

    def _emit_waits(self, eng, deps):
        kn = self.known[eng]
        for k, v in deps.items():
            if k == eng and eng == "pe":
                continue
            if kn.get(k, 0) >= v:
                continue
            kn[k] = v
            self.q[eng].append(("w", k, v))
            self.n_wait += 1

    def op(self, eng, fn, R=(), W=()):
        deps = self._deps(R, W)
        self._emit_waits(eng, deps)
        self.cnt[eng] += 1
        tok = (eng, self.cnt[eng])
        self.q[eng].append(("o", fn, eng, 1))
        self._record(tok, R, W)
        self.n_op += 1
        return tok

    def dma(self, eng, out, in_, R=(), W=(), **kw):
        deps = self._deps(R, W)
        j = self.drr[eng]
        self.drr[eng] = (j + 1) % NDSEM
        key = ("d", eng, j)
        prev = self.dcnt.get(key, 0)
        if prev:
            deps[key] = max(deps.get(key, 0), prev * 16)
        self._emit_waits(eng, deps)
        self.dcnt[key] = prev + 1
        tok = (key, (prev + 1) * 16)
        self.q[eng].append(("o", lambda e: e.dma_start(out=out, in_=in_, **kw), key, 16))
        self._record(tok, R, W)
        self.n_op += 1
        return tok

    def finish(self, eng="sp"):
        deps = {k: c * 16 for k, c in self.dcnt.items()}
        self._emit_waits(eng, deps)

    def emit(self):
        nc = self.nc
        with contextlib.ExitStack() as es:
            keys = list(COMPUTE) + list(self.dcnt.keys())
            for k in keys:
                nm = k if isinstance(k, str) else "d_%s_%d" % (k[1], k[2])
                self.sems[k] = es.enter_context(nc.semaphore("s_" + nm))
            block = es.enter_context(nc.Block())
            handles = {"pe": block.tensor, "act": block.scalar, "dve": block.vector,
                       "pool": block.gpsimd, "sp": block.sync}
            sems = self.sems
            for eng in self.engs:
                items = self.q[eng]

                def body(e, items=items):
                    for it in items:
                        if it[0] == "w":
                            e.wait_ge(sems[it[1]], it[2])
                        else:
                            it[1](e).then_inc(sems[it[2]], it[3])
                handles[eng](body)


class Rot:
    def __init__(self, nc, name, shape, dtype, n, psum=False):
        self.tiles = []
        for i in range(n):
            if psum:
                t = nc.alloc_psum_tensor("%s%d" % (name, i), shape, dtype)
            else:
                t = nc.alloc_sbuf_tensor("%s%d" % (name, i), shape, dtype)
            self.tiles.append(t)
        self.name = name
        self.i = 0

    def next(self):
        i = self.i % len(self.tiles)
        self.i += 1
        return self.tiles[i], (self.name, i)


class Ctx:
    def __init__(self, nc, n_ps=8):
        self.nc = nc
        self.P = Prog(nc)
        P = self.P
        self.ps = Rot(nc, "ps", [128, 512], F32, n_ps, psum=True)
        self.ones_bf = nc.alloc_sbuf_tensor("ones_bf", [128, 128], BF16)
        self.ones_f = nc.alloc_sbuf_tensor("ones_f", [128, 128], F32)
        self.ident_f = nc.alloc_sbuf_tensor("ident_f", [128, 128], F32)
        self.eps_t = nc.alloc_sbuf_tensor("eps_t", [128, 1], F32)
        P.op("pool", lambda e: e.memset(self.ones_bf[:], 1.0), W=["ones_bf"])
        P.op("pool", lambda e: e.memset(self.ones_f[:], 1.0), W=["ones_f"])
        P.op("pool", lambda e: e.memset(self.eps_t[:], EPS), W=["eps_t"])
        P.op("pool", lambda e: e.memset(self.ident_f[:], 1.0), W=["ident_f"])
        P.op("pool", lambda e: e.affine_select(out=self.ident_f[:], in_=self.ident_f[:], pattern=[[-1, 128]],
                                               compare_op=ALU.is_equal, fill=0.0, base=0, channel_multiplier=1),
             R=["ident_f"], W=["ident_f"])
        self.sq = Rot(nc, "sq", [128, 8, 512], BF16, 1)
        self.rt = Rot(nc, "rt", [128, 512], F32, 2)


def emit_rmsnorm_fm(C, hT, hkeys, nk, tok0, ntok, gain_sb, gkey, xnT, xkeys, xtok0, D, npart=128):
    P = C.P
    sq, sqk = C.sq.next()
    P.op("act", lambda e: e.activation(out=sq[:npart, 0:nk, 0:ntok], in_=hT[:npart, 0:nk, tok0:tok0 + ntok], func=AF.Square),
         R=list(hkeys), W=[sqk])
    ps, psk = C.ps.next()
    for k in range(nk):
        P.op("pe", lambda e, k=k: e.matmul(ps[:npart, 0:ntok], lhsT=C.ones_bf[:npart, :npart], rhs=sq[:npart, k, 0:ntok],
                                          start=(k == 0), stop=(k == nk - 1)),
             R=[sqk, "ones_bf"], W=[psk])
    rt, rtk = C.rt.next()
    P.op("act", lambda e: e.activation(out=rt[:npart, 0:ntok], in_=ps[:npart, 0:ntok], func=AF.Sqrt,
                                       bias=C.eps_t[:npart, 0:1], scale=1.0 / D),
         R=[psk, "eps_t"], W=[rtk])
    P.op("dve", lambda e: e.reciprocal(out=rt[:npart, 0:ntok], in_=rt[:npart, 0:ntok]), R=[rtk], W=[rtk])
    for k in range(nk):
        P.op("dve", lambda e, k=k: e.scalar_tensor_tensor(out=xnT[:npart, k, xtok0:xtok0 + ntok],
                                                          in0=hT[:npart, k, tok0:tok0 + ntok],
                                                          scalar=gain_sb[:npart, k:k + 1], in1=rt[:npart, 0:ntok],
                                                          op0=ALU.mult, op1=ALU.mult),
             R=[hkeys[k], rtk, gkey], W=[xkeys[k]])


def load_vec_fm(C, name, dram_vec_ap, n):
    nk = max(1, n // 128)
    npart = min(128, n)
    t = C.nc.alloc_sbuf_tensor(name, [128, nk], F32)
    C.P.dma("sp", t[:npart, :], dram_vec_ap.rearrange("(k p) -> p k", p=npart), W=[name], allow_slow_non_contiguous=True)
    return t


def emit_ffn(C, xnT, xkeys, hT, hkeys, T, wg, wu, wd, pools, gate_bc=None):
    P = C.P
    NTG = T // 512
    hidT, hidkeys = pools["hidT"], pools["hidkeys"]
    for wb in range(7):
        wgt, wgk = pools["wgu"].next()
        P.dma("pool", wgt[:], wg[:, wb * 512:(wb + 1) * 512].rearrange("(k p) n -> p k n", p=128), W=[wgk])
        wut, wuk = pools["wgu"].next()
        P.dma("pool", wut[:], wu[:, wb * 512:(wb + 1) * 512].rearrange("(k p) n -> p k n", p=128), W=[wuk])
        for m in range(4):
            mm = wb * 4 + m
            for tg in range(NTG):
                pg, pgk = C.ps.next()
                for k in range(8):
                    P.op("pe", lambda e, k=k, pg=pg, wgt=wgt, m=m, tg=tg: e.matmul(
                        pg[:, :], lhsT=wgt[:, k, m * 128:(m + 1) * 128], rhs=xnT[:, k, tg * 512:(tg + 1) * 512],
                        start=(k == 0), stop=(k == 7)), R=[wgk, xkeys(k, tg)], W=[pgk])
                pu, puk = C.ps.next()
                for k in range(8):
                    P.op("pe", lambda e, k=k, pu=pu, wut=wut, m=m, tg=tg: e.matmul(
                        pu[:, :], lhsT=wut[:, k, m * 128:(m + 1) * 128], rhs=xnT[:, k, tg * 512:(tg + 1) * 512],
                        start=(k == 0), stop=(k == 7)), R=[wuk, xkeys(k, tg)], W=[puk])
                sg, sgk = pools["sg"].next()
                P.op("act", lambda e, sg=sg, pg=pg: e.activation(out=sg[:, :], in_=pg[:, :], func=AF.Silu), R=[pgk], W=[sgk])
                P.op("dve", lambda e, sg=sg, pu=pu, mm=mm, tg=tg: e.tensor_tensor(
                    out=hidT[:, mm, tg * 512:(tg + 1) * 512], in0=sg[:, :], in1=pu[:, :], op=ALU.mult),
                    R=[sgk, puk], W=[hidkeys[mm] + (tg,)])
    for dm in range(8):
        wdt, wdk = pools["wd"].next()
        P.dma("pool", wdt[:], wd[:, dm * 128:(dm + 1) * 128].rearrange("(k p) n -> p k n", p=128), W=[wdk])
        for tg in range(NTG):
            po, pok = C.ps.next()
            for k in range(28):
                P.op("pe", lambda e, k=k, po=po, wdt=wdt, tg=tg: e.matmul(
                    po[:, :], lhsT=wdt[:, k, :], rhs=hidT[:, k, tg * 512:(tg + 1) * 512],
                    start=(k == 0), stop=(k == 27)), R=[wdk, hidkeys[k] + (tg,)], W=[pok])
            if gate_bc is None:
                P.op("dve", lambda e, po=po, dm=dm, tg=tg: e.tensor_tensor(
                    out=hT[:, dm, tg * 512:(tg + 1) * 512], in0=hT[:, dm, tg * 512:(tg + 1) * 512], in1=po[:, :], op=ALU.add),
                    R=[pok, hkeys(dm, tg)], W=[hkeys(dm, tg)])
            else:
                gt, gkf = gate_bc
                tmp, tmpk = pools["sg"].next()
                P.op("dve", lambda e, po=po, tmp=tmp, gt=gt, tg=tg: e.tensor_tensor(
                    out=tmp[:, :], in0=po[:, :], in1=gt[:, tg * 512:(tg + 1) * 512], op=ALU.mult),
                    R=[pok, gkf(tg)], W=[tmpk])
                P.op("dve", lambda e, tmp=tmp, dm=dm, tg=tg: e.tensor_tensor(
                    out=hT[:, dm, tg * 512:(tg + 1) * 512], in0=hT[:, dm, tg * 512:(tg + 1) * 512], in1=tmp[:, :], op=ALU.add),
                    R=[tmpk, hkeys(dm, tg)], W=[hkeys(dm, tg)])


def ffn_pools(nc, T):
    return {
        "hidT": nc.alloc_sbuf_tensor("hidT", [128, 28, T], BF16),
        "hidkeys": [("hid", k) for k in range(28)],
        "wgu": Rot(nc, "wgu", [128, 8, 512], BF16, 4),
        "wd": Rot(nc, "wd", [128, 28, 128], BF16, 2),
        "sg": Rot(nc, "sg", [128, 512], F32, 3),
    }


def emit_load_tm_to_fm(C, src_dram, hT, hkeys, ntiles, stage):
    P = C.P
    for t in range(ntiles):
        st, stk = stage.next()
        P.dma("sp", st[:], src_dram[t * 128:(t + 1) * 128, :], W=[stk])
        for half in range(2):
            ps, psk = C.ps.next()
            for kk in range(4):
                k = half * 4 + kk
                P.op("pe", lambda e, ps=ps, st=st, k=k, kk=kk: e.transpose(out=ps[:, kk * 128:(kk + 1) * 128], in_=st[:, k * 128:(k + 1) * 128],
                                                                           identity=C.ident_f[:]),
                     R=[stk, "ident_f"], W=[psk])
            P.op("dve" if half == 0 else "act",
                 (lambda e, ps=ps, half=half, t=t: e.tensor_copy(out=hT[:, half * 4:half * 4 + 4, t * 128:(t + 1) * 128],
                                                                 in_=ps[:, :].rearrange("p (k n) -> p k n", k=4))) if half == 0 else
                 (lambda e, ps=ps, half=half, t=t: e.copy(out=hT[:, half * 4:half * 4 + 4, t * 128:(t + 1) * 128],
                                                          in_=ps[:, :].rearrange("p (k n) -> p k n", k=4))),
                 R=[psk], W=[hkeys(half * 4 + kk, t // 4) for kk in range(4)])


def emit_store_fm_to_tm(C, hT, hkeys, dst_dram, ntiles, stage):
    P = C.P
    for t in range(ntiles):
        st, stk = stage.next()
        for half in range(2):
            ps, psk = C.ps.next()
            for kk in range(4):
                k = half * 4 + kk
                P.op("pe", lambda e, ps=ps, k=k, kk=kk, t=t: e.transpose(out=ps[:, kk * 128:(kk + 1) * 128], in_=hT[:, k, t * 128:(t + 1) * 128],
                                                                         identity=C.ident_f[:]),
                     R=[hkeys(k, t // 4), "ident_f"], W=[psk])
            if half == 0:
                P.op("dve", lambda e, ps=ps, st=st: e.tensor_copy(out=st[:, 0:512], in_=ps[:, :]), R=[psk], W=[stk + (0,)])
            else:
                P.op("act", lambda e, ps=ps, st=st: e.copy(out=st[:, 512:1024], in_=ps[:, :]), R=[psk], W=[stk + (1,)])
        P.dma("sp", dst_dram[t * 128:(t + 1) * 128, :], st[:], R=[stk + (0,), stk + (1,)], W=[("out", t)])


def build_L3(T):
    nc = bass.Bass("TRN2", target_bir_lowering=False)
    x = nc.dram_tensor("x", [T, 1024], F32, kind="ExternalInput").ap()
    mT = nc.dram_tensor("mT", [1024, T], F32, kind="ExternalInput").ap()
    w_out = nc.dram_tensor("w_out", [1024, 1024], F32, kind="ExternalInput").ap()
    n1 = nc.dram_tensor("dffn_norm", [1024], F32, kind="ExternalInput").ap()
    wg = nc.dram_tensor("wg", [1024, D_FF], F32, kind="ExternalInput").ap()
    wu = nc.dram_tensor("wu", [1024, D_FF], F32, kind="ExternalInput").ap()
    wd = nc.dram_tensor("wd", [D_FF, 1024], F32, kind="ExternalInput").ap()
    n2 = nc.dram_tensor("ssm_norm", [1024], F32, kind="ExternalInput").ap()
    w_sin = nc.dram_tensor("w_sin", [1024, 512], F32, kind="ExternalInput").ap()
    h2T = nc.dram_tensor("h2T", [1024, T], F32, kind="ExternalOutput").ap()
    uT = nc.dram_tensor("uT", [512, T], F32, kind="ExternalOutput").ap()
    C = Ctx(nc)
    P = C.P
    TG = 1024
    hT = nc.alloc_sbuf_tensor("hT", [128, 8, TG], F32)
    xnT = nc.alloc_sbuf_tensor("xnT", [128, 8, TG], BF16)
    pools = ffn_pools(nc, TG)
    stage = Rot(nc, "stage", [128, 1024], F32, 2)
    mts = Rot(nc, "mts", [128, 8, 512], BF16, 2)
    uo = Rot(nc, "uo", [128, 512], F32, 2)
    g1 = load_vec_fm(C, "g1", n1, 1024)
    g2 = load_vec_fm(C, "g2", n2, 1024)
    hk = lambda k, tg: ("h", k, tg)
    xk = lambda k, tg: ("xn", k, tg)
    for grp in range(T // TG):
        t0 = grp * TG
        emit_load_tm_to_fm(C, x[t0:t0 + TG, :], hT, hk, TG // 128, stage)
        wo = []
        for hf in range(2):
            wt, wk = pools["wgu"].next()
            P.dma("pool", wt[:], w_out[:, hf * 512:(hf + 1) * 512].rearrange("(k p) n -> p k n", p=128), W=[wk])
            wo.append((wt, wk))
        for tg in range(TG // 512):
            mt, mk = mts.next()
            P.dma("pool", mt[:], mT[:, t0 + tg * 512:t0 + (tg + 1) * 512].rearrange("(k p) n -> p k n", p=128), W=[mk])
            for dm in range(8):
                wt, wk = wo[dm // 4]
                po, pok = C.ps.next()
                for k in range(8):
                    P.op("pe", lambda e, k=k, po=po, wt=wt, mt=mt, dm=dm: e.matmul(
                        po[:, :], lhsT=wt[:, k, (dm % 4) * 128:(dm % 4 + 1) * 128], rhs=mt[:, k, :],
                        start=(k == 0), stop=(k == 7)), R=[wk, mk], W=[pok])
                P.op("dve", lambda e, po=po, dm=dm, tg=tg: e.tensor_tensor(
                    out=hT[:, dm, tg * 512:(tg + 1) * 512], in0=hT[:, dm, tg * 512:(tg + 1) * 512], in1=po[:, :], op=ALU.add),
                    R=[pok, hk(dm, tg)], W=[hk(dm, tg)])
        for tg in range(TG // 512):
            emit_rmsnorm_fm(C, hT, [hk(k, tg) for k in range(8)], 8, tg * 512, 512, g1, "g1",
                            xnT, [xk(k, tg) for k in range(8)], tg * 512, 1024)
        emit_ffn(C, xnT, xk, hT, hk, TG, wg, wu, wd, pools)
        for k in range(8):
            P.dma("sp", h2T[k * 128:(k + 1) * 128, t0:t0 + TG], hT[:, k, :], R=[hk(k, tg) for tg in range(TG // 512)], W=[("h2o", k)])
        for tg in range(TG // 512):
            emit_rmsnorm_fm(C, hT, [hk(k, tg) for k in range(8)], 8, tg * 512, 512, g2, "g2",
                            xnT, [xk(k, tg) for k in range(8)], tg * 512, 1024)
        wt, wk = pools["wgu"].next()
        P.dma("pool", wt[:], w_sin.rearrange("(k p) n -> p k n", p=128), W=[wk])
        for tg in range(TG // 512):
            for c in range(4):
                po, pok = C.ps.next()
                for k in range(8):
                    P.op("pe", lambda e, k=k, po=po, wt=wt, c=c, tg=tg: e.matmul(
                        po[:, :], lhsT=wt[:, k, c * 128:(c + 1) * 128], rhs=xnT[:, k, tg * 512:(tg + 1) * 512],
                        start=(k == 0), stop=(k == 7)), R=[wk, xk(k, tg)], W=[pok])
                ut, uk = uo.next()
                P.op("act", lambda e, ut=ut, po=po: e.copy(out=ut[:, :], in_=po[:, :]), R=[pok], W=[uk])
                P.dma("sp", uT[c * 128:(c + 1) * 128, t0 + tg * 512:t0 + (tg + 1) * 512], ut[:, :], R=[uk], W=[("uo", c, tg, grp)])
    P.finish("sp")
    P.emit()
    return nc


def build_L5(T, n_exp=8):
    nc = bass.Bass("TRN2", target_bir_lowering=False)
    h2T = nc.dram_tensor("h2T", [1024, T], F32, kind="ExternalInput").ap()
    yT = nc.dram_tensor("yT", [512, T], F32, kind="ExternalInput").ap()
    w_glu = nc.dram_tensor("w_glu", [512, 2048], F32, kind="ExternalInput").ap()
    n1 = nc.dram_tensor("moe_norm", [1024], F32, kind="ExternalInput").ap()
    w_r = nc.dram_tensor("w_r", [1024, 8], F32, kind="ExternalInput").ap()
    wg = nc.dram_tensor("wg", [8, 1024, D_FF], F32, kind="ExternalInput").ap()
    wu = nc.dram_tensor("wu", [8, 1024, D_FF], F32, kind="ExternalInput").ap()
    wd = nc.dram_tensor("wd", [8, D_FF, 1024], F32, kind="ExternalInput").ap()
    out = nc.dram_tensor("out", [T, 1024], F32, kind="ExternalOutput").ap()
    C = Ctx(nc)
    P = C.P
    TG = 1024
    NTG = TG // 512
    hT = nc.alloc_sbuf_tensor("hT", [128, 8, TG], F32)
    xnT = nc.alloc_sbuf_tensor("xnT", [128, 8, TG], BF16)
    pools = ffn_pools(nc, TG)
    stage = Rot(nc, "stage", [128, 1024], F32, 1)
    yts = Rot(nc, "yts", [128, 4, 512], BF16, 1)
    wglu = nc.alloc_sbuf_tensor("wglu", [128, 4, 2048], BF16)
    wrg = nc.alloc_sbuf_tensor("wrg", [128, 8, 8], F32)
    sel = nc.alloc_sbuf_tensor("sel", [8, 8, 128], F32)
    gT = nc.alloc_sbuf_tensor("gT", [8, TG], F32)
    gbc = nc.alloc_sbuf_tensor("gbc", [128, TG], F32)
    sm = Rot(nc, "sm", [128, 64], F32, 2)
    g1 = load_vec_fm(C, "g1", n1, 1024)
    P.dma("pool", wglu[:], w_glu.rearrange("(k p) n -> p k n", p=128), W=["wglu"])
    P.dma("sp", wrg[:], w_r.rearrange("(k p) n -> p k n", p=128), W=["wrg"], allow_slow_non_contiguous=True)
    for k in range(8):
        P.op("dve", lambda e, k=k: e.tensor_scalar(out=wrg[:, k, :], in0=wrg[:, k, :], scalar1=g1[:, k:k + 1], scalar2=None, op0=ALU.mult),
             R=["wrg", "g1"], W=["wrg"])
    P.op("pool", lambda e: e.memset(sel[:], 1.0), W=["sel"])
    P.op("pool", lambda e: e.affine_select(out=sel[:], in_=sel[:], pattern=[[-1, 8], [0, 128]], compare_op=ALU.is_equal, fill=0.0,
                                           base=0, channel_multiplier=1), R=["sel"], W=["sel"])
    hk = lambda k, tg: ("h", k, tg)
    xk = lambda k, tg: ("xn", k, tg)
    for grp in range(T // TG):
        t0 = grp * TG
        for k in range(8):
            P.dma("sp", hT[:, k, :], h2T[k * 128:(k + 1) * 128, t0:t0 + TG], W=[hk(k, tg) for tg in range(NTG)])
        for tg in range(NTG):
            yt, yk = yts.next()
            P.dma("pool", yt[:], yT[:, t0 + tg * 512:t0 + (tg + 1) * 512].rearrange("(k p) n -> p k n", p=128), W=[yk])
            for dm in range(8):
                p1, p1k = C.ps.next()
                for k in range(4):
                    P.op("pe", lambda e, k=k, p1=p1, yt=yt, dm=dm: e.matmul(p1[:, :], lhsT=wglu[:, k, dm * 128:(dm + 1) * 128], rhs=yt[:, k, :],
                                                                          start=(k == 0), stop=(k == 3)), R=["wglu", yk], W=[p1k])
                p2, p2k = C.ps.next()
                for k in range(4):
                    P.op("pe", lambda e, k=k, p2=p2, yt=yt, dm=dm: e.matmul(p2[:, :], lhsT=wglu[:, k, 1024 + dm * 128:1024 + (dm + 1) * 128], rhs=yt[:, k, :],
                                                                          start=(k == 0), stop=(k == 3)), R=["wglu", yk], W=[p2k])
                sg, sgk = pools["sg"].next()
                P.op("act", lambda e, sg=sg, p2=p2: e.activation(out=sg[:, :], in_=p2[:, :], func=AF.Sigmoid), R=[p2k], W=[sgk])
                P.op("dve", lambda e, sg=sg, p1=p1: e.tensor_tensor(out=sg[:, :], in0=sg[:, :], in1=p1[:, :], op=ALU.mult), R=[sgk, p1k], W=[sgk])
                P.op("dve", lambda e, sg=sg, dm=dm, tg=tg: e.tensor_tensor(out=hT[:, dm, tg * 512:(tg + 1) * 512], in0=hT[:, dm, tg * 512:(tg + 1) * 512],
                                                                       in1=sg[:, :], op=ALU.add), R=[sgk, hk(dm, tg)], W=[hk(dm, tg)])
        for tg in range(NTG):
            emit_rmsnorm_fm(C, hT, [hk(k, tg) for k in range(8)], 8, tg * 512, 512, g1, "g1",
                            xnT, [xk(k, tg) for k in range(8)], tg * 512, 1024)
        for tt in range(TG // 128):
            tg = tt // 4
            pl, plk = C.ps.next()
            for k in range(8):
                P.op("pe", lambda e, k=k, pl=pl, tt=tt: e.matmul(pl[:, 0:8], lhsT=hT[:, k, tt * 128:(tt + 1) * 128], rhs=wrg[:, k, :],
                                                             start=(k == 0), stop=(k == 7)), R=[hk(k, tg), "wrg"], W=[plk])
            pss, pssk = C.ps.next()
            sq, sqk = C.sq.next()
            P.op("act", lambda e, sq=sq, tt=tt: e.activation(out=sq[:, :, 0:128], in_=hT[:, :, tt * 128:(tt + 1) * 128], func=AF.Square),
                 R=[hk(k, tg) for k in range(8)], W=[sqk])
            for k in range(8):
                P.op("pe", lambda e, k=k, pss=pss, sq=sq: e.matmul(pss[:, 0:1], lhsT=sq[:, k, 0:128], rhs=C.ones_bf[:, 0:1],
                                                               start=(k == 0), stop=(k == 7)), R=[sqk, "ones_bf"], W=[pssk])
            s, sk = sm.next()
            P.op("act", lambda e, s=s, pss=pss: e.activation(out=s[:, 0:1], in_=pss[:, 0:1], func=AF.Sqrt, bias=C.eps_t[:, 0:1], scale=1.0 / 1024),
                 R=[pssk, "eps_t"], W=[sk])
            P.op("dve", lambda e, s=s: e.reciprocal(out=s[:, 0:1], in_=s[:, 0:1]), R=[sk], W=[sk])
            P.op("dve", lambda e, s=s, pl=pl: e.tensor_scalar(out=s[:, 8:16], in0=pl[:, 0:8], scalar1=s[:, 0:1], scalar2=None, op0=ALU.mult),
                 R=[sk, plk], W=[sk])
            P.op("dve", lambda e, s=s: e.max(out=s[:, 16:24], in_=s[:, 8:16]), R=[sk], W=[sk])
            P.op("dve", lambda e, s=s: e.tensor_scalar(out=s[:, 24:25], in0=s[:, 16:17], scalar1=-1.0, scalar2=None, op0=ALU.mult), R=[sk], W=[sk])
            P.op("act", lambda e, s=s: e.activation(out=s[:, 32:40], in_=s[:, 8:16], func=AF.Exp, bias=s[:, 24:25], scale=1.0), R=[sk], W=[sk])
            P.op("dve", lambda e, s=s: e.tensor_scalar(out=s[:, 40:48], in0=s[:, 8:16], scalar1=s[:, 17:18], scalar2=None, op0=ALU.is_ge), R=[sk], W=[sk])
            P.op("dve", lambda e, s=s: e.tensor_tensor(out=s[:, 32:40], in0=s[:, 32:40], in1=s[:, 40:48], op=ALU.mult), R=[sk], W=[sk])
            P.op("dve", lambda e, s=s: e.reduce_sum(out=s[:, 48:49], in_=s[:, 32:40], axis=AX.X), R=[sk], W=[sk])
            P.op("dve", lambda e, s=s: e.reciprocal(out=s[:, 48:49], in_=s[:, 48:49]), R=[sk], W=[sk])
            P.op("dve", lambda e, s=s: e.tensor_scalar(out=s[:, 32:40], in0=s[:, 32:40], scalar1=s[:, 48:49], scalar2=None, op0=ALU.mult), R=[sk], W=[sk])
            pt, ptk = C.ps.next()
            P.op("pe", lambda e, pt=pt, s=s: e.transpose(out=pt[0:8, 0:128], in_=s[:, 32:40], identity=C.ident_f[:]), R=[sk, "ident_f"], W=[ptk])
            P.op("act", lambda e, pt=pt, tt=tt: e.copy(out=gT[0:8, tt * 128:(tt + 1) * 128], in_=pt[0:8, 0:128]), R=[ptk], W=[("gT", tg)])
        for ex in range(n_exp):
            for tg in range(NTG):
                pb, pbk = C.ps.next()
                P.op("pe", lambda e, pb=pb, ex=ex, tg=tg: e.matmul(pb[:, :], lhsT=sel[0:8, ex, :], rhs=gT[0:8, tg * 512:(tg + 1) * 512],
                                                               start=True, stop=True), R=["sel", ("gT", tg)], W=[pbk])
                P.op("act", lambda e, pb=pb, tg=tg: e.copy(out=gbc[:, tg * 512:(tg + 1) * 512], in_=pb[:, :]), R=[pbk], W=[("gbc", tg)])
            emit_ffn(C, xnT, xk, hT, hk, TG, wg[ex], wu[ex], wd[ex], pools, gate_bc=(gbc, lambda tg: ("gbc", tg)))
        emit_store_fm_to_tm(C, hT, hk, out[t0:t0 + TG, :], TG // 128, stage)
    P.finish("sp")
    P.emit()
    return nc


def build_L1(T):
    nc = bass.Bass("TRN2", target_bir_lowering=False)
    dt = lambda n, s, k="ExternalInput": nc.dram_tensor(n, s, F32, kind=k).ap()
    x = dt("x", [T, 1024])
    n0 = dt("att_norm", [1024])
    w_in = dt("w_in", [1024, 1952])
    nq = dt("q_lat_norm", [256])
    w_qup = dt("w_q_up", [256, 768])
    nkv = dt("kv_lat_norm", [128])
    w_kvup = dt("w_kv_up", [128, 1024])
    gq = dt("q_norm", [96])
    gk = dt("k_norm", [96])
    cq_t = dt("cq_t", [96, T]); sq_t = dt("sq_t", [96, T])
    ck_t = dt("ck_t", [96, T]); sk_t = dt("sk_t", [96, T])
    pm = dt("pm", [96, 96])
    dtb = lambda n, s: nc.dram_tensor(n, s, BF16, kind="ExternalOutput").ap()
    sbqT = dtb("sbqT", [512, T])
    sbkT = dtb("sbkT", [512, T])
    sbv = dtb("sbv", [T, 512])
    mqT = dtb("mqT", [8, 96, T])
    mkT = dtb("mkT", [8, 96, T])
    mv = dtb("mv", [T, 512])
    C = Ctx(nc)
    P = C.P
    A = nc.alloc_sbuf_tensor
    hT = A("hT", [128, 8, 512], F32)
    xnT = A("xnT", [128, 8, 512], BF16)
    win = A("win", [128, 8, 1952], BF16)
    wkr = A("wkr", [128, 8, 96], BF16)
    wqup = A("wqup", [128, 2, 768], BF16)
    wkn = A("wkn", [128, 8, 96], BF16)
    wkv = A("wkv", [128, 8, 64], BF16)
    pmt = A("pmt", [96, 96], BF16)
    lat = A("lat", [128, 3, 512], F32)
    latn = A("latn", [128, 3, 512], BF16)
    krp = A("krp", [96, 512], F32)
    hrs = Rot(nc, "hrs", [96, 1, 512], F32, 8)
    hns = Rot(nc, "hns", [96, 1, 512], BF16, 8)
    sqh = Rot(nc, "sqh", [96, 512], BF16, 8)
    rth = Rot(nc, "rth", [96, 512], F32, 8)
    tabs = A("tabs", [96, 4, 512], F32)
    t1 = Rot(nc, "t1", [96, 512], F32, 8)
    t2 = Rot(nc, "t2", [96, 512], F32, 8)
    ob = Rot(nc, "ob", [128, 512], BF16, 3)
    t3 = Rot(nc, "t3", [96, 512], BF16, 8)
    stage = Rot(nc, "stage", [128, 1024], F32, 2)
    g0 = load_vec_fm(C, "g0", n0, 1024)
    gql = load_vec_fm(C, "gql", nq, 256)
    gkl = load_vec_fm(C, "gkl", nkv, 128)
    gqh = load_vec_fm(C, "gqh", gq, 96)
    gkh = load_vec_fm(C, "gkh", gk, 96)
    P.dma("pool", win[:], w_in.rearrange("(k p) n -> p k n", p=128), W=["win"])
    P.op("pool", lambda e: e.memset(wkr[:], 0.0), W=["wkr"])
    P.dma("pool", wkr[:, :, 64:96], w_in[:, 1920:1952].rearrange("(k p) n -> p k n", p=128), R=["wkr"], W=["wkr"])
    P.dma("pool", wqup[:], w_qup.rearrange("(k p) n -> p k n", p=128), W=["wqup"])
    P.op("pool", lambda e: e.memset(wkn[:], 0.0), W=["wkn"])
    P.dma("pool", wkn[:, :, 0:64], w_kvup.rearrange("k (h c) -> k h c", c=128)[:, :, 0:64], R=["wkn"], W=["wkn"])
    P.dma("pool", wkv[:], w_kvup.rearrange("k (h c) -> k h c", c=128)[:, :, 64:128], W=["wkv"])
    P.dma("pool", pmt[:], pm, W=["pmt"])
    hk = lambda k, tg: ("h", k)
    for tg in range(T // 512):
        t0 = tg * 512
        emit_load_tm_to_fm(C, x[t0:t0 + 512, :], hT, hk, 4, stage)
        emit_rmsnorm_fm(C, hT, [hk(k, 0) for k in range(8)], 8, 0, 512, g0, "g0", xnT, [("xn", k) for k in range(8)], 0, 1024)
        XR = [("xn", k) for k in range(8)]
        for i, tab in enumerate((cq_t, sq_t, ck_t, sk_t)):
            P.dma("sp", tabs[:, i, :], tab[:, t0:t0 + 512], W=[("tabs", i)])

        def proj_fm(col0, ncols, wt=win, wkey="win"):
            po, pok = C.ps.next()
            for k in range(8):
                P.op("pe", lambda e, k=k, po=po: e.matmul(po[0:ncols, :], lhsT=wt[:, k, col0:col0 + ncols], rhs=xnT[:, k, :],
                                                          start=(k == 0), stop=(k == 7)), R=[wkey] + XR, W=[pok])
            return po, pok
        for c in range(8):
            po, pok = proj_fm(c * 128, 128)
            o, okk = ob.next()
            P.op("act", lambda e, o=o, po=po, c=c: e.mul(out=o[:, :], in_=po[:, :], mul=(0.125 if c < 4 else 1.0)), R=[pok], W=[okk])
            dst = sbqT if c < 4 else sbkT
            P.dma("sp", dst[(c % 4) * 128:(c % 4 + 1) * 128, t0:t0 + 512], o[:, :], R=[okk], W=[("o1", c, tg)])
        for tt in range(4):
            po, pok = C.ps.next()
            for k in range(8):
                P.op("pe", lambda e, k=k, po=po, tt=tt: e.matmul(po[:, :], lhsT=xnT[:, k, tt * 128:(tt + 1) * 128], rhs=win[:, k, 1024:1536],
                                                             start=(k == 0), stop=(k == 7)), R=["win"] + XR, W=[pok])
            o, okk = ob.next()
            P.op("dve", lambda e, o=o, po=po: e.tensor_copy(out=o[:, :], in_=po[:, :]), R=[pok], W=[okk])
            P.dma("sp", sbv[t0 + tt * 128:t0 + (tt + 1) * 128, :], o[:, :], R=[okk], W=[("o2", tt, tg)])
        for c in range(3):
            po, pok = proj_fm(1536 + c * 128, 128)
            P.op("act", lambda e, po=po, c=c: e.copy(out=lat[:, c, :], in_=po[:, :]), R=[pok], W=[("lat", c)])
        emit_rmsnorm_fm(C, lat, [("lat", 0), ("lat", 1)], 2, 0, 512, gql, "gql", latn, [("latn", 0), ("latn", 1)], 0, 256)
        emit_rmsnorm_fm(C, lat[:, 2:3, :], [("lat", 2)], 1, 0, 512, gkl, "gkl", latn[:, 2:3, :], [("latn", 2)], 0, 128)
        po, pok = proj_fm(0, 96, wt=wkr, wkey="wkr")
        P.op("act", lambda e, po=po: e.copy(out=krp[:, :], in_=po[0:96, :]), R=[pok], W=["krp"])
        for tt in range(4):
            po, pok = C.ps.next()
            P.op("pe", lambda e, po=po, tt=tt: e.matmul(po[:, :], lhsT=latn[:, 2, tt * 128:(tt + 1) * 128], rhs=wkv[:, :, :],
                                                    start=True, stop=True), R=["wkv", ("latn", 2)], W=[pok])
            o, okk = ob.next()
            P.op("dve", lambda e, o=o, po=po: e.tensor_copy(out=o[:, :], in_=po[:, :]), R=[pok], W=[okk])
            P.dma("sp", mv[t0 + tt * 128:t0 + (tt + 1) * 128, :], o[:, :], R=[okk], W=[("o3", tt, tg)])
        def chain(h, which):
            hr_, hrk = hrs.next()
            hn_, hnk = hns.next()
            po, pok = C.ps.next()
            if which == 0:
                for k in range(2):
                    P.op("pe", lambda e, k=k: e.matmul(po[0:96, :], lhsT=wqup[:, k, h * 96:(h + 1) * 96], rhs=latn[:, k, :],
                                                       start=(k == 0), stop=(k == 1)), R=["wqup", ("latn", 0), ("latn", 1)], W=[pok])
                yield
                P.op("act", lambda e: e.copy(out=hr_[:, 0, :], in_=po[0:96, :]), R=[pok], W=[hrk])
            else:
                P.op("pe", lambda e: e.matmul(po[0:96, :], lhsT=wkn[:, h, :], rhs=latn[:, 2, :], start=True, stop=True),
                     R=["wkn", ("latn", 2)], W=[pok])
                yield
                P.op("dve", lambda e: e.tensor_tensor(out=hr_[:, 0, :], in0=po[0:96, :], in1=krp[:, :], op=ALU.add), R=[pok, "krp"], W=[hrk])
            yield
            gain, gkey = (gqh, "gqh") if which == 0 else (gkh, "gkh")
            sq_, sqk = sqh.next()
            P.op("act", lambda e: e.activation(out=sq_[:, :], in_=hr_[:, 0, :], func=AF.Square), R=[hrk], W=[sqk])
            yield
            ps, psk = C.ps.next()
            P.op("pe", lambda e: e.matmul(ps[0:96, :], lhsT=C.ones_bf[0:96, 0:96], rhs=sq_[:, :], start=True, stop=True), R=[sqk, "ones_bf"], W=[psk])
            yield
            rt_, rtk = rth.next()
            P.op("act", lambda e: e.activation(out=rt_[:, :], in_=ps[0:96, :], func=AF.Sqrt, bias=C.eps_t[0:96, 0:1], scale=1.0 / 96),
                 R=[psk, "eps_t"], W=[rtk])
            yield
            P.op("dve", lambda e: e.reciprocal(out=rt_[:, :], in_=rt_[:, :]), R=[rtk], W=[rtk])
            yield
            P.op("dve", lambda e: e.scalar_tensor_tensor(out=hn_[:, 0, :], in0=hr_[:, 0, :], scalar=gain[0:96, 0:1], in1=rt_[:, :],
                                                         op0=ALU.mult, op1=ALU.mult), R=[hrk, rtk, gkey], W=[hnk])
            yield
            pp, ppk = C.ps.next()
            P.op("pe", lambda e: e.matmul(pp[0:96, :], lhsT=pmt[:, :], rhs=hn_[:, 0, :], start=True, stop=True), R=["pmt", hnk], W=[ppk])
            a, ak = t1.next()
            ci, si = (0, 1) if which == 0 else (2, 3)
            P.op("pool", lambda e: e.tensor_tensor(out=a[:, :], in0=hn_[:, 0, :], in1=tabs[:, ci, :], op=ALU.mult), R=[hnk, ("tabs", ci)], W=[ak])
            yield
            b, bk = t2.next()
            P.op("dve", lambda e: e.tensor_tensor(out=b[:, :], in0=pp[0:96, :], in1=tabs[:, si, :], op=ALU.mult), R=[ppk, ("tabs", si)], W=[bk])
            yield
            a3, a3k = t3.next()
            P.op("pool", lambda e: e.tensor_tensor(out=a3[:, :], in0=a[:, :], in1=b[:, :], op=ALU.add), R=[ak, bk], W=[a3k])
            dst = mqT if which == 0 else mkT
            P.dma("sp", dst[h, :, t0:t0 + 512], a3[:, :], R=[a3k], W=[("o4", h, which, tg)])

        todo = [(h, which) for h in range(8) for which in range(2)]
        for g0_ in range(0, len(todo), 8):
            gens = [chain(h, which) for (h, which) in todo[g0_:g0_ + 8]]
            while gens:
                alive = []
                for g_ in gens:
                    try:
                        next(g_)
                        alive.append(g_)
                    except StopIteration:
                        pass
                gens = alive
    P.finish("sp")
    P.emit()
    return nc


def rope_tables(S):
    inv_freq = (10000.0 ** (-np.arange(0, 32, 2, dtype=np.float32) / np.float32(32))).astype(np.float32)
    ang = (np.arange(S, dtype=np.float32)[:, None] * inv_freq[None, :]).astype(np.float32)
    cos = np.cos(ang).astype(np.float32).T
    sin = np.sin(ang).astype(np.float32).T
    Ct = np.ones((96, S), np.float32); St = np.zeros((96, S), np.float32)
    Ct[64:80] = cos; Ct[80:96] = cos
    St[64:80] = sin; St[80:96] = sin
    pm = np.zeros((96, 96), np.float32)
    for i in range(16):
        pm[80 + i, 64 + i] = -1.0
        pm[64 + i, 80 + i] = 1.0
    return Ct, St, pm


def build_L2(S, n_sb=2, n_mla=2):
    nc = bass.Bass("TRN2", target_bir_lowering=False)
    dt = lambda n, s, k="ExternalInput": nc.dram_tensor(n, s, F32, kind=k).ap()
    dtb = lambda n, s: nc.dram_tensor(n, s, BF16, kind="ExternalInput").ap()
    sbqT = dtb("sbqT", [2, 64, S]); sbkT = dtb("sbkT", [2, 64, S]); sbv = dtb("sbv", [2, S, 64])
    mqT = dtb("mqT", [2, 96, S]); mkT = dtb("mkT", [2, 96, S]); mv = dtb("mv", [2, S, 64])
    oT = dt("oT", [4, 64, S], "ExternalOutput")
    C = Ctx(nc, n_ps=3)
    P = C.P
    A = nc.alloc_sbuf_tensor
    NB = S // 128
    NQG = S // 512
    argp = Rot(nc, "argp", [128, 512], F32, 3, psum=True)
    acc = Rot(nc, "acc", [128, 512], F32, 2, psum=True)
    qTs = [A("qT%d" % i, [128, S], BF16) for i in range(2)]
    kTs = [A("kT%d" % i, [128, S], BF16) for i in range(2)]
    vas = [A("va%d" % i, [128, NB, 128], BF16) for i in range(2)]
    mle = A("mle", [128, 4, 512], BF16)
    mlt = A("mlt", [128, 4, 512], BF16)
    for bi in range(2):
        for c4 in range(4):
            sl = slice(c4 * (S // 4), (c4 + 1) * (S // 4))
            P.op("pool", lambda e, sl=sl, bi=bi: e.memset(qTs[bi][:, sl], 0.0), W=[("qT", bi, c4)])
            P.op("pool", lambda e, sl=sl, bi=bi: e.memset(kTs[bi][:, sl], 0.0), W=[("kT", bi, c4)])
        P.op("pool", lambda e, bi=bi: e.memset(vas[bi][:], 0.0), W=[("va", bi)])
        P.op("pool", lambda e, bi=bi: e.memset(vas[bi][:, :, 64:65], 1.0), R=[("va", bi)], W=[("va", bi)])
    nuin = A("nuin", [128, 128], BF16)
    nones = A("nones", [128, 128], BF16)
    et = Rot(nc, "et", [128, 512], F32, 2)
    spt = Rot(nc, "spt", [128, 512], BF16, 3)
    wt = Rot(nc, "wt", [128, 512], BF16, 3)
    Rt = Rot(nc, "Rt", [128, 512], BF16, 3)
    ot = Rot(nc, "ot", [128, 512], F32, 2)
    rr = A("rr", [128, 512], F32)
    bcs = A("bcs", [64, 512], F32)
    P.op("pool", lambda e: e.memset(mle[:], 1.0), W=["mle"])
    P.op("pool", lambda e: e.memset(mlt[:], 1.0), W=["mlt"])
    P.op("pool", lambda e: e.memset(nuin[:], -1.0), W=["nuin"])
    P.op("pool", lambda e: e.memset(nones[:], -1.0), W=["nones"])
    for d in range(4):
        P.op("pool", lambda e, d=d: e.affine_select(out=mle[:, d, :], in_=mle[:, d, :], pattern=[[1, 512]], compare_op=ALU.is_ge, fill=0.0,
                                                    base=-128 * d, channel_multiplier=-1), R=["mle"], W=["mle"])
        P.op("pool", lambda e, d=d: e.affine_select(out=mlt[:, d, :], in_=mlt[:, d, :], pattern=[[1, 512]], compare_op=ALU.is_gt, fill=0.0,
                                                    base=-128 * d, channel_multiplier=-1), R=["mlt"], W=["mlt"])
    P.op("pool", lambda e: e.affine_select(out=nuin[:], in_=nuin[:], pattern=[[-1, 128]], compare_op=ALU.is_ge, fill=0.0,
                                           base=0, channel_multiplier=1), R=["nuin"], W=["nuin"])
    CW = S // 4
    NH = n_sb + n_mla

    def head_cfg(hd):
        is_sb = hd < n_sb
        hh = hd if is_sb else hd - n_sb
        return is_sb, hh, (64 if is_sb else 96), ((sbqT, sbkT, sbv) if is_sb else (mqT, mkT, mv))

    def emit_loads(hd):
        is_sb, hh, dq, (qsrc, ksrc, vsrc) = head_cfg(hd)
        bi = hd % 2
        for c4 in range(4):
            sl = slice(c4 * CW, (c4 + 1) * CW)
            P.dma("sp", qTs[bi][0:dq, sl], qsrc[hh, :, sl], W=[("qT", bi, c4)])
            P.dma("sp", kTs[bi][0:dq, sl], ksrc[hh, :, sl], W=[("kT", bi, c4)])
        P.dma("sp", vas[bi][:, :, 0:64], vsrc[hh].rearrange("(kb p) c -> p kb c", p=128), R=[("va", bi)], W=[("va", bi)])

    emit_loads(0)
    for hd in range(NH):
        is_sb, hh, dq, _ = head_cfg(hd)
        bi = hd % 2
        qT, kT, va = qTs[bi], kTs[bi], vas[bi]
        vak = ("va", bi)
        qkeys = lambda qg, bi=bi: [("qT", bi, c) for c in range((qg * 512) // CW, (qg * 512 + 511) // CW + 1)]
        kkeys = lambda kb, bi=bi: [("kT", bi, c) for c in range((kb * 128) // CW, (kb * 128 + 127) // CW + 1)]
        if hd + 1 < NH:
            emit_loads(hd + 1)
        blocks = []
        for qg in range(NQG):
            nkb = 4 * (qg + 1)
            order = range(nkb - 1, -1, -1) if is_sb else range(nkb)
            for i, kb in enumerate(order):
                blocks.append((qg, i, kb, nkb))
        nblk = len(blocks)
        st = {}
        qstate = {}

        def stage_z(t):
            qg, i, kb, nkb = blocks[t]
            kTl, qTl, val, dql, sbl, hdl = kT, qT, va, dq, is_sb, hd
            zp, zpk = C.ps.next()
            kq = 128
            P.op("pe", lambda e: e.matmul(zp[:, :], lhsT=kTl[0:kq, kb * 128:(kb + 1) * 128], rhs=qTl[0:kq, qg * 512:(qg + 1) * 512],
                                          start=True, stop=True), R=qkeys(qg) + kkeys(kb), W=[zpk])
            st[t] = dict(zp=zp, zpk=zpk)

        def stage_a_sb(t):
            qg, i, kb, nkb = blocks[t]
            kTl, qTl, val, dql, sbl, hdl = kT, qT, va, dq, is_sb, hd
            d = kb - 4 * qg
            s_ = st[t]
            zp, zpk = s_["zp"], s_["zpk"]
            e_, ek = et.next()
            P.op("act", lambda e: e.activation(out=e_[:, :], in_=zp[:, :], func=AF.Exp), R=[zpk], W=[ek])
            s_["e"] = (e_, ek)

        def stage_a2_sb(t):
            qg, i, kb, nkb = blocks[t]
            kTl, qTl, val, dql, sbl, hdl = kT, qT, va, dq, is_sb, hd
            d = kb - 4 * qg
            s_ = st[t]
            e_, ek = s_["e"]
            sp, spk = spt.next()
            P.op("act", lambda e: e.activation(out=sp[:, :], in_=e_[:, :], func=AF.Ln, bias=C.ones_f[:, 0:1], scale=1.0),
                 R=[ek, "ones_f"], W=[spk])
            if d >= 0:
                P.op("pool", lambda e: e.tensor_tensor(out=sp[:, :], in0=sp[:, :], in1=mlt[:, d, :], op=ALU.mult), R=[spk, "mlt"], W=[spk])
            ap_, apk = argp.next()
            P.op("pe", lambda e: e.matmul(ap_[:, :], lhsT=kTl[:, kb * 128:(kb + 1) * 128], rhs=qTl[:, qg * 512:(qg + 1) * 512],
                                          start=True, stop=False), R=qkeys(qg) + kkeys(kb), W=[apk])
            P.op("pe", lambda e: e.matmul(ap_[:, :], lhsT=nuin[:, :], rhs=sp[:, :], start=False, stop=(i == 0)), R=[spk, "nuin"], W=[apk])
            if i > 0:
                Rp, Rpk = qstate[qg]["R"]
                P.op("pe", lambda e: e.matmul(ap_[:, :], lhsT=nones[:, :], rhs=Rp[:, :], start=False, stop=True), R=[Rpk, "nones"], W=[apk])
            if kb > 0:
                Rn, Rnk = Rt.next()
                if i == 0:
                    P.op("pool", lambda e: e.tensor_copy(out=Rn[:, :], in_=sp[:, :]), R=[spk], W=[Rnk])
                else:
                    Rp, Rpk = qstate[qg]["R"]
                    P.op("pool", lambda e: e.tensor_tensor(out=Rn[:, :], in0=Rp[:, :], in1=sp[:, :], op=ALU.add), R=[spk, Rpk], W=[Rnk])
                qstate.setdefault(qg, {})["R"] = (Rn, Rnk)
            s_["arg"] = (ap_, apk)

        def stage_b(t):
            qg, i, kb, nkb = blocks[t]
            kTl, qTl, val, dql, sbl, hdl = kT, qT, va, dq, is_sb, hd
            d = kb - 4 * qg
            s_ = st[t]
            if i == 0:
                qstate.setdefault(qg, {})["acc"] = acc.next()
            op_, opk = qstate[qg]["acc"]
            src, srck = s_["arg"] if is_sb else (s_["zp"], s_["zpk"])
            w_, wk_ = wt.next()
            P.op("act", lambda e: e.activation(out=w_[:, :], in_=src[:, :], func=AF.Exp), R=[srck], W=[wk_])
            if d >= 0:
                mk_ = mlt if is_sb else mle
                P.op("dve", lambda e: e.tensor_tensor(out=w_[:, :], in0=w_[:, :], in1=mk_[:, d, :], op=ALU.mult),
                     R=[wk_, "mlt" if is_sb else "mle"], W=[wk_])
            last = (i == nkb - 1)
            nv = 64 if is_sb else 65
            P.op("pe", lambda e: e.matmul(op_[:, :], lhsT=val[:, kb, :], rhs=w_[:, :], start=(i == 0), stop=last), R=[wk_, vak], W=[opk])
            if last:
                o_, ok_ = ot.next()
                if is_sb:
                    P.op("dve", lambda e: e.tensor_copy(out=o_[0:64, :], in_=op_[0:64, :]), R=[opk], W=[ok_])
                else:
                    P.op("dve", lambda e: e.reciprocal(out=rr[64:65, :], in_=op_[64:65, :]), R=[opk], W=["rr"])
                    bc, bck = argp.next()
                    P.op("pe", lambda e: e.matmul(bc[0:64, :], lhsT=C.ones_f[64:65, 0:64], rhs=rr[64:65, :], start=True, stop=True),
                         R=["rr", "ones_f"], W=[bck])
                    P.op("dve", lambda e: e.tensor_copy(out=bcs[:, :], in_=bc[0:64, :]), R=[bck], W=["bcs"])
                    P.op("dve", lambda e: e.tensor_tensor(out=o_[0:64, :], in0=op_[0:64, :], in1=bcs[:, :], op=ALU.mult), R=[opk, "bcs"], W=[ok_])
                P.dma("sp", oT[hdl, :, qg * 512:(qg + 1) * 512], o_[0:64, :], R=[ok_], W=[("oo", hd, qg)])
            del st[t]

        for t in range(-2, nblk):
            if 0 <= t + 2 < nblk:
                stage_z(t + 2)
            if is_sb:
                if 0 <= t + 1 < nblk:
                    stage_a_sb(t + 1)
                if 0 <= t:
                    stage_b(t)
                if 0 <= t + 1 < nblk:
                    stage_a2_sb(t + 1)
            else:
                if 0 <= t:
                    stage_b(t)
    P.finish("sp")
    P.emit()
    return nc


TWO_PI = 6.283185307179586
PI = 3.141592653589793
LCH = 512


def build_L4(S):
    nc = bass.Bass("TRN2", target_bir_lowering=False)
    dt = lambda n, s, k="ExternalInput": nc.dram_tensor(n, s, F32, kind=k).ap()
    uT = dt("uT", [128, S])
    a_re = dt("a_re", [128, 4]); a_im = dt("a_im", [128, 4]); ldt = dt("ldt", [128, 4])
    b_re = dt("b_re", [4, 128, 16]); b_im = dt("b_im", [4, 128, 16])
    ct_re = dt("ct_re", [4, 128, 16]); ct_im = dt("ct_im", [4, 128, 16])
    dsk = dt("dsk", [128])
    yT = dt("yT", [128, S], "ExternalOutput")
    C = Ctx(nc)
    P = C.P
    A = nc.alloc_sbuf_tensor
    NCH = S // LCH
    ub = A("ub", [128, S], BF16)
    P.dma("pool", ub[:], uT, W=["ub"])
    par = A("par", [128, 16, 4], F32)
    AR, AI, DT, ARD, TH, LRE, LIM, NUM, DEN, CRE, CIM, TMP, TMP2, MRE, MIM, NMIM = range(16)
    pk = lambda i: ("par", i)
    P.dma("sp", par[:, AR, :], a_re, W=[pk(AR)])
    P.dma("sp", par[:, AI, :], a_im, W=[pk(AI)])
    P.dma("sp", par[:, DT, :], ldt, W=[pk(DT)])
    dvec = load_vec_fm(C, "dvec", dsk, 128)
    cpi = A("cpi", [128, 1], F32)
    P.op("pool", lambda e: e.memset(cpi[:], PI), W=["cpi"])
    bst = A("bst", [128, 4, 4, 16], F32)
    for i, src in enumerate((b_re, b_im, ct_re, ct_im)):
        P.dma("sp", bst[:, i, :, :], src.rearrange("j p c -> p j c"), W=[("bst", i)], allow_slow_non_contiguous=True)
    io_i = A("io_i", [128, LCH], I32)
    io_f = A("io_f", [128, LCH], F32)
    P.op("pool", lambda e: e.iota(io_i[:], pattern=[[1, LCH]], base=0, channel_multiplier=0), W=["io_i"])
    P.op("dve", lambda e: e.tensor_copy(out=io_f[:], in_=io_i[:]), R=["io_i"], W=["io_f"])
    onesL = A("onesL", [128, LCH], F32)
    P.op("pool", lambda e: e.memset(onesL[:], 1.0), W=["onesL"])

    def ts(out, in0, s1, s2, o0, o1=None, R=(), W=()):
        if o1 is None:
            P.op("dve", lambda e: e.tensor_scalar(out=out, in0=in0, scalar1=s1, scalar2=None, op0=o0), R=R, W=W)
        else:
            P.op("dve", lambda e: e.tensor_scalar(out=out, in0=in0, scalar1=s1, scalar2=s2, op0=o0, op1=o1), R=R, W=W)

    def tt(out, in0, in1, o, R=(), W=(), eng="dve"):
        P.op(eng, lambda e: e.tensor_tensor(out=out, in0=in0, in1=in1, op=o), R=R, W=W)

    pv = lambda i: par[:, i, :]
    P.op("act", lambda e: e.activation(out=pv(DT), in_=pv(DT), func=AF.Exp), R=[pk(DT)], W=[pk(DT)])
    ts(pv(AR), pv(AR), -1e-4, None, ALU.min, R=[pk(AR)], W=[pk(AR)])
    tt(pv(ARD), pv(AR), pv(DT), ALU.mult, R=[pk(AR), pk(DT)], W=[pk(ARD)])
    tt(pv(TH), pv(AI), pv(DT), ALU.mult, R=[pk(AI), pk(DT)], W=[pk(TH)])
    tab = A("tab", [128, 4, 4, LCH], F32)
    scr = Rot(nc, "scr", [128, LCH], F32, 8)
    scri = Rot(nc, "scri", [128, LCH], I32, 2)
    nard = A("nard", [128, 4], F32)
    ts(nard[:, :], pv(ARD), -1.0, None, ALU.mult, R=[pk(ARD)], W=["nard"])
    def sin_of(ang, angk):
        t, tk = scr.next()
        ki, kik = scri.next()
        ts(t[:, :], ang[:, :], 1.0 / TWO_PI, None, ALU.mult, R=[angk], W=[tk])
        P.op("dve", lambda e: e.tensor_copy(out=ki[:, :], in_=t[:, :]), R=[tk], W=[kik])
        P.op("dve", lambda e: e.tensor_copy(out=t[:, :], in_=ki[:, :]), R=[kik], W=[tk])
        P.op("dve", lambda e: e.scalar_tensor_tensor(out=ang[:, :], in0=t[:, :], scalar=-TWO_PI, in1=ang[:, :], op0=ALU.mult, op1=ALU.add),
             R=[tk, angk], W=[angk])
        ts(t[:, :], ang[:, :], PI, -TWO_PI, ALU.is_gt, ALU.mult, R=[angk], W=[tk])
        tt(ang[:, :], ang[:, :], t[:, :], ALU.add, R=[angk, tk], W=[angk])
        ts(t[:, :], ang[:, :], -PI, TWO_PI, ALU.is_lt, ALU.mult, R=[angk], W=[tk])
        tt(ang[:, :], ang[:, :], t[:, :], ALU.add, R=[angk, tk], W=[angk])
        ts(ang[:, :], ang[:, :], PI, -PI, ALU.min, ALU.max, R=[angk], W=[angk])
        P.op("act", lambda e: e.activation(out=t[:, :], in_=ang[:, :], func=AF.Sin), R=[angk], W=[tk])
        return t, tk

    for j in range(4):
        ang, angk = scr.next()
        ts(ang[:, :], io_f[:, :], par[:, TH, j:j + 1], None, ALU.mult, R=["io_f", pk(TH)], W=[angk])
        sn, snk = sin_of(ang, angk)
        ang2, ang2k = scr.next()
        ts(ang2[:, :], io_f[:, :], par[:, TH, j:j + 1], PI / 2, ALU.mult, ALU.add, R=["io_f", pk(TH)], W=[ang2k])
        cs, csk = sin_of(ang2, ang2k)
        mg, mgk = scr.next()
        P.op("act", lambda e, mg=mg, j=j: e.activation(out=mg[:, :], in_=io_f[:, :], func=AF.Exp, scale=par[:, ARD, j:j + 1]),
             R=["io_f", pk(ARD)], W=[mgk])
        tt(tab[:, 2, j, :], mg[:, :], cs[:, :], ALU.mult, R=[mgk, csk], W=[("tab", 2, j)])
        tt(tab[:, 3, j, :], mg[:, :], sn[:, :], ALU.mult, R=[mgk, snk], W=[("tab", 3, j)])
        mg2, mg2k = scr.next()
        P.op("act", lambda e, mg2=mg2, j=j: e.activation(out=mg2[:, :], in_=io_f[:, :], func=AF.Exp, scale=nard[:, j:j + 1]),
             R=["io_f", "nard"], W=[mg2k])
        tt(tab[:, 0, j, :], mg2[:, :], cs[:, :], ALU.mult, R=[mg2k, csk], W=[("tab", 0, j)])
        P.op("dve", lambda e, mg2=mg2, sn=sn, j=j: e.scalar_tensor_tensor(out=tab[:, 1, j, :], in0=mg2[:, :], scalar=-1.0, in1=sn[:, :],
                                                                          op0=ALU.mult, op1=ALU.mult), R=[mg2k, snk], W=[("tab", 1, j)])
    for j in range(4):
        P.op("dve", lambda e, j=j: e.tensor_copy(out=par[:, LRE, j:j + 1], in_=tab[:, 2, j, 1:2]), R=[("tab", 2, j)], W=[pk(LRE)])
        P.op("dve", lambda e, j=j: e.tensor_copy(out=par[:, LIM, j:j + 1], in_=tab[:, 3, j, 1:2]), R=[("tab", 3, j)], W=[pk(LIM)])
    for j in range(4):
        l5r = tab[:, 2, j, LCH - 1:LCH]; l5i = tab[:, 3, j, LCH - 1:LCH]
        RK = [pk(LRE), pk(LIM), ("tab", 2, j), ("tab", 3, j)]
        tt(par[:, TMP, j:j + 1], par[:, LRE, j:j + 1], l5r, ALU.mult, R=RK, W=[pk(TMP)])
        tt(par[:, TMP2, j:j + 1], par[:, LIM, j:j + 1], l5i, ALU.mult, R=RK, W=[pk(TMP2)])
        tt(par[:, MRE, j:j + 1], par[:, TMP, j:j + 1], par[:, TMP2, j:j + 1], ALU.subtract, R=[pk(TMP), pk(TMP2)], W=[pk(MRE)])
        tt(par[:, TMP, j:j + 1], par[:, LRE, j:j + 1], l5i, ALU.mult, R=RK + [pk(MRE)], W=[pk(TMP)])
        tt(par[:, TMP2, j:j + 1], par[:, LIM, j:j + 1], l5r, ALU.mult, R=RK + [pk(MRE)], W=[pk(TMP2)])
        tt(par[:, MIM, j:j + 1], par[:, TMP, j:j + 1], par[:, TMP2, j:j + 1], ALU.add, R=[pk(TMP), pk(TMP2)], W=[pk(MIM)])
    ts(pv(NMIM), pv(MIM), -1.0, None, ALU.mult, R=[pk(MIM)], W=[pk(NMIM)])
    ts(pv(NUM), pv(LRE), -1.0, None, ALU.add, R=[pk(LRE)], W=[pk(NUM)])
    tt(pv(DEN), pv(AR), pv(AR), ALU.mult, R=[pk(AR)], W=[pk(DEN)])
    tt(pv(TMP), pv(AI), pv(AI), ALU.mult, R=[pk(AI), pk(MIM), pk(MRE)], W=[pk(TMP)])
    tt(pv(DEN), pv(DEN), pv(TMP), ALU.add, R=[pk(DEN), pk(TMP)], W=[pk(DEN)])
    P.op("dve", lambda e: e.reciprocal(out=pv(DEN), in_=pv(DEN)), R=[pk(DEN)], W=[pk(DEN)])
    tt(pv(TMP), pv(NUM), pv(AR), ALU.mult, R=[pk(NUM), pk(AR), pk(DEN)], W=[pk(TMP)])
    tt(pv(TMP2), pv(LIM), pv(AI), ALU.mult, R=[pk(LIM), pk(AI), pk(NMIM)], W=[pk(TMP2)])
    tt(pv(CRE), pv(TMP), pv(TMP2), ALU.add, R=[pk(TMP), pk(TMP2)], W=[pk(CRE)])
    tt(pv(CRE), pv(CRE), pv(DEN), ALU.mult, R=[pk(CRE), pk(DEN)], W=[pk(CRE)])
    tt(pv(TMP), pv(LIM), pv(AR), ALU.mult, R=[pk(LIM), pk(AR), pk(CRE)], W=[pk(TMP)])
    tt(pv(TMP2), pv(NUM), pv(AI), ALU.mult, R=[pk(NUM), pk(AI), pk(CRE)], W=[pk(TMP2)])
    tt(pv(CIM), pv(TMP), pv(TMP2), ALU.subtract, R=[pk(TMP), pk(TMP2)], W=[pk(CIM)])
    tt(pv(CIM), pv(CIM), pv(DEN), ALU.mult, R=[pk(CIM), pk(DEN)], W=[pk(CIM)])
    bfull = A("bfull", [128, 2, 4, 128], F32)
    P.op("pool", lambda e: e.memset(bfull[:], 0.0), W=["bfull"])
    BT = A("BT", [128, 2, 4, 128], BF16)
    CTt = A("CTt", [128, 2, 4, 128], BF16)
    P.op("pool", lambda e: e.memset(CTt[:], 0.0), W=["CTt"])
    t16 = Rot(nc, "t16", [128, 16], F32, 4)
    for j in range(4):
        for g in range(2):
            ps_ = slice(g * 64, (g + 1) * 64)
            c0 = 32 * j + 16 * g
            for which in range(2):
                ta, tak = t16.next()
                tb, tbk = t16.next()
                s_a = bst[ps_, 0 if which == 0 else 1, j, :]
                s_b = bst[ps_, 1 if which == 0 else 0, j, :]
                ts(ta[ps_, :], s_a, par[ps_, CRE, j:j + 1], None, ALU.mult, R=[("bst", 0), ("bst", 1), pk(CRE)], W=[tak])
                ts(tb[ps_, :], s_b, par[ps_, CIM, j:j + 1], None, ALU.mult, R=[("bst", 0), ("bst", 1), pk(CIM)], W=[tbk])
                tt(bfull[ps_, which, j, c0:c0 + 16], ta[ps_, :], tb[ps_, :], ALU.subtract if which == 0 else ALU.add,
                   R=[tak, tbk, "bfull"], W=["bfull"])
            P.op("dve", lambda e, ps_=ps_, j=j, c0=c0: e.tensor_copy(out=CTt[ps_, 0, j, c0:c0 + 16], in_=bst[ps_, 2, j, :]),
                 R=[("bst", 2), "CTt"], W=["CTt"])
            ts(CTt[ps_, 1, j, c0:c0 + 16], bst[ps_, 3, j, :], -1.0, None, ALU.mult, R=[("bst", 3), "CTt"], W=["CTt"])
    for j in range(4):
        for which in range(2):
            pt, ptk = C.ps.next()
            P.op("pe", lambda e, pt=pt, which=which, j=j: e.transpose(out=pt[:, 0:128], in_=bfull[:, which, j, :], identity=C.ident_f[:]),
                 R=["bfull", "ident_f"], W=[ptk])
            P.op("act", lambda e, pt=pt, which=which, j=j: e.copy(out=BT[:, which, j, :], in_=pt[:, 0:128]), R=[ptk], W=[("BT", which, j)])
    G = A("G", [128, NCH + 1, 4, 2], F32)
    P.op("pool", lambda e: e.memset(G[:], 0.0), W=["G"])
    pa = Rot(nc, "pa", [128, LCH], F32, 8)
    pb = Rot(nc, "pb", [128, LCH], F32, 8)
    Pre = Rot(nc, "Pre", [128, LCH], F32, 3)
    Pim = Rot(nc, "Pim", [128, LCH], F32, 3)
    Sre = Rot(nc, "Sre", [128, LCH], F32, 3)
    Sim = Rot(nc, "Sim", [128, LCH], F32, 3)
    hre = Rot(nc, "hre", [128, LCH], BF16, 8)
    him = Rot(nc, "him", [128, LCH], BF16, 8)
    yv = Rot(nc, "yv", [128, LCH], F32, 2)
    gt = Rot(nc, "gt", [128, LCH], F32, 2)
    sml = Rot(nc, "sml", [128, 2], F32, 4)
    items = [(ch, j) for ch in range(NCH) for j in range(4)]
    stt = {}
    hs_by_ch = {}

    def stX(t):
        ch, j = items[t]
        c0 = ch * LCH
        bre, brek = C.ps.next()
        P.op("pe", lambda e: e.matmul(bre[:, :], lhsT=BT[:, 0, j, :], rhs=ub[:, c0:c0 + LCH], start=True, stop=True),
             R=[("BT", 0, j), "ub"], W=[brek])
        bim, bimk = C.ps.next()
        P.op("pe", lambda e: e.matmul(bim[:, :], lhsT=BT[:, 1, j, :], rhs=ub[:, c0:c0 + LCH], start=True, stop=True),
             R=[("BT", 1, j), "ub"], W=[bimk])
        a1, a1k = pa.next(); a2, a2k = pa.next(); a3, a3k = pa.next(); a4, a4k = pa.next()
        tt(a1[:, :], bre[:, :], tab[:, 0, j, :], ALU.mult, R=[brek, ("tab", 0, j)], W=[a1k])
        tt(a2[:, :], bim[:, :], tab[:, 1, j, :], ALU.mult, R=[bimk, ("tab", 1, j)], W=[a2k])
        tt(a3[:, :], bim[:, :], tab[:, 0, j, :], ALU.mult, R=[bimk, ("tab", 0, j)], W=[a3k])
        tt(a4[:, :], bre[:, :], tab[:, 1, j, :], ALU.mult, R=[brek, ("tab", 1, j)], W=[a4k])
        pr, prk = Pre.next(); pi_, pik = Pim.next()
        tt(pr[:, :], a1[:, :], a2[:, :], ALU.subtract, R=[a1k, a2k], W=[prk], eng="pool")
        tt(pi_[:, :], a3[:, :], a4[:, :], ALU.add, R=[a3k, a4k], W=[pik], eng="pool")
        stt[t] = dict(pr=(pr, prk), pi=(pi_, pik))

    def stY(t):
        ch, j = items[t]
        pr, prk = stt[t]["pr"]; pi_, pik = stt[t]["pi"]
        sr, srk = Sre.next(); si, sik = Sim.next()
        P.op("dve", lambda e: e.tensor_tensor_scan(out=sr[:, :], data0=onesL[:, :], data1=pr[:, :], initial=G[:, ch, j, 0:1],
                                                   op0=ALU.mult, op1=ALU.add), R=[prk, "onesL", ("G", ch, j), "G"], W=[srk])
        P.op("dve", lambda e: e.tensor_tensor_scan(out=si[:, :], data0=onesL[:, :], data1=pi_[:, :], initial=G[:, ch, j, 1:2],
                                                   op0=ALU.mult, op1=ALU.add), R=[pik, "onesL", ("G", ch, j), "G"], W=[sik])
        sm_, smk = sml.next()
        ts(sm_[:, 0:1], sr[:, LCH - 1:LCH], par[:, MRE, j:j + 1], None, ALU.mult, R=[srk, pk(MRE)], W=[smk])
        ts(sm_[:, 1:2], si[:, LCH - 1:LCH], par[:, MRE, j:j + 1], None, ALU.mult, R=[sik, pk(MRE)], W=[smk])
        P.op("dve", lambda e: e.scalar_tensor_tensor(out=G[:, ch + 1, j, 0:1], in0=si[:, LCH - 1:LCH], scalar=par[:, NMIM, j:j + 1],
                                                     in1=sm_[:, 0:1], op0=ALU.mult, op1=ALU.add),
             R=[smk, sik, pk(NMIM), "G"], W=[("G", ch + 1, j, 0)])
        P.op("dve", lambda e: e.scalar_tensor_tensor(out=G[:, ch + 1, j, 1:2], in0=sr[:, LCH - 1:LCH], scalar=par[:, MIM, j:j + 1],
                                                     in1=sm_[:, 1:2], op0=ALU.mult, op1=ALU.add),
             R=[smk, srk, pk(MIM), "G", ("G", ch + 1, j, 0)], W=[("G", ch + 1, j)])
        b1, b1k = pb.next(); b2, b2k = pb.next(); b3, b3k = pb.next(); b4, b4k = pb.next()
        tt(b1[:, :], sr[:, :], tab[:, 2, j, :], ALU.mult, R=[srk, ("tab", 2, j)], W=[b1k], eng="pool")
        tt(b2[:, :], si[:, :], tab[:, 3, j, :], ALU.mult, R=[sik, ("tab", 3, j)], W=[b2k], eng="pool")
        tt(b3[:, :], si[:, :], tab[:, 2, j, :], ALU.mult, R=[sik, ("tab", 2, j)], W=[b3k], eng="pool")
        tt(b4[:, :], sr[:, :], tab[:, 3, j, :], ALU.mult, R=[srk, ("tab", 3, j)], W=[b4k], eng="pool")
        stt[t]["b"] = (b1, b1k, b2, b2k, b3, b3k, b4, b4k)

    def stW(t):
        ch, j = items[t]
        c0 = ch * LCH
        b1, b1k, b2, b2k, b3, b3k, b4, b4k = stt[t]["b"]
        hr_, hrk = hre.next(); hi_, hik = him.next()
        tt(hr_[:, :], b1[:, :], b2[:, :], ALU.subtract, R=[b1k, b2k], W=[hrk])
        tt(hi_[:, :], b3[:, :], b4[:, :], ALU.add, R=[b3k, b4k], W=[hik])
        hs_by_ch.setdefault(ch, []).append((hr_, hrk, hi_, hik))
        del stt[t]
        if j == 3:
            hs = hs_by_ch.pop(ch)
            yp, ypk = C.ps.next()
            for jj in range(4):
                h_r, h_rk, h_i, h_ik = hs[jj]
                P.op("pe", lambda e, h_r=h_r, jj=jj: e.matmul(yp[:, :], lhsT=CTt[:, 0, jj, :], rhs=h_r[:, :], start=(jj == 0), stop=False),
                     R=["CTt", h_rk], W=[ypk])
                P.op("pe", lambda e, h_i=h_i, jj=jj: e.matmul(yp[:, :], lhsT=CTt[:, 1, jj, :], rhs=h_i[:, :], start=False, stop=(jj == 3)),
                     R=["CTt", h_ik], W=[ypk])
            y_, yk_ = yv.next()
            P.op("dve", lambda e: e.scalar_tensor_tensor(out=y_[:, :], in0=ub[:, c0:c0 + LCH], scalar=dvec[:, 0:1], in1=yp[:, :],
                                                         op0=ALU.mult, op1=ALU.add), R=[ypk, "ub", "dvec"], W=[yk_])
            g_, gk_ = gt.next()
            tt(g_[:, :], y_[:, :], y_[:, :], ALU.mult, R=[yk_], W=[gk_], eng="pool")
            P.op("pool", lambda e: e.tensor_scalar(out=g_[:, :], in0=g_[:, :], scalar1=0.044715, scalar2=1.0, op0=ALU.mult, op1=ALU.add),
                 R=[gk_], W=[gk_])
            tt(g_[:, :], g_[:, :], y_[:, :], ALU.mult, R=[gk_, yk_], W=[gk_], eng="pool")
            P.op("act", lambda e: e.activation(out=g_[:, :], in_=g_[:, :], func=AF.Sigmoid, scale=1.5957691216057308), R=[gk_], W=[gk_])
            tt(y_[:, :], y_[:, :], g_[:, :], ALU.mult, R=[gk_, yk_], W=[yk_], eng="pool")
            P.dma("sp", yT[:, c0:c0 + LCH], y_[:, :], R=[yk_], W=[("yo", ch)])

    nit = len(items)
    for t in range(-2, nit):
        if 0 <= t + 2 < nit:
            stX(t + 2)
        if 0 <= t + 1 < nit:
            stY(t + 1)
        if 0 <= t:
            stW(t)
    P.finish("sp")
    P.emit()
    return nc


def s5_core_inputs(ins, b, gq, uT_b):
    gs = slice(8 * gq, 8 * gq + 8)
    def st(a):
        return np.ascontiguousarray(a[gs].reshape(4, 128).T)
    d = dict(uT=np.ascontiguousarray(uT_b[128 * gq:128 * gq + 128]),
             a_re=st(ins["ssm_a_re"][0]), a_im=st(ins["ssm_a_im"][0]),
             ldt=np.ascontiguousarray(np.repeat(ins["ssm_log_dt"][0][gs].reshape(4, 2, 1), 64, axis=2).reshape(4, 128).T),
             b_re=np.ascontiguousarray(ins["ssm_b_re"][0][gs].reshape(4, 128, 16)),
             b_im=np.ascontiguousarray(ins["ssm_b_im"][0][gs].reshape(4, 128, 16)),
             ct_re=np.ascontiguousarray(ins["ssm_c_re"][0][gs].transpose(0, 2, 1).reshape(4, 128, 16)),
             ct_im=np.ascontiguousarray(ins["ssm_c_im"][0][gs].transpose(0, 2, 1).reshape(4, 128, 16)),
             dsk=np.ascontiguousarray(ins["ssm_d"][0][128 * gq:128 * gq + 128]))
    return d


SEQ = 8192
BATCH = 2
TPC = BATCH * SEQ // NCORES
CPB = NCORES // BATCH


def _run(nc, in_maps):
    res = run_bass_kernel_spmd(nc, in_maps, core_ids=list(range(NCORES)))
    return res.results


def kernel(**ins):
    ins = {k: np.ascontiguousarray(np.asarray(v, dtype=np.float32)) for k, v in ins.items()}
    x = ins["x"].reshape(BATCH * SEQ, D_MODEL)
    ca = np.ascontiguousarray
    Ct, St, pm = rope_tables(SEQ)
    sc = np.float32(96 ** -0.5)
    Cq, Sq = ca(Ct * sc), ca(St * sc)
    nc1 = build_L1(TPC)
    maps = []
    for c in range(NCORES):
        p0 = (c % CPB) * TPC
        sl = slice(p0, p0 + TPC)
        maps.append(dict(x=x[c * TPC:(c + 1) * TPC], att_norm=ins["att_norm"][0], w_in=ins["att_w_in"][0],
                         q_lat_norm=ins["att_q_latent_norm"][0], w_q_up=ins["att_w_q_up"][0],
                         kv_lat_norm=ins["att_kv_latent_norm"][0], w_kv_up=ins["att_w_kv_up"][0],
                         q_norm=ins["att_q_norm"][0], k_norm=ins["att_k_norm"][0],
                         cq_t=ca(Cq[:, sl]), sq_t=ca(Sq[:, sl]), ck_t=ca(Ct[:, sl]), sk_t=ca(St[:, sl]), pm=pm))
    r1 = _run(nc1, maps)
    del nc1
    cat = lambda name, b, axis: np.concatenate([r1[b * CPB + i][name] for i in range(CPB)], axis=axis)
    nc2 = build_L2(SEQ)
    maps = []
    for b in range(BATCH):
        sbqT = cat("sbqT", b, 1); sbkT = cat("sbkT", b, 1); sbv = cat("sbv", b, 0)
        mqT = cat("mqT", b, 2); mkT = cat("mkT", b, 2); mv = cat("mv", b, 0)
        for g in range(CPB):
            maps.append(dict(sbqT=ca(sbqT[128 * g:128 * g + 128].reshape(2, 64, SEQ)),
                             sbkT=ca(sbkT[128 * g:128 * g + 128].reshape(2, 64, SEQ)),
                             sbv=ca(sbv[:, 128 * g:128 * g + 128].reshape(SEQ, 2, 64).transpose(1, 0, 2)),
                             mqT=ca(mqT[2 * g:2 * g + 2]), mkT=ca(mkT[2 * g:2 * g + 2]),
                             mv=ca(mv[:, 128 * g:128 * g + 128].reshape(SEQ, 2, 64).transpose(1, 0, 2))))
    r2 = _run(nc2, maps)
    del nc2, r1
    mT = []
    for b in range(BATCH):
        m = np.empty((1024, SEQ), np.float32)
        for g in range(CPB):
            o = r2[b * CPB + g]["oT"]
            m[128 * g:128 * g + 128] = o[0:2].reshape(128, SEQ)
            m[512 + 128 * g:512 + 128 * g + 128] = o[2:4].reshape(128, SEQ)
        mT.append(m)
    nc3 = build_L3(TPC)
    maps = []
    for c in range(NCORES):
        p0 = (c % CPB) * TPC
        maps.append(dict(x=x[c * TPC:(c + 1) * TPC], mT=ca(mT[c // CPB][:, p0:p0 + TPC]), w_out=ins["att_w_out"][0],
                         dffn_norm=ins["dffn_norm"][0], wg=ins["dffn_w_gate"][0], wu=ins["dffn_w_up"][0], wd=ins["dffn_w_down"][0],
                         ssm_norm=ins["ssm_norm"][0], w_sin=ins["ssm_w_in"][0]))
    r3 = _run(nc3, maps)
    del nc3, r2
    nc4 = build_L4(SEQ)
    maps = []
    for b in range(BATCH):
        uT_b = np.concatenate([r3[b * CPB + i]["uT"] for i in range(CPB)], axis=1)
        for gq in range(CPB):
            maps.append(s5_core_inputs(ins, b, gq, uT_b))
    r4 = _run(nc4, maps)
    del nc4
    nc5 = build_L5(TPC)
    maps = []
    for c in range(NCORES):
        b = c // CPB
        p0 = (c % CPB) * TPC
        yT = np.concatenate([r4[b * CPB + gq]["yT"][:, p0:p0 + TPC] for gq in range(CPB)], axis=0)
        maps.append(dict(h2T=r3[c]["h2T"], yT=ca(yT), w_glu=ins["ssm_w_glu"][0], moe_norm=ins["moe_norm"][0], w_r=ins["moe_router"][0],
                         wg=ins["moe_w_gate"][0], wu=ins["moe_w_up"][0], wd=ins["moe_w_down"][0]))
    r5 = _run(nc5, maps)
    out = np.concatenate([r5[c]["out"] for c in range(NCORES)], axis=0).reshape(BATCH, SEQ, D_MODEL)
    return out.astype(np.float32)
```

```python
import contextlib
import numpy as np
import concourse.bass as bass
import concourse.mybir as mybir
from concourse.bass_utils import run_bass_kernel_spmd

F32 = mybir.dt.float32
BF16 = mybir.dt.bfloat16
I32 = mybir.dt.int32
AF = mybir.ActivationFunctionType
ALU = mybir.AluOpType
AX = mybir.AxisListType

D_MODEL = 1024
D_FF = 3584
EPS = 1e-6
NCORES = 8

COMPUTE = ("pe", "act", "dve", "pool")
NDSEM = 8


class Prog:
    def __init__(self, nc):
        self.nc = nc
        self.engs = ("pe", "act", "dve", "pool", "sp")
        self.q = {e: [] for e in self.engs}
        self.cnt = {e: 0 for e in COMPUTE}
        self.known = {e: {} for e in self.engs}
        self.res = {}
        self.dcnt = {}
        self.drr = {e: 0 for e in self.engs}
        self.sems = {}
        self.n_wait = 0
        self.n_op = 0

    def _deps(self, R, W):
        deps = {}
        for r in R:
            st = self.res.get(r)
            if st is not None and st[0] is not None:
                k, v = st[0]
                if deps.get(k, 0) < v:
                    deps[k] = v
        for w in W:
            st = self.res.get(w)
            if st is not None:
                if st[0] is not None:
                    k, v = st[0]
                    if deps.get(k, 0) < v:
                        deps[k] = v
                for k, v in st[1].items():
                    if deps.get(k, 0) < v:
                        deps[k] = v
        return deps

    def _record(self, tok, R, W):
        k, v = tok
        for r in R:
            st = self.res.get(r)
            if st is None:
                st = [None, {}]
                self.res[r] = st
            if st[1].get(k, 0) < v:
                st[1][k] = v
        for w in W:
            self.res[w] = [tok, {}]

    def _emit_waits(self, eng, deps):
        kn = self.known[eng]
        for k, v in deps.items():
            if k == eng and eng == "pe":
                continue
            if kn.get(k, 0) >= v:
                continue
            kn[k] = v
            self.q[eng].append(("w", k, v))
            self.n_wait += 1

    def op(self, eng, fn, R=(), W=()):
        deps = self._deps(R, W)
        self._emit_waits(eng, deps)
        self.cnt[eng] += 1
        tok = (eng, self.cnt[eng])
        self.q[eng].append(("o", fn, eng, 1))
        self._record(tok, R, W)
        self.n_op += 1
        return tok

    def dma(self, eng, out, in_, R=(), W=(), **kw):
        deps = self._deps(R, W)
        j = self.drr[eng]
        self.drr[eng] = (j + 1) % NDSEM
        key = ("d", eng, j)
        prev = self.dcnt.get(key, 0)
        if prev:
            deps[key] = max(deps.get(key, 0), prev * 16)
        self._emit_waits(eng, deps)
        self.dcnt[key] = prev + 1
        tok = (key, (prev + 1) * 16)
        self.q[eng].append(("o", lambda e: e.dma_start(out=out, in_=in_, **kw), key, 16))
        self._record(tok, R, W)
        self.n_op += 1
        return tok

    def finish(self, eng="sp"):
        deps = {k: c * 16 for k, c in self.dcnt.items()}
        self._emit_waits(eng, deps)

    def emit(self):
        nc = self.nc
        with contextlib.ExitStack() as es:
            keys = list(COMPUTE) + list(self.dcnt.keys())
            for k in keys:
                nm = k if isinstance(k, str) else "d_%s_%d" % (k[1], k[2])
                self.sems[k] = es.enter_context(nc.semaphore("s_" + nm))
            block = es.enter_context(nc.Block())
            handles = {"pe": block.tensor, "act": block.scalar, "dve": block.vector,
                       "pool": block.gpsimd, "sp": block.sync}
            sems = self.sems
            for eng in self.engs:
                items = self.q[eng]

                def body(e, items=items):
                    for it in items:
                        if it[0] == "w":
                            e.wait_ge(sems[it[1]], it[2])
                        else:
                            it[1](e).then_inc(sems[it[2]], it[3])
                handles[eng](body)


class Rot:
    def __init__(self, nc, name, shape, dtype, n, psum=False):
        self.tiles = []
        for i in range(n):
            if psum:
                t = nc.alloc_psum_tensor("%s%d" % (name, i), shape, dtype)
            else:
                t = nc.alloc_sbuf_tensor("%s%d" % (name, i), shape, dtype)
            self.tiles.append(t)
        self.name = name
        self.i = 0

    def next(self):
        i = self.i % len(self.tiles)
        self.i += 1
        return self.tiles[i], (self.name, i)


class Ctx:
    def __init__(self, nc, n_ps=8):
        self.nc = nc
        self.P = Prog(nc)
        P = self.P
        self.ps = Rot(nc, "ps", [128, 512], F32, n_ps, psum=True)
        self.ones_bf = nc.alloc_sbuf_tensor("ones_bf", [128, 128], BF16)
        self.ones_f = nc.alloc_sbuf_tensor("ones_f", [128, 128], F32)
        self.ident_f = nc.alloc_sbuf_tensor("ident_f", [128, 128], F32)
        self.eps_t = nc.alloc_sbuf_tensor("eps_t", [128, 1], F32)
        P.op("pool", lambda e: e.memset(self.ones_bf[:], 1.0), W=["ones_bf"])
        P.op("pool", lambda e: e.memset(self.ones_f[:], 1.0), W=["ones_f"])
        P.op("pool", lambda e: e.memset(self.eps_t[:], EPS), W=["eps_t"])
        P.op("pool", lambda e: e.memset(self.ident_f[:], 1.0), W=["ident_f"])
        P.op("pool", lambda e: e.affine_select(out=self.ident_f[:], in_=self.ident_f[:], pattern=[[-1, 128]],
                                               compare_op=ALU.is_equal, fill=0.0, base=0, channel_multiplier=1),
             R=["ident_f"], W=["ident_f"])
        self.sq = Rot(nc, "sq", [128, 8, 512], BF16, 1)
        self.rt = Rot(nc, "rt", [128, 512], F32, 2)


def emit_rmsnorm_fm(C, hT, hkeys, nk, tok0, ntok, gain_sb, gkey, xnT, xkeys, xtok0, D, npart=128):
    P = C.P
    sq, sqk = C.sq.next()
    P.op("act", lambda e: e.activation(out=sq[:npart, 0:nk, 0:ntok], in_=hT[:npart, 0:nk, tok0:tok0 + ntok], func=AF.Square),
         R=list(hkeys), W=[sqk])
    ps, psk = C.ps.next()
    for k in range(nk):
        P.op("pe", lambda e, k=k: e.matmul(ps[:npart, 0:ntok], lhsT=C.ones_bf[:npart, :npart], rhs=sq[:npart, k, 0:ntok],
                                          start=(k == 0), stop=(k == nk - 1)),
             R=[sqk, "ones_bf"], W=[psk])
    rt, rtk = C.rt.next()
    P.op("act", lambda e: e.activation(out=rt[:npart, 0:ntok], in_=ps[:npart, 0:ntok], func=AF.Sqrt,
                                       bias=C.eps_t[:npart, 0:1], scale=1.0 / D),
         R=[psk, "eps_t"], W=[rtk])
    P.op("dve", lambda e: e.reciprocal(out=rt[:npart, 0:ntok], in_=rt[:npart, 0:ntok]), R=[rtk], W=[rtk])
    for k in range(nk):
        P.op("dve", lambda e, k=k: e.scalar_tensor_tensor(out=xnT[:npart, k, xtok0:xtok0 + ntok],
                                                          in0=hT[:npart, k, tok0:tok0 + ntok],
                                                          scalar=gain_sb[:npart, k:k + 1], in1=rt[:npart, 0:ntok],
                                                          op0=ALU.mult, op1=ALU.mult),
             R=[hkeys[k], rtk, gkey], W=[xkeys[k]])


def load_vec_fm(C, name, dram_vec_ap, n):
    nk = max(1, n // 128)
    npart = min(128, n)
    t = C.nc.alloc_sbuf_tensor(name, [128, nk], F32)
    C.P.dma("sp", t[:npart, :], dram_vec_ap.rearrange("(k p) -> p k", p=npart), W=[name], allow_slow_non_contiguous=True)
    return t


def emit_ffn(C, xnT, xkeys, hT, hkeys, T, wg, wu, wd, pools, gate_bc=None):
    P = C.P
    NTG = T // 512
    hidT, hidkeys = pools["hidT"], pools["hidkeys"]
    for wb in range(7):
        wgt, wgk = pools["wgu"].next()
        P.dma("pool", wgt[:], wg[:, wb * 512:(wb + 1) * 512].rearrange("(k p) n -> p k n", p=128), W=[wgk])
        wut, wuk = pools["wgu"].next()
        P.dma("pool", wut[:], wu[:, wb * 512:(wb + 1) * 512].rearrange("(k p) n -> p k n", p=128), W=[wuk])
        for m in range(4):
            mm = wb * 4 + m
            for tg in range(NTG):
                pg, pgk = C.ps.next()
                for k in range(8):
                    P.op("pe", lambda e, k=k, pg=pg, wgt=wgt, m=m, tg=tg: e.matmul(
                        pg[:, :], lhsT=wgt[:, k, m * 128:(m + 1) * 128], rhs=xnT[:, k, tg * 512:(tg + 1) * 512],
                        start=(k == 0), stop=(k == 7)), R=[wgk, xkeys(k, tg)], W=[pgk])
                pu, puk = C.ps.next()
                for k in range(8):
                    P.op("pe", lambda e, k=k, pu=pu, wut=wut, m=m, tg=tg: e.matmul(
                        pu[:, :], lhsT=wut[:, k, m * 128:(m + 1) * 128], rhs=xnT[:, k, tg * 512:(tg + 1) * 512],
                        start=(k == 0), stop=(k == 7)), R=[wuk, xkeys(k, tg)], W=[puk])
                sg, sgk = pools["sg"].next()
                P.op("act", lambda e, sg=sg, pg=pg: e.activation(out=sg[:, :], in_=pg[:, :], func=AF.Silu), R=[pgk], W=[sgk])
                P.op("dve", lambda e, sg=sg, pu=pu, mm=mm, tg=tg: e.tensor_tensor(
                    out=hidT[:, mm, tg * 512:(tg + 1) * 512], in0=sg[:, :], in1=pu[:, :], op=ALU.mult),
                    R=[sgk, puk], W=[hidkeys[mm] + (tg,)])
    for dm in range(8):
        wdt, wdk = pools["wd"].next()
        P.dma("pool", wdt[:], wd[:, dm * 128:(dm + 1) * 128].rearrange("(k p) n -> p k n", p=128), W=[wdk])
        for tg in range(NTG):
            po, pok = C.ps.next()
            for k in range(28):
                P.op("pe", lambda e, k=k, po=po, wdt=wdt, tg=tg: e.matmul(
                    po[:, :], lhsT=wdt[:, k, :], rhs=hidT[:, k, tg * 512:(tg + 1) * 512],
                    start=(k == 0), stop=(k == 27)), R=[wdk, hidkeys[k] + (tg,)], W=[pok])
            if gate_bc is None:
                P.op("dve", lambda e, po=po, dm=dm, tg=tg: e.tensor_tensor(
                    out=hT[:, dm, tg * 512:(tg + 1) * 512], in0=hT[:, dm, tg * 512:(tg + 1) * 512], in1=po[:, :], op=ALU.add),
                    R=[pok, hkeys(dm, tg)], W=[hkeys(dm, tg)])
            else:
                gt, gkf = gate_bc
                tmp, tmpk = pools["sg"].next()
                P.op("dve", lambda e, po=po, tmp=tmp, gt=gt, tg=tg: e.tensor_tensor(
                    out=tmp[:, :], in0=po[:, :], in1=gt[:, tg * 512:(tg + 1) * 512], op=ALU.mult),
                    R=[pok, gkf(tg)], W=[tmpk])
                P.op("dve", lambda e, tmp=tmp, dm=dm, tg=tg: e.tensor_tensor(
                    out=hT[:, dm, tg * 512:(tg + 1) * 512], in0=hT[:, dm, tg * 512:(tg + 1) * 512], in1=tmp[:, :], op=ALU.add),
                    R=[tmpk, hkeys(dm, tg)], W=[hkeys(dm, tg)])


def ffn_pools(nc, T):
    return {
        "hidT": nc.alloc_sbuf_tensor("hidT", [128, 28, T], BF16),
        "hidkeys": [("hid", k) for k in range(28)],
        "wgu": Rot(nc, "wgu", [128, 8, 512], BF16, 4),
        "wd": Rot(nc, "wd", [128, 28, 128], BF16, 2),
        "sg": Rot(nc, "sg", [128, 512], F32, 3),
    }


def emit_load_tm_to_fm(C, src_dram, hT, hkeys, ntiles, stage):
    P = C.P
    for t in range(ntiles):
        st, stk = stage.next()
        P.dma("sp", st[:], src_dram[t * 128:(t + 1) * 128, :], W=[stk])
        for half in range(2):
            ps, psk = C.ps.next()
            for kk in range(4):
                k = half * 4 + kk
                P.op("pe", lambda e, ps=ps, st=st, k=k, kk=kk: e.transpose(out=ps[:, kk * 128:(kk + 1) * 128], in_=st[:, k * 128:(k + 1) * 128],
                                                                           identity=C.ident_f[:]),
                     R=[stk, "ident_f"], W=[psk])
            P.op("dve" if half == 0 else "act",
                 (lambda e, ps=ps, half=half, t=t: e.tensor_copy(out=hT[:, half * 4:half * 4 + 4, t * 128:(t + 1) * 128],
                                                                 in_=ps[:, :].rearrange("p (k n) -> p k n", k=4))) if half == 0 else
                 (lambda e, ps=ps, half=half, t=t: e.copy(out=hT[:, half * 4:half * 4 + 4, t * 128:(t + 1) * 128],
                                                          in_=ps[:, :].rearrange("p (k n) -> p k n", k=4))),
                 R=[psk], W=[hkeys(half * 4 + kk, t // 4) for kk in range(4)])


def emit_store_fm_to_tm(C, hT, hkeys, dst_dram, ntiles, stage):
    P = C.P
    for t in range(ntiles):
        st, stk = stage.next()
        for half in range(2):
            ps, psk = C.ps.next()
            for kk in range(4):
                k = half * 4 + kk
                P.op("pe", lambda e, ps=ps, k=k, kk=kk, t=t: e.transpose(out=ps[:, kk * 128:(kk + 1) * 128], in_=hT[:, k, t * 128:(t + 1) * 128],
                                                                         identity=C.ident_f[:]),
                     R=[hkeys(k, t // 4), "ident_f"], W=[psk])
            if half == 0:
                P.op("dve", lambda e, ps=ps, st=st: e.tensor_copy(out=st[:, 0:512], in_=ps[:, :]), R=[psk], W=[stk + (0,)])
            else:
                P.op("act", lambda e, ps=ps, st=st: e.copy(out=st[:, 512:1024], in_=ps[:, :]), R=[psk], W=[stk + (1,)])
        P.dma("sp", dst_dram[t * 128:(t + 1) * 128, :], st[:], R=[stk + (0,), stk + (1,)], W=[("out", t)])


def build_L3(T):
    nc = bass.Bass("TRN2", target_bir_lowering=False)
    x = nc.dram_tensor("x", [T, 1024], F32, kind="ExternalInput").ap()
    mT = nc.dram_tensor("mT", [1024, T], F32, kind="ExternalInput").ap()
    w_out = nc.dram_tensor("w_out", [1024, 1024], F32, kind="ExternalInput").ap()
    n1 = nc.dram_tensor("dffn_norm", [1024], F32, kind="ExternalInput").ap()
    wg = nc.dram_tensor("wg", [1024, D_FF], F32, kind="ExternalInput").ap()
    wu = nc.dram_tensor("wu", [1024, D_FF], F32, kind="ExternalInput").ap()
    wd = nc.dram_tensor("wd", [D_FF, 1024], F32, kind="ExternalInput").ap()
    n2 = nc.dram_tensor("ssm_norm", [1024], F32, kind="ExternalInput").ap()
    w_sin = nc.dram_tensor("w_sin", [1024, 512], F32, kind="ExternalInput").ap()
    h2T = nc.dram_tensor("h2T", [1024, T], F32, kind="ExternalOutput").ap()
    uT = nc.dram_tensor("uT", [512, T], F32, kind="ExternalOutput").ap()
    C = Ctx(nc)
    P = C.P
    TG = 1024
    hT = nc.alloc_sbuf_tensor("hT", [128, 8, TG], F32)
    xnT = nc.alloc_sbuf_tensor("xnT", [128, 8, TG], BF16)
    pools = ffn_pools(nc, TG)
    stage = Rot(nc, "stage", [128, 1024], F32, 2)
    mts = Rot(nc, "mts", [128, 8, 512], BF16, 2)
    uo = Rot(nc, "uo", [128, 512], F32, 2)
    g1 = load_vec_fm(C, "g1", n1, 1024)
    g2 = load_vec_fm(C, "g2", n2, 1024)
    hk = lambda k, tg: ("h", k, tg)
    xk = lambda k, tg: ("xn", k, tg)
    for grp in range(T // TG):
        t0 = grp * TG
        emit_load_tm_to_fm(C, x[t0:t0 + TG, :], hT, hk, TG // 128, stage)
        wo = []
        for hf in range(2):
            wt, wk = pools["wgu"].next()
            P.dma("pool", wt[:], w_out[:, hf * 512:(hf + 1) * 512].rearrange("(k p) n -> p k n", p=128), W=[wk])
            wo.append((wt, wk))
        for tg in range(TG // 512):
            mt, mk = mts.next()
            P.dma("pool", mt[:], mT[:, t0 + tg * 512:t0 + (tg + 1) * 512].rearrange("(k p) n -> p k n", p=128), W=[mk])
            for dm in range(8):
                wt, wk = wo[dm // 4]
                po, pok = C.ps.next()
                for k in range(8):
                    P.op("pe", lambda e, k=k, po=po, wt=wt, mt=mt, dm=dm: e.matmul(
                        po[:, :], lhsT=wt[:, k, (dm % 4) * 128:(dm % 4 + 1) * 128], rhs=mt[:, k, :],
                        start=(k == 0), stop=(k == 7)), R=[wk, mk], W=[pok])
                P.op("dve", lambda e, po=po, dm=dm, tg=tg: e.tensor_tensor(
                    out=hT[:, dm, tg * 512:(tg + 1) * 512], in0=hT[:, dm, tg * 512:(tg + 1) * 512], in1=po[:, :], op=ALU.add),
                    R=[pok, hk(dm, tg)], W=[hk(dm, tg)])
        for tg in range(TG // 512):
            emit_rmsnorm_fm(C, hT, [hk(k, tg) for k in range(8)], 8, tg * 512, 512, g1, "g1",
                            xnT, [xk(k, tg) for k in range(8)], tg * 512, 1024)
        emit_ffn(C, xnT, xk, hT, hk, TG, wg, wu, wd, pools)
        for k in range(8):
            P.dma("sp", h2T[k * 128:(k + 1) * 128, t0:t0 + TG], hT[:, k, :], R=[hk(k, tg) for tg in range(TG // 512)], W=[("h2o", k)])
        for tg in range(TG // 512):
            emit_rmsnorm_fm(C, hT, [hk(k, tg) for k in range(8)], 8, tg * 512, 512, g2, "g2",
                            xnT, [xk(k, tg) for k in range(8)], tg * 512, 1024)
        wt, wk = pools["wgu"].next()
        P.dma("pool", wt[:], w_sin.rearrange("(k p) n -> p k n", p=128), W=[wk])
        for tg in range(TG // 512):
            for c in range(4):
                po, pok = C.ps.next()
                for k in range(8):
                    P.op("pe", lambda e, k=k, po=po, wt=wt, c=c, tg=tg: e.matmul(
                        po[:, :], lhsT=wt[:, k, c * 128:(c + 1) * 128], rhs=xnT[:, k, tg * 512:(tg + 1) * 512],
                        start=(k == 0), stop=(k == 7)), R=[wk, xk(k, tg)], W=[pok])
                ut, uk = uo.next()
                P.op("act", lambda e, ut=ut, po=po: e.copy(out=ut[:, :], in_=po[:, :]), R=[pok], W=[uk])
                P.dma("sp", uT[c * 128:(c + 1) * 128, t0 + tg * 512:t0 + (tg + 1) * 512], ut[:, :], R=[uk], W=[("uo", c, tg, grp)])
    P.finish("sp")
    P.emit()
    return nc


def build_L5(T, n_exp=8):
    nc = bass.Bass("TRN2", target_bir_lowering=False)
    h2T = nc.dram_tensor("h2T", [1024, T], F32, kind="ExternalInput").ap()
    yT = nc.dram_tensor("yT", [512, T], F32, kind="ExternalInput").ap()
    w_glu = nc.dram_tensor("w_glu", [512, 2048], F32, kind="ExternalInput").ap()
    n1 = nc.dram_tensor("moe_norm", [1024], F32, kind="ExternalInput").ap()
    w_r = nc.dram_tensor("w_r", [1024, 8], F32, kind="ExternalInput").ap()
    wg = nc.dram_tensor("wg", [8, 1024, D_FF], F32, kind="ExternalInput").ap()
    wu = nc.dram_tensor("wu", [8, 1024, D_FF], F32, kind="ExternalInput").ap()
    wd = nc.dram_tensor("wd", [8, D_FF, 1024], F32, kind="ExternalInput").ap()
    out = nc.dram_tensor("out", [T, 1024], F32, kind="ExternalOutput").ap()
    C = Ctx(nc)
    P = C.P
    TG = 1024
    NTG = TG // 512
    hT = nc.alloc_sbuf_tensor("hT", [128, 8, TG], F32)
    xnT = nc.alloc_sbuf_tensor("xnT", [128, 8, TG], BF16)
    pools = ffn_pools(nc, TG)
    stage = Rot(nc, "stage", [128, 1024], F32, 1)
    yts = Rot(nc, "yts", [128, 4, 512], BF16, 1)
    wglu = nc.alloc_sbuf_tensor("wglu", [128, 4, 2048], BF16)
    wrg = nc.alloc_sbuf_tensor("wrg", [128, 8, 8], F32)
    sel = nc.alloc_sbuf_tensor("sel", [8, 8, 128], F32)
    gT = nc.alloc_sbuf_tensor("gT", [8, TG], F32)
    gbc = nc.alloc_sbuf_tensor("gbc", [128, TG], F32)
    sm = Rot(nc, "sm", [128, 64], F32, 2)
    g1 = load_vec_fm(C, "g1", n1, 1024)
    P.dma("pool", wglu[:], w_glu.rearrange("(k p) n -> p k n", p=128), W=["wglu"])
    P.dma("sp", wrg[:], w_r.rearrange("(k p) n -> p k n", p=128), W=["wrg"], allow_slow_non_contiguous=True)
    for k in range(8):
        P.op("dve", lambda e, k=k: e.tensor_scalar(out=wrg[:, k, :], in0=wrg[:, k, :], scalar1=g1[:, k:k + 1], scalar2=None, op0=ALU.mult),
             R=["wrg", "g1"], W=["wrg"])
    P.op("pool", lambda e: e.memset(sel[:], 1.0), W=["sel"])
    P.op("pool", lambda e: e.affine_select(out=sel[:], in_=sel[:], pattern=[[-1, 8], [0, 128]], compare_op=ALU.is_equal, fill=0.0,
                                           base=0, channel_multiplier=1), R=["sel"], W=["sel"])
    hk = lambda k, tg: ("h", k, tg)
    xk = lambda k, tg: ("xn", k, tg)
    for grp in range(T // TG):
        t0 = grp * TG
        for k in range(8):
            P.dma("sp", hT[:, k, :], h2T[k * 128:(k + 1) * 128, t0:t0 + TG], W=[hk(k, tg) for tg in range(NTG)])
        for tg in range(NTG):
            yt, yk = yts.next()
            P.dma("pool", yt[:], yT[:, t0 + tg * 512:t0 + (tg + 1) * 512].rearrange("(k p) n -> p k n", p=128), W=[yk])
            for dm in range(8):
                p1, p1k = C.ps.next()
                for k in range(4):
                    P.op("pe", lambda e, k=k, p1=p1, yt=yt, dm=dm: e.matmul(p1[:, :], lhsT=wglu[:, k, dm * 128:(dm + 1) * 128], rhs=yt[:, k, :],
                                                                          start=(k == 0), stop=(k == 3)), R=["wglu", yk], W=[p1k])
                p2, p2k = C.ps.next()
                for k in range(4):
                    P.op("pe", lambda e, k=k, p2=p2, yt=yt, dm=dm: e.matmul(p2[:, :], lhsT=wglu[:, k, 1024 + dm * 128:1024 + (dm + 1) * 128], rhs=yt[:, k, :],
                                                                          start=(k == 0), stop=(k == 3)), R=["wglu", yk], W=[p2k])
                sg, sgk = pools["sg"].next()
                P.op("act", lambda e, sg=sg, p2=p2: e.activation(out=sg[:, :], in_=p2[:, :], func=AF.Sigmoid), R=[p2k], W=[sgk])
                P.op("dve", lambda e, sg=sg, p1=p1: e.tensor_tensor(out=sg[:, :], in0=sg[:, :], in1=p1[:, :], op=ALU.mult), R=[sgk, p1k], W=[sgk])
                P.op("dve", lambda e, sg=sg, dm=dm, tg=tg: e.tensor_tensor(out=hT[:, dm, tg * 512:(tg + 1) * 512], in0=hT[:, dm, tg * 512:(tg + 1) * 512],
                                                                       in1=sg[:, :], op=ALU.add), R=[sgk, hk(dm, tg)], W=[hk(dm, tg)])
        for tg in range(NTG):
            emit_rmsnorm_fm(C, hT, [hk(k, tg) for k in range(8)], 8, tg * 512, 512, g1, "g1",
                            xnT, [xk(k, tg) for k in range(8)], tg * 512, 1024)
        for tt in range(TG // 128):
            tg = tt // 4
            pl, plk = C.ps.next()
            for k in range(8):
                P.op("pe", lambda e, k=k, pl=pl, tt=tt: e.matmul(pl[:, 0:8], lhsT=hT[:, k, tt * 128:(tt + 1) * 128], rhs=wrg[:, k, :],
                                                             start=(k == 0), stop=(k == 7)), R=[hk(k, tg), "wrg"], W=[plk])
            pss, pssk = C.ps.next()
            sq, sqk = C.sq.next()
            P.op("act", lambda e, sq=sq, tt=tt: e.activation(out=sq[:, :, 0:128], in_=hT[:, :, tt * 128:(tt + 1) * 128], func=AF.Square),
                 R=[hk(k, tg) for k in range(8)], W=[sqk])
            for k in range(8):
                P.op("pe", lambda e, k=k, pss=pss, sq=sq: e.matmul(pss[:, 0:1], lhsT=sq[:, k, 0:128], rhs=C.ones_bf[:, 0:1],
                                                               start=(k == 0), stop=(k == 7)), R=[sqk, "ones_bf"], W=[pssk])
            s, sk = sm.next()
            P.op("act", lambda e, s=s, pss=pss: e.activation(out=s[:, 0:1], in_=pss[:, 0:1], func=AF.Sqrt, bias=C.eps_t[:, 0:1], scale=1.0 / 1024),
                 R=[pssk, "eps_t"], W=[sk])
            P.op("dve", lambda e, s=s: e.reciprocal(out=s[:, 0:1], in_=s[:, 0:1]), R=[sk], W=[sk])
            P.op("dve", lambda e, s=s, pl=pl: e.tensor_scalar(out=s[:, 8:16], in0=pl[:, 0:8], scalar1=s[:, 0:1], scalar2=None, op0=ALU.mult),
                 R=[sk, plk], W=[sk])
            P.op("dve", lambda e, s=s: e.max(out=s[:, 16:24], in_=s[:, 8:16]), R=[sk], W=[sk])
            P.op("dve", lambda e, s=s: e.tensor_scalar(out=s[:, 24:25], in0=s[:, 16:17], scalar1=-1.0, scalar2=None, op0=ALU.mult), R=[sk], W=[sk])
            P.op("act", lambda e, s=s: e.activation(out=s[:, 32:40], in_=s[:, 8:16], func=AF.Exp, bias=s[:, 24:25], scale=1.0), R=[sk], W=[sk])
            P.op("dve", lambda e, s=s: e.tensor_scalar(out=s[:, 40:48], in0=s[:, 8:16], scalar1=s[:, 17:18], scalar2=None, op0=ALU.is_ge), R=[sk], W=[sk])
            P.op("dve", lambda e, s=s: e.tensor_tensor(out=s[:, 32:40], in0=s[:, 32:40], in1=s[:, 40:48], op=ALU.mult), R=[sk], W=[sk])
            P.op("dve", lambda e, s=s: e.reduce_sum(out=s[:, 48:49], in_=s[:, 32:40], axis=AX.X), R=[sk], W=[sk])
            P.op("dve", lambda e, s=s: e.reciprocal(out=s[:, 48:49], in_=s[:, 48:49]), R=[sk], W=[sk])
            P.op("dve", lambda e, s=s: e.tensor_scalar(out=s[:, 32:40], in0=s[:, 32:40], scalar1=s[:, 48:49], scalar2=None, op0=ALU.mult), R=[sk], W=[sk])
            pt, ptk = C.ps.next()
            P.op("pe", lambda e, pt=pt, s=s: e.transpose(out=pt[0:8, 0:128], in_=s[:, 32:40], identity=C.ident_f[:]), R=[sk, "ident_f"], W=[ptk])
            P.op("act", lambda e, pt=pt, tt=tt: e.copy(out=gT[0:8, tt * 128:(tt + 1) * 128], in_=pt[0:8, 0:128]), R=[ptk], W=[("gT", tg)])
        for ex in range(n_exp):
            for tg in range(NTG):
                pb, pbk = C.ps.next()
                P.op("pe", lambda e, pb=pb, ex=ex, tg=tg: e.matmul(pb[:, :], lhsT=sel[0:8, ex, :], rhs=gT[0:8, tg * 512:(tg + 1) * 512],
                                                               start=True, stop=True), R=["sel", ("gT", tg)], W=[pbk])
                P.op("act", lambda e, pb=pb, tg=tg: e.copy(out=gbc[:, tg * 512:(tg + 1) * 512], in_=pb[:, :]), R=[pbk], W=[("gbc", tg)])
            emit_ffn(C, xnT, xk, hT, hk, TG, wg[ex], wu[ex], wd[ex], pools, gate_bc=(gbc, lambda tg: ("gbc", tg)))
        emit_store_fm_to_tm(C, hT, hk, out[t0:t0 + TG, :], TG // 128, stage)
    P.finish("sp")
    P.emit()
    return nc


def build_L1(T):
    nc = bass.Bass("TRN2", target_bir_lowering=False)
    dt = lambda n, s, k="ExternalInput": nc.dram_tensor(n, s, F32, kind=k).ap()
    x = dt("x", [T, 1024])
    n0 = dt("att_norm", [1024])
    w_in = dt("w_in", [1024, 1952])
    nq = dt("q_lat_norm", [256])
    w_qup = dt("w_q_up", [256, 768])
    nkv = dt("kv_lat_norm", [128])
    w_kvup = dt("w_kv_up", [128, 1024])
    gq = dt("q_norm", [96])
    gk = dt("k_norm", [96])
    cq_t = dt("cq_t", [96, T]); sq_t = dt("sq_t", [96, T])
    ck_t = dt("ck_t", [96, T]); sk_t = dt("sk_t", [96, T])
    pm = dt("pm", [96, 96])
    dtb = lambda n, s: nc.dram_tensor(n, s, BF16, kind="ExternalOutput").ap()
    sbqT = dtb("sbqT", [512, T])
    sbkT = dtb("sbkT", [512, T])
    sbv = dtb("sbv", [T, 512])
    mqT = dtb("mqT", [8, 96, T])
    mkT = dtb("mkT", [8, 96, T])
    mv = dtb("mv", [T, 512])
    C = Ctx(nc)
    P = C.P
    A = nc.alloc_sbuf_tensor
    hT = A("hT", [128, 8, 512], F32)
    xnT = A("xnT", [128, 8, 512], BF16)
    win = A("win", [128, 8, 1952], BF16)
    wkr = A("wkr", [128, 8, 96], BF16)
    wqup = A("wqup", [128, 2, 768], BF16)
    wkn = A("wkn", [128, 8, 96], BF16)
    wkv = A("wkv", [128, 8, 64], BF16)
    pmt = A("pmt", [96, 96], BF16)
    lat = A("lat", [128, 3, 512], F32)
    latn = A("latn", [128, 3, 512], BF16)
    krp = A("krp", [96, 512], F32)
    hrs = Rot(nc, "hrs", [96, 1, 512], F32, 8)
    hns = Rot(nc, "hns", [96, 1, 512], BF16, 8)
    sqh = Rot(nc, "sqh", [96, 512], BF16, 8)
    rth = Rot(nc, "rth", [96, 512], F32, 8)
    tabs = A("tabs", [96, 4, 512], F32)
    t1 = Rot(nc, "t1", [96, 512], F32, 8)
    t2 = Rot(nc, "t2", [96, 512], F32, 8)
    ob = Rot(nc, "ob", [128, 512], BF16, 3)
    t3 = Rot(nc, "t3", [96, 512], BF16, 8)
    stage = Rot(nc, "stage", [128, 1024], F32, 2)
    g0 = load_vec_fm(C, "g0", n0, 1024)
    gql = load_vec_fm(C, "gql", nq, 256)
    gkl = load_vec_fm(C, "gkl", nkv, 128)
    gqh = load_vec_fm(C, "gqh", gq, 96)
    gkh = load_vec_fm(C, "gkh", gk, 96)
    P.dma("pool", win[:], w_in.rearrange("(k p) n -> p k n", p=128), W=["win"])
    P.op("pool", lambda e: e.memset(wkr[:], 0.0), W=["wkr"])
    P.dma("pool", wkr[:, :, 64:96], w_in[:, 1920:1952].rearrange("(k p) n -> p k n", p=128), R=["wkr"], W=["wkr"])
    P.dma("pool", wqup[:], w_qup.rearrange("(k p) n -> p k n", p=128), W=["wqup"])
    P.op("pool", lambda e: e.memset(wkn[:], 0.0), W=["wkn"])
    P.dma("pool", wkn[:, :, 0:64], w_kvup.rearrange("k (h c) -> k h c", c=128)[:, :, 0:64], R=["wkn"], W=["wkn"])
    P.dma("pool", wkv[:], w_kvup.rearrange("k (h c) -> k h c", c=128)[:, :, 64:128], W=["wkv"])
    P.dma("pool", pmt[:], pm, W=["pmt"])
    hk = lambda k, tg: ("h", k)
    for tg in range(T // 512):
        t0 = tg * 512
        emit_load_tm_to_fm(C, x[t0:t0 + 512, :], hT, hk, 4, stage)
        emit_rmsnorm_fm(C, hT, [hk(k, 0) for k in range(8)], 8, 0, 512, g0, "g0", xnT, [("xn", k) for k in range(8)], 0, 1024)
        XR = [("xn", k) for k in range(8)]
        for i, tab in enumerate((cq_t, sq_t, ck_t, sk_t)):
            P.dma("sp", tabs[:, i, :], tab[:, t0:t0 + 512], W=[("tabs", i)])

        def proj_fm(col0, ncols, wt=win, wkey="win"):
            po, pok = C.ps.next()
            for k in range(8):
                P.op("pe", lambda e, k=k, po=po: e.matmul(po[0:ncols, :], lhsT=wt[:, k, col0:col0 + ncols], rhs=xnT[:, k, :],
                                                          start=(k == 0), stop=(k == 7)), R=[wkey] + XR, W=[pok])
            return po, pok
        for c in range(8):
            po, pok = proj_fm(c * 128, 128)
            o, okk = ob.next()
            P.op("act", lambda e, o=o, po=po, c=c: e.mul(out=o[:, :], in_=po[:, :], mul=(0.125 if c < 4 else 1.0)), R=[pok], W=[okk])
            dst = sbqT if c < 4 else sbkT
            P.dma("sp", dst[(c % 4) * 128:(c % 4 + 1) * 128, t0:t0 + 512], o[:, :], R=[okk], W=[("o1", c, tg)])
        for tt in range(4):
            po, pok = C.ps.next()
            for k in range(8):
                P.op("pe", lambda e, k=k, po=po, tt=tt: e.matmul(po[:, :], lhsT=xnT[:, k, tt * 128:(tt + 1) * 128], rhs=win[:, k, 1024:1536],
                                                             start=(k == 0), stop=(k == 7)), R=["win"] + XR, W=[pok])
            o, okk = ob.next()
            P.op("dve", lambda e, o=o, po=po: e.tensor_copy(out=o[:, :], in_=po[:, :]), R=[pok], W=[okk])
            P.dma("sp", sbv[t0 + tt * 128:t0 + (tt + 1) * 128, :], o[:, :], R=[okk], W=[("o2", tt, tg)])
        for c in range(3):
            po, pok = proj_fm(1536 + c * 128, 128)
            P.op("act", lambda e, po=po, c=c: e.copy(out=lat[:, c, :], in_=po[:, :]), R=[pok], W=[("lat", c)])
        emit_rmsnorm_fm(C, lat, [("lat", 0), ("lat", 1)], 2, 0, 512, gql, "gql", latn, [("latn", 0), ("latn", 1)], 0, 256)
        emit_rmsnorm_fm(C, lat[:, 2:3, :], [("lat", 2)], 1, 0, 512, gkl, "gkl", latn[:, 2:3, :], [("latn", 2)], 0, 128)
        po, pok = proj_fm(0, 96, wt=wkr, wkey="wkr")
        P.op("act", lambda e, po=po: e.copy(out=krp[:, :], in_=po[0:96, :]), R=[pok], W=["krp"])
        for tt in range(4):
            po, pok = C.ps.next()
            P.op("pe", lambda e, po=po, tt=tt: e.matmul(po[:, :], lhsT=latn[:, 2, tt * 128:(tt + 1) * 128], rhs=wkv[:, :, :],
                                                    start=True, stop=True), R=["wkv", ("latn", 2)], W=[pok])
            o, okk = ob.next()
            P.op("dve", lambda e, o=o, po=po: e.tensor_copy(out=o[:, :], in_=po[:, :]), R=[pok], W=[okk])
            P.dma("sp", mv[t0 + tt * 128:t0 + (tt + 1) * 128, :], o[:, :], R=[okk], W=[("o3", tt, tg)])
        def chain(h, which):
            hr_, hrk = hrs.next()
            hn_, hnk = hns.next()
            po, pok = C.ps.next()
            if which == 0:
                for k in range(2):
                    P.op("pe", lambda e, k=k: e.matmul(po[0:96, :], lhsT=wqup[:, k, h * 96:(h + 1) * 96], rhs=latn[:, k, :],
                                                       start=(k == 0), stop=(k == 1)), R=["wqup", ("latn", 0), ("latn", 1)], W=[pok])
                yield
                P.op("act", lambda e: e.copy(out=hr_[:, 0, :], in_=po[0:96, :]), R=[pok], W=[hrk])
            else:
                P.op("pe", lambda e: e.matmul(po[0:96, :], lhsT=wkn[:, h, :], rhs=latn[:, 2, :], start=True, stop=True),
                     R=["wkn", ("latn", 2)], W=[pok])
                yield
                P.op("dve", lambda e: e.tensor_tensor(out=hr_[:, 0, :], in0=po[0:96, :], in1=krp[:, :], op=ALU.add), R=[pok, "krp"], W=[hrk])
            yield
            gain, gkey = (gqh, "gqh") if which == 0 else (gkh, "gkh")
            sq_, sqk = sqh.next()
            P.op("act", lambda e: e.activation(out=sq_[:, :], in_=hr_[:, 0, :], func=AF.Square), R=[hrk], W=[sqk])
            yield
            ps, psk = C.ps.next()
            P.op("pe", lambda e: e.matmul(ps[0:96, :], lhsT=C.ones_bf[0:96, 0:96], rhs=sq_[:, :], start=True, stop=True), R=[sqk, "ones_bf"], W=[psk])
            yield
            rt_, rtk = rth.next()
            P.op("act", lambda e: e.activation(out=rt_[:, :], in_=ps[0:96, :], func=AF.Sqrt, bias=C.eps_t[0:96, 0:1], scale=1.0 / 96),
                 R=[psk, "eps_t"], W=[rtk])
            yield
            P.op("dve", lambda e: e.reciprocal(out=rt_[:, :], in_=rt_[:, :]), R=[rtk], W=[rtk])
            yield
            P.op("dve", lambda e: e.scalar_tensor_tensor(out=hn_[:, 0, :], in0=hr_[:, 0, :], scalar=gain[0:96, 0:1], in1=rt_[:, :],
                                                         op0=ALU.mult, op1=ALU.mult), R=[hrk, rtk, gkey], W=[hnk])
            yield
            pp, ppk = C.ps.next()
            P.op("pe", lambda e: e.matmul(pp[0:96, :], lhsT=pmt[:, :], rhs=hn_[:, 0, :], start=True, stop=True), R=["pmt", hnk], W=[ppk])
            a, ak = t1.next()
            ci, si = (0, 1) if which == 0 else (2, 3)
            P.op("pool", lambda e: e.tensor_tensor(out=a[:, :], in0=hn_[:, 0, :], in1=tabs[:, ci, :], op=ALU.mult), R=[hnk, ("tabs", ci)], W=[ak])
            yield
            b, bk = t2.next()
            P.op("dve", lambda e: e.tensor_tensor(out=b[:, :], in0=pp[0:96, :], in1=tabs[:, si, :], op=ALU.mult), R=[ppk, ("tabs", si)], W=[bk])
            yield
            a3, a3k = t3.next()
            P.op("pool", lambda e: e.tensor_tensor(out=a3[:, :], in0=a[:, :], in1=b[:, :], op=ALU.add), R=[ak, bk], W=[a3k])
            dst = mqT if which == 0 else mkT
            P.dma("sp", dst[h, :, t0:t0 + 512], a3[:, :], R=[a3k], W=[("o4", h, which, tg)])

        todo = [(h, which) for h in range(8) for which in range(2)]
        for g0_ in range(0, len(todo), 8):
            gens = [chain(h, which) for (h, which) in todo[g0_:g0_ + 8]]
            while gens:
                alive = []
                for g_ in gens:
                    try:
                        next(g_)
                        alive.append(g_)
                    except StopIteration:
                        pass
                gens = alive
    P.finish("sp")
    P.emit()
    return nc


def rope_tables(S):
    inv_freq = (10000.0 ** (-np.arange(0, 32, 2, dtype=np.float32) / np.float32(32))).astype(np.float32)
    ang = (np.arange(S, dtype=np.float32)[:, None] * inv_freq[None, :]).astype(np.float32)
    cos = np.cos(ang).astype(np.float32).T
    sin = np.sin(ang).astype(np.float32).T
    Ct = np.ones((96, S), np.float32); St = np.zeros((96, S), np.float32)
    Ct[64:80] = cos; Ct[80:96] = cos
    St[64:80] = sin; St[80:96] = sin
    pm = np.zeros((96, 96), np.float32)
    for i in range(16):
        pm[80 + i, 64 + i] = -1.0
        pm[64 + i, 80 + i] = 1.0
    return Ct, St, pm


def build_L2(S, n_sb=2, n_mla=2):
    nc = bass.Bass("TRN2", target_bir_lowering=False)
    dt = lambda n, s, k="ExternalInput": nc.dram_tensor(n, s, F32, kind=k).ap()
    dtb = lambda n, s: nc.dram_tensor(n, s, BF16, kind="ExternalInput").ap()
    sbqT = dtb("sbqT", [2, 64, S]); sbkT = dtb("sbkT", [2, 64, S]); sbv = dtb("sbv", [2, S, 64])
    mqT = dtb("mqT", [2, 96, S]); mkT = dtb("mkT", [2, 96, S]); mv = dtb("mv", [2, S, 64])
    oT = dt("oT", [4, 64, S], "ExternalOutput")
    C = Ctx(nc, n_ps=0)
    P = C.P
    A = nc.alloc_sbuf_tensor
    NB = S // 128
    NQG = S // 512
    zps = Rot(nc, "zps", [128, 2, 512], F32, 1, psum=True)
    argp = Rot(nc, "argp", [128, 2, 512], F32, 2, psum=True)
    acc = Rot(nc, "acc", [128, 512], F32, 2, psum=True)
    qTs = [A("qT%d" % i, [128, S], BF16) for i in range(2)]
    kTs = [A("kT%d" % i, [128, S], BF16) for i in range(2)]
    vas = [A("va%d" % i, [128, NB, 128], BF16) for i in range(2)]
    mle = A("mle", [128, 4, 512], BF16)
    mltr = A("mltr", [128, 4, 512], BF16)
    for bi in range(2):
        for c4 in range(4):
            sl = slice(c4 * (S // 4), (c4 + 1) * (S // 4))
            P.op("pool", lambda e, sl=sl, bi=bi: e.memset(qTs[bi][:, sl], 0.0), W=[("qT", bi, c4)])
            P.op("pool", lambda e, sl=sl, bi=bi: e.memset(kTs[bi][:, sl], 0.0), W=[("kT", bi, c4)])
        P.op("pool", lambda e, bi=bi: e.memset(vas[bi][:], 0.0), W=[("va", bi)])
        P.op("pool", lambda e, bi=bi: e.memset(vas[bi][:, :, 64:65], 1.0), R=[("va", bi)], W=[("va", bi)])
    nuin = A("nuin", [128, 128], BF16)
    nones = A("nones", [128, 128], BF16)
    et = Rot(nc, "et", [128, 2, 512], F32, 4)
    xt = Rot(nc, "xt", [128, 2, 512], F32, 2)
    spt = Rot(nc, "spt", [128, 2, 512], BF16, 3)
    wt = Rot(nc, "wt", [128, 2, 512], BF16, 3)
    Rt = Rot(nc, "Rt", [128, 2, 512], BF16, 3)
    ot = Rot(nc, "ot", [128, 512], F32, 2)
    rr = A("rr", [128, 512], F32)
    bcs = A("bcs", [64, 512], F32)
    P.op("pool", lambda e: e.memset(mle[:], 1.0), W=["mle"])
    P.op("pool", lambda e: e.memset(mltr[:], 1.0), W=["mltr"])
    P.op("pool", lambda e: e.memset(nuin[:], -1.0), W=["nuin"])
    P.op("pool", lambda e: e.memset(nones[:], -1.0), W=["nones"])
    for d in range(4):
        P.op("pool", lambda e, d=d: e.affine_select(out=mle[:, d, :], in_=mle[:, d, :], pattern=[[1, 512]], compare_op=ALU.is_ge, fill=0.0,
                                                    base=-128 * d, channel_multiplier=-1), R=["mle"], W=["mle"])
        P.op("pool", lambda e, d=d: e.affine_select(out=mltr[:, 3 - d, :], in_=mltr[:, 3 - d, :], pattern=[[1, 512]], compare_op=ALU.is_gt,
                                                    fill=0.0, base=-128 * d, channel_multiplier=-1), R=["mltr"], W=["mltr"])
    P.op("pool", lambda e: e.affine_select(out=nuin[:], in_=nuin[:], pattern=[[-1, 128]], compare_op=ALU.is_ge, fill=0.0,
                                           base=0, channel_multiplier=1), R=["nuin"], W=["nuin"])
    CW = S // 4
    NH = n_sb + n_mla

    def head_cfg(hd):
        is_sb = hd < n_sb
        hh = hd if is_sb else hd - n_sb
        return is_sb, hh, (64 if is_sb else 96), ((sbqT, sbkT, sbv) if is_sb else (mqT, mkT, mv))

    def emit_loads(hd):
        is_sb, hh, dq, (qsrc, ksrc, vsrc) = head_cfg(hd)
        bi = hd % 2
        for c4 in range(4):
            sl = slice(c4 * CW, (c4 + 1) * CW)
            P.dma("sp", qTs[bi][0:dq, sl], qsrc[hh, :, sl], W=[("qT", bi, c4)])
            P.dma("sp", kTs[bi][0:dq, sl], ksrc[hh, :, sl], W=[("kT", bi, c4)])
        P.dma("sp", vas[bi][:, :, 0:64], vsrc[hh].rearrange("(kb p) c -> p kb c", p=128), R=[("va", bi)], W=[("va", bi)])

    emit_loads(0)
    for hd in range(NH):
        is_sb, hh, dq, _ = head_cfg(hd)
        bi = hd % 2
        qT, kT, va = qTs[bi], kTs[bi], vas[bi]
        vak = ("va", bi)
        qkeys = lambda qg, bi=bi: [("qT", bi, c) for c in range((qg * 512) // CW, (qg * 512 + 511) // CW + 1)]
        kkeys = lambda kb, bi=bi: [("kT", bi, c) for c in range((kb * 128) // CW, (kb * 128 + 127) // CW + 1)]
        if hd + 1 < NH:
            emit_loads(hd + 1)
        blocks = []
        for qg in range(NQG):
            nkb = 4 * (qg + 1)
            order = list(range(nkb - 1, -1, -1)) if is_sb else list(range(nkb))
            for i2 in range(nkb // 2):
                blocks.append((qg, i2, order[2 * i2], order[2 * i2 + 1], nkb // 2))
        nblk = len(blocks)
        st = {}
        qstate = {}

        mla_z = [(zps.tiles[0], ("zps", 0)), (argp.tiles[0], ("argp", 0)), (argp.tiles[1], ("argp", 1))]

        def stage_z(t):
            qg, i2, kbA, kbB, npr = blocks[t]
            kTl, qTl = kT, qT
            zp, zpk = zps.next() if is_sb else mla_z[t % 3]
            for hf, kb in enumerate((kbA, kbB)):
                P.op("pe", lambda e, hf=hf, kb=kb: e.matmul(zp[:, hf, :], lhsT=kTl[:, kb * 128:(kb + 1) * 128], rhs=qTl[:, qg * 512:(qg + 1) * 512],
                                                            start=True, stop=True), R=qkeys(qg) + kkeys(kb), W=[zpk + (hf,)])
            st[t] = dict(zp=zp, zpk=zpk)

        def stage_a_sb(t):
            s_ = st[t]
            zp, zpk = s_["zp"], s_["zpk"]
            e_, ek = et.next()
            P.op("act", lambda e: e.activation(out=e_[:, :, :], in_=zp[:, :, :], func=AF.Exp), R=[zpk + (0,), zpk + (1,)], W=[ek])
            s_["e"] = (e_, ek)

        def stage_a2_sb(t):
            qg, i2, kbA, kbB, npr = blocks[t]
            dA = kbA - 4 * qg
            s_ = st[t]
            e_, ek = s_["e"]
            sp, spk = spt.next()
            P.op("act", lambda e: e.activation(out=sp[:, :, :], in_=e_[:, :, :], func=AF.Ln, bias=C.ones_f[:, 0:1], scale=1.0),
                 R=[ek, "ones_f"], W=[spk])
            if dA >= 0:
                r0 = 3 - dA
                P.op("dve", lambda e: e.tensor_tensor(out=sp[:, :, :], in0=sp[:, :, :], in1=mltr[:, r0:r0 + 2, :], op=ALU.mult),
                     R=[spk, "mltr"], W=[spk])
            Rn, Rnk = Rt.next()
            if i2 == 0:
                P.op("dve", lambda e: e.tensor_copy(out=Rn[:, 0, :], in_=sp[:, 0, :]), R=[spk], W=[Rnk + (0,)])
            else:
                Rp, Rpk = qstate[qg]["R"]
                P.op("dve", lambda e: e.tensor_tensor(out=Rn[:, 0, :], in0=Rp[:, 1, :], in1=sp[:, 0, :], op=ALU.add),
                     R=[spk, Rpk + (1,)], W=[Rnk + (0,)])
            ap_, apk = argp.next()
            for hf, kb in enumerate((kbA, kbB)):
                first = (i2 == 0 and hf == 0)
                P.op("pe", lambda e, hf=hf, first=first: e.matmul(ap_[:, hf, :], lhsT=nuin[:, :], rhs=sp[:, hf, :], start=True, stop=first),
                     R=[spk, "nuin"], W=[apk + (hf,)])
                if not first:
                    if hf == 0:
                        Rp, Rpk = qstate[qg]["R"]
                        P.op("pe", lambda e, Rp=Rp: e.matmul(ap_[:, 0, :], lhsT=nones[:, :], rhs=Rp[:, 1, :], start=False, stop=True),
                             R=[Rpk + (1,), "nones"], W=[apk + (0,)])
                    else:
                        P.op("pe", lambda e: e.matmul(ap_[:, 1, :], lhsT=nones[:, :], rhs=Rn[:, 0, :], start=False, stop=True),
                             R=[Rnk + (0,), "nones"], W=[apk + (1,)])
            if kbB > 0:
                P.op("dve", lambda e: e.tensor_tensor(out=Rn[:, 1, :], in0=Rn[:, 0, :], in1=sp[:, 1, :], op=ALU.add),
                     R=[spk, Rnk + (0,)], W=[Rnk + (1,)])
            qstate.setdefault(qg, {})["R"] = (Rn, Rnk)
            s_["arg"] = (ap_, apk)

        def stage_b(t):
            qg, i2, kbA, kbB, npr = blocks[t]
            val, hdl, sbl = va, hd, is_sb
            s_ = st[t]
            if i2 == 0:
                qstate.setdefault(qg, {})["acc"] = acc.next()
            op_, opk = qstate[qg]["acc"]
            w_, wk_ = wt.next()
            if sbl:
                src, srck = s_["arg"]
                e_, ek = s_["e"]
                x_, xk_ = xt.next()
                P.op("act", lambda e: e.activation(out=x_[:, :, :], in_=src[:, :, :], func=AF.Exp), R=[srck + (0,), srck + (1,)], W=[xk_])
                P.op("dve", lambda e: e.tensor_tensor(out=w_[:, :, :], in0=e_[:, :, :], in1=x_[:, :, :], op=ALU.mult), R=[ek, xk_], W=[wk_])
            else:
                src, srck = s_["zp"], s_["zpk"]
                P.op("act", lambda e: e.activation(out=w_[:, :, :], in_=src[:, :, :], func=AF.Exp), R=[srck + (0,), srck + (1,)], W=[wk_])
            dA = kbA - 4 * qg
            if sbl and dA >= 0:
                r0 = 3 - dA
                P.op("dve", lambda e: e.tensor_tensor(out=w_[:, :, :], in0=w_[:, :, :], in1=mltr[:, r0:r0 + 2, :], op=ALU.mult),
                     R=[wk_, "mltr"], W=[wk_])
            if (not sbl) and dA >= 0:
                P.op("dve", lambda e: e.tensor_tensor(out=w_[:, :, :], in0=w_[:, :, :], in1=mle[:, dA:dA + 2, :], op=ALU.mult),
                     R=[wk_, "mle"], W=[wk_])
            last = (i2 == npr - 1)
            for hf, kb in enumerate((kbA, kbB)):
                P.op("pe", lambda e, hf=hf, kb=kb: e.matmul(op_[:, :], lhsT=val[:, kb, :], rhs=w_[:, hf, :], start=(i2 == 0 and hf == 0),
                                                            stop=(last and hf == 1)), R=[wk_, vak], W=[opk])
            if last:
                o_, ok_ = ot.next()
                if sbl:
                    P.op("dve", lambda e: e.tensor_copy(out=o_[0:64, :], in_=op_[0:64, :]), R=[opk], W=[ok_])
                else:
                    P.op("dve", lambda e: e.reciprocal(out=rr[64:65, :], in_=op_[64:65, :]), R=[opk], W=["rr"])
                    bc, bck = acc.next()
                    P.op("pe", lambda e: e.matmul(bc[0:64, :], lhsT=C.ones_f[64:65, 0:64], rhs=rr[64:65, :], start=True, stop=True),
                         R=["rr", "ones_f"], W=[bck])
                    P.op("dve", lambda e: e.tensor_copy(out=bcs[:, :], in_=bc[0:64, :]), R=[bck], W=["bcs"])
                    P.op("dve", lambda e: e.tensor_tensor(out=o_[0:64, :], in0=op_[0:64, :], in1=bcs[:, :], op=ALU.mult), R=[opk, "bcs"], W=[ok_])
                P.dma("sp", oT[hdl, :, qg * 512:(qg + 1) * 512], o_[0:64, :], R=[ok_], W=[("oo", hdl, qg)])
            del st[t]

        if is_sb:
            stage_z(0)
            if nblk > 1:
                pass
            for t in range(-1, nblk + 1):
                if 0 <= t + 1 < nblk:
                    stage_a_sb(t + 1)
                if 0 <= t + 2 < nblk:
                    stage_z(t + 2)
                if 0 <= t - 1 < nblk:
                    stage_b(t - 1)
                if 0 <= t + 1 < nblk:
                    stage_a2_sb(t + 1)
        else:
            for t in range(-2, nblk):
                if 0 <= t + 2 < nblk:
                    stage_z(t + 2)
                if 0 <= t:
                    stage_b(t)
    P.finish("sp")
    P.emit()
    return nc


TWO_PI = 6.283185307179586
PI = 3.141592653589793
LCH = 512


def build_L4(S):
    nc = bass.Bass("TRN2", target_bir_lowering=False)
    dt = lambda n, s, k="ExternalInput": nc.dram_tensor(n, s, F32, kind=k).ap()
    uT = dt("uT", [128, S])
    a_re = dt("a_re", [128, 4]); a_im = dt("a_im", [128, 4]); ldt = dt("ldt", [128, 4])
    b_re = dt("b_re", [4, 128, 16]); b_im = dt("b_im", [4, 128, 16])
    ct_re = dt("ct_re", [4, 128, 16]); ct_im = dt("ct_im", [4, 128, 16])
    dsk = dt("dsk", [128])
    yT = dt("yT", [128, S], "ExternalOutput")
    C = Ctx(nc)
    P = C.P
    A = nc.alloc_sbuf_tensor
    NCH = S // LCH
    ub = A("ub", [128, S], BF16)
    P.dma("pool", ub[:], uT, W=["ub"])
    par = A("par", [128, 16, 4], F32)
    AR, AI, DT, ARD, TH, LRE, LIM, NUM, DEN, CRE, CIM, TMP, TMP2, MRE, MIM, NMIM = range(16)
    pk = lambda i: ("par", i)
    P.dma("sp", par[:, AR, :], a_re, W=[pk(AR)])
    P.dma("sp", par[:, AI, :], a_im, W=[pk(AI)])
    P.dma("sp", par[:, DT, :], ldt, W=[pk(DT)])
    dvec = load_vec_fm(C, "dvec", dsk, 128)
    cpi = A("cpi", [128, 1], F32)
    P.op("pool", lambda e: e.memset(cpi[:], PI), W=["cpi"])
    bst = A("bst", [128, 4, 4, 16], F32)
    for i, src in enumerate((b_re, b_im, ct_re, ct_im)):
        P.dma("sp", bst[:, i, :, :], src.rearrange("j p c -> p j c"), W=[("bst", i)], allow_slow_non_contiguous=True)
    io_i = A("io_i", [128, LCH], I32)
    io_f = A("io_f", [128, LCH], F32)
    P.op("pool", lambda e: e.iota(io_i[:], pattern=[[1, LCH]], base=0, channel_multiplier=0), W=["io_i"])
    P.op("dve", lambda e: e.tensor_copy(out=io_f[:], in_=io_i[:]), R=["io_i"], W=["io_f"])
    onesL = A("onesL", [128, LCH], F32)
    P.op("pool", lambda e: e.memset(onesL[:], 1.0), W=["onesL"])

    def ts(out, in0, s1, s2, o0, o1=None, R=(), W=()):
        if o1 is None:
            P.op("dve", lambda e: e.tensor_scalar(out=out, in0=in0, scalar1=s1, scalar2=None, op0=o0), R=R, W=W)
        else:
            P.op("dve", lambda e: e.tensor_scalar(out=out, in0=in0, scalar1=s1, scalar2=s2, op0=o0, op1=o1), R=R, W=W)

    def tt(out, in0, in1, o, R=(), W=(), eng="dve"):
        P.op(eng, lambda e: e.tensor_tensor(out=out, in0=in0, in1=in1, op=o), R=R, W=W)

    pv = lambda i: par[:, i, :]
    P.op("act", lambda e: e.activation(out=pv(DT), in_=pv(DT), func=AF.Exp), R=[pk(DT)], W=[pk(DT)])
    ts(pv(AR), pv(AR), -1e-4, None, ALU.min, R=[pk(AR)], W=[pk(AR)])
    tt(pv(ARD), pv(AR), pv(DT), ALU.mult, R=[pk(AR), pk(DT)], W=[pk(ARD)])
    tt(pv(TH), pv(AI), pv(DT), ALU.mult, R=[pk(AI), pk(DT)], W=[pk(TH)])
    tab = A("tab", [128, 4, 4, LCH], F32)
    scr = Rot(nc, "scr", [128, LCH], F32, 8)
    scri = Rot(nc, "scri", [128, LCH], I32, 2)
    nard = A("nard", [128, 4], F32)
    ts(nard[:, :], pv(ARD), -1.0, None, ALU.mult, R=[pk(ARD)], W=["nard"])
    def sin_of(ang, angk):
        t, tk = scr.next()
        ki, kik = scri.next()
        ts(t[:, :], ang[:, :], 1.0 / TWO_PI, None, ALU.mult, R=[angk], W=[tk])
        P.op("dve", lambda e: e.tensor_copy(out=ki[:, :], in_=t[:, :]), R=[tk], W=[kik])
        P.op("dve", lambda e: e.tensor_copy(out=t[:, :], in_=ki[:, :]), R=[kik], W=[tk])
        P.op("dve", lambda e: e.scalar_tensor_tensor(out=ang[:, :], in0=t[:, :], scalar=-TWO_PI, in1=ang[:, :], op0=ALU.mult, op1=ALU.add),
             R=[tk, angk], W=[angk])
        ts(t[:, :], ang[:, :], PI, -TWO_PI, ALU.is_gt, ALU.mult, R=[angk], W=[tk])
        tt(ang[:, :], ang[:, :], t[:, :], ALU.add, R=[angk, tk], W=[angk])
        ts(t[:, :], ang[:, :], -PI, TWO_PI, ALU.is_lt, ALU.mult, R=[angk], W=[tk])
        tt(ang[:, :], ang[:, :], t[:, :], ALU.add, R=[angk, tk], W=[angk])
        ts(ang[:, :], ang[:, :], PI, -PI, ALU.min, ALU.max, R=[angk], W=[angk])
        P.op("act", lambda e: e.activation(out=t[:, :], in_=ang[:, :], func=AF.Sin), R=[angk], W=[tk])
        return t, tk

    for j in range(4):
        ang, angk = scr.next()
        ts(ang[:, :], io_f[:, :], par[:, TH, j:j + 1], None, ALU.mult, R=["io_f", pk(TH)], W=[angk])
        sn, snk = sin_of(ang, angk)
        ang2, ang2k = scr.next()
        ts(ang2[:, :], io_f[:, :], par[:, TH, j:j + 1], PI / 2, ALU.mult, ALU.add, R=["io_f", pk(TH)], W=[ang2k])
        cs, csk = sin_of(ang2, ang2k)
        mg, mgk = scr.next()
        P.op("act", lambda e, mg=mg, j=j: e.activation(out=mg[:, :], in_=io_f[:, :], func=AF.Exp, scale=par[:, ARD, j:j + 1]),
             R=["io_f", pk(ARD)], W=[mgk])
        tt(tab[:, 2, j, :], mg[:, :], cs[:, :], ALU.mult, R=[mgk, csk], W=[("tab", 2, j)])
        tt(tab[:, 3, j, :], mg[:, :], sn[:, :], ALU.mult, R=[mgk, snk], W=[("tab", 3, j)])
        mg2, mg2k = scr.next()
        P.op("act", lambda e, mg2=mg2, j=j: e.activation(out=mg2[:, :], in_=io_f[:, :], func=AF.Exp, scale=nard[:, j:j + 1]),
             R=["io_f", "nard"], W=[mg2k])
        tt(tab[:, 0, j, :], mg2[:, :], cs[:, :], ALU.mult, R=[mg2k, csk], W=[("tab", 0, j)])
        P.op("dve", lambda e, mg2=mg2, sn=sn, j=j: e.scalar_tensor_tensor(out=tab[:, 1, j, :], in0=mg2[:, :], scalar=-1.0, in1=sn[:, :],
                                                                          op0=ALU.mult, op1=ALU.mult), R=[mg2k, snk], W=[("tab", 1, j)])
    for j in range(4):
        P.op("dve", lambda e, j=j: e.tensor_copy(out=par[:, LRE, j:j + 1], in_=tab[:, 2, j, 1:2]), R=[("tab", 2, j)], W=[pk(LRE)])
        P.op("dve", lambda e, j=j: e.tensor_copy(out=par[:, LIM, j:j + 1], in_=tab[:, 3, j, 1:2]), R=[("tab", 3, j)], W=[pk(LIM)])
    for j in range(4):
        l5r = tab[:, 2, j, LCH - 1:LCH]; l5i = tab[:, 3, j, LCH - 1:LCH]
        RK = [pk(LRE), pk(LIM), ("tab", 2, j), ("tab", 3, j)]
        tt(par[:, TMP, j:j + 1], par[:, LRE, j:j + 1], l5r, ALU.mult, R=RK, W=[pk(TMP)])
        tt(par[:, TMP2, j:j + 1], par[:, LIM, j:j + 1], l5i, ALU.mult, R=RK, W=[pk(TMP2)])
        tt(par[:, MRE, j:j + 1], par[:, TMP, j:j + 1], par[:, TMP2, j:j + 1], ALU.subtract, R=[pk(TMP), pk(TMP2)], W=[pk(MRE)])
        tt(par[:, TMP, j:j + 1], par[:, LRE, j:j + 1], l5i, ALU.mult, R=RK + [pk(MRE)], W=[pk(TMP)])
        tt(par[:, TMP2, j:j + 1], par[:, LIM, j:j + 1], l5r, ALU.mult, R=RK + [pk(MRE)], W=[pk(TMP2)])
        tt(par[:, MIM, j:j + 1], par[:, TMP, j:j + 1], par[:, TMP2, j:j + 1], ALU.add, R=[pk(TMP), pk(TMP2)], W=[pk(MIM)])
    ts(pv(NMIM), pv(MIM), -1.0, None, ALU.mult, R=[pk(MIM)], W=[pk(NMIM)])
    ts(pv(NUM), pv(LRE), -1.0, None, ALU.add, R=[pk(LRE)], W=[pk(NUM)])
    tt(pv(DEN), pv(AR), pv(AR), ALU.mult, R=[pk(AR)], W=[pk(DEN)])
    tt(pv(TMP), pv(AI), pv(AI), ALU.mult, R=[pk(AI), pk(MIM), pk(MRE)], W=[pk(TMP)])
    tt(pv(DEN), pv(DEN), pv(TMP), ALU.add, R=[pk(DEN), pk(TMP)], W=[pk(DEN)])
    P.op("dve", lambda e: e.reciprocal(out=pv(DEN), in_=pv(DEN)), R=[pk(DEN)], W=[pk(DEN)])
    tt(pv(TMP), pv(NUM), pv(AR), ALU.mult, R=[pk(NUM), pk(AR), pk(DEN)], W=[pk(TMP)])
    tt(pv(TMP2), pv(LIM), pv(AI), ALU.mult, R=[pk(LIM), pk(AI), pk(NMIM)], W=[pk(TMP2)])
    tt(pv(CRE), pv(TMP), pv(TMP2), ALU.add, R=[pk(TMP), pk(TMP2)], W=[pk(CRE)])
    tt(pv(CRE), pv(CRE), pv(DEN), ALU.mult, R=[pk(CRE), pk(DEN)], W=[pk(CRE)])
    tt(pv(TMP), pv(LIM), pv(AR), ALU.mult, R=[pk(LIM), pk(AR), pk(CRE)], W=[pk(TMP)])
    tt(pv(TMP2), pv(NUM), pv(AI), ALU.mult, R=[pk(NUM), pk(AI), pk(CRE)], W=[pk(TMP2)])
    tt(pv(CIM), pv(TMP), pv(TMP2), ALU.subtract, R=[pk(TMP), pk(TMP2)], W=[pk(CIM)])
    tt(pv(CIM), pv(CIM), pv(DEN), ALU.mult, R=[pk(CIM), pk(DEN)], W=[pk(CIM)])
    bfull = A("bfull", [128, 2, 4, 128], F32)
    P.op("pool", lambda e: e.memset(bfull[:], 0.0), W=["bfull"])
    BT = A("BT", [128, 2, 4, 128], BF16)
    CTt = A("CTt", [128, 2, 4, 128], BF16)
    P.op("pool", lambda e: e.memset(CTt[:], 0.0), W=["CTt"])
    t16 = Rot(nc, "t16", [128, 16], F32, 4)
    for j in range(4):
        for g in range(2):
            ps_ = slice(g * 64, (g + 1) * 64)
            c0 = 32 * j + 16 * g
            for which in range(2):
                ta, tak = t16.next()
                tb, tbk = t16.next()
                s_a = bst[ps_, 0 if which == 0 else 1, j, :]
                s_b = bst[ps_, 1 if which == 0 else 0, j, :]
                ts(ta[ps_, :], s_a, par[ps_, CRE, j:j + 1], None, ALU.mult, R=[("bst", 0), ("bst", 1), pk(CRE)], W=[tak])
                ts(tb[ps_, :], s_b, par[ps_, CIM, j:j + 1], None, ALU.mult, R=[("bst", 0), ("bst", 1), pk(CIM)], W=[tbk])
                tt(bfull[ps_, which, j, c0:c0 + 16], ta[ps_, :], tb[ps_, :], ALU.subtract if which == 0 else ALU.add,
                   R=[tak, tbk, "bfull"], W=["bfull"])
            P.op("dve", lambda e, ps_=ps_, j=j, c0=c0: e.tensor_copy(out=CTt[ps_, 0, j, c0:c0 + 16], in_=bst[ps_, 2, j, :]),
                 R=[("bst", 2), "CTt"], W=["CTt"])
            ts(CTt[ps_, 1, j, c0:c0 + 16], bst[ps_, 3, j, :], -1.0, None, ALU.mult, R=[("bst", 3), "CTt"], W=["CTt"])
    for j in range(4):
        for which in range(2):
            pt, ptk = C.ps.next()
            P.op("pe", lambda e, pt=pt, which=which, j=j: e.transpose(out=pt[:, 0:128], in_=bfull[:, which, j, :], identity=C.ident_f[:]),
                 R=["bfull", "ident_f"], W=[ptk])
            P.op("act", lambda e, pt=pt, which=which, j=j: e.copy(out=BT[:, which, j, :], in_=pt[:, 0:128]), R=[ptk], W=[("BT", which, j)])
    G = A("G", [128, NCH + 1, 4, 2], F32)
    P.op("pool", lambda e: e.memset(G[:], 0.0), W=["G"])
    pa = Rot(nc, "pa", [128, LCH], F32, 8)
    pb = Rot(nc, "pb", [128, LCH], F32, 8)
    Pre = Rot(nc, "Pre", [128, LCH], F32, 3)
    Pim = Rot(nc, "Pim", [128, LCH], F32, 3)
    Sre = Rot(nc, "Sre", [128, LCH], F32, 3)
    Sim = Rot(nc, "Sim", [128, LCH], F32, 3)
    hre = Rot(nc, "hre", [128, LCH], BF16, 8)
    him = Rot(nc, "him", [128, LCH], BF16, 8)
    yv = Rot(nc, "yv", [128, LCH], F32, 2)
    gt = Rot(nc, "gt", [128, LCH], F32, 2)
    sml = Rot(nc, "sml", [128, 2], F32, 4)
    items = [(ch, j) for ch in range(NCH) for j in range(4)]
    stt = {}
    hs_by_ch = {}

    def stX(t):
        ch, j = items[t]
        c0 = ch * LCH
        bre, brek = C.ps.next()
        P.op("pe", lambda e: e.matmul(bre[:, :], lhsT=BT[:, 0, j, :], rhs=ub[:, c0:c0 + LCH], start=True, stop=True),
             R=[("BT", 0, j), "ub"], W=[brek])
        bim, bimk = C.ps.next()
        P.op("pe", lambda e: e.matmul(bim[:, :], lhsT=BT[:, 1, j, :], rhs=ub[:, c0:c0 + LCH], start=True, stop=True),
             R=[("BT", 1, j), "ub"], W=[bimk])
        a1, a1k = pa.next(); a2, a2k = pa.next(); a3, a3k = pa.next(); a4, a4k = pa.next()
        tt(a1[:, :], bre[:, :], tab[:, 0, j, :], ALU.mult, R=[brek, ("tab", 0, j)], W=[a1k])
        tt(a2[:, :], bim[:, :], tab[:, 1, j, :], ALU.mult, R=[bimk, ("tab", 1, j)], W=[a2k])
        tt(a3[:, :], bim[:, :], tab[:, 0, j, :], ALU.mult, R=[bimk, ("tab", 0, j)], W=[a3k])
        tt(a4[:, :], bre[:, :], tab[:, 1, j, :], ALU.mult, R=[brek, ("tab", 1, j)], W=[a4k])
        pr, prk = Pre.next(); pi_, pik = Pim.next()
        tt(pr[:, :], a1[:, :], a2[:, :], ALU.subtract, R=[a1k, a2k], W=[prk], eng="pool")
        tt(pi_[:, :], a3[:, :], a4[:, :], ALU.add, R=[a3k, a4k], W=[pik], eng="pool")
        stt[t] = dict(pr=(pr, prk), pi=(pi_, pik))

    def stY(t):
        ch, j = items[t]
        pr, prk = stt[t]["pr"]; pi_, pik = stt[t]["pi"]
        sr, srk = Sre.next(); si, sik = Sim.next()
        P.op("dve", lambda e: e.tensor_tensor_scan(out=sr[:, :], data0=onesL[:, :], data1=pr[:, :], initial=G[:, ch, j, 0:1],
                                                   op0=ALU.mult, op1=ALU.add), R=[prk, "onesL", ("G", ch, j), "G"], W=[srk])
        P.op("dve", lambda e: e.tensor_tensor_scan(out=si[:, :], data0=onesL[:, :], data1=pi_[:, :], initial=G[:, ch, j, 1:2],
                                                   op0=ALU.mult, op1=ALU.add), R=[pik, "onesL", ("G", ch, j), "G"], W=[sik])
        sm_, smk = sml.next()
        ts(sm_[:, 0:1], sr[:, LCH - 1:LCH], par[:, MRE, j:j + 1], None, ALU.mult, R=[srk, pk(MRE)], W=[smk])
        ts(sm_[:, 1:2], si[:, LCH - 1:LCH], par[:, MRE, j:j + 1], None, ALU.mult, R=[sik, pk(MRE)], W=[smk])
        P.op("dve", lambda e: e.scalar_tensor_tensor(out=G[:, ch + 1, j, 0:1], in0=si[:, LCH - 1:LCH], scalar=par[:, NMIM, j:j + 1],
                                                     in1=sm_[:, 0:1], op0=ALU.mult, op1=ALU.add),
             R=[smk, sik, pk(NMIM), "G"], W=[("G", ch + 1, j, 0)])
        P.op("dve", lambda e: e.scalar_tensor_tensor(out=G[:, ch + 1, j, 1:2], in0=sr[:, LCH - 1:LCH], scalar=par[:, MIM, j:j + 1],
                                                     in1=sm_[:, 1:2], op0=ALU.mult, op1=ALU.add),
             R=[smk, srk, pk(MIM), "G", ("G", ch + 1, j, 0)], W=[("G", ch + 1, j)])
        b1, b1k = pb.next(); b2, b2k = pb.next(); b3, b3k = pb.next(); b4, b4k = pb.next()
        tt(b1[:, :], sr[:, :], tab[:, 2, j, :], ALU.mult, R=[srk, ("tab", 2, j)], W=[b1k], eng="pool")
        tt(b2[:, :], si[:, :], tab[:, 3, j, :], ALU.mult, R=[sik, ("tab", 3, j)], W=[b2k], eng="pool")
        tt(b3[:, :], si[:, :], tab[:, 2, j, :], ALU.mult, R=[sik, ("tab", 2, j)], W=[b3k], eng="pool")
        tt(b4[:, :], sr[:, :], tab[:, 3, j, :], ALU.mult, R=[srk, ("tab", 3, j)], W=[b4k], eng="pool")
        stt[t]["b"] = (b1, b1k, b2, b2k, b3, b3k, b4, b4k)

    def stW(t):
        ch, j = items[t]
        c0 = ch * LCH
        b1, b1k, b2, b2k, b3, b3k, b4, b4k = stt[t]["b"]
        hr_, hrk = hre.next(); hi_, hik = him.next()
        tt(hr_[:, :], b1[:, :], b2[:, :], ALU.subtract, R=[b1k, b2k], W=[hrk])
        tt(hi_[:, :], b3[:, :], b4[:, :], ALU.add, R=[b3k, b4k], W=[hik])
        hs_by_ch.setdefault(ch, []).append((hr_, hrk, hi_, hik))
        del stt[t]
        if j == 3:
            hs = hs_by_ch.pop(ch)
            yp, ypk = C.ps.next()
            for jj in range(4):
                h_r, h_rk, h_i, h_ik = hs[jj]
                P.op("pe", lambda e, h_r=h_r, jj=jj: e.matmul(yp[:, :], lhsT=CTt[:, 0, jj, :], rhs=h_r[:, :], start=(jj == 0), stop=False),
                     R=["CTt", h_rk], W=[ypk])
                P.op("pe", lambda e, h_i=h_i, jj=jj: e.matmul(yp[:, :], lhsT=CTt[:, 1, jj, :], rhs=h_i[:, :], start=False, stop=(jj == 3)),
                     R=["CTt", h_ik], W=[ypk])
            y_, yk_ = yv.next()
            P.op("dve", lambda e: e.scalar_tensor_tensor(out=y_[:, :], in0=ub[:, c0:c0 + LCH], scalar=dvec[:, 0:1], in1=yp[:, :],
                                                         op0=ALU.mult, op1=ALU.add), R=[ypk, "ub", "dvec"], W=[yk_])
            g_, gk_ = gt.next()
            tt(g_[:, :], y_[:, :], y_[:, :], ALU.mult, R=[yk_], W=[gk_], eng="pool")
            P.op("pool", lambda e: e.tensor_scalar(out=g_[:, :], in0=g_[:, :], scalar1=0.044715, scalar2=1.0, op0=ALU.mult, op1=ALU.add),
                 R=[gk_], W=[gk_])
            tt(g_[:, :], g_[:, :], y_[:, :], ALU.mult, R=[gk_, yk_], W=[gk_], eng="pool")
            P.op("act", lambda e: e.activation(out=g_[:, :], in_=g_[:, :], func=AF.Sigmoid, scale=1.5957691216057308), R=[gk_], W=[gk_])
            tt(y_[:, :], y_[:, :], g_[:, :], ALU.mult, R=[gk_, yk_], W=[yk_], eng="pool")
            P.dma("sp", yT[:, c0:c0 + LCH], y_[:, :], R=[yk_], W=[("yo", ch)])

    nit = len(items)
    for t in range(-2, nit):
        if 0 <= t + 2 < nit:
            stX(t + 2)
        if 0 <= t + 1 < nit:
            stY(t + 1)
        if 0 <= t:
            stW(t)
    P.finish("sp")
    P.emit()
    return nc


def s5_core_inputs(ins, b, gq, uT_b):
    gs = slice(8 * gq, 8 * gq + 8)
    def st(a):
        return np.ascontiguousarray(a[gs].reshape(4, 128).T)
    d = dict(uT=np.ascontiguousarray(uT_b[128 * gq:128 * gq + 128]),
             a_re=st(ins["ssm_a_re"][0]), a_im=st(ins["ssm_a_im"][0]),
             ldt=np.ascontiguousarray(np.repeat(ins["ssm_log_dt"][0][gs].reshape(4, 2, 1), 64, axis=2).reshape(4, 128).T),
             b_re=np.ascontiguousarray(ins["ssm_b_re"][0][gs].reshape(4, 128, 16)),
             b_im=np.ascontiguousarray(ins["ssm_b_im"][0][gs].reshape(4, 128, 16)),
             ct_re=np.ascontiguousarray(ins["ssm_c_re"][0][gs].transpose(0, 2, 1).reshape(4, 128, 16)),
             ct_im=np.ascontiguousarray(ins["ssm_c_im"][0][gs].transpose(0, 2, 1).reshape(4, 128, 16)),
             dsk=np.ascontiguousarray(ins["ssm_d"][0][128 * gq:128 * gq + 128]))
    return d


SEQ = 8192
BATCH = 2
TPC = BATCH * SEQ // NCORES
CPB = NCORES // BATCH


def _run(nc, in_maps):
    res = run_bass_kernel_spmd(nc, in_maps, core_ids=list(range(NCORES)))
    return res.results


def kernel(**ins):
    ins = {k: np.ascontiguousarray(np.asarray(v, dtype=np.float32)) for k, v in ins.items()}
    x = ins["x"].reshape(BATCH * SEQ, D_MODEL)
    ca = np.ascontiguousarray
    Ct, St, pm = rope_tables(SEQ)
    sc = np.float32(96 ** -0.5)
    Cq, Sq = ca(Ct * sc), ca(St * sc)
    nc1 = build_L1(TPC)
    maps = []
    for c in range(NCORES):
        p0 = (c % CPB) * TPC
        sl = slice(p0, p0 + TPC)
        maps.append(dict(x=x[c * TPC:(c + 1) * TPC], att_norm=ins["att_norm"][0], w_in=ins["att_w_in"][0],
                         q_lat_norm=ins["att_q_latent_norm"][0], w_q_up=ins["att_w_q_up"][0],
                         kv_lat_norm=ins["att_kv_latent_norm"][0], w_kv_up=ins["att_w_kv_up"][0],
                         q_norm=ins["att_q_norm"][0], k_norm=ins["att_k_norm"][0],
                         cq_t=ca(Cq[:, sl]), sq_t=ca(Sq[:, sl]), ck_t=ca(Ct[:, sl]), sk_t=ca(St[:, sl]), pm=pm))
    r1 = _run(nc1, maps)
    del nc1
    cat = lambda name, b, axis: np.concatenate([r1[b * CPB + i][name] for i in range(CPB)], axis=axis)
    nc2 = build_L2(SEQ)
    maps = []
    for b in range(BATCH):
        sbqT = cat("sbqT", b, 1); sbkT = cat("sbkT", b, 1); sbv = cat("sbv", b, 0)
        mqT = cat("mqT", b, 2); mkT = cat("mkT", b, 2); mv = cat("mv", b, 0)
        for g in range(CPB):
            maps.append(dict(sbqT=ca(sbqT[128 * g:128 * g + 128].reshape(2, 64, SEQ)),
                             sbkT=ca(sbkT[128 * g:128 * g + 128].reshape(2, 64, SEQ)),
                             sbv=ca(sbv[:, 128 * g:128 * g + 128].reshape(SEQ, 2, 64).transpose(1, 0, 2)),
                             mqT=ca(mqT[2 * g:2 * g + 2]), mkT=ca(mkT[2 * g:2 * g + 2]),
                             mv=ca(mv[:, 128 * g:128 * g + 128].reshape(SEQ, 2, 64).transpose(1, 0, 2))))
    r2 = _run(nc2, maps)
    del nc2, r1
    mT = []
    for b in range(BATCH):
        m = np.empty((1024, SEQ), np.float32)
        for g in range(CPB):
            o = r2[b * CPB + g]["oT"]
            m[128 * g:128 * g + 128] = o[0:2].reshape(128, SEQ)
            m[512 + 128 * g:512 + 128 * g + 128] = o[2:4].reshape(128, SEQ)
        mT.append(m)
    nc3 = build_L3(TPC)
    maps = []
    for c in range(NCORES):
        p0 = (c % CPB) * TPC
        maps.append(dict(x=x[c * TPC:(c + 1) * TPC], mT=ca(mT[c // CPB][:, p0:p0 + TPC]), w_out=ins["att_w_out"][0],
                         dffn_norm=ins["dffn_norm"][0], wg=ins["dffn_w_gate"][0], wu=ins["dffn_w_up"][0], wd=ins["dffn_w_down"][0],
                         ssm_norm=ins["ssm_norm"][0], w_sin=ins["ssm_w_in"][0]))
    r3 = _run(nc3, maps)
    del nc3, r2
    nc4 = build_L4(SEQ)
    maps = []
    for b in range(BATCH):
        uT_b = np.concatenate([r3[b * CPB + i]["uT"] for i in range(CPB)], axis=1)
        for gq in range(CPB):
            maps.append(s5_core_inputs(ins, b, gq, uT_b))
    r4 = _run(nc4, maps)
    del nc4
    nc5 = build_L5(TPC)
    maps = []
    for c in range(NCORES):
        b = c // CPB
        p0 = (c % CPB) * TPC
        yT = np.concatenate([r4[b * CPB + gq]["yT"][:, p0:p0 + TPC] for gq in range(CPB)], axis=0)
        maps.append(dict(h2T=r3[c]["h2T"], yT=ca(yT), w_glu=ins["ssm_w_glu"][0], moe_norm=ins["moe_norm"][0], w_r=ins["moe_router"][0],
                         wg=ins["moe_w_gate"][0], wu=ins["moe_w_up"][0], wd=ins["moe_w_down"][0]))
    r5 = _run(nc5, maps)
    out = np.concatenate([r5[c]["out"] for c in range(NCORES)], axis=0).reshape(BATCH, SEQ, D_MODEL)
    return out.astype(np.float32)
```

```python
import contextlib
import numpy as np
import concourse.bass as bass
import concourse.mybir as mybir
from concourse.bass_utils import run_bass_kernel_spmd

F32 = mybir.dt.float32
BF16 = mybir.dt.bfloat16
I32 = mybir.dt.int32
AF = mybir.ActivationFunctionType
ALU = mybir.AluOpType
AX = mybir.AxisListType

D_MODEL = 1024
D_FF = 3584
EPS = 1e-6
NCORES = 8

COMPUTE = ("pe", "act", "dve", "pool")
NDSEM = 8


class Prog:
    def __init__(self, nc):
        self.nc = nc
        self.engs = ("pe", "act", "dve", "pool", "sp")
        self.q = {e: [] for e in self.engs}
        self.cnt = {e: 0 for e in COMPUTE}
        self.known = {e: {} for e in self.engs}
        self.res = {}
        self.dcnt = {}
        self.drr = {e: 0 for e in self.engs}
        self.sems = {}
        self.n_wait = 0
        self.n_op = 0

    def _deps(self, R, W):
        deps = {}
        for r in R:
            st = self.res.get(r)
            if st is not None and st[0] is not None:
                k, v = st[0]
                if deps.get(k, 0) < v:
                    deps[k] = v
        for w in W:
            st = self.res.get(w)
            if st is not None:
                if st[0] is not None:
                    k, v = st[0]
                    if deps.get(k, 0) < v:
                        deps[k] = v
                for k, v in st[1].items():
                    if deps.get(k, 0) < v:
                        deps[k] = v
        return deps

    def _record(self, tok, R, W):
        k, v = tok
        for r in R:
            st = self.res.get(r)
            if st is None:
                st = [None, {}]
                self.res[r] = st
            if st[1].get(k, 0) < v:
                st[1][k] = v
        for w in W:
            self.res[w] = [tok, {}]

    def _emit_waits(self, eng, deps):
        kn = self.known[eng]
        for k, v in deps.items():
            if k == eng and eng == "pe":
                continue
            if kn.get(k, 0) >= v:
                continue
            kn[k] = v
            self.q[eng].append(("w", k, v))
            self.n_wait += 1

    def op(self, eng, fn, R=(), W=()):
        deps = self._deps(R, W)
        self._emit_waits(eng, deps)
        self.cnt[eng] += 1
        tok = (eng, self.cnt[eng])
        self.q[eng].append(("o", fn, eng, 1))
        self._record(tok, R, W)
        self.n_op += 1
        return tok

    def dma(self, eng, out, in_, R=(), W=(), **kw):
        deps = self._deps(R, W)
        j = self.drr[eng]
        self.drr[eng] = (j + 1) % NDSEM
        key = ("d", eng, j)
        prev = self.dcnt.get(key, 0)
        if prev:
            deps[key] = max(deps.get(key, 0), prev * 16)
        self._emit_waits(eng, deps)
        self.dcnt[key] = prev + 1
        tok = (key, (prev + 1) * 16)
        self.q[eng].append(("o", lambda e: e.dma_start(out=out, in_=in_, **kw), key, 16))
        self._record(tok, R, W)
        self.n_op += 1
        return tok

    def finish(self, eng="sp"):
        deps = {k: c * 16 for k, c in self.dcnt.items()}
        self._emit_waits(eng, deps)

    def emit(self):
        nc = self.nc
        with contextlib.ExitStack() as es:
            keys = list(COMPUTE) + list(self.dcnt.keys())
            for k in keys:
                nm = k if isinstance(k, str) else "d_%s_%d" % (k[1], k[2])
                self.sems[k] = es.enter_context(nc.semaphore("s_" + nm))
            block = es.enter_context(nc.Block())
            handles = {"pe": block.tensor, "act": block.scalar, "dve": block.vector,
                       "pool": block.gpsimd, "sp": block.sync}
            sems = self.sems
            for eng in self.engs:
                items = self.q[eng]

                def body(e, items=items):
                    for it in items:
                        if it[0] == "w":
                            e.wait_ge(sems[it[1]], it[2])
                        else:
                            it[1](e).then_inc(sems[it[2]], it[3])
                handles[eng](body)


class Rot:
    def __init__(self, nc, name, shape, dtype, n, psum=False):
        self.tiles = []
        for i in range(n):
            if psum:
                t = nc.alloc_psum_tensor("%s%d" % (name, i), shape, dtype)
            else:
                t = nc.alloc_sbuf_tensor("%s%d" % (name, i), shape, dtype)
            self.tiles.append(t)
        self.name = name
        self.i = 0

    def next(self):
        i = self.i % len(self.tiles)
        self.i += 1
        return self.tiles[i], (self.name, i)


class Ctx:
    def __init__(self, nc, n_ps=8):
        self.nc = nc
        self.P = Prog(nc)
        P = self.P
        self.ps = Rot(nc, "ps", [128, 512], F32, n_ps, psum=True)
        self.ones_bf = nc.alloc_sbuf_tensor("ones_bf", [128, 128], BF16)
        self.ones_f = nc.alloc_sbuf_tensor("ones_f", [128, 128], F32)
        self.ident_f = nc.alloc_sbuf_tensor("ident_f", [128, 128], F32)
        self.eps_t = nc.alloc_sbuf_tensor("eps_t", [128, 1], F32)
        P.op("pool", lambda e: e.memset(self.ones_bf[:], 1.0), W=["ones_bf"])
        P.op("pool", lambda e: e.memset(self.ones_f[:], 1.0), W=["ones_f"])
        P.op("pool", lambda e: e.memset(self.eps_t[:], EPS), W=["eps_t"])
        P.op("pool", lambda e: e.memset(self.ident_f[:], 1.0), W=["ident_f"])
        P.op("pool", lambda e: e.affine_select(out=self.ident_f[:], in_=self.ident_f[:], pattern=[[-1, 128]],
                                               compare_op=ALU.is_equal, fill=0.0, base=0, channel_multiplier=1),
             R=["ident_f"], W=["ident_f"])
        self.sq = Rot(nc, "sq", [128, 8, 512], BF16, 1)
        self.rt = Rot(nc, "rt", [128, 512], F32, 2)


def emit_rmsnorm_fm(C, hT, hkeys, nk, tok0, ntok, gain_sb, gkey, xnT, xkeys, xtok0, D, npart=128):
    P = C.P
    sq, sqk = C.sq.next()
    P.op("act", lambda e: e.activation(out=sq[:npart, 0:nk, 0:ntok], in_=hT[:npart, 0:nk, tok0:tok0 + ntok], func=AF.Square),
         R=list(hkeys), W=[sqk])
    ps, psk = C.ps.next()
    for k in range(nk):
        P.op("pe", lambda e, k=k: e.matmul(ps[:npart, 0:ntok], lhsT=C.ones_bf[:npart, :npart], rhs=sq[:npart, k, 0:ntok],
                                          start=(k == 0), stop=(k == nk - 1)),
             R=[sqk, "ones_bf"], W=[psk])
    rt, rtk = C.rt.next()
    P.op("act", lambda e: e.activation(out=rt[:npart, 0:ntok], in_=ps[:npart, 0:ntok], func=AF.Ln,
                                       bias=C.eps_t[:npart, 0:1], scale=1.0 / D),
         R=[psk, "eps_t"], W=[rtk])
    P.op("act", lambda e: e.activation(out=rt[:npart, 0:ntok], in_=rt[:npart, 0:ntok], func=AF.Exp, scale=-0.5), R=[rtk], W=[rtk])
    for k in range(nk):
        P.op("dve", lambda e, k=k: e.scalar_tensor_tensor(out=xnT[:npart, k, xtok0:xtok0 + ntok],
                                                          in0=hT[:npart, k, tok0:tok0 + ntok],
                                                          scalar=gain_sb[:npart, k:k + 1], in1=rt[:npart, 0:ntok],
                                                          op0=ALU.mult, op1=ALU.mult),
             R=[hkeys[k], rtk, gkey], W=[xkeys[k]])


def load_vec_fm(C, name, dram_vec_ap, n):
    nk = max(1, n // 128)
    npart = min(128, n)
    t = C.nc.alloc_sbuf_tensor(name, [128, nk], F32)
    C.P.dma("sp", t[:npart, :], dram_vec_ap.rearrange("(k p) -> p k", p=npart), W=[name], allow_slow_non_contiguous=True)
    return t


def emit_ffn(C, xnT, xkeys, hT, hkeys, T, wg, wu, wd, pools, gate_bc=None):
    P = C.P
    NTG = T // 512
    hidT, hidkeys = pools["hidT"], pools["hidkeys"]
    for wb in range(7):
        wgt, wgk = pools["wgu"].next()
        P.dma("pool", wgt[:], wg[:, wb * 512:(wb + 1) * 512].rearrange("(k p) n -> p k n", p=128), W=[wgk])
        wut, wuk = pools["wgu"].next()
        P.dma("pool", wut[:], wu[:, wb * 512:(wb + 1) * 512].rearrange("(k p) n -> p k n", p=128), W=[wuk])
        for m in range(4):
            mm = wb * 4 + m
            for tg in range(NTG):
                pg, pgk = C.ps.next()
                for k in range(8):
                    P.op("pe", lambda e, k=k, pg=pg, wgt=wgt, m=m, tg=tg: e.matmul(
                        pg[:, :], lhsT=wgt[:, k, m * 128:(m + 1) * 128], rhs=xnT[:, k, tg * 512:(tg + 1) * 512],
                        start=(k == 0), stop=(k == 7)), R=[wgk, xkeys(k, tg)], W=[pgk])
                pu, puk = C.ps.next()
                for k in range(8):
                    P.op("pe", lambda e, k=k, pu=pu, wut=wut, m=m, tg=tg: e.matmul(
                        pu[:, :], lhsT=wut[:, k, m * 128:(m + 1) * 128], rhs=xnT[:, k, tg * 512:(tg + 1) * 512],
                        start=(k == 0), stop=(k == 7)), R=[wuk, xkeys(k, tg)], W=[puk])
                sg, sgk = pools["sg"].next()
                P.op("act", lambda e, sg=sg, pg=pg: e.activation(out=sg[:, :], in_=pg[:, :], func=AF.Silu), R=[pgk], W=[sgk])
                P.op("dve", lambda e, sg=sg, pu=pu, mm=mm, tg=tg: e.tensor_tensor(
                    out=hidT[:, mm, tg * 512:(tg + 1) * 512], in0=sg[:, :], in1=pu[:, :], op=ALU.mult),
                    R=[sgk, puk], W=[hidkeys[mm] + (tg,)])
    for dm in range(8):
        wdt, wdk = pools["wd"].next()
        P.dma("pool", wdt[:], wd[:, dm * 128:(dm + 1) * 128].rearrange("(k p) n -> p k n", p=128), W=[wdk])
        for tg in range(NTG):
            po, pok = C.ps.next()
            for k in range(28):
                P.op("pe", lambda e, k=k, po=po, wdt=wdt, tg=tg: e.matmul(
                    po[:, :], lhsT=wdt[:, k, :], rhs=hidT[:, k, tg * 512:(tg + 1) * 512],
                    start=(k == 0), stop=(k == 27)), R=[wdk, hidkeys[k] + (tg,)], W=[pok])
            if gate_bc is None:
                P.op("dve", lambda e, po=po, dm=dm, tg=tg: e.tensor_tensor(
                    out=hT[:, dm, tg * 512:(tg + 1) * 512], in0=hT[:, dm, tg * 512:(tg + 1) * 512], in1=po[:, :], op=ALU.add),
                    R=[pok, hkeys(dm, tg)], W=[hkeys(dm, tg)])
            else:
                gt, gkf = gate_bc
                tmp, tmpk = pools["sg"].next()
                P.op("dve", lambda e, po=po, tmp=tmp, gt=gt, tg=tg: e.tensor_tensor(
                    out=tmp[:, :], in0=po[:, :], in1=gt[:, tg * 512:(tg + 1) * 512], op=ALU.mult),
                    R=[pok, gkf(tg)], W=[tmpk])
                P.op("dve", lambda e, tmp=tmp, dm=dm, tg=tg: e.tensor_tensor(
                    out=hT[:, dm, tg * 512:(tg + 1) * 512], in0=hT[:, dm, tg * 512:(tg + 1) * 512], in1=tmp[:, :], op=ALU.add),
                    R=[tmpk, hkeys(dm, tg)], W=[hkeys(dm, tg)])


def ffn_pools(nc, T):
    return {
        "hidT": nc.alloc_sbuf_tensor("hidT", [128, 28, T], BF16),
        "hidkeys": [("hid", k) for k in range(28)],
        "wgu": Rot(nc, "wgu", [128, 8, 512], BF16, 4),
        "wd": Rot(nc, "wd", [128, 28, 128], BF16, 2),
        "sg": Rot(nc, "sg", [128, 512], F32, 3),
    }


def emit_load_tm_to_fm(C, src_dram, hT, hkeys, ntiles, stage):
    P = C.P
    for t in range(ntiles):
        st, stk = stage.next()
        P.dma("sp", st[:], src_dram[t * 128:(t + 1) * 128, :], W=[stk])
        for half in range(2):
            ps, psk = C.ps.next()
            for kk in range(4):
                k = half * 4 + kk
                P.op("pe", lambda e, ps=ps, st=st, k=k, kk=kk: e.transpose(out=ps[:, kk * 128:(kk + 1) * 128], in_=st[:, k * 128:(k + 1) * 128],
                                                                           identity=C.ident_f[:]),
                     R=[stk, "ident_f"], W=[psk])
            P.op("dve" if half == 0 else "act",
                 (lambda e, ps=ps, half=half, t=t: e.tensor_copy(out=hT[:, half * 4:half * 4 + 4, t * 128:(t + 1) * 128],
                                                                 in_=ps[:, :].rearrange("p (k n) -> p k n", k=4))) if half == 0 else
                 (lambda e, ps=ps, half=half, t=t: e.copy(out=hT[:, half * 4:half * 4 + 4, t * 128:(t + 1) * 128],
                                                          in_=ps[:, :].rearrange("p (k n) -> p k n", k=4))),
                 R=[psk], W=[hkeys(half * 4 + kk, t // 4) for kk in range(4)])


def emit_store_fm_to_tm(C, hT, hkeys, dst_dram, ntiles, stage):
    P = C.P
    for t in range(ntiles):
        st, stk = stage.next()
        for half in range(2):
            ps, psk = C.ps.next()
            for kk in range(4):
                k = half * 4 + kk
                P.op("pe", lambda e, ps=ps, k=k, kk=kk, t=t: e.transpose(out=ps[:, kk * 128:(kk + 1) * 128], in_=hT[:, k, t * 128:(t + 1) * 128],
                                                                         identity=C.ident_f[:]),
                     R=[hkeys(k, t // 4), "ident_f"], W=[psk])
            if half == 0:
                P.op("dve", lambda e, ps=ps, st=st: e.tensor_copy(out=st[:, 0:512], in_=ps[:, :]), R=[psk], W=[stk + (0,)])
            else:
                P.op("act", lambda e, ps=ps, st=st: e.copy(out=st[:, 512:1024], in_=ps[:, :]), R=[psk], W=[stk + (1,)])
        P.dma("sp", dst_dram[t * 128:(t + 1) * 128, :], st[:], R=[stk + (0,), stk + (1,)], W=[("out", t)])


def build_L3(T):
    nc = bass.Bass("TRN2", target_bir_lowering=False)
    x = nc.dram_tensor("x", [T, 1024], F32, kind="ExternalInput").ap()
    mT = nc.dram_tensor("mT", [1024, T], F32, kind="ExternalInput").ap()
    w_out = nc.dram_tensor("w_out", [1024, 1024], F32, kind="ExternalInput").ap()
    n1 = nc.dram_tensor("dffn_norm", [1024], F32, kind="ExternalInput").ap()
    wg = nc.dram_tensor("wg", [1024, D_FF], F32, kind="ExternalInput").ap()
    wu = nc.dram_tensor("wu", [1024, D_FF], F32, kind="ExternalInput").ap()
    wd = nc.dram_tensor("wd", [D_FF, 1024], F32, kind="ExternalInput").ap()
    n2 = nc.dram_tensor("ssm_norm", [1024], F32, kind="ExternalInput").ap()
    w_sin = nc.dram_tensor("w_sin", [1024, 512], F32, kind="ExternalInput").ap()
    h2T = nc.dram_tensor("h2T", [1024, T], F32, kind="ExternalOutput").ap()
    uT = nc.dram_tensor("uT", [512, T], F32, kind="ExternalOutput").ap()
    C = Ctx(nc)
    P = C.P
    TG = 1024
    hT = nc.alloc_sbuf_tensor("hT", [128, 8, TG], F32)
    xnT = nc.alloc_sbuf_tensor("xnT", [128, 8, TG], BF16)
    pools = ffn_pools(nc, TG)
    stage = Rot(nc, "stage", [128, 1024], F32, 2)
    mts = Rot(nc, "mts", [128, 8, 512], BF16, 2)
    uo = Rot(nc, "uo", [128, 512], F32, 2)
    g1 = load_vec_fm(C, "g1", n1, 1024)
    g2 = load_vec_fm(C, "g2", n2, 1024)
    hk = lambda k, tg: ("h", k, tg)
    xk = lambda k, tg: ("xn", k, tg)
    for grp in range(T // TG):
        t0 = grp * TG
        emit_load_tm_to_fm(C, x[t0:t0 + TG, :], hT, hk, TG // 128, stage)
        wo = []
        for hf in range(2):
            wt, wk = pools["wgu"].next()
            P.dma("pool", wt[:], w_out[:, hf * 512:(hf + 1) * 512].rearrange("(k p) n -> p k n", p=128), W=[wk])
            wo.append((wt, wk))
        for tg in range(TG // 512):
            mt, mk = mts.next()
            P.dma("pool", mt[:], mT[:, t0 + tg * 512:t0 + (tg + 1) * 512].rearrange("(k p) n -> p k n", p=128), W=[mk])
            for dm in range(8):
                wt, wk = wo[dm // 4]
                po, pok = C.ps.next()
                for k in range(8):
                    P.op("pe", lambda e, k=k, po=po, wt=wt, mt=mt, dm=dm: e.matmul(
                        po[:, :], lhsT=wt[:, k, (dm % 4) * 128:(dm % 4 + 1) * 128], rhs=mt[:, k, :],
                        start=(k == 0), stop=(k == 7)), R=[wk, mk], W=[pok])
                P.op("dve", lambda e, po=po, dm=dm, tg=tg: e.tensor_tensor(
                    out=hT[:, dm, tg * 512:(tg + 1) * 512], in0=hT[:, dm, tg * 512:(tg + 1) * 512], in1=po[:, :], op=ALU.add),
                    R=[pok, hk(dm, tg)], W=[hk(dm, tg)])
        for tg in range(TG // 512):
            emit_rmsnorm_fm(C, hT, [hk(k, tg) for k in range(8)], 8, tg * 512, 512, g1, "g1",
                            xnT, [xk(k, tg) for k in range(8)], tg * 512, 1024)
        emit_ffn(C, xnT, xk, hT, hk, TG, wg, wu, wd, pools)
        for k in range(8):
            P.dma("sp", h2T[k * 128:(k + 1) * 128, t0:t0 + TG], hT[:, k, :], R=[hk(k, tg) for tg in range(TG // 512)], W=[("h2o", k)])
        for tg in range(TG // 512):
            emit_rmsnorm_fm(C, hT, [hk(k, tg) for k in range(8)], 8, tg * 512, 512, g2, "g2",
                            xnT, [xk(k, tg) for k in range(8)], tg * 512, 1024)
        wt, wk = pools["wgu"].next()
        P.dma("pool", wt[:], w_sin.rearrange("(k p) n -> p k n", p=128), W=[wk])
        for tg in range(TG // 512):
            for c in range(4):
                po, pok = C.ps.next()
                for k in range(8):
                    P.op("pe", lambda e, k=k, po=po, wt=wt, c=c, tg=tg: e.matmul(
                        po[:, :], lhsT=wt[:, k, c * 128:(c + 1) * 128], rhs=xnT[:, k, tg * 512:(tg + 1) * 512],
                        start=(k == 0), stop=(k == 7)), R=[wk, xk(k, tg)], W=[pok])
                ut, uk = uo.next()
                P.op("act", lambda e, ut=ut, po=po: e.copy(out=ut[:, :], in_=po[:, :]), R=[pok], W=[uk])
                P.dma("sp", uT[c * 128:(c + 1) * 128, t0 + tg * 512:t0 + (tg + 1) * 512], ut[:, :], R=[uk], W=[("uo", c, tg, grp)])
    P.finish("sp")
    P.emit()
    return nc


def build_L5(T, n_exp=8):
    nc = bass.Bass("TRN2", target_bir_lowering=False)
    h2T = nc.dram_tensor("h2T", [1024, T], F32, kind="ExternalInput").ap()
    yT = nc.dram_tensor("yT", [512, T], F32, kind="ExternalInput").ap()
    w_glu = nc.dram_tensor("w_glu", [512, 2048], F32, kind="ExternalInput").ap()
    n1 = nc.dram_tensor("moe_norm", [1024], F32, kind="ExternalInput").ap()
    w_r = nc.dram_tensor("w_r", [1024, 8], F32, kind="ExternalInput").ap()
    wg = nc.dram_tensor("wg", [8, 1024, D_FF], F32, kind="ExternalInput").ap()
    wu = nc.dram_tensor("wu", [8, 1024, D_FF], F32, kind="ExternalInput").ap()
    wd = nc.dram_tensor("wd", [8, D_FF, 1024], F32, kind="ExternalInput").ap()
    out = nc.dram_tensor("out", [T, 1024], F32, kind="ExternalOutput").ap()
    C = Ctx(nc)
    P = C.P
    TG = 1024
    NTG = TG // 512
    hT = nc.alloc_sbuf_tensor("hT", [128, 8, TG], F32)
    xnT = nc.alloc_sbuf_tensor("xnT", [128, 8, TG], BF16)
    pools = ffn_pools(nc, TG)
    stage = Rot(nc, "stage", [128, 1024], F32, 1)
    yts = Rot(nc, "yts", [128, 4, 512], BF16, 1)
    wglu = nc.alloc_sbuf_tensor("wglu", [128, 4, 2048], BF16)
    wrg = nc.alloc_sbuf_tensor("wrg", [128, 8, 8], F32)
    sel = nc.alloc_sbuf_tensor("sel", [8, 8, 128], F32)
    gT = nc.alloc_sbuf_tensor("gT", [8, TG], F32)
    gbc = nc.alloc_sbuf_tensor("gbc", [128, TG], F32)
    sm = Rot(nc, "sm", [128, 64], F32, 2)
    g1 = load_vec_fm(C, "g1", n1, 1024)
    P.dma("pool", wglu[:], w_glu.rearrange("(k p) n -> p k n", p=128), W=["wglu"])
    P.dma("sp", wrg[:], w_r.rearrange("(k p) n -> p k n", p=128), W=["wrg"], allow_slow_non_contiguous=True)
    for k in range(8):
        P.op("dve", lambda e, k=k: e.tensor_scalar(out=wrg[:, k, :], in0=wrg[:, k, :], scalar1=g1[:, k:k + 1], scalar2=None, op0=ALU.mult),
             R=["wrg", "g1"], W=["wrg"])
    P.op("pool", lambda e: e.memset(sel[:], 1.0), W=["sel"])
    P.op("pool", lambda e: e.affine_select(out=sel[:], in_=sel[:], pattern=[[-1, 8], [0, 128]], compare_op=ALU.is_equal, fill=0.0,
                                           base=0, channel_multiplier=1), R=["sel"], W=["sel"])
    hk = lambda k, tg: ("h", k, tg)
    xk = lambda k, tg: ("xn", k, tg)
    for grp in range(T // TG):
        t0 = grp * TG
        for k in range(8):
            P.dma("sp", hT[:, k, :], h2T[k * 128:(k + 1) * 128, t0:t0 + TG], W=[hk(k, tg) for tg in range(NTG)])
        for tg in range(NTG):
            yt, yk = yts.next()
            P.dma("pool", yt[:], yT[:, t0 + tg * 512:t0 + (tg + 1) * 512].rearrange("(k p) n -> p k n", p=128), W=[yk])
            for dm in range(8):
                p1, p1k = C.ps.next()
                for k in range(4):
                    P.op("pe", lambda e, k=k, p1=p1, yt=yt, dm=dm: e.matmul(p1[:, :], lhsT=wglu[:, k, dm * 128:(dm + 1) * 128], rhs=yt[:, k, :],
                                                                          start=(k == 0), stop=(k == 3)), R=["wglu", yk], W=[p1k])
                p2, p2k = C.ps.next()
                for k in range(4):
                    P.op("pe", lambda e, k=k, p2=p2, yt=yt, dm=dm: e.matmul(p2[:, :], lhsT=wglu[:, k, 1024 + dm * 128:1024 + (dm + 1) * 128], rhs=yt[:, k, :],
                                                                          start=(k == 0), stop=(k == 3)), R=["wglu", yk], W=[p2k])
                sg, sgk = pools["sg"].next()
                P.op("act", lambda e, sg=sg, p2=p2: e.activation(out=sg[:, :], in_=p2[:, :], func=AF.Sigmoid), R=[p2k], W=[sgk])
                P.op("dve", lambda e, sg=sg, p1=p1: e.tensor_tensor(out=sg[:, :], in0=sg[:, :], in1=p1[:, :], op=ALU.mult), R=[sgk, p1k], W=[sgk])
                P.op("dve", lambda e, sg=sg, dm=dm, tg=tg: e.tensor_tensor(out=hT[:, dm, tg * 512:(tg + 1) * 512], in0=hT[:, dm, tg * 512:(tg + 1) * 512],
                                                                       in1=sg[:, :], op=ALU.add), R=[sgk, hk(dm, tg)], W=[hk(dm, tg)])
        for tg in range(NTG):
            emit_rmsnorm_fm(C, hT, [hk(k, tg) for k in range(8)], 8, tg * 512, 512, g1, "g1",
                            xnT, [xk(k, tg) for k in range(8)], tg * 512, 1024)
        for tt in range(TG // 128):
            tg = tt // 4
            pl, plk = C.ps.next()
            for k in range(8):
                P.op("pe", lambda e, k=k, pl=pl, tt=tt: e.matmul(pl[:, 0:8], lhsT=hT[:, k, tt * 128:(tt + 1) * 128], rhs=wrg[:, k, :],
                                                             start=(k == 0), stop=(k == 7)), R=[hk(k, tg), "wrg"], W=[plk])
            pss, pssk = C.ps.next()
            sq, sqk = C.sq.next()
            P.op("act", lambda e, sq=sq, tt=tt: e.activation(out=sq[:, :, 0:128], in_=hT[:, :, tt * 128:(tt + 1) * 128], func=AF.Square),
                 R=[hk(k, tg) for k in range(8)], W=[sqk])
            for k in range(8):
                P.op("pe", lambda e, k=k, pss=pss, sq=sq: e.matmul(pss[:, 0:1], lhsT=sq[:, k, 0:128], rhs=C.ones_bf[:, 0:1],
                                                               start=(k == 0), stop=(k == 7)), R=[sqk, "ones_bf"], W=[pssk])
            s, sk = sm.next()
            P.op("act", lambda e, s=s, pss=pss: e.activation(out=s[:, 0:1], in_=pss[:, 0:1], func=AF.Sqrt, bias=C.eps_t[:, 0:1], scale=1.0 / 1024),
                 R=[pssk, "eps_t"], W=[sk])
            P.op("dve", lambda e, s=s: e.reciprocal(out=s[:, 0:1], in_=s[:, 0:1]), R=[sk], W=[sk])
            P.op("dve", lambda e, s=s, pl=pl: e.tensor_scalar(out=s[:, 8:16], in0=pl[:, 0:8], scalar1=s[:, 0:1], scalar2=None, op0=ALU.mult),
                 R=[sk, plk], W=[sk])
            P.op("dve", lambda e, s=s: e.max(out=s[:, 16:24], in_=s[:, 8:16]), R=[sk], W=[sk])
            P.op("dve", lambda e, s=s: e.tensor_scalar(out=s[:, 24:25], in0=s[:, 16:17], scalar1=-1.0, scalar2=None, op0=ALU.mult), R=[sk], W=[sk])
            P.op("act", lambda e, s=s: e.activation(out=s[:, 32:40], in_=s[:, 8:16], func=AF.Exp, bias=s[:, 24:25], scale=1.0), R=[sk], W=[sk])
            P.op("dve", lambda e, s=s: e.tensor_scalar(out=s[:, 40:48], in0=s[:, 8:16], scalar1=s[:, 17:18], scalar2=None, op0=ALU.is_ge), R=[sk], W=[sk])
            P.op("dve", lambda e, s=s: e.tensor_tensor(out=s[:, 32:40], in0=s[:, 32:40], in1=s[:, 40:48], op=ALU.mult), R=[sk], W=[sk])
            P.op("dve", lambda e, s=s: e.reduce_sum(out=s[:, 48:49], in_=s[:, 32:40], axis=AX.X), R=[sk], W=[sk])
            P.op("dve", lambda e, s=s: e.reciprocal(out=s[:, 48:49], in_=s[:, 48:49]), R=[sk], W=[sk])
            P.op("dve", lambda e, s=s: e.tensor_scalar(out=s[:, 32:40], in0=s[:, 32:40], scalar1=s[:, 48:49], scalar2=None, op0=ALU.mult), R=[sk], W=[sk])
            pt, ptk = C.ps.next()
            P.op("pe", lambda e, pt=pt, s=s: e.transpose(out=pt[0:8, 0:128], in_=s[:, 32:40], identity=C.ident_f[:]), R=[sk, "ident_f"], W=[ptk])
            P.op("act", lambda e, pt=pt, tt=tt: e.copy(out=gT[0:8, tt * 128:(tt + 1) * 128], in_=pt[0:8, 0:128]), R=[ptk], W=[("gT", tg)])
        for ex in range(n_exp):
            for tg in range(NTG):
                pb, pbk = C.ps.next()
                P.op("pe", lambda e, pb=pb, ex=ex, tg=tg: e.matmul(pb[:, :], lhsT=sel[0:8, ex, :], rhs=gT[0:8, tg * 512:(tg + 1) * 512],
                                                               start=True, stop=True), R=["sel", ("gT", tg)], W=[pbk])
                P.op("act", lambda e, pb=pb, tg=tg: e.copy(out=gbc[:, tg * 512:(tg + 1) * 512], in_=pb[:, :]), R=[pbk], W=[("gbc", tg)])
            emit_ffn(C, xnT, xk, hT, hk, TG, wg[ex], wu[ex], wd[ex], pools, gate_bc=(gbc, lambda tg: ("gbc", tg)))
        emit_store_fm_to_tm(C, hT, hk, out[t0:t0 + TG, :], TG // 128, stage)
    P.finish("sp")
    P.emit()
    return nc


def build_L1(T):
    nc = bass.Bass("TRN2", target_bir_lowering=False)
    dt = lambda n, s, k="ExternalInput": nc.dram_tensor(n, s, F32, kind=k).ap()
    x = dt("x", [T, 1024])
    n0 = dt("att_norm", [1024])
    w_in = dt("w_in", [1024, 1952])
    nq = dt("q_lat_norm", [256])
    w_qup = dt("w_q_up", [256, 768])
    nkv = dt("kv_lat_norm", [128])
    w_kvup = dt("w_kv_up", [128, 1024])
    gq = dt("q_norm", [96])
    gk = dt("k_norm", [96])
    cq_t = dt("cq_t", [96, T]); sq_t = dt("sq_t", [96, T])
    ck_t = dt("ck_t", [96, T]); sk_t = dt("sk_t", [96, T])
    pm = dt("pm", [96, 96])
    dtb = lambda n, s: nc.dram_tensor(n, s, BF16, kind="ExternalOutput").ap()
    sbqT = dtb("sbqT", [512, T])
    sbkT = dtb("sbkT", [512, T])
    sbv = dtb("sbv", [T, 512])
    mqT = dtb("mqT", [8, 96, T])
    mkT = dtb("mkT", [8, 96, T])
    mv = dtb("mv", [T, 512])
    C = Ctx(nc)
    P = C.P
    A = nc.alloc_sbuf_tensor
    hT = A("hT", [128, 8, 512], F32)
    xnT = A("xnT", [128, 8, 512], BF16)
    win = A("win", [128, 8, 1952], BF16)
    wkr = A("wkr", [128, 8, 96], BF16)
    wqup = A("wqup", [128, 2, 768], BF16)
    wkn = A("wkn", [128, 8, 96], BF16)
    wkv = A("wkv", [128, 8, 64], BF16)
    pmt = A("pmt", [96, 96], BF16)
    lat = A("lat", [128, 3, 512], F32)
    latn = A("latn", [128, 3, 512], BF16)
    krp = A("krp", [96, 512], F32)
    hrs = Rot(nc, "hrs", [96, 1, 512], F32, 8)
    hns = Rot(nc, "hns", [96, 1, 512], BF16, 8)
    sqh = Rot(nc, "sqh", [96, 512], BF16, 8)
    rth = Rot(nc, "rth", [96, 512], F32, 8)
    tabs = A("tabs", [96, 4, 512], F32)
    t1 = Rot(nc, "t1", [96, 512], F32, 8)
    t2 = Rot(nc, "t2", [96, 512], F32, 8)
    ob = Rot(nc, "ob", [128, 512], BF16, 3)
    t3 = Rot(nc, "t3", [96, 512], BF16, 8)
    stage = Rot(nc, "stage", [128, 1024], F32, 2)
    g0 = load_vec_fm(C, "g0", n0, 1024)
    gql = load_vec_fm(C, "gql", nq, 256)
    gkl = load_vec_fm(C, "gkl", nkv, 128)
    gqh = load_vec_fm(C, "gqh", gq, 96)
    gkh = load_vec_fm(C, "gkh", gk, 96)
    P.dma("pool", win[:], w_in.rearrange("(k p) n -> p k n", p=128), W=["win"])
    P.op("pool", lambda e: e.memset(wkr[:], 0.0), W=["wkr"])
    P.dma("pool", wkr[:, :, 64:96], w_in[:, 1920:1952].rearrange("(k p) n -> p k n", p=128), R=["wkr"], W=["wkr"])
    P.dma("pool", wqup[:], w_qup.rearrange("(k p) n -> p k n", p=128), W=["wqup"])
    P.op("pool", lambda e: e.memset(wkn[:], 0.0), W=["wkn"])
    P.dma("pool", wkn[:, :, 0:64], w_kvup.rearrange("k (h c) -> k h c", c=128)[:, :, 0:64], R=["wkn"], W=["wkn"])
    P.dma("pool", wkv[:], w_kvup.rearrange("k (h c) -> k h c", c=128)[:, :, 64:128], W=["wkv"])
    P.dma("pool", pmt[:], pm, W=["pmt"])
    hk = lambda k, tg: ("h", k)
    for tg in range(T // 512):
        t0 = tg * 512
        emit_load_tm_to_fm(C, x[t0:t0 + 512, :], hT, hk, 4, stage)
        emit_rmsnorm_fm(C, hT, [hk(k, 0) for k in range(8)], 8, 0, 512, g0, "g0", xnT, [("xn", k) for k in range(8)], 0, 1024)
        XR = [("xn", k) for k in range(8)]
        for i, tab in enumerate((cq_t, sq_t, ck_t, sk_t)):
            P.dma("sp", tabs[:, i, :], tab[:, t0:t0 + 512], W=[("tabs", i)])

        def proj_fm(col0, ncols, wt=win, wkey="win"):
            po, pok = C.ps.next()
            for k in range(8):
                P.op("pe", lambda e, k=k, po=po: e.matmul(po[0:ncols, :], lhsT=wt[:, k, col0:col0 + ncols], rhs=xnT[:, k, :],
                                                          start=(k == 0), stop=(k == 7)), R=[wkey] + XR, W=[pok])
            return po, pok
        for c in range(8):
            po, pok = proj_fm(c * 128, 128)
            o, okk = ob.next()
            P.op("act", lambda e, o=o, po=po, c=c: e.mul(out=o[:, :], in_=po[:, :], mul=(0.125 if c < 4 else 1.0)), R=[pok], W=[okk])
            dst = sbqT if c < 4 else sbkT
            P.dma("sp", dst[(c % 4) * 128:(c % 4 + 1) * 128, t0:t0 + 512], o[:, :], R=[okk], W=[("o1", c, tg)])
        for tt in range(4):
            po, pok = C.ps.next()
            for k in range(8):
                P.op("pe", lambda e, k=k, po=po, tt=tt: e.matmul(po[:, :], lhsT=xnT[:, k, tt * 128:(tt + 1) * 128], rhs=win[:, k, 1024:1536],
                                                             start=(k == 0), stop=(k == 7)), R=["win"] + XR, W=[pok])
            o, okk = ob.next()
            P.op("dve", lambda e, o=o, po=po: e.tensor_copy(out=o[:, :], in_=po[:, :]), R=[pok], W=[okk])
            P.dma("sp", sbv[t0 + tt * 128:t0 + (tt + 1) * 128, :], o[:, :], R=[okk], W=[("o2", tt, tg)])
        for c in range(3):
            po, pok = proj_fm(1536 + c * 128, 128)
            P.op("act", lambda e, po=po, c=c: e.copy(out=lat[:, c, :], in_=po[:, :]), R=[pok], W=[("lat", c)])
        emit_rmsnorm_fm(C, lat, [("lat", 0), ("lat", 1)], 2, 0, 512, gql, "gql", latn, [("latn", 0), ("latn", 1)], 0, 256)
        emit_rmsnorm_fm(C, lat[:, 2:3, :], [("lat", 2)], 1, 0, 512, gkl, "gkl", latn[:, 2:3, :], [("latn", 2)], 0, 128)
        po, pok = proj_fm(0, 96, wt=wkr, wkey="wkr")
        P.op("act", lambda e, po=po: e.copy(out=krp[:, :], in_=po[0:96, :]), R=[pok], W=["krp"])
        for tt in range(4):
            po, pok = C.ps.next()
            P.op("pe", lambda e, po=po, tt=tt: e.matmul(po[:, :], lhsT=latn[:, 2, tt * 128:(tt + 1) * 128], rhs=wkv[:, :, :],
                                                    start=True, stop=True), R=["wkv", ("latn", 2)], W=[pok])
            o, okk = ob.next()
            P.op("dve", lambda e, o=o, po=po: e.tensor_copy(out=o[:, :], in_=po[:, :]), R=[pok], W=[okk])
            P.dma("sp", mv[t0 + tt * 128:t0 + (tt + 1) * 128, :], o[:, :], R=[okk], W=[("o3", tt, tg)])
        def chain(h, which):
            hr_, hrk = hrs.next()
            hn_, hnk = hns.next()
            po, pok = C.ps.next()
            if which == 0:
                for k in range(2):
                    P.op("pe", lambda e, k=k: e.matmul(po[0:96, :], lhsT=wqup[:, k, h * 96:(h + 1) * 96], rhs=latn[:, k, :],
                                                       start=(k == 0), stop=(k == 1)), R=["wqup", ("latn", 0), ("latn", 1)], W=[pok])
                yield
                P.op("act", lambda e: e.copy(out=hr_[:, 0, :], in_=po[0:96, :]), R=[pok], W=[hrk])
            else:
                P.op("pe", lambda e: e.matmul(po[0:96, :], lhsT=wkn[:, h, :], rhs=latn[:, 2, :], start=True, stop=True),
                     R=["wkn", ("latn", 2)], W=[pok])
                yield
                P.op("dve", lambda e: e.tensor_tensor(out=hr_[:, 0, :], in0=po[0:96, :], in1=krp[:, :], op=ALU.add), R=[pok, "krp"], W=[hrk])
            yield
            gain, gkey = (gqh, "gqh") if which == 0 else (gkh, "gkh")
            sq_, sqk = sqh.next()
            P.op("act", lambda e: e.activation(out=sq_[:, :], in_=hr_[:, 0, :], func=AF.Square), R=[hrk], W=[sqk])
            yield
            ps, psk = C.ps.next()
            P.op("pe", lambda e: e.matmul(ps[0:96, :], lhsT=C.ones_bf[0:96, 0:96], rhs=sq_[:, :], start=True, stop=True), R=[sqk, "ones_bf"], W=[psk])
            yield
            rt_, rtk = rth.next()
            P.op("act", lambda e: e.activation(out=rt_[:, :], in_=ps[0:96, :], func=AF.Ln, bias=C.eps_t[0:96, 0:1], scale=1.0 / 96),
                 R=[psk, "eps_t"], W=[rtk])
            yield
            P.op("act", lambda e: e.activation(out=rt_[:, :], in_=rt_[:, :], func=AF.Exp, scale=-0.5), R=[rtk], W=[rtk])
            yield
            P.op("dve", lambda e: e.scalar_tensor_tensor(out=hn_[:, 0, :], in0=hr_[:, 0, :], scalar=gain[0:96, 0:1], in1=rt_[:, :],
                                                         op0=ALU.mult, op1=ALU.mult), R=[hrk, rtk, gkey], W=[hnk])
            yield
            pp, ppk = C.ps.next()
            P.op("pe", lambda e: e.matmul(pp[0:96, :], lhsT=pmt[:, :], rhs=hn_[:, 0, :], start=True, stop=True), R=["pmt", hnk], W=[ppk])
            a, ak = t1.next()
            ci, si = (0, 1) if which == 0 else (2, 3)
            P.op("pool", lambda e: e.tensor_tensor(out=a[:, :], in0=hn_[:, 0, :], in1=tabs[:, ci, :], op=ALU.mult), R=[hnk, ("tabs", ci)], W=[ak])
            yield
            b, bk = t2.next()
            P.op("dve", lambda e: e.tensor_tensor(out=b[:, :], in0=pp[0:96, :], in1=tabs[:, si, :], op=ALU.mult), R=[ppk, ("tabs", si)], W=[bk])
            yield
            a3, a3k = t3.next()
            P.op("dve", lambda e: e.tensor_tensor(out=a3[:, :], in0=a[:, :], in1=b[:, :], op=ALU.add), R=[ak, bk], W=[a3k])
            dst = mqT if which == 0 else mkT
            P.dma("sp", dst[h, :, t0:t0 + 512], a3[:, :], R=[a3k], W=[("o4", h, which, tg)])

        todo = [(h, which) for h in range(8) for which in range(2)]
        for g0_ in range(0, len(todo), 8):
            gens = [chain(h, which) for (h, which) in todo[g0_:g0_ + 8]]
            while gens:
                alive = []
                for g_ in gens:
                    try:
                        next(g_)
                        alive.append(g_)
                    except StopIteration:
                        pass
                gens = alive
    P.finish("sp")
    P.emit()
    return nc


def rope_tables(S):
    inv_freq = (10000.0 ** (-np.arange(0, 32, 2, dtype=np.float32) / np.float32(32))).astype(np.float32)
    ang = (np.arange(S, dtype=np.float32)[:, None] * inv_freq[None, :]).astype(np.float32)
    cos = np.cos(ang).astype(np.float32).T
    sin = np.sin(ang).astype(np.float32).T
    Ct = np.ones((96, S), np.float32); St = np.zeros((96, S), np.float32)
    Ct[64:80] = cos; Ct[80:96] = cos
    St[64:80] = sin; St[80:96] = sin
    pm = np.zeros((96, 96), np.float32)
    for i in range(16):
        pm[80 + i, 64 + i] = -1.0
        pm[64 + i, 80 + i] = 1.0
    return Ct, St, pm


def build_L2(S, n_sb=2, n_mla=2):
    nc = bass.Bass("TRN2", target_bir_lowering=False)
    dt = lambda n, s, k="ExternalInput": nc.dram_tensor(n, s, F32, kind=k).ap()
    dtb = lambda n, s: nc.dram_tensor(n, s, BF16, kind="ExternalInput").ap()
    sbqT = dtb("sbqT", [2, 64, S]); sbkT = dtb("sbkT", [2, 64, S]); sbv = dtb("sbv", [2, S, 64])
    mqT = dtb("mqT", [2, 96, S]); mkT = dtb("mkT", [2, 96, S]); mv = dtb("mv", [2, S, 64])
    oT = dt("oT", [4, 64, S], "ExternalOutput")
    rscr = nc.dram_tensor("rscr", [4 * (S // 512), 512], F32, kind="Internal").ap()
    C = Ctx(nc, n_ps=0)
    P = C.P
    A = nc.alloc_sbuf_tensor
    NB = S // 128
    NQG = S // 512
    zps = Rot(nc, "zps", [128, 2, 512], F32, 1, psum=True)
    argp = Rot(nc, "argp", [128, 2, 512], F32, 2, psum=True)
    acc = Rot(nc, "acc", [128, 512], F32, 2, psum=True)
    qTs = [A("qT%d" % i, [128, S], BF16) for i in range(2)]
    kTs = [A("kT%d" % i, [128, S], BF16) for i in range(2)]
    vas = [A("va%d" % i, [128, NB, 128], BF16) for i in range(2)]
    mle = A("mle", [128, 4, 512], BF16)
    mltr = A("mltr", [128, 4, 512], BF16)
    for bi in range(2):
        for c4 in range(4):
            sl = slice(c4 * (S // 4), (c4 + 1) * (S // 4))
            P.op("pool", lambda e, sl=sl, bi=bi: e.memset(qTs[bi][:, sl], 0.0), W=[("qT", bi, c4)])
            P.op("pool", lambda e, sl=sl, bi=bi: e.memset(kTs[bi][:, sl], 0.0), W=[("kT", bi, c4)])
        P.op("pool", lambda e, bi=bi: e.memset(vas[bi][:], 0.0), W=[("va", bi)])
        P.op("pool", lambda e, bi=bi: e.memset(vas[bi][:, :, 64:65], 1.0), R=[("va", bi)], W=[("va", bi)])
    nuin = A("nuin", [128, 128], BF16)
    nones = A("nones", [128, 128], BF16)
    et = Rot(nc, "et", [128, 2, 512], F32, 4)
    xt = Rot(nc, "xt", [128, 2, 512], F32, 2)
    spt = Rot(nc, "spt", [128, 2, 512], BF16, 3)
    wt = Rot(nc, "wt", [128, 2, 512], BF16, 3)
    Rt = Rot(nc, "Rt", [128, 2, 512], BF16, 3)
    ot = Rot(nc, "ot", [128, 512], F32, 2)
    rr = A("rr", [128, 512], F32)
    bcs = A("bcs", [64, 512], F32)
    P.op("pool", lambda e: e.memset(mle[:], 1.0), W=["mle"])
    P.op("pool", lambda e: e.memset(mltr[:], 1.0), W=["mltr"])
    P.op("pool", lambda e: e.memset(nuin[:], -1.0), W=["nuin"])
    P.op("pool", lambda e: e.memset(nones[:], -1.0), W=["nones"])
    for d in range(4):
        P.op("pool", lambda e, d=d: e.affine_select(out=mle[:, d, :], in_=mle[:, d, :], pattern=[[1, 512]], compare_op=ALU.is_ge, fill=0.0,
                                                    base=-128 * d, channel_multiplier=-1), R=["mle"], W=["mle"])
        P.op("pool", lambda e, d=d: e.affine_select(out=mltr[:, 3 - d, :], in_=mltr[:, 3 - d, :], pattern=[[1, 512]], compare_op=ALU.is_gt,
                                                    fill=0.0, base=-128 * d, channel_multiplier=-1), R=["mltr"], W=["mltr"])
    P.op("pool", lambda e: e.affine_select(out=nuin[:], in_=nuin[:], pattern=[[-1, 128]], compare_op=ALU.is_ge, fill=0.0,
                                           base=0, channel_multiplier=1), R=["nuin"], W=["nuin"])
    CW = S // 4
    NH = n_sb + n_mla

    def head_cfg(hd):
        is_sb = hd < n_sb
        hh = hd if is_sb else hd - n_sb
        return is_sb, hh, (64 if is_sb else 96), ((sbqT, sbkT, sbv) if is_sb else (mqT, mkT, mv))

    def emit_loads(hd):
        is_sb, hh, dq, (qsrc, ksrc, vsrc) = head_cfg(hd)
        bi = hd % 2
        for c4 in range(4):
            sl = slice(c4 * CW, (c4 + 1) * CW)
            P.dma("sp", qTs[bi][0:dq, sl], qsrc[hh, :, sl], W=[("qT", bi, c4)])
            P.dma("sp", kTs[bi][0:dq, sl], ksrc[hh, :, sl], W=[("kT", bi, c4)])
        P.dma("sp", vas[bi][:, :, 0:64], vsrc[hh].rearrange("(kb p) c -> p kb c", p=128), R=[("va", bi)], W=[("va", bi)])

    emit_loads(0)
    for hd in range(NH):
        is_sb, hh, dq, _ = head_cfg(hd)
        bi = hd % 2
        qT, kT, va = qTs[bi], kTs[bi], vas[bi]
        vak = ("va", bi)
        qkeys = lambda qg, bi=bi: [("qT", bi, c) for c in range((qg * 512) // CW, (qg * 512 + 511) // CW + 1)]
        kkeys = lambda kb, bi=bi: [("kT", bi, c) for c in range((kb * 128) // CW, (kb * 128 + 127) // CW + 1)]
        if hd + 1 < NH:
            emit_loads(hd + 1)
        blocks = []
        for qg in range(NQG):
            nkb = 4 * (qg + 1)
            order = list(range(nkb - 1, -1, -1)) if is_sb else list(range(nkb))
            for i2 in range(nkb // 2):
                blocks.append((qg, i2, order[2 * i2], order[2 * i2 + 1], nkb // 2))
        nblk = len(blocks)
        st = {}
        qstate = {}

        mla_z = [(zps.tiles[0], ("zps", 0)), (argp.tiles[0], ("argp", 0)), (argp.tiles[1], ("argp", 1))]

        def stage_z(t):
            qg, i2, kbA, kbB, npr = blocks[t]
            kTl, qTl = kT, qT
            zp, zpk = zps.next() if is_sb else mla_z[t % 3]
            for hf, kb in enumerate((kbA, kbB)):
                P.op("pe", lambda e, hf=hf, kb=kb: e.matmul(zp[:, hf, :], lhsT=kTl[:, kb * 128:(kb + 1) * 128], rhs=qTl[:, qg * 512:(qg + 1) * 512],
                                                            start=True, stop=True), R=qkeys(qg) + kkeys(kb), W=[zpk + (hf,)])
            st[t] = dict(zp=zp, zpk=zpk)

        def stage_a_sb(t):
            s_ = st[t]
            zp, zpk = s_["zp"], s_["zpk"]
            e_, ek = et.next()
            P.op("act", lambda e: e.activation(out=e_[:, :, :], in_=zp[:, :, :], func=AF.Exp), R=[zpk + (0,), zpk + (1,)], W=[ek])
            s_["e"] = (e_, ek)

        def stage_a2_sb(t):
            qg, i2, kbA, kbB, npr = blocks[t]
            dA = kbA - 4 * qg
            s_ = st[t]
            e_, ek = s_["e"]
            sp, spk = spt.next()
            P.op("act", lambda e: e.activation(out=sp[:, :, :], in_=e_[:, :, :], func=AF.Ln, bias=C.ones_f[:, 0:1], scale=1.0),
                 R=[ek, "ones_f"], W=[spk])
            if dA >= 0:
                r0 = 3 - dA
                P.op("dve", lambda e: e.tensor_tensor(out=sp[:, :, :], in0=sp[:, :, :], in1=mltr[:, r0:r0 + 2, :], op=ALU.mult),
                     R=[spk, "mltr"], W=[spk])
            Rn, Rnk = Rt.next()
            if i2 == 0:
                P.op("dve", lambda e: e.tensor_copy(out=Rn[:, 0, :], in_=sp[:, 0, :]), R=[spk], W=[Rnk + (0,)])
            else:
                Rp, Rpk = qstate[qg]["R"]
                P.op("dve", lambda e: e.tensor_tensor(out=Rn[:, 0, :], in0=Rp[:, 1, :], in1=sp[:, 0, :], op=ALU.add),
                     R=[spk, Rpk + (1,)], W=[Rnk + (0,)])
            ap_, apk = argp.next()
            for hf, kb in enumerate((kbA, kbB)):
                first = (i2 == 0 and hf == 0)
                P.op("pe", lambda e, hf=hf, first=first: e.matmul(ap_[:, hf, :], lhsT=nuin[:, :], rhs=sp[:, hf, :], start=True, stop=first),
                     R=[spk, "nuin"], W=[apk + (hf,)])
                if not first:
                    if hf == 0:
                        Rp, Rpk = qstate[qg]["R"]
                        P.op("pe", lambda e, Rp=Rp: e.matmul(ap_[:, 0, :], lhsT=nones[:, :], rhs=Rp[:, 1, :], start=False, stop=True),
                             R=[Rpk + (1,), "nones"], W=[apk + (0,)])
                    else:
                        P.op("pe", lambda e: e.matmul(ap_[:, 1, :], lhsT=nones[:, :], rhs=Rn[:, 0, :], start=False, stop=True),
                             R=[Rnk + (0,), "nones"], W=[apk + (1,)])
            if kbB > 0:
                P.op("dve", lambda e: e.tensor_tensor(out=Rn[:, 1, :], in0=Rn[:, 0, :], in1=sp[:, 1, :], op=ALU.add),
                     R=[spk, Rnk + (0,)], W=[Rnk + (1,)])
            qstate.setdefault(qg, {})["R"] = (Rn, Rnk)
            s_["arg"] = (ap_, apk)

        def stage_b(t):
            qg, i2, kbA, kbB, npr = blocks[t]
            val, hdl, sbl = va, hd, is_sb
            s_ = st[t]
            if i2 == 0:
                qstate.setdefault(qg, {})["acc"] = acc.next()
            op_, opk = qstate[qg]["acc"]
            w_, wk_ = wt.next()
            if sbl:
                src, srck = s_["arg"]
                e_, ek = s_["e"]
                x_, xk_ = xt.next()
                P.op("act", lambda e: e.activation(out=x_[:, :, :], in_=src[:, :, :], func=AF.Exp), R=[srck + (0,), srck + (1,)], W=[xk_])
                P.op("dve", lambda e: e.tensor_tensor(out=w_[:, :, :], in0=e_[:, :, :], in1=x_[:, :, :], op=ALU.mult), R=[ek, xk_], W=[wk_])
            else:
                src, srck = s_["zp"], s_["zpk"]
                P.op("act", lambda e: e.activation(out=w_[:, :, :], in_=src[:, :, :], func=AF.Exp), R=[srck + (0,), srck + (1,)], W=[wk_])
            dA = kbA - 4 * qg
            if sbl and dA >= 0:
                r0 = 3 - dA
                P.op("dve", lambda e: e.tensor_tensor(out=w_[:, :, :], in0=w_[:, :, :], in1=mltr[:, r0:r0 + 2, :], op=ALU.mult),
                     R=[wk_, "mltr"], W=[wk_])
            if (not sbl) and dA >= 0:
                P.op("dve", lambda e: e.tensor_tensor(out=w_[:, :, :], in0=w_[:, :, :], in1=mle[:, dA:dA + 2, :], op=ALU.mult),
                     R=[wk_, "mle"], W=[wk_])
            last = (i2 == npr - 1)
            for hf, kb in enumerate((kbA, kbB)):
                P.op("pe", lambda e, hf=hf, kb=kb: e.matmul(op_[:, :], lhsT=val[:, kb, :], rhs=w_[:, hf, :], start=(i2 == 0 and hf == 0),
                                                            stop=(last and hf == 1)), R=[wk_, vak], W=[opk])
            if last:
                o_, ok_ = ot.next()
                if sbl:
                    P.op("dve", lambda e: e.tensor_copy(out=o_[0:64, :], in_=op_[0:64, :]), R=[opk], W=[ok_])
                else:
                    P.op("act", lambda e: e.activation(out=rr[64:65, :], in_=op_[64:65, :], func=AF.Ln), R=[opk], W=["rr"])
                    P.op("act", lambda e: e.activation(out=rr[64:65, :], in_=rr[64:65, :], func=AF.Exp, scale=-1.0), R=["rr"], W=["rr"])
                    row = hdl * NQG + qg
                    P.dma("sp", rscr[row:row + 1, :], rr[64:65, :], R=["rr"], W=[("rscr", row)])
                    P.dma("sp", bcs[:, :], rscr[row:row + 1, :].partition_broadcast(64), R=[("rscr", row)], W=["bcs"])
                    P.op("dve", lambda e: e.tensor_tensor(out=o_[0:64, :], in0=op_[0:64, :], in1=bcs[:, :], op=ALU.mult), R=[opk, "bcs"], W=[ok_])
                P.dma("sp", oT[hdl, :, qg * 512:(qg + 1) * 512], o_[0:64, :], R=[ok_], W=[("oo", hdl, qg)])
            del st[t]

        if is_sb:
            stage_z(0)
            if nblk > 1:
                pass
            for t in range(-1, nblk + 1):
                if 0 <= t + 1 < nblk:
                    stage_a_sb(t + 1)
                if 0 <= t + 2 < nblk:
                    stage_z(t + 2)
                if 0 <= t - 1 < nblk:
                    stage_b(t - 1)
                if 0 <= t + 1 < nblk:
                    stage_a2_sb(t + 1)
        else:
            for t in range(-2, nblk):
                if 0 <= t + 2 < nblk:
                    stage_z(t + 2)
                if 0 <= t:
                    stage_b(t)
    P.finish("sp")
    P.emit()
    return nc


TWO_PI = 6.283185307179586
PI = 3.141592653589793
LCH = 512


def build_L4(S):
    nc = bass.Bass("TRN2", target_bir_lowering=False)
    dt = lambda n, s, k="ExternalInput": nc.dram_tensor(n, s, F32, kind=k).ap()
    uT = dt("uT", [128, S])
    a_re = dt("a_re", [128, 4]); a_im = dt("a_im", [128, 4]); ldt = dt("ldt", [128, 4])
    b_re = dt("b_re", [4, 128, 16]); b_im = dt("b_im", [4, 128, 16])
    ct_re = dt("ct_re", [4, 128, 16]); ct_im = dt("ct_im", [4, 128, 16])
    dsk = dt("dsk", [128])
    yT = dt("yT", [128, S], "ExternalOutput")
    C = Ctx(nc)
    P = C.P
    A = nc.alloc_sbuf_tensor
    NCH = S // LCH
    ub = A("ub", [128, S], BF16)
    P.dma("pool", ub[:], uT, W=["ub"])
    par = A("par", [128, 16, 4], F32)
    AR, AI, DT, ARD, TH, LRE, LIM, NUM, DEN, CRE, CIM, TMP, TMP2, MRE, MIM, NMIM = range(16)
    pk = lambda i: ("par", i)
    P.dma("sp", par[:, AR, :], a_re, W=[pk(AR)])
    P.dma("sp", par[:, AI, :], a_im, W=[pk(AI)])
    P.dma("sp", par[:, DT, :], ldt, W=[pk(DT)])
    dvec = load_vec_fm(C, "dvec", dsk, 128)
    cpi = A("cpi", [128, 1], F32)
    P.op("pool", lambda e: e.memset(cpi[:], PI), W=["cpi"])
    bst = A("bst", [128, 4, 4, 16], F32)
    for i, src in enumerate((b_re, b_im, ct_re, ct_im)):
        P.dma("sp", bst[:, i, :, :], src.rearrange("j p c -> p j c"), W=[("bst", i)], allow_slow_non_contiguous=True)
    io_i = A("io_i", [128, LCH], I32)
    io_f = A("io_f", [128, LCH], F32)
    P.op("pool", lambda e: e.iota(io_i[:], pattern=[[1, LCH]], base=0, channel_multiplier=0), W=["io_i"])
    P.op("dve", lambda e: e.tensor_copy(out=io_f[:], in_=io_i[:]), R=["io_i"], W=["io_f"])
    onesL = A("onesL", [128, LCH], F32)
    P.op("pool", lambda e: e.memset(onesL[:], 1.0), W=["onesL"])

    def ts(out, in0, s1, s2, o0, o1=None, R=(), W=()):
        if o1 is None:
            P.op("dve", lambda e: e.tensor_scalar(out=out, in0=in0, scalar1=s1, scalar2=None, op0=o0), R=R, W=W)
        else:
            P.op("dve", lambda e: e.tensor_scalar(out=out, in0=in0, scalar1=s1, scalar2=s2, op0=o0, op1=o1), R=R, W=W)

    def tt(out, in0, in1, o, R=(), W=(), eng="dve"):
        P.op(eng, lambda e: e.tensor_tensor(out=out, in0=in0, in1=in1, op=o), R=R, W=W)

    pv = lambda i: par[:, i, :]
    P.op("act", lambda e: e.activation(out=pv(DT), in_=pv(DT), func=AF.Exp), R=[pk(DT)], W=[pk(DT)])
    ts(pv(AR), pv(AR), -1e-4, None, ALU.min, R=[pk(AR)], W=[pk(AR)])
    tt(pv(ARD), pv(AR), pv(DT), ALU.mult, R=[pk(AR), pk(DT)], W=[pk(ARD)])
    tt(pv(TH), pv(AI), pv(DT), ALU.mult, R=[pk(AI), pk(DT)], W=[pk(TH)])
    tab = A("tab", [128, 4, 4, LCH], F32)
    scr = Rot(nc, "scr", [128, LCH], F32, 8)
    scri = Rot(nc, "scri", [128, LCH], I32, 2)
    nard = A("nard", [128, 4], F32)
    ts(nard[:, :], pv(ARD), -1.0, None, ALU.mult, R=[pk(ARD)], W=["nard"])
    def sin_of(ang, angk):
        t, tk = scr.next()
        ki, kik = scri.next()
        ts(t[:, :], ang[:, :], 1.0 / TWO_PI, None, ALU.mult, R=[angk], W=[tk])
        P.op("dve", lambda e: e.tensor_copy(out=ki[:, :], in_=t[:, :]), R=[tk], W=[kik])
        P.op("dve", lambda e: e.tensor_copy(out=t[:, :], in_=ki[:, :]), R=[kik], W=[tk])
        P.op("dve", lambda e: e.scalar_tensor_tensor(out=ang[:, :], in0=t[:, :], scalar=-TWO_PI, in1=ang[:, :], op0=ALU.mult, op1=ALU.add),
             R=[tk, angk], W=[angk])
        ts(t[:, :], ang[:, :], PI, -TWO_PI, ALU.is_gt, ALU.mult, R=[angk], W=[tk])
        tt(ang[:, :], ang[:, :], t[:, :], ALU.add, R=[angk, tk], W=[angk])
        ts(t[:, :], ang[:, :], -PI, TWO_PI, ALU.is_lt, ALU.mult, R=[angk], W=[tk])
        tt(ang[:, :], ang[:, :], t[:, :], ALU.add, R=[angk, tk], W=[angk])
        ts(ang[:, :], ang[:, :], PI, -PI, ALU.min, ALU.max, R=[angk], W=[angk])
        P.op("act", lambda e: e.activation(out=t[:, :], in_=ang[:, :], func=AF.Sin), R=[angk], W=[tk])
        return t, tk

    for j in range(4):
        ang, angk = scr.next()
        ts(ang[:, :], io_f[:, :], par[:, TH, j:j + 1], None, ALU.mult, R=["io_f", pk(TH)], W=[angk])
        sn, snk = sin_of(ang, angk)
        ang2, ang2k = scr.next()
        ts(ang2[:, :], io_f[:, :], par[:, TH, j:j + 1], PI / 2, ALU.mult, ALU.add, R=["io_f", pk(TH)], W=[ang2k])
        cs, csk = sin_of(ang2, ang2k)
        mg, mgk = scr.next()
        P.op("act", lambda e, mg=mg, j=j: e.activation(out=mg[:, :], in_=io_f[:, :], func=AF.Exp, scale=par[:, ARD, j:j + 1]),
             R=["io_f", pk(ARD)], W=[mgk])
        tt(tab[:, 2, j, :], mg[:, :], cs[:, :], ALU.mult, R=[mgk, csk], W=[("tab", 2, j)])
        tt(tab[:, 3, j, :], mg[:, :], sn[:, :], ALU.mult, R=[mgk, snk], W=[("tab", 3, j)])
        mg2, mg2k = scr.next()
        P.op("act", lambda e, mg2=mg2, j=j: e.activation(out=mg2[:, :], in_=io_f[:, :], func=AF.Exp, scale=nard[:, j:j + 1]),
             R=["io_f", "nard"], W=[mg2k])
        tt(tab[:, 0, j, :], mg2[:, :], cs[:, :], ALU.mult, R=[mg2k, csk], W=[("tab", 0, j)])
        P.op("dve", lambda e, mg2=mg2, sn=sn, j=j: e.scalar_tensor_tensor(out=tab[:, 1, j, :], in0=mg2[:, :], scalar=-1.0, in1=sn[:, :],
                                                                          op0=ALU.mult, op1=ALU.mult), R=[mg2k, snk], W=[("tab", 1, j)])
    for j in range(4):
        P.op("dve", lambda e, j=j: e.tensor_copy(out=par[:, LRE, j:j + 1], in_=tab[:, 2, j, 1:2]), R=[("tab", 2, j)], W=[pk(LRE)])
        P.op("dve", lambda e, j=j: e.tensor_copy(out=par[:, LIM, j:j + 1], in_=tab[:, 3, j, 1:2]), R=[("tab", 3, j)], W=[pk(LIM)])
    for j in range(4):
        l5r = tab[:, 2, j, LCH - 1:LCH]; l5i = tab[:, 3, j, LCH - 1:LCH]
        RK = [pk(LRE), pk(LIM), ("tab", 2, j), ("tab", 3, j)]
        tt(par[:, TMP, j:j + 1], par[:, LRE, j:j + 1], l5r, ALU.mult, R=RK, W=[pk(TMP)])
        tt(par[:, TMP2, j:j + 1], par[:, LIM, j:j + 1], l5i, ALU.mult, R=RK, W=[pk(TMP2)])
        tt(par[:, MRE, j:j + 1], par[:, TMP, j:j + 1], par[:, TMP2, j:j + 1], ALU.subtract, R=[pk(TMP), pk(TMP2)], W=[pk(MRE)])
        tt(par[:, TMP, j:j + 1], par[:, LRE, j:j + 1], l5i, ALU.mult, R=RK + [pk(MRE)], W=[pk(TMP)])
        tt(par[:, TMP2, j:j + 1], par[:, LIM, j:j + 1], l5r, ALU.mult, R=RK + [pk(MRE)], W=[pk(TMP2)])
        tt(par[:, MIM, j:j + 1], par[:, TMP, j:j + 1], par[:, TMP2, j:j + 1], ALU.add, R=[pk(TMP), pk(TMP2)], W=[pk(MIM)])
    ts(pv(NMIM), pv(MIM), -1.0, None, ALU.mult, R=[pk(MIM)], W=[pk(NMIM)])
    ts(pv(NUM), pv(LRE), -1.0, None, ALU.add, R=[pk(LRE)], W=[pk(NUM)])
    tt(pv(DEN), pv(AR), pv(AR), ALU.mult, R=[pk(AR)], W=[pk(DEN)])
    tt(pv(TMP), pv(AI), pv(AI), ALU.mult, R=[pk(AI), pk(MIM), pk(MRE)], W=[pk(TMP)])
    tt(pv(DEN), pv(DEN), pv(TMP), ALU.add, R=[pk(DEN), pk(TMP)], W=[pk(DEN)])
    P.op("dve", lambda e: e.reciprocal(out=pv(DEN), in_=pv(DEN)), R=[pk(DEN)], W=[pk(DEN)])
    tt(pv(TMP), pv(NUM), pv(AR), ALU.mult, R=[pk(NUM), pk(AR), pk(DEN)], W=[pk(TMP)])
    tt(pv(TMP2), pv(LIM), pv(AI), ALU.mult, R=[pk(LIM), pk(AI), pk(NMIM)], W=[pk(TMP2)])
    tt(pv(CRE), pv(TMP), pv(TMP2), ALU.add, R=[pk(TMP), pk(TMP2)], W=[pk(CRE)])
    tt(pv(CRE), pv(CRE), pv(DEN), ALU.mult, R=[pk(CRE), pk(DEN)], W=[pk(CRE)])
    tt(pv(TMP), pv(LIM), pv(AR), ALU.mult, R=[pk(LIM), pk(AR), pk(CRE)], W=[pk(TMP)])
    tt(pv(TMP2), pv(NUM), pv(AI), ALU.mult, R=[pk(NUM), pk(AI), pk(CRE)], W=[pk(TMP2)])
    tt(pv(CIM), pv(TMP), pv(TMP2), ALU.subtract, R=[pk(TMP), pk(TMP2)], W=[pk(CIM)])
    tt(pv(CIM), pv(CIM), pv(DEN), ALU.mult, R=[pk(CIM), pk(DEN)], W=[pk(CIM)])
    bfull = A("bfull", [128, 2, 4, 128], F32)
    P.op("pool", lambda e: e.memset(bfull[:], 0.0), W=["bfull"])
    BT = A("BT", [128, 2, 4, 128], BF16)
    CTt = A("CTt", [128, 2, 4, 128], BF16)
    P.op("pool", lambda e: e.memset(CTt[:], 0.0), W=["CTt"])
    t16 = Rot(nc, "t16", [128, 16], F32, 4)
    for j in range(4):
        for g in range(2):
            ps_ = slice(g * 64, (g + 1) * 64)
            c0 = 32 * j + 16 * g
            for which in range(2):
                ta, tak = t16.next()
                tb, tbk = t16.next()
                s_a = bst[ps_, 0 if which == 0 else 1, j, :]
                s_b = bst[ps_, 1 if which == 0 else 0, j, :]
                ts(ta[ps_, :], s_a, par[ps_, CRE, j:j + 1], None, ALU.mult, R=[("bst", 0), ("bst", 1), pk(CRE)], W=[tak])
                ts(tb[ps_, :], s_b, par[ps_, CIM, j:j + 1], None, ALU.mult, R=[("bst", 0), ("bst", 1), pk(CIM)], W=[tbk])
                tt(bfull[ps_, which, j, c0:c0 + 16], ta[ps_, :], tb[ps_, :], ALU.subtract if which == 0 else ALU.add,
                   R=[tak, tbk, "bfull"], W=["bfull"])
            P.op("dve", lambda e, ps_=ps_, j=j, c0=c0: e.tensor_copy(out=CTt[ps_, 0, j, c0:c0 + 16], in_=bst[ps_, 2, j, :]),
                 R=[("bst", 2), "CTt"], W=["CTt"])
            ts(CTt[ps_, 1, j, c0:c0 + 16], bst[ps_, 3, j, :], -1.0, None, ALU.mult, R=[("bst", 3), "CTt"], W=["CTt"])
    for j in range(4):
        for which in range(2):
            pt, ptk = C.ps.next()
            P.op("pe", lambda e, pt=pt, which=which, j=j: e.transpose(out=pt[:, 0:128], in_=bfull[:, which, j, :], identity=C.ident_f[:]),
                 R=["bfull", "ident_f"], W=[ptk])
            P.op("act", lambda e, pt=pt, which=which, j=j: e.copy(out=BT[:, which, j, :], in_=pt[:, 0:128]), R=[ptk], W=[("BT", which, j)])
    G = A("G", [128, NCH + 1, 4, 2], F32)
    P.op("pool", lambda e: e.memset(G[:], 0.0), W=["G"])
    pa = Rot(nc, "pa", [128, LCH], F32, 8)
    pb = Rot(nc, "pb", [128, LCH], F32, 8)
    Pre = Rot(nc, "Pre", [128, LCH], F32, 3)
    Pim = Rot(nc, "Pim", [128, LCH], F32, 3)
    Sre = Rot(nc, "Sre", [128, LCH], F32, 3)
    Sim = Rot(nc, "Sim", [128, LCH], F32, 3)
    hre = Rot(nc, "hre", [128, LCH], BF16, 8)
    him = Rot(nc, "him", [128, LCH], BF16, 8)
    yv = Rot(nc, "yv", [128, LCH], F32, 2)
    gt = Rot(nc, "gt", [128, LCH], F32, 2)
    sml = Rot(nc, "sml", [128, 2], F32, 4)
    items = [(ch, j) for ch in range(NCH) for j in range(4)]
    stt = {}
    hs_by_ch = {}

    def stX(t):
        ch, j = items[t]
        c0 = ch * LCH
        bre, brek = C.ps.next()
        P.op("pe", lambda e: e.matmul(bre[:, :], lhsT=BT[:, 0, j, :], rhs=ub[:, c0:c0 + LCH], start=True, stop=True),
             R=[("BT", 0, j), "ub"], W=[brek])
        bim, bimk = C.ps.next()
        P.op("pe", lambda e: e.matmul(bim[:, :], lhsT=BT[:, 1, j, :], rhs=ub[:, c0:c0 + LCH], start=True, stop=True),
             R=[("BT", 1, j), "ub"], W=[bimk])
        a1, a1k = pa.next(); a2, a2k = pa.next(); a3, a3k = pa.next(); a4, a4k = pa.next()
        tt(a1[:, :], bre[:, :], tab[:, 0, j, :], ALU.mult, R=[brek, ("tab", 0, j)], W=[a1k])
        tt(a2[:, :], bim[:, :], tab[:, 1, j, :], ALU.mult, R=[bimk, ("tab", 1, j)], W=[a2k])
        tt(a3[:, :], bim[:, :], tab[:, 0, j, :], ALU.mult, R=[bimk, ("tab", 0, j)], W=[a3k])
        tt(a4[:, :], bre[:, :], tab[:, 1, j, :], ALU.mult, R=[brek, ("tab", 1, j)], W=[a4k])
        pr, prk = Pre.next(); pi_, pik = Pim.next()
        tt(pr[:, :], a1[:, :], a2[:, :], ALU.subtract, R=[a1k, a2k], W=[prk], eng="pool")
        tt(pi_[:, :], a3[:, :], a4[:, :], ALU.add, R=[a3k, a4k], W=[pik], eng="pool")
        stt[t] = dict(pr=(pr, prk), pi=(pi_, pik))

    def stY(t):
        ch, j = items[t]
        pr, prk = stt[t]["pr"]; pi_, pik = stt[t]["pi"]
        sr, srk = Sre.next(); si, sik = Sim.next()
        P.op("dve", lambda e: e.tensor_tensor_scan(out=sr[:, :], data0=onesL[:, :], data1=pr[:, :], initial=G[:, ch, j, 0:1],
                                                   op0=ALU.mult, op1=ALU.add), R=[prk, "onesL", ("G", ch, j), "G"], W=[srk])
        P.op("dve", lambda e: e.tensor_tensor_scan(out=si[:, :], data0=onesL[:, :], data1=pi_[:, :], initial=G[:, ch, j, 1:2],
                                                   op0=ALU.mult, op1=ALU.add), R=[pik, "onesL", ("G", ch, j), "G"], W=[sik])
        sm_, smk = sml.next()
        ts(sm_[:, 0:1], sr[:, LCH - 1:LCH], par[:, MRE, j:j + 1], None, ALU.mult, R=[srk, pk(MRE)], W=[smk])
        ts(sm_[:, 1:2], si[:, LCH - 1:LCH], par[:, MRE, j:j + 1], None, ALU.mult, R=[sik, pk(MRE)], W=[smk])
        P.op("dve", lambda e: e.scalar_tensor_tensor(out=G[:, ch + 1, j, 0:1], in0=si[:, LCH - 1:LCH], scalar=par[:, NMIM, j:j + 1],
                                                     in1=sm_[:, 0:1], op0=ALU.mult, op1=ALU.add),
             R=[smk, sik, pk(NMIM), "G"], W=[("G", ch + 1, j, 0)])
        P.op("dve", lambda e: e.scalar_tensor_tensor(out=G[:, ch + 1, j, 1:2], in0=sr[:, LCH - 1:LCH], scalar=par[:, MIM, j:j + 1],
                                                     in1=sm_[:, 1:2], op0=ALU.mult, op1=ALU.add),
             R=[smk, srk, pk(MIM), "G", ("G", ch + 1, j, 0)], W=[("G", ch + 1, j)])
        b1, b1k = pb.next(); b2, b2k = pb.next(); b3, b3k = pb.next(); b4, b4k = pb.next()
        tt(b1[:, :], sr[:, :], tab[:, 2, j, :], ALU.mult, R=[srk, ("tab", 2, j)], W=[b1k], eng="pool")
        tt(b2[:, :], si[:, :], tab[:, 3, j, :], ALU.mult, R=[sik, ("tab", 3, j)], W=[b2k], eng="pool")
        tt(b3[:, :], si[:, :], tab[:, 2, j, :], ALU.mult, R=[sik, ("tab", 2, j)], W=[b3k], eng="pool")
        tt(b4[:, :], sr[:, :], tab[:, 3, j, :], ALU.mult, R=[srk, ("tab", 3, j)], W=[b4k], eng="pool")
        stt[t]["b"] = (b1, b1k, b2, b2k, b3, b3k, b4, b4k)

    def stW(t):
        ch, j = items[t]
        c0 = ch * LCH
        b1, b1k, b2, b2k, b3, b3k, b4, b4k = stt[t]["b"]
        hr_, hrk = hre.next(); hi_, hik = him.next()
        tt(hr_[:, :], b1[:, :], b2[:, :], ALU.subtract, R=[b1k, b2k], W=[hrk])
        tt(hi_[:, :], b3[:, :], b4[:, :], ALU.add, R=[b3k, b4k], W=[hik])
        hs_by_ch.setdefault(ch, []).append((hr_, hrk, hi_, hik))
        del stt[t]
        if j == 3:
            hs = hs_by_ch.pop(ch)
            yp, ypk = C.ps.next()
            for jj in range(4):
                h_r, h_rk, h_i, h_ik = hs[jj]
                P.op("pe", lambda e, h_r=h_r, jj=jj: e.matmul(yp[:, :], lhsT=CTt[:, 0, jj, :], rhs=h_r[:, :], start=(jj == 0), stop=False),
                     R=["CTt", h_rk], W=[ypk])
                P.op("pe", lambda e, h_i=h_i, jj=jj: e.matmul(yp[:, :], lhsT=CTt[:, 1, jj, :], rhs=h_i[:, :], start=False, stop=(jj == 3)),
                     R=["CTt", h_ik], W=[ypk])
            y_, yk_ = yv.next()
            P.op("dve", lambda e: e.scalar_tensor_tensor(out=y_[:, :], in0=ub[:, c0:c0 + LCH], scalar=dvec[:, 0:1], in1=yp[:, :],
                                                         op0=ALU.mult, op1=ALU.add), R=[ypk, "ub", "dvec"], W=[yk_])
            g_, gk_ = gt.next()
            tt(g_[:, :], y_[:, :], y_[:, :], ALU.mult, R=[yk_], W=[gk_], eng="pool")
            P.op("pool", lambda e: e.tensor_scalar(out=g_[:, :], in0=g_[:, :], scalar1=0.044715, scalar2=1.0, op0=ALU.mult, op1=ALU.add),
                 R=[gk_], W=[gk_])
            tt(g_[:, :], g_[:, :], y_[:, :], ALU.mult, R=[gk_, yk_], W=[gk_], eng="pool")
            P.op("act", lambda e: e.activation(out=g_[:, :], in_=g_[:, :], func=AF.Sigmoid, scale=1.5957691216057308), R=[gk_], W=[gk_])
            tt(y_[:, :], y_[:, :], g_[:, :], ALU.mult, R=[gk_, yk_], W=[yk_], eng="pool")
            P.dma("sp", yT[:, c0:c0 + LCH], y_[:, :], R=[yk_], W=[("yo", ch)])

    nit = len(items)
    for t in range(-2, nit):
        if 0 <= t + 2 < nit:
            stX(t + 2)
        if 0 <= t + 1 < nit:
            stY(t + 1)
        if 0 <= t:
            stW(t)
    P.finish("sp")
    P.emit()
    return nc


def s5_core_inputs(ins, b, gq, uT_b):
    gs = slice(8 * gq, 8 * gq + 8)
    def st(a):
        return np.ascontiguousarray(a[gs].reshape(4, 128).T)
    d = dict(uT=np.ascontiguousarray(uT_b[128 * gq:128 * gq + 128]),
             a_re=st(ins["ssm_a_re"][0]), a_im=st(ins["ssm_a_im"][0]),
             ldt=np.ascontiguousarray(np.repeat(ins["ssm_log_dt"][0][gs].reshape(4, 2, 1), 64, axis=2).reshape(4, 128).T),
             b_re=np.ascontiguousarray(ins["ssm_b_re"][0][gs].reshape(4, 128, 16)),
             b_im=np.ascontiguousarray(ins["ssm_b_im"][0][gs].reshape(4, 128, 16)),
             ct_re=np.ascontiguousarray(ins["ssm_c_re"][0][gs].transpose(0, 2, 1).reshape(4, 128, 16)),
             ct_im=np.ascontiguousarray(ins["ssm_c_im"][0][gs].transpose(0, 2, 1).reshape(4, 128, 16)),
             dsk=np.ascontiguousarray(ins["ssm_d"][0][128 * gq:128 * gq + 128]))
    return d


SEQ = 8192
BATCH = 2
TPC = BATCH * SEQ // NCORES
CPB = NCORES // BATCH


def _run(nc, in_maps):
    res = run_bass_kernel_spmd(nc, in_maps, core_ids=list(range(NCORES)))
    return res.results


def kernel(**ins):
    ins = {k: np.ascontiguousarray(np.asarray(v, dtype=np.float32)) for k, v in ins.items()}
    x = ins["x"].reshape(BATCH * SEQ, D_MODEL)
    ca = np.ascontiguousarray
    Ct, St, pm = rope_tables(SEQ)
    sc = np.float32(96 ** -0.5)
    Cq, Sq = ca(Ct * sc), ca(St * sc)
    nc1 = build_L1(TPC)
    maps = []
    for c in range(NCORES):
        p0 = (c % CPB) * TPC
        sl = slice(p0, p0 + TPC)
        maps.append(dict(x=x[c * TPC:(c + 1) * TPC], att_norm=ins["att_norm"][0], w_in=ins["att_w_in"][0],
                         q_lat_norm=ins["att_q_latent_norm"][0], w_q_up=ins["att_w_q_up"][0],
                         kv_lat_norm=ins["att_kv_latent_norm"][0], w_kv_up=ins["att_w_kv_up"][0],
                         q_norm=ins["att_q_norm"][0], k_norm=ins["att_k_norm"][0],
                         cq_t=ca(Cq[:, sl]), sq_t=ca(Sq[:, sl]), ck_t=ca(Ct[:, sl]), sk_t=ca(St[:, sl]), pm=pm))
    r1 = _run(nc1, maps)
    del nc1
    cat = lambda name, b, axis: np.concatenate([r1[b * CPB + i][name] for i in range(CPB)], axis=axis)
    nc2 = build_L2(SEQ)
    maps = []
    for b in range(BATCH):
        sbqT = cat("sbqT", b, 1); sbkT = cat("sbkT", b, 1); sbv = cat("sbv", b, 0)
        mqT = cat("mqT", b, 2); mkT = cat("mkT", b, 2); mv = cat("mv", b, 0)
        for g in range(CPB):
            maps.append(dict(sbqT=ca(sbqT[128 * g:128 * g + 128].reshape(2, 64, SEQ)),
                             sbkT=ca(sbkT[128 * g:128 * g + 128].reshape(2, 64, SEQ)),
                             sbv=ca(sbv[:, 128 * g:128 * g + 128].reshape(SEQ, 2, 64).transpose(1, 0, 2)),
                             mqT=ca(mqT[2 * g:2 * g + 2]), mkT=ca(mkT[2 * g:2 * g + 2]),
                             mv=ca(mv[:, 128 * g:128 * g + 128].reshape(SEQ, 2, 64).transpose(1, 0, 2))))
    r2 = _run(nc2, maps)
    del nc2, r1
    mT = []
    for b in range(BATCH):
        m = np.empty((1024, SEQ), np.float32)
        for g in range(CPB):
            o = r2[b * CPB + g]["oT"]
            m[128 * g:128 * g + 128] = o[0:2].reshape(128, SEQ)
            m[512 + 128 * g:512 + 128 * g + 128] = o[2:4].reshape(128, SEQ)
        mT.append(m)
    nc3 = build_L3(TPC)
    maps = []
    for c in range(NCORES):
        p0 = (c % CPB) * TPC
        maps.append(dict(x=x[c * TPC:(c + 1) * TPC], mT=ca(mT[c // CPB][:, p0:p0 + TPC]), w_out=ins["att_w_out"][0],
                         dffn_norm=ins["dffn_norm"][0], wg=ins["dffn_w_gate"][0], wu=ins["dffn_w_up"][0], wd=ins["dffn_w_down"][0],
                         ssm_norm=ins["ssm_norm"][0], w_sin=ins["ssm_w_in"][0]))
    r3 = _run(nc3, maps)
    del nc3, r2
    nc4 = build_L4(SEQ)
    maps = []
    for b in range(BATCH):
        uT_b = np.concatenate([r3[b * CPB + i]["uT"] for i in range(CPB)], axis=1)
        for gq in range(CPB):
            maps.append(s5_core_inputs(ins, b, gq, uT_b))
    r4 = _run(nc4, maps)
    del nc4
    nc5 = build_L5(TPC)
    maps = []
    for c in range(NCORES):
        b = c // CPB
        p0 = (c % CPB) * TPC
        yT = np.concatenate([r4[b * CPB + gq]["yT"][:, p0:p0 + TPC] for gq in range(CPB)], axis=0)
        maps.append(dict(h2T=r3[c]["h2T"], yT=ca(yT), w_glu=ins["ssm_w_glu"][0], moe_norm=ins["moe_norm"][0], w_r=ins["moe_router"][0],
                         wg=ins["moe_w_gate"][0], wu=ins["moe_w_up"][0], wd=ins["moe_w_down"][0]))
    r5 = _run(nc5, maps)
    out = np.concatenate([r5[c]["out"] for c in range(NCORES)], axis=0).reshape(BATCH, SEQ, D_MODEL)
    return out.astype(np.float32)
```
